# Optimizing a Trainium2 kernel written in Bass

```python
import math
import jax, jax.numpy as jnp
from jax import lax
import numpy as np

D_MODEL = 1024
BATCH = 16
SEQ = 2048
DEPTH = 1
DEC_BATCH = 4
DEC_SEQ = 8192
PAST_LEN = 128

GRID_W = 64
Q_BLOCK = 128
HA = 4
HD_A = 64
HB = 8
KV_B = 2
HD_B = 64
ROPE_THETA = 10000.0
N_BUCKETS = 32
MAX_DISTANCE = 128
A_Q = HA * 2 * HD_A
A_K = HA * 2 * HD_A
A_V = HA * 2 * HD_A
B_Q = HB * HD_B
B_K = KV_B * HD_B
B_V = KV_B * HD_B
D_PROJ = A_Q + A_K + A_V + B_Q + B_K + B_V
D_MIX_OUT = HA * 2 * HD_A + HB * HD_B
N_EXPERTS = 256
TOP_K = 8
N_GROUPS = 8
TOPK_GROUPS = 4
D_EXPERT = 256
D_SHARED = 256
ROUTE_SCALE = 2.5
MOE_BLOCK = 128
EPS = 1e-6

kernel_name = 'hybrid_diffattn_gqa_axialrope_moe_encoder'


def rms_norm(x, g):
    xf = x.astype(jnp.float32)
    y = xf * lax.rsqrt(jnp.mean(xf * xf, axis=-1, keepdims=True) + EPS)
    return (y * g.astype(jnp.float32)).astype(x.dtype)


def t5_bucket(rel):
    nb = N_BUCKETS // 2
    max_exact = nb // 2
    ret = jnp.where(rel > 0, nb, 0)
    n = jnp.abs(rel)
    nf = jnp.maximum(n, 1).astype(jnp.float32)
    large = max_exact + (jnp.log(nf / max_exact) / math.log(MAX_DISTANCE / max_exact) * (nb - max_exact)).astype(jnp.int32)
    large = jnp.minimum(large, nb - 1)
    return ret + jnp.where(n < max_exact, n, large)


def to_blocks(t):
    b, s = t.shape[:2]
    return t.reshape(b, s // Q_BLOCK, Q_BLOCK, *t.shape[2:]).swapaxes(0, 1)


def from_blocks(t):
    t = t.swapaxes(0, 1)
    return t.reshape(t.shape[0], t.shape[1] * t.shape[2], *t.shape[3:])


def diff_attention(q1, q2, k1, k2, v, lam, rel_bias):
    s_len = q1.shape[1]
    nblk = s_len // Q_BLOCK
    scale = HD_A ** -0.5
    kpos = jnp.arange(s_len, dtype=jnp.int32)

    def block(args):
        q1b, q2b, q0 = args
        qpos = q0 + jnp.arange(Q_BLOCK, dtype=jnp.int32)
        bias = rel_bias[t5_bucket(kpos[None, :] - qpos[:, None])]
        bias = jnp.transpose(bias, (2, 0, 1)).astype(jnp.float32)
        s1 = jnp.einsum('bqhd,bkhd->bhqk', q1b, k1).astype(jnp.float32) * scale + bias
        s2 = jnp.einsum('bqhd,bkhd->bhqk', q2b, k2).astype(jnp.float32) * scale + bias
        p = jax.nn.softmax(s1, axis=-1) - lam * jax.nn.softmax(s2, axis=-1)
        return jnp.einsum('bhqk,bkhe->bqhe', p.astype(v.dtype), v)

    starts = jnp.arange(nblk, dtype=jnp.int32) * Q_BLOCK
    out = lax.map(block, (to_blocks(q1), to_blocks(q2), starts))
    return from_blocks(out)


def axial_rope_tables(s_len):
    rows = s_len // GRID_W
    row_id = jnp.repeat(jnp.arange(rows, dtype=jnp.float32), GRID_W)
    col_id = jnp.tile(jnp.arange(GRID_W, dtype=jnp.float32), rows)
    half = HD_B // 2
    inv = ROPE_THETA ** (-jnp.arange(0, half, 2, dtype=jnp.float32) / half)
    ang_r = row_id[:, None] * inv[None, :]
    ang_c = col_id[:, None] * inv[None, :]
    return jnp.cos(ang_r), jnp.sin(ang_r), jnp.cos(ang_c), jnp.sin(ang_c)


def rope_rotate(x, cos, sin):
    x1, x2 = jnp.split(x.astype(jnp.float32), 2, axis=-1)
    c = cos[None, :, None, :]
    s = sin[None, :, None, :]
    return jnp.concatenate([x1 * c - x2 * s, x1 * s + x2 * c], axis=-1)


def apply_axial_rope(x, tabs):
    cr, sr, cc, sc = tabs
    half = HD_B // 2
    y = jnp.concatenate([rope_rotate(x[..., :half], cr, sr), rope_rotate(x[..., half:], cc, sc)], axis=-1)
    return y.astype(x.dtype)


def gqa_attention(q, k, v):
    b, s_len = q.shape[:2]
    g = HB // KV_B
    scale = HD_B ** -0.5
    qg = q.reshape(b, s_len, KV_B, g, HD_B)

    def block(qb):
        s = jnp.einsum('bqngd,bknd->bngqk', qb, k).astype(jnp.float32) * scale
        p = jax.nn.softmax(s, axis=-1)
        return jnp.einsum('bngqk,bknd->bqngd', p.astype(v.dtype), v)

    out = from_blocks(lax.map(block, to_blocks(qg)))
    return out.reshape(b, s_len, HB * HD_B)


def token_mixers(h, layer, rel_bias, w_in, lambda_q1, lambda_k1, lambda_q2, lambda_k2,
                 g_subln, g_qnorm, g_knorm, w_out):
    b, s_len, _ = h.shape
    proj = h @ w_in[layer]
    o1 = A_Q
    o2 = o1 + A_K
    o3 = o2 + A_V
    o4 = o3 + B_Q
    o5 = o4 + B_K
    qa = proj[..., :o1].reshape(b, s_len, HA, 2, HD_A)
    ka = proj[..., o1:o2].reshape(b, s_len, HA, 2, HD_A)
    va = proj[..., o2:o3].reshape(b, s_len, HA, 2 * HD_A)
    qb = proj[..., o3:o4].reshape(b, s_len, HB, HD_B)
    kb = proj[..., o4:o5].reshape(b, s_len, KV_B, HD_B)
    vb = proj[..., o5:].reshape(b, s_len, KV_B, HD_B)

    lam_init = 0.8 - 0.6 * math.exp(-0.3 * layer)
    lam = (jnp.exp(jnp.sum(lambda_q1[layer].astype(jnp.float32) * lambda_k1[layer].astype(jnp.float32)))
           - jnp.exp(jnp.sum(lambda_q2[layer].astype(jnp.float32) * lambda_k2[layer].astype(jnp.float32)))
           + lam_init)
    oa = diff_attention(qa[..., 0, :], qa[..., 1, :], ka[..., 0, :], ka[..., 1, :], va, lam, rel_bias)
    oa = rms_norm(oa, g_subln[layer]) * (1.0 - lam_init)
    oa = oa.reshape(b, s_len, HA * 2 * HD_A)

    tabs = axial_rope_tables(s_len)
    qb = apply_axial_rope(rms_norm(qb, g_qnorm[layer]), tabs)
    kb = apply_axial_rope(rms_norm(kb, g_knorm[layer]), tabs)
    ob = gqa_attention(qb, kb, vb)

    return jnp.concatenate([oa, ob], axis=-1) @ w_out[layer]


def swiglu(x, wg, wu, wd):
    return (jax.nn.silu(x @ wg) * (x @ wu)) @ wd


def routed_experts(xf, idx, gate, w_eg, w_eu, w_ed):
    t_len, d = xf.shape
    a_len = t_len * TOP_K
    e_flat = idx.reshape(-1)
    tok_flat = jnp.repeat(jnp.arange(t_len, dtype=jnp.int32), TOP_K)
    g_flat = gate.reshape(-1)
    order = jnp.argsort(e_flat)
    e_sorted = e_flat[order]
    counts = jnp.bincount(e_flat, length=N_EXPERTS)
    padded = (counts + MOE_BLOCK - 1) // MOE_BLOCK * MOE_BLOCK
    pad_end = jnp.cumsum(padded)
    pad_start = pad_end - padded
    start = jnp.cumsum(counts) - counts
    dest = pad_start[e_sorted] + jnp.arange(a_len, dtype=jnp.int32) - start[e_sorted]
    n_blocks = a_len // MOE_BLOCK + N_EXPERTS
    n_slots = n_blocks * MOE_BLOCK
    slot_tok = jnp.full((n_slots,), t_len, jnp.int32).at[dest].set(tok_flat[order])
    slot_gate = jnp.zeros((n_slots,), jnp.float32).at[dest].set(g_flat[order])
    blk_expert = jnp.minimum(jnp.searchsorted(pad_end, jnp.arange(n_blocks) * MOE_BLOCK, side='right'),
                             N_EXPERTS - 1)
    x_pad = jnp.concatenate([xf, jnp.zeros((1, d), xf.dtype)], axis=0)

    def block(args):
        tb, gb, e = args
        xb = x_pad[tb]
        yb = swiglu(xb, w_eg[e], w_eu[e], w_ed[e])
        return yb * gb[:, None].astype(yb.dtype)

    y = lax.map(block, (slot_tok.reshape(n_blocks, MOE_BLOCK), slot_gate.reshape(n_blocks, MOE_BLOCK), blk_expert))
    out = jnp.zeros((t_len + 1, d), xf.dtype).at[slot_tok].add(y.reshape(n_slots, d))
    return out[:t_len]


def moe_ffn(h, layer, w_router, router_bias, w_exp_gate, w_exp_up, w_exp_down, w_sh_gate, w_sh_up, w_sh_down):
    b, s_len, d = h.shape
    xf = h.reshape(-1, d)
    t_len = xf.shape[0]
    scores = jax.nn.sigmoid((xf @ w_router[layer]).astype(jnp.float32))
    sel = scores + router_bias[layer].astype(jnp.float32)
    grp = sel.reshape(t_len, N_GROUPS, N_EXPERTS // N_GROUPS)
    grp_score = lax.top_k(grp, 2)[0].sum(-1)
    _, top_g = lax.top_k(grp_score, TOPK_GROUPS)
    gmask = jax.nn.one_hot(top_g, N_GROUPS, dtype=jnp.float32).sum(1)
    emask = jnp.repeat(gmask, N_EXPERTS // N_GROUPS, axis=1) > 0
    _, idx = lax.top_k(jnp.where(emask, sel, -jnp.inf), TOP_K)
    w = jnp.take_along_axis(scores, idx, axis=-1)
    w = w / jnp.sum(w, axis=-1, keepdims=True) * ROUTE_SCALE
    routed = routed_experts(xf, idx, w, w_exp_gate[layer], w_exp_up[layer], w_exp_down[layer])
    shared = swiglu(xf, w_sh_gate[layer], w_sh_up[layer], w_sh_down[layer])
    return (routed + shared).reshape(b, s_len, d)


def trunk(x, c, rel_bias, w_ada, b_ada, g_norm1, w_in, lambda_q1, lambda_k1, lambda_q2, lambda_k2,
          g_subln, g_qnorm, g_knorm, w_out, g_norm2, w_router, router_bias, w_exp_gate, w_exp_up,
          w_exp_down, w_sh_gate, w_sh_up, w_sh_down, g_final):
    for layer in range(DEPTH):
        mod = jax.nn.silu(c) @ w_ada[layer] + b_ada[layer]
        sh1, sc1, gt1, sh2, sc2, gt2 = jnp.split(mod[:, None, :], 6, axis=-1)
        h = rms_norm(x, g_norm1[layer]) * (1.0 + sc1) + sh1
        x = x + gt1 * token_mixers(h, layer, rel_bias, w_in, lambda_q1, lambda_k1, lambda_q2, lambda_k2,
                                   g_subln, g_qnorm, g_knorm, w_out)
        h = rms_norm(x, g_norm2[layer]) * (1.0 + sc2) + sh2
        x = x + gt2 * moe_ffn(h, layer, w_router, router_bias, w_exp_gate, w_exp_up, w_exp_down,
                              w_sh_gate, w_sh_up, w_sh_down)
    return rms_norm(x, g_final)


def setup_inputs(seed: int = 0) -> dict:
    key = jax.random.key(seed)
    ks = jax.random.split(key, 32)
    f32 = jnp.float32
    D = D_MODEL

    def nrm(k, shape, scale):
        return jax.random.normal(k, shape, f32) * scale

    return {
        'x_prompt': nrm(ks[0], (BATCH, SEQ, D), 1.0),
        'x_sample': nrm(ks[1], (DEC_BATCH, DEC_SEQ, D), 1.0),
        'c_prompt': nrm(ks[2], (BATCH, D), 1.0),
        'c_sample': nrm(ks[3], (DEC_BATCH, D), 1.0),
        'rel_bias': nrm(ks[4], (N_BUCKETS, HA), 0.5),
        'w_ada': nrm(ks[5], (DEPTH, D, 6 * D), 0.2 * D ** -0.5),
        'b_ada': nrm(ks[6], (DEPTH, 6 * D), 0.02),
        'g_norm1': 1.0 + nrm(ks[7], (DEPTH, D), 0.02),
        'w_in': nrm(ks[8], (DEPTH, D, D_PROJ), D ** -0.5),
        'lambda_q1': nrm(ks[9], (DEPTH, HD_A), 0.1),
        'lambda_k1': nrm(ks[10], (DEPTH, HD_A), 0.1),
        'lambda_q2': nrm(ks[11], (DEPTH, HD_A), 0.1),
        'lambda_k2': nrm(ks[12], (DEPTH, HD_A), 0.1),
        'g_subln': 1.0 + nrm(ks[13], (DEPTH, 2 * HD_A), 0.02),
        'g_qnorm': 1.0 + nrm(ks[14], (DEPTH, HD_B), 0.02),
        'g_knorm': 1.0 + nrm(ks[15], (DEPTH, HD_B), 0.02),
        'w_out': nrm(ks[16], (DEPTH, D_MIX_OUT, D), D_MIX_OUT ** -0.5),
        'g_norm2': 1.0 + nrm(ks[17], (DEPTH, D), 0.02),
        'w_router': nrm(ks[18], (DEPTH, D, N_EXPERTS), D ** -0.5),
        'router_bias': nrm(ks[19], (DEPTH, N_EXPERTS), 0.01),
        'w_exp_gate': nrm(ks[20], (DEPTH, N_EXPERTS, D, D_EXPERT), D ** -0.5),
        'w_exp_up': nrm(ks[21], (DEPTH, N_EXPERTS, D, D_EXPERT), D ** -0.5),
        'w_exp_down': nrm(ks[22], (DEPTH, N_EXPERTS, D_EXPERT, D), D_EXPERT ** -0.5),
        'w_sh_gate': nrm(ks[23], (DEPTH, D, D_SHARED), D ** -0.5),
        'w_sh_up': nrm(ks[24], (DEPTH, D, D_SHARED), D ** -0.5),
        'w_sh_down': nrm(ks[25], (DEPTH, D_SHARED, D), D_SHARED ** -0.5),
        'g_final': 1.0 + nrm(ks[26], (D,), 0.02),
    }


def reference(x_prompt, x_sample, c_prompt, c_sample, rel_bias, w_ada, b_ada, g_norm1, w_in,
              lambda_q1, lambda_k1, lambda_q2, lambda_k2, g_subln, g_qnorm, g_knorm, w_out, g_norm2,
              w_router, router_bias, w_exp_gate, w_exp_up, w_exp_down, w_sh_gate, w_sh_up, w_sh_down,
              g_final):
    y_prompt = trunk(x_prompt, c_prompt, rel_bias, w_ada, b_ada, g_norm1, w_in, lambda_q1, lambda_k1,
                     lambda_q2, lambda_k2, g_subln, g_qnorm, g_knorm, w_out, g_norm2, w_router, router_bias,
                     w_exp_gate, w_exp_up, w_exp_down, w_sh_gate, w_sh_up, w_sh_down, g_final)
    y_sample = trunk(x_sample, c_sample, rel_bias, w_ada, b_ada, g_norm1, w_in, lambda_q1, lambda_k1,
                     lambda_q2, lambda_k2, g_subln, g_qnorm, g_knorm, w_out, g_norm2, w_router, router_bias,
                     w_exp_gate, w_exp_up, w_exp_down, w_sh_gate, w_sh_up, w_sh_down, g_final)
    return (y_prompt, y_sample)
```

```python
import math
from contextlib import ExitStack
import numpy as np
import concourse.bass as bass
import concourse.mybir as mybir
from concourse.bass_utils import run_bass_kernel_spmd

F32 = mybir.dt.float32
BF16 = mybir.dt.bfloat16
I32 = mybir.dt.int32
AF = mybir.ActivationFunctionType
ALU = mybir.AluOpType
AX = mybir.AxisListType

D = 1024
DP = 2304
NE = 256
EPS = 1e-6
ENGS = ("pe", "act", "dve", "pool", "sp")


class Buf:
    __slots__ = ("name", "writers", "readers")

    def __init__(self, name):
        self.name = name
        self.writers = []
        self.readers = []


class Op:
    __slots__ = ("eng", "fn", "deps", "is_dma", "sem", "val", "signals")

    def __init__(self, eng, fn, is_dma):
        self.eng = eng
        self.fn = fn
        self.deps = []
        self.is_dma = is_dma
        self.sem = None
        self.val = 0
        self.signals = False


class Sched:
    def __init__(self, nc, stack):
        self.nc = nc
        self.stack = stack
        self.ops = {e: [] for e in ENGS}
        self.esem = {e: stack.enter_context(nc.semaphore("es_" + e)) for e in ENGS}
        self.dma_sems = {}
        self.pending = {e: [] for e in ENGS}
        self.last_dma = {}

    def barrier(self):
        deps = []
        for e in ENGS:
            for o in reversed(self.ops[e]):
                if not o.is_dma:
                    deps.append(o)
                    break
        deps.extend(self.last_dma.values())
        for e in ENGS:
            self.pending[e] = list(deps)

    def _dma_sem(self, key):
        if key not in self.dma_sems:
            s = self.stack.enter_context(self.nc.semaphore("ds%d" % len(self.dma_sems)))
            self.dma_sems[key] = [s, 0]
        return self.dma_sems[key]

    def op(self, eng, fn, reads=(), writes=(), accs=(), dma_key=None):
        is_dma = dma_key is not None
        o = Op(eng, fn, is_dma)
        deps = []
        for b in reads:
            deps.extend(b.writers)
        for b in writes:
            deps.extend(b.writers)
            deps.extend(b.readers)
        for b in accs:
            deps.extend(b.readers)
            for w in b.writers:
                if w.is_dma and is_dma:
                    continue
                deps.append(w)
        if self.pending[eng]:
            deps.extend(self.pending[eng])
            self.pending[eng] = []
        seen = set()
        for d in deps:
            if id(d) in seen or d is o:
                continue
            seen.add(id(d))
            if (not d.is_dma) and (not is_dma) and d.eng == "pe" and eng == "pe":
                continue
            o.deps.append(d)
            d.signals = True
        for b in reads:
            b.readers.append(o)
        for b in writes:
            b.writers = [o]
            b.readers = []
        for b in accs:
            b.writers.append(o)
            b.readers = []
        if is_dma:
            s = self._dma_sem(dma_key)
            s[1] += 16
            o.sem = s[0]
            o.val = s[1]
            o.signals = True
            self.last_dma[dma_key] = o
        self.ops[eng].append(o)
        return o

    def emit(self, final_ops):
        nc = self.nc
        for e in ENGS:
            c = 0
            for o in self.ops[e]:
                if not o.is_dma and o.signals:
                    c += 1
                    o.sem = self.esem[e]
                    o.val = c
        sched = self

        def run(engname, eh):
            waited = {}
            for o in sched.ops[engname]:
                for d in o.deps:
                    k = id(d.sem)
                    if waited.get(k, 0) >= d.val:
                        continue
                    eh.wait_ge(d.sem, d.val)
                    waited[k] = d.val
                ins = o.fn(eh)
                if o.is_dma:
                    ins.then_inc(o.sem, 16)
                elif o.signals:
                    ins.then_inc(o.sem, 1)
            if engname == "sp":
                for d in final_ops:
                    if waited.get(id(d.sem), 0) < d.val:
                        eh.wait_ge(d.sem, d.val)
                        waited[id(d.sem)] = d.val

        allsems = list(self.esem.values()) + [v[0] for v in self.dma_sems.values()]
        with nc.Block() as blk0:
            @blk0.sync
            def _(e):
                for s_ in allsems:
                    e.sem_clear(s_)

        with nc.Block() as block:
            @block.tensor
            def _(e):
                run("pe", e)

            @block.scalar
            def _(e):
                run("act", e)

            @block.vector
            def _(e):
                run("dve", e)

            @block.gpsimd
            def _(e):
                run("pool", e)

            @block.sync
            def _(e):
                run("sp", e)


def t5_bucket_np(rel):
    nb = 16
    max_exact = 8
    ret = np.where(rel > 0, nb, 0)
    n = np.abs(rel)
    nf = np.maximum(n, 1).astype(np.float32)
    large = max_exact + (np.log(nf / np.float32(max_exact)) / np.float32(math.log(128 / max_exact))
                         * np.float32(nb - max_exact)).astype(np.int32)
    large = np.minimum(large, nb - 1)
    return ret + np.where(n < max_exact, n, large)


def host_constants():
    c = {}
    c["ident"] = np.eye(128, dtype=np.float32)
    c["jrev"] = np.eye(128, dtype=np.float32)[::-1].copy()
    tri = np.zeros((128, 128), np.float32)
    for a in range(128):
        tri[a, a + 1:] = 1.0
    c["tri"] = tri
    tril = np.zeros((128, 128), np.float32)
    for a in range(128):
        tril[a, a:] = 1.0
    c["tril"] = tril
    i = np.arange(1280)
    bk = t5_bucket_np((639 - i).astype(np.int32))
    oh = np.zeros((32, 1280), np.float32)
    oh[bk, i] = 1.0
    c["ohr"] = oh
    c["iota_e"] = np.broadcast_to(np.arange(256, dtype=np.float32)[None, :], (128, 256)).copy()
    c["iota_b"] = np.broadcast_to(np.arange(1024, dtype=np.float32)[None, :], (128, 1024)).copy()
    c["iota_p"] = np.arange(128, dtype=np.float32).reshape(128, 1).copy()
    return c


def rope_table(pos):
    t = np.asarray(pos)
    c = {}
    half = 32
    inv = (10000.0 ** (-np.arange(0, half, 2, dtype=np.float32) / half)).astype(np.float32)
    row = (t // 64).astype(np.float32)
    col = (t % 64).astype(np.float32)
    ar = row[:, None] * inv[None, :]
    ac = col[:, None] * inv[None, :]
    return np.stack([np.cos(ar), np.sin(ar), np.cos(ac), np.sin(ac)], axis=1).astype(np.float32)


WEIGHT_NAMES = ["rel_bias", "w_ada", "b_ada", "g_norm1", "w_in", "lambda_q1", "lambda_k1", "lambda_q2",
                "lambda_k2", "g_subln", "g_qnorm", "g_knorm", "w_out", "g_norm2", "w_router",
                "router_bias", "w_exp_gate", "w_exp_up", "w_exp_down", "w_sh_gate", "w_sh_up",
                "w_sh_down", "g_final"]


def build(jobs, dbg=False):
    J = len(jobs)
    T = sum(j_[2] for j_ in jobs)
    NT = T // 128
    NBLK = T * 8 // 128 + NE
    SMAX = max(j[0] for j in jobs)
    nc = bass.Bass("TRN2", target_bir_lowering=False)
    st = ExitStack()
    S = Sched(nc, st)

    def din(name, shape, dt=F32):
        return nc.dram_tensor(name, list(shape), dt, kind="ExternalInput")

    def dscr(name, shape, dt):
        return nc.dram_tensor(name, list(shape), dt, kind="ExternalOutput" if dbg else "Internal")

    xs = [din("xs%d" % j, [jobs[j][0], D]) for j in range(J)]
    cT_d = din("cT", [128, 8, J])
    W = {}
    wshapes = dict(rel_bias=[32, 4], w_ada=[D, 6 * D], b_ada=[6 * D], g_norm1=[D], w_in=[D, DP],
                   lambda_q1=[64], lambda_k1=[64], lambda_q2=[64], lambda_k2=[64], g_subln=[128],
                   g_qnorm=[64], g_knorm=[64], w_out=[D, D], g_norm2=[D], w_router=[D, NE],
                   router_bias=[NE], w_exp_gate=[NE, D, 256], w_exp_up=[NE, D, 256],
                   w_exp_down=[NE, 256, D], w_sh_gate=[D, 256], w_sh_up=[D, 256], w_sh_down=[256, D],
                   g_final=[D])
    for n in WEIGHT_NAMES:
        W[n] = din(n, wshapes[n])
    cshapes = dict(ident=[128, 128], jrev=[128, 128], tri=[128, 128], tril=[128, 128], ohr=[32, 1280],
                   iota_e=[128, 256], iota_b=[128, 1024], iota_p=[128, 1], hfv=[128, 2])
    C = {n: din("c_" + n, s) for n, s in cshapes.items()}
    ROPE = [din("rope%d" % j, [jobs[j][0], 4, 16]) for j in range(J)]
    y_out = nc.dram_tensor("y", [T, D], F32, kind="ExternalOutput")

    QA = [dscr("QA%d" % j, [4, 128, jobs[j][2]], BF16) for j in range(J)]
    KA = [dscr("KA%d" % j, [4, 128, jobs[j][0]], BF16) for j in range(J)]
    VA = [dscr("VA%d" % j, [jobs[j][0], 512], BF16) for j in range(J)]
    QB = [dscr("QB%d" % j, [4, 128, jobs[j][2]], BF16) for j in range(J)]
    KB = [dscr("KB%d" % j, [2, 128, jobs[j][0]], BF16) for j in range(J)]
    VB = [dscr("VB%d" % j, [jobs[j][0], 2, 65], BF16) for j in range(J)]
    OT = dscr("OT", [8, 128, T], BF16)
    X1 = dscr("X1", [T, D], F32)
    H2 = dscr("H2", [T, D], BF16)
    XS = dscr("XSLOT", [NBLK * 128, D], BF16)
    YS = dscr("YSLOT", [NBLK * 128, D], BF16)
    GRD = dscr("GRD", [4, 1280], F32)
    WBGU = nc.dram_tensor("WBGU", [NE * 128, 4096], BF16, kind="Internal")
    WBD = nc.dram_tensor("WBD", [NE * 128, 2048], BF16, kind="Internal")

    sb_bytes = [0]

    stk = [st]

    def sb(name, shape, dt):
        return stk[-1].enter_context(nc.sbuf_tensor(name, list(shape), dt))

    def ps(name, shape, dt=F32):
        return st.enter_context(nc.psum_tensor(name, list(shape), dt))

    MODR = dscr("MODR", [J, 4, D], F32)
    b_MODR = Buf("MODR")
    ident_f = sb("ident_f", [128, 128], F32); b_ident_f = Buf("ident_f")
    ident_b = sb("ident_b", [128, 128], BF16); b_ident_b = Buf("ident_b")
    ones_b = sb("ones_b", [128, 128], BF16); b_ones_b = Buf("ones_b")
    ones_f = sb("ones_f", [128, 128], F32); b_ones_f = Buf("ones_f")
    tri_b = sb("tri_b", [128, 128], BF16); b_tri = Buf("tri_b")
    tril_f = sb("tril_f", [128, 128], F32); b_tril = Buf("tril_f")
    iota_e = sb("iota_e", [128, 256], F32); b_iota_e = Buf("iota_e")
    iota_b = sb("iota_b", [128, 1024], F32); b_iota_b = Buf("iota_b")
    iota_p = sb("iota_p", [128, 1], F32); b_iota_p = Buf("iota_p")
    epsb = sb("epsb", [128, 1], F32); b_epsb = Buf("epsb")
    G1T = sb("G1T", [128, 8, J], F32); b_G1T = Buf("G1T")
    SH1T = sb("SH1T", [128, 8, J], F32); b_SH1T = Buf("SH1T")
    lsm = sb("lsm", [128, 8], F32); b_lsm = Buf("lsm")
    gsub = sb("gsub", [128, 1], F32); b_gsub = Buf("gsub")
    gq = sb("gq", [128, 64], F32); b_gq = Buf("gq")
    gk = sb("gk", [128, 64], F32); b_gk = Buf("gk")
    bcst = sb("bcst", [128, 2, 4], F32); b_bcst = Buf("bcst")

    PSB = [ps("psb%d" % i, [128, 512], F32) for i in range(8)]
    b_PSB = [Buf("psb%d" % i) for i in range(8)]

    def ld(eng, out_ap, in_ap, wbuf, key, reads=(), acc=False, slow=False):
        fn = (lambda e: e.dma_start(out=out_ap, in_=in_ap, allow_slow_non_contiguous=True)) if slow else \
             (lambda e: e.dma_start(out=out_ap, in_=in_ap))
        if acc:
            return S.op(eng, fn, reads=reads, accs=[wbuf], dma_key=key)
        return S.op(eng, fn, reads=reads, writes=[wbuf], dma_key=key)

    ld("sp", ident_f[:], C["ident"].ap(), b_ident_f, "ident_f")
    ld("sp", tril_f[:], C["tril"].ap(), b_tril, "tril_f")
    ld("sp", iota_e[:], C["iota_e"].ap(), b_iota_e, "iota_e")
    ld("sp", iota_b[:], C["iota_b"].ap(), b_iota_b, "iota_b")
    ld("sp", iota_p[:], C["iota_p"].ap(), b_iota_p, "iota_p")
    ld("pool", ident_b[:], C["ident"].ap(), b_ident_b, "ident_b")
    ld("pool", tri_b[:], C["tri"].ap(), b_tri, "tri_b")
    S.op("dve", lambda e: e.memset(ones_b[:], 1.0), writes=[b_ones_b])
    S.op("dve", lambda e: e.memset(ones_f[:], 1.0), writes=[b_ones_f])
    S.op("dve", lambda e: e.memset(epsb[:], EPS), writes=[b_epsb])

    pst = ExitStack()
    stk.append(pst)
    big = sb("big", [128, 4096], F32); b_big = Buf("big")
    cT = sb("cT_sb", [128, 8, J], F32); b_cT = Buf("cT")
    scT = sb("scT", [128, 8, J], F32); b_scT = Buf("scT")
    ld("sp", cT[:], cT_d.ap(), b_cT, "cT")
    S.op("act", lambda e: e.activation(out=scT[:], in_=cT[:], func=AF.Silu), reads=[b_cT], writes=[b_scT])
    badaT = sb("badaT", [128, 16], F32); b_badaT = Buf("badaT")
    g1T = sb("g1T", [128, 8], F32); b_g1T = Buf("g1T")
    ld("sp", badaT[:], W["b_ada"].ap()[0:2048].rearrange("(c p) -> p c", p=128), b_badaT, "badaT", slow=True)
    ld("sp", g1T[:], W["g_norm1"].ap().rearrange("(c p) -> p c", p=128), b_g1T, "g1T", slow=True)
    scbc = sb("scbc", [128, 8, 128], F32); b_scbc = Buf("scbc")
    wada_v = W["w_ada"].ap().rearrange("(kc p) n -> p kc n", p=128)
    wst = big[:, 0:4096].rearrange("p (kc n) -> p kc n", kc=8)
    for cc in range(4):
        ld("sp", wst, wada_v[:, :, cc * 512:(cc + 1) * 512], b_big, "big_ld")
        for sub in range(4):
            col = cc * 4 + sub
            pb = b_PSB[col % 2]
            pt = PSB[col % 2]
            for kc in range(8):
                S.op("pe", lambda e, kc=kc, sub=sub, pt=pt: e.matmul(pt[:, 0:J], lhsT=wst[:, kc, sub * 128:(sub + 1) * 128],
                                                                   rhs=scT[:, kc, :], start=(kc == 0), stop=(kc == 7)),
                     reads=[b_big, b_scT], writes=[pb] if kc == 0 else (), accs=[pb] if kc > 0 else ())
            if col < 8:
                S.op("dve", lambda e, col=col, pt=pt: e.tensor_scalar(out=SH1T[:, col, :], in0=pt[:, 0:J], scalar1=badaT[:, col:col + 1],
                                                                    scalar2=None, op0=ALU.add),
                     reads=[pb, b_badaT], accs=[b_SH1T])
            else:
                c8 = col - 8
                S.op("dve", lambda e, col=col, c8=c8, pt=pt: e.tensor_scalar(out=G1T[:, c8, :], in0=pt[:, 0:J], scalar1=badaT[:, col:col + 1],
                                                                           scalar2=1.0, op0=ALU.add, op1=ALU.add),
                     reads=[pb, b_badaT], accs=[b_G1T])
                S.op("dve", lambda e, c8=c8: e.tensor_scalar(out=G1T[:, c8, :], in0=G1T[:, c8, :], scalar1=g1T[:, c8:c8 + 1],
                                                             scalar2=None, op0=ALU.mult),
                     reads=[b_g1T], accs=[b_G1T])
    brow = sb("brow", [128, D], F32); b_brow = Buf("brow")
    g2row = sb("g2row", [128, D], F32); b_g2row = Buf("g2row")
    mtmp = sb("mtmp", [128, D], F32); b_mtmp = Buf("mtmp")
    ld("sp", g2row[:], bass.AP(W["g_norm2"], 0, [[0, 128], [1, D]]), b_g2row, "g2row")
    vmap = {2: 0, 3: 2, 4: 1, 5: 3}
    for v6 in (2, 3, 4, 5):
        ld("sp", brow[:], bass.AP(W["b_ada"], v6 * D, [[0, 128], [1, D]]), b_brow, "brow")
        for j in range(J):
            for hc in range(2):
                ld("sp", wst, wada_v[:, :, v6 * D + hc * 512: v6 * D + (hc + 1) * 512], b_big, "big_ld")
                S.op("dve", lambda e, j=j: e.tensor_copy(scbc[:], scT[:, :, j:j + 1].to_broadcast([128, 8, 128])),
                     reads=[b_scT], writes=[b_scbc])
                pb = b_PSB[2 + hc]
                pt = PSB[2 + hc]
                for kc in range(8):
                    S.op("pe", lambda e, kc=kc, pt=pt: e.matmul(pt[:, :], lhsT=scbc[:, kc, :], rhs=wst[:, kc, :],
                                                              start=(kc == 0), stop=(kc == 7)),
                         reads=[b_big, b_scbc], writes=[pb] if kc == 0 else (), accs=[pb] if kc > 0 else ())
                sl = slice(hc * 512, (hc + 1) * 512)
                S.op("dve", lambda e, pt=pt, sl=sl: e.tensor_tensor(out=mtmp[:, sl], in0=pt[:, :], in1=brow[:, sl], op=ALU.add),
                     reads=[pb, b_brow], writes=[b_mtmp] if hc == 0 else (), accs=[b_mtmp] if hc == 1 else ())
            if v6 == 4:
                S.op("dve", lambda e: e.scalar_tensor_tensor(out=mtmp[:], in0=mtmp[:], scalar=1.0, in1=g2row[:],
                                                             op0=ALU.add, op1=ALU.mult),
                     reads=[b_g2row], accs=[b_mtmp])
            S.op("sp", lambda e, j=j, v6=v6: e.dma_start(out=MODR.ap()[j, vmap[v6]:vmap[v6] + 1, :], in_=mtmp[0:1, :]),
                 reads=[b_mtmp], accs=[b_MODR], dma_key="mtmp_st")
    lamv = sb("lamv", [128, 4, 64], F32); b_lamv = Buf("lamv")
    for i, n in enumerate(["lambda_q1", "lambda_k1", "lambda_q2", "lambda_k2"]):
        ld("sp", lamv[:, i, :], bass.AP(W[n], 0, [[0, 128], [1, 64]]), b_lamv, "lamv", acc=(i > 0))
    ljunk = sb("ljunk", [128, 64], F32); b_ljunk = Buf("ljunk")
    for i in range(2):
        S.op("dve", lambda e, i=i: e.tensor_tensor(out=ljunk[:], in0=lamv[:, 2 * i, :], in1=lamv[:, 2 * i + 1, :], op=ALU.mult),
             reads=[b_lamv], writes=[b_ljunk])
        S.op("dve", lambda e, i=i: e.tensor_reduce(out=lsm[:, i:i + 1], in_=ljunk[:], axis=AX.X, op=ALU.add),
             reads=[b_ljunk], accs=[b_lsm])
    S.op("act", lambda e: e.activation(out=lsm[:, 2:4], in_=lsm[:, 0:2], func=AF.Exp), reads=[b_lsm], accs=[b_lsm])
    S.op("dve", lambda e: e.tensor_tensor(out=lsm[:, 4:5], in0=lsm[:, 3:4], in1=lsm[:, 2:3], op=ALU.subtract),
         reads=[b_lsm], accs=[b_lsm])
    S.op("dve", lambda e: e.tensor_scalar(out=lsm[:, 5:6], in0=lsm[:, 4:5], scalar1=-0.2, scalar2=None, op0=ALU.add),
         reads=[b_lsm], accs=[b_lsm])
    NLAM = lsm[:, 5:6]
    ld("sp", gsub[:], W["g_subln"].ap().rearrange("(p o) -> p o", o=1), b_gsub, "gsub")
    S.op("dve", lambda e: e.tensor_scalar(out=gsub[:], in0=gsub[:], scalar1=0.8, scalar2=None, op0=ALU.mult),
         reads=[], writes=[b_gsub])
    ld("sp", gq[:], bass.AP(W["g_qnorm"], 0, [[0, 128], [1, 64]]), b_gq, "gq")
    ld("sp", gk[:], bass.AP(W["g_knorm"], 0, [[0, 128], [1, 64]]), b_gk, "gk")
    ld("sp", bcst[:, 0, :], bass.AP(W["rel_bias"], 15 * 4, [[0, 128], [1, 4]]), b_bcst, "bcst")
    ld("sp", bcst[:, 1, :], bass.AP(W["rel_bias"], 31 * 4, [[0, 128], [1, 4]]), b_bcst, "bcst", acc=True)
    rb = sb("rb", [32, 4], F32); b_rb = Buf("rb")
    ohr = sb("ohr", [32, 1280], F32); b_ohr = Buf("ohr")
    ld("sp", rb[:], W["rel_bias"].ap(), b_rb, "rb")
    ld("sp", ohr[:], C["ohr"].ap(), b_ohr, "ohr")
    grs = sb("grs", [4, 1280], F32); b_grs = Buf("grs")
    for i, (a_, n_) in enumerate([(0, 512), (512, 512), (1024, 256)]):
        S.op("pe", lambda e, a_=a_, n_=n_, i=i: e.matmul(PSB[4 + i][0:4, 0:n_], lhsT=rb[:, :], rhs=ohr[:, a_:a_ + n_], start=True, stop=True),
             reads=[b_rb, b_ohr], writes=[b_PSB[4 + i]])
        S.op("dve", lambda e, a_=a_, n_=n_, i=i: e.tensor_copy(grs[:, a_:a_ + n_], PSB[4 + i][0:4, 0:n_]),
             reads=[b_PSB[4 + i]], accs=[b_grs])
    b_GRD = Buf("GRD")
    S.op("sp", lambda e: e.dma_start(out=GRD.ap(), in_=grs[:]), reads=[b_grs], writes=[b_GRD], dma_key="grs_st")
    pst.close()
    stk.pop()
    S.barrier()

    ctx = dict(WBGU=WBGU, WBD=WBD, ROPE=ROPE, nc=nc, S=S, st=st, sb=sb, stk=stk, jobs=jobs, J=J, T=T, NT=NT, NBLK=NBLK, xs=xs, W=W, C=C, y_out=y_out,
               QA=QA, KA=KA, VA=VA, QB=QB, KB=KB, VB=VB, OT=OT, X1=X1, H2=H2, XS=XS, YS=YS, GRD=GRD, MODR=MODR,
               PSB=PSB, b_PSB=b_PSB, ident_f=ident_f, b_ident_f=b_ident_f, ident_b=ident_b, b_ident_b=b_ident_b,
               ones_b=ones_b, b_ones_b=b_ones_b, ones_f=ones_f, b_ones_f=b_ones_f, tri_b=tri_b, b_tri=b_tri,
               tril_f=tril_f, b_tril=b_tril, iota_e=iota_e, b_iota_e=b_iota_e, iota_b=iota_b, b_iota_b=b_iota_b,
               iota_p=iota_p, b_iota_p=b_iota_p, epsb=epsb, b_epsb=b_epsb,
               G1T=G1T, b_G1T=b_G1T, SH1T=SH1T, b_SH1T=b_SH1T,
               NLAM=NLAM, b_lsm=b_lsm, gsub=gsub, b_gsub=b_gsub, gq=gq, b_gq=b_gq, gk=gk, b_gk=b_gk,
               bcst=bcst, b_bcst=b_bcst, ld=ld, dbg=dbg)
    return ctx


def stage_P(cx):
    nc, S, sb, jobs, J = cx["nc"], cx["S"], cx["sb"], cx["jobs"], cx["J"]
    PSB, b_PSB = cx["PSB"], cx["b_PSB"]
    sst = ExitStack(); cx["stk"].append(sst)
    win = sb("win", [128, 8, DP], BF16); b_win = Buf("win")
    for half in range(2):
        cx["ld"]("pool", win[:, :, half * 1152:(half + 1) * 1152],
                 cx["W"]["w_in"].ap().rearrange("(kc p) n -> p kc n", p=128)[:, :, half * 1152:(half + 1) * 1152],
                 b_win, "win", acc=(half == 1))
    xt = [sb("xt%d" % i, [128, D], F32) for i in range(2)]; b_xt = [Buf("xt%d" % i) for i in range(2)]
    sq = sb("sqj", [128, D], BF16); b_sq = Buf("sqj")
    st1 = sb("st1", [128, 8], F32); b_st1 = Buf("st1")
    xn = sb("xn", [128, D], BF16); b_xn = Buf("xn")
    hT = sb("hT", [128, 8, 512], BF16); b_hT = Buf("hT")
    fmst = sb("fmst", [128, 8, 512], BF16); b_fmst = Buf("fmst")
    vast = sb("vast", [128, 4, 512], BF16); b_vast = Buf("vast")
    qbf = sb("qbf", [128, 512], F32); b_qbf = Buf("qbf")
    kvf = sb("kvf", [128, 256], F32); b_kvf = Buf("kvf")
    rt = sb("ropet", [128, 4, 16], F32); b_rt = Buf("ropet")
    tq = [sb("tq%d" % i, [128, 512], F32) for i in range(3)]; b_tq = [Buf("tq%d" % i) for i in range(3)]
    nst = sb("nst", [128, 16], F32); b_nst = Buf("nst")
    qbb = sb("qbb", [128, 512], BF16); b_qbb = Buf("qbb")
    kbb = sb("kbb", [128, 2, 2, 64], BF16); b_kbb = Buf("kbb")
    qbst = sb("qbst", [128, 4, 512], BF16); b_qbst = Buf("qbst")
    kbst = sb("kbst", [128, 2, 512], BF16); b_kbst = Buf("kbst")
    vbst = sb("vbst", [128, 4, 2, 65], BF16); b_vbst = Buf("vbst")
    S.op("dve", lambda e: e.memset(vbst[:], 1.0), writes=[b_vbst])
    b_D = cx["b_D"] = {}
    gq, gk = cx["gq"], cx["gk"]
    for j in range(J):
        Sk, q0, Sq = jobs[j][:3]
        for n in ("QA", "KA", "VA", "QB", "KB", "VB"):
            b_D[(n, j)] = Buf("%s%d" % (n, j))
        G1 = cx["G1T"][:, :, j:j + 1]
        SH1 = cx["SH1T"][:, :, j:j + 1]
        for ch in range(Sk // 512):
            t0 = ch * 512
            own = (t0 >= q0) and (t0 < q0 + Sq)
            for ti in range(4):
                r0 = t0 + ti * 128
                x_ = xt[ti % 2]; bx = b_xt[ti % 2]
                cx["ld"]("sp", x_[:], cx["xs"][j].ap()[r0:r0 + 128, :], bx, "xt%d" % (ti % 2))
                S.op("act", lambda e, x_=x_: e.activation(out=sq[:], in_=x_[:], func=AF.Square, accum_out=st1[:, 0:1]),
                     reads=[bx], writes=[b_sq, b_st1])
                S.op("act", lambda e: e.activation(out=st1[:, 1:2], in_=st1[:, 0:1], func=AF.Sqrt, scale=1.0 / D, bias=cx["epsb"][:, 0:1]),
                     reads=[cx["b_epsb"]], accs=[b_st1])
                S.op("dve", lambda e: e.reciprocal(out=st1[:, 2:3], in_=st1[:, 1:2]), accs=[b_st1])
                S.op("dve", lambda e, x_=x_: e.tensor_scalar(out=xn[:], in0=x_[:], scalar1=st1[:, 2:3], scalar2=None, op0=ALU.mult),
                     reads=[bx, b_st1], writes=[b_xn])
                pT = PSB[0][:, :].bitcast(BF16).rearrange("p (kc t) -> p kc t", kc=8)
                for kc in range(8):
                    S.op("pe", lambda e, kc=kc, pT=pT: e.transpose(out=pT[:, kc, :], in_=xn[:, kc * 128:(kc + 1) * 128], identity=cx["ident_b"][:]),
                         reads=[b_xn, cx["b_ident_b"]], writes=[b_PSB[0]] if kc == 0 else (), accs=[b_PSB[0]] if kc > 0 else ())
                hs = hT[:, :, ti * 128:(ti + 1) * 128]
                S.op("dve", lambda e, hs=hs, pT=pT, G1=G1: e.tensor_tensor(out=hs, in0=pT, in1=G1.to_broadcast([128, 8, 128]), op=ALU.mult),
                     reads=[b_PSB[0], cx["b_G1T"]], writes=[b_hT] if ti == 0 else (), accs=[b_hT] if ti > 0 else ())
                S.op("dve", lambda e, hs=hs, SH1=SH1: e.tensor_tensor(out=hs, in0=hs, in1=SH1.to_broadcast([128, 8, 128]), op=ALU.add),
                     reads=[cx["b_SH1T"]], accs=[b_hT])
            chunks = ([(h, h) for h in range(4)] if own else []) + [(4 + h, 4 + h) for h in range(4)]
            for i, (slot, wc) in enumerate(chunks):
                pb = b_PSB[1 + (i % 2)]; pt = PSB[1 + (i % 2)]
                for kc in range(8):
                    S.op("pe", lambda e, kc=kc, wc=wc, pt=pt: e.matmul(pt[:, :], lhsT=win[:, kc, wc * 128:(wc + 1) * 128], rhs=hT[:, kc, :],
                                                                     start=(kc == 0), stop=(kc == 7)),
                         reads=[b_win, b_hT], writes=[pb] if kc == 0 else (), accs=[pb] if kc > 0 else ())
                S.op("act", lambda e, slot=slot, pt=pt: e.activation(out=fmst[:, slot, :], in_=pt[:, :], func=AF.Identity),
                     reads=[pb], writes=[b_fmst] if i == 0 else (), accs=[b_fmst] if i > 0 else ())
            if own:
                S.op("act", lambda e, j=j, t0=t0, q0=q0: e.dma_start(out=cx["QA"][j].ap()[:, :, t0 - q0:t0 - q0 + 512].rearrange("h p t -> p h t"),
                                                                  in_=fmst[:, 0:4, :]),
                     reads=[b_fmst], accs=[b_D[("QA", j)]], dma_key="fmst_st")
            S.op("act", lambda e, j=j, t0=t0: e.dma_start(out=cx["KA"][j].ap()[:, :, t0:t0 + 512].rearrange("h p t -> p h t"), in_=fmst[:, 4:8, :]),
                 reads=[b_fmst], accs=[b_D[("KA", j)]], dma_key="fmst_st")
            for ti in range(4):
                r0 = t0 + ti * 128
                for kc in range(8):
                    S.op("pe", lambda e, kc=kc, ti=ti: e.matmul(PSB[3][:, :], lhsT=hT[:, kc, ti * 128:(ti + 1) * 128], rhs=win[:, kc, 1024:1536],
                                                               start=(kc == 0), stop=(kc == 7)),
                         reads=[b_win, b_hT], writes=[b_PSB[3]] if kc == 0 else (), accs=[b_PSB[3]] if kc > 0 else ())
                S.op("act", lambda e, ti=ti: e.activation(out=vast[:, ti, :], in_=PSB[3][:, :], func=AF.Identity),
                     reads=[b_PSB[3]], writes=[b_vast] if ti == 0 else (), accs=[b_vast] if ti > 0 else ())
                for kc in range(8):
                    S.op("pe", lambda e, kc=kc, ti=ti: e.matmul(PSB[4][:, 0:256], lhsT=hT[:, kc, ti * 128:(ti + 1) * 128], rhs=win[:, kc, 2048:2304],
                                                               start=(kc == 0), stop=(kc == 7)),
                         reads=[b_win, b_hT], writes=[b_PSB[4]] if kc == 0 else (), accs=[b_PSB[4]] if kc > 0 else ())
                S.op("dve", lambda e: e.tensor_copy(kvf[:], PSB[4][:, 0:256]), reads=[b_PSB[4]], writes=[b_kvf])
                S.op("dve", lambda e, ti=ti: e.tensor_copy(vbst[:, ti, :, 0:64], kvf[:, 128:256].rearrange("p (h d) -> p h d", h=2)),
                     reads=[b_kvf], accs=[b_vbst])
                cx["ld"]("sp", rt[:], cx["ROPE"][j].ap()[r0:r0 + 128, :, :], b_rt, "ropet")

                def norm_rope(src, H, gtile, b_g, dst_ap, b_dst, b_src):
                    HW = H * 64
                    s3 = src[:, 0:HW].rearrange("p (h d) -> p h d", h=H)
                    a3 = tq[0][:, 0:HW].rearrange("p (h d) -> p h d", h=H)
                    S.op("dve", lambda e: e.tensor_tensor(out=tq[0][:, 0:HW], in0=src[:, 0:HW], in1=src[:, 0:HW], op=ALU.mult),
                         reads=[b_src], writes=[b_tq[0]])
                    S.op("dve", lambda e: e.tensor_reduce(out=nst[:, 0:H], in_=a3, axis=AX.X, op=ALU.add),
                         reads=[b_tq[0]], writes=[b_nst])
                    S.op("act", lambda e: e.activation(out=nst[:, 8:8 + H], in_=nst[:, 0:H], func=AF.Sqrt, scale=1.0 / 64, bias=cx["epsb"][:, 0:1]),
                         reads=[cx["b_epsb"]], accs=[b_nst])
                    S.op("dve", lambda e: e.reciprocal(out=nst[:, 0:H], in_=nst[:, 8:8 + H]), accs=[b_nst])
                    S.op("dve", lambda e: e.tensor_tensor(out=a3, in0=s3, in1=nst[:, 0:H].unsqueeze(2).to_broadcast([128, H, 64]), op=ALU.mult),
                         reads=[b_src, b_nst], writes=[b_tq[0]])
                    S.op("dve", lambda e: e.tensor_tensor(out=a3, in0=a3, in1=gtile[:, :].unsqueeze(1).to_broadcast([128, H, 64]), op=ALU.mult),
                         reads=[b_g], accs=[b_tq[0]])
                    xv = tq[0][:, 0:HW].rearrange("p (h f two d) -> p h f two d", h=H, f=2, two=2)
                    t1 = tq[1][:, 0:HW // 2].rearrange("p (h f d) -> p h f d", h=H, f=2)
                    t2 = tq[2][:, 0:HW // 2].rearrange("p (h f d) -> p h f d", h=H, f=2)
                    cosb = rt[:, 0:4:2, :].unsqueeze(1).to_broadcast([128, H, 2, 16])
                    sinb = rt[:, 1:4:2, :].unsqueeze(1).to_broadcast([128, H, 2, 16])
                    dv = dst_ap.rearrange("p h (f two d) -> p h f two d", f=2, two=2)
                    x1 = xv[:, :, :, 0, :]; x2 = xv[:, :, :, 1, :]
                    S.op("dve", lambda e: e.tensor_tensor(out=t1, in0=x1, in1=cosb, op=ALU.mult), reads=[b_tq[0], b_rt], writes=[b_tq[1]])
                    S.op("dve", lambda e: e.tensor_tensor(out=t2, in0=x2, in1=sinb, op=ALU.mult), reads=[b_tq[0], b_rt], writes=[b_tq[2]])
                    S.op("dve", lambda e: e.tensor_tensor(out=dv[:, :, :, 0, :], in0=t1, in1=t2, op=ALU.subtract),
                         reads=[b_tq[1], b_tq[2]], accs=[b_dst])
                    S.op("dve", lambda e: e.tensor_tensor(out=t1, in0=x1, in1=sinb, op=ALU.mult), reads=[b_tq[0], b_rt], writes=[b_tq[1]])
                    S.op("dve", lambda e: e.tensor_tensor(out=t2, in0=x2, in1=cosb, op=ALU.mult), reads=[b_tq[0], b_rt], writes=[b_tq[2]])
                    S.op("dve", lambda e: e.tensor_tensor(out=dv[:, :, :, 1, :], in0=t1, in1=t2, op=ALU.add),
                         reads=[b_tq[1], b_tq[2]], accs=[b_dst])

                norm_rope(kvf, 2, gk, cx["b_gk"], kbb[:, :, 0, :], b_kbb, b_kvf)
                S.op("dve", lambda e: e.tensor_copy(kbb[:, :, 1, :], kbb[:, :, 0, :]), accs=[b_kbb])
                pK = PSB[5][:, :].bitcast(BF16)[:, 0:256].rearrange("p (h t) -> p h t", h=2)
                for h in range(2):
                    S.op("pe", lambda e, h=h, pK=pK: e.transpose(out=pK[:, h, :], in_=kbb[:, h, :, :].rearrange("p a d -> p (a d)"), identity=cx["ident_b"][:]),
                         reads=[b_kbb, cx["b_ident_b"]], writes=[b_PSB[5]] if h == 0 else (), accs=[b_PSB[5]] if h > 0 else ())
                S.op("dve", lambda e, ti=ti, pK=pK: e.tensor_copy(kbst[:, :, ti * 128:(ti + 1) * 128], pK), reads=[b_PSB[5]],
                     writes=[b_kbst] if ti == 0 else (), accs=[b_kbst] if ti > 0 else ())
                if own:
                    for kc in range(8):
                        S.op("pe", lambda e, kc=kc, ti=ti: e.matmul(PSB[6][:, :], lhsT=hT[:, kc, ti * 128:(ti + 1) * 128], rhs=win[:, kc, 1536:2048],
                                                                   start=(kc == 0), stop=(kc == 7)),
                             reads=[b_win, b_hT], writes=[b_PSB[6]] if kc == 0 else (), accs=[b_PSB[6]] if kc > 0 else ())
                    S.op("dve", lambda e: e.tensor_copy(qbf[:], PSB[6][:, :]), reads=[b_PSB[6]], writes=[b_qbf])
                    norm_rope(qbf, 8, gq, cx["b_gq"], qbb[:, :].rearrange("p (h d) -> p h d", h=8), b_qbb, b_qbf)
                    pQ = PSB[7][:, :].bitcast(BF16)[:, 0:512].rearrange("p (c t) -> p c t", c=4)
                    for c in range(4):
                        S.op("pe", lambda e, c=c, pQ=pQ: e.transpose(out=pQ[:, c, :], in_=qbb[:, c * 128:(c + 1) * 128], identity=cx["ident_b"][:]),
                             reads=[b_qbb, cx["b_ident_b"]], writes=[b_PSB[7]] if c == 0 else (), accs=[b_PSB[7]] if c > 0 else ())
                    S.op("dve", lambda e, ti=ti, pQ=pQ: e.tensor_copy(qbst[:, :, ti * 128:(ti + 1) * 128], pQ), reads=[b_PSB[7]],
                         writes=[b_qbst] if ti == 0 else (), accs=[b_qbst] if ti > 0 else ())
            S.op("act", lambda e, j=j, t0=t0: e.dma_start(out=cx["VA"][j].ap()[t0:t0 + 512, :].rearrange("(a p) n -> p a n", p=128), in_=vast[:]),
                 reads=[b_vast], accs=[b_D[("VA", j)]], dma_key="vast_st")
            S.op("act", lambda e, j=j, t0=t0: e.dma_start(out=cx["VB"][j].ap()[t0:t0 + 512, :, :].rearrange("(a p) h d -> p a h d", p=128), in_=vbst[:]),
                 reads=[b_vbst], accs=[b_D[("VB", j)]], dma_key="vbst_st")
            S.op("act", lambda e, j=j, t0=t0: e.dma_start(out=cx["KB"][j].ap()[:, :, t0:t0 + 512].rearrange("h p t -> p h t"), in_=kbst[:]),
                 reads=[b_kbst], accs=[b_D[("KB", j)]], dma_key="kbst_st")
            if own:
                S.op("act", lambda e, j=j, t0=t0, q0=q0: e.dma_start(out=cx["QB"][j].ap()[:, :, t0 - q0:t0 - q0 + 512].rearrange("c p t -> p c t"), in_=qbst[:]),
                     reads=[b_qbst], accs=[b_D[("QB", j)]], dma_key="qbst_st")
    sst.close(); cx["stk"].pop()
    S.barrier()


def stage_T(cx):
    nc, S, sb, jobs, J = cx["nc"], cx["S"], cx["sb"], cx["jobs"], cx["J"]
    PSB, b_PSB = cx["PSB"], cx["b_PSB"]
    b_D = cx["b_D"]
    ld = cx["ld"]
    sst = ExitStack(); cx["stk"].append(sst)
    SKM = max(j_[0] for j_ in jobs); SQM = max(j_[2] for j_ in jobs)
    jrev = sb("jrev", [128, 128], F32); b_jrev = Buf("jrev")
    ld("sp", jrev[:], cx["C"]["jrev"].ap(), b_jrev, "jrev")
    hk = sb("hk", [128, 1152], F32); b_hk = Buf("hk")
    STR = sb("STR", [128, 4, 1152], F32); b_STR = Buf("STR")
    for h in range(4):
        ld("sp", hk[:], bass.AP(cx["GRD"], h * 1280, [[1, 128], [1, 1152]]), b_hk, "hk")
        for i, (a_, n_) in enumerate([(0, 512), (512, 512), (1024, 128)]):
            S.op("pe", lambda e, a_=a_, n_=n_, i=i: e.matmul(PSB[i][:, 0:n_], lhsT=jrev[:, :], rhs=hk[:, a_:a_ + n_], start=True, stop=True),
                 reads=[b_jrev, b_hk], writes=[b_PSB[i]])
            S.op("dve", lambda e, a_=a_, n_=n_, i=i, h=h: e.tensor_copy(STR[:, h, a_:a_ + n_], PSB[i][:, 0:n_]),
                 reads=[b_PSB[i]], accs=[b_STR])
    W = cx["W"]
    b_WB = cx["b_WB"] = Buf("WB")
    srcs = (W["w_exp_gate"].ap().rearrange("e (p kc) f -> (e p) (kc f)", kc=8),
            W["w_exp_up"].ap().rearrange("e (p kc) f -> (e p) (kc f)", kc=8),
            W["w_exp_down"].ap().rearrange("e (p fc) d -> (e p) (fc d)", fc=2))
    RCH = 512
    cast_jobs = []
    for r0_ in range(0, NE * 128, RCH):
        for wi in range(3):
            cast_jobs.append((r0_, wi))

    def issue_casts(n, gate_buf):
        for _ in range(n):
            if not cast_jobs:
                return
            r0_, wi = cast_jobs.pop(0)
            S.op("pool", lambda e, r0_=r0_, wi=wi: e.dma_start(out=(cx["WBGU"].ap()[r0_:r0_ + RCH, wi * 2048:(wi + 1) * 2048] if wi < 2 else cx["WBD"].ap()[r0_:r0_ + RCH, :]), in_=srcs[wi][r0_:r0_ + RCH, :]),
                 reads=[gate_buf], accs=[b_WB], dma_key="wb_cast")
    NB2 = 2
    kT = [sb("kT%d" % i, [128, SKM], BF16) for i in range(NB2)]; b_kT = [Buf("kT%d" % i) for i in range(NB2)]
    vh = [sb("vh%d" % i, [128, SKM // 128, 128], BF16) for i in range(NB2)]; b_vh = [Buf("vh%d" % i) for i in range(NB2)]
    qT = [sb("qT%d" % i, [128, SQM], BF16) for i in range(NB2)]; b_qT = [Buf("qT%d" % i) for i in range(NB2)]
    NPT = 6
    pT = [sb("pT%d" % i, [128, 512], BF16) for i in range(NPT)]; b_pT = [Buf("pT%d" % i) for i in range(NPT)]
    sbi = [sb("sbi%d" % i, [128, 512], F32) for i in range(2)]; b_sbi = [Buf("sbi%d" % i) for i in range(2)]
    ft = [sb("ft%d" % i, [128, 512], F32) for i in range(5)]; b_ft = [Buf("ft%d" % i) for i in range(5)]
    sqb = sb("sqb", [128, 512], BF16); b_sqb = Buf("sqb")
    oTs = [sb("oTs%d" % i, [128, 512], BF16) for i in range(2)]; b_oTs = [Buf("oTs%d" % i) for i in range(2)]
    b_OT = cx["b_OT"] = Buf("OT")
    cnt = dict(pt=0, sbi=0, hd=0, ots=0, fin=0)
    tok0 = 0
    dacc = [[sb("dacc%d_%d" % (i, m), [128, 512], F32) for m in range(2)] for i in range(2)]
    b_dacc = [[Buf("dacc%d_%d" % (i, m)) for m in range(2)] for i in range(2)]
    dacc_pend = []
    pending = []

    def run_pending(kb, force=False):
        if not pending:
            return
        ph = pending[0]
        trig = (1, 2, 8)
        while ph and (force or kb >= trig[3 - len(ph)]):
            ph.pop(0)()
        if not ph:
            pending.pop(0)

    hfv = sb("hfv", [128, 2], F32); b_hfv = Buf("hfv")
    ld("sp", hfv[:], cx["C"]["hfv"].ap(), b_hfv, "hfv")
    BC2 = sb("BC2", [128, 8], F32); b_BC2 = Buf("BC2")
    SP1 = sb("SP1", [128, 4, 512], F32); b_SP1 = Buf("SP1")
    SP2 = sb("SP2", [128, 4, 512], F32); b_SP2 = Buf("SP2")
    bc = cx["bcst"]
    hb0 = sb("hb0", [128, 8], F32); b_hb0 = Buf("hb0")
    S.op("dve", lambda e: e.tensor_scalar(out=hb0[:, 0:4], in0=bc[:, 0, :], scalar1=hfv[:, 0:1], scalar2=None, op0=ALU.mult),
         reads=[cx["b_bcst"], b_hfv], writes=[b_hb0])
    S.op("dve", lambda e: e.tensor_scalar(out=hb0[:, 4:8], in0=bc[:, 1, :], scalar1=hfv[:, 1:2], scalar2=None, op0=ALU.mult),
         reads=[cx["b_bcst"], b_hfv], accs=[b_hb0])
    S.op("dve", lambda e: e.tensor_tensor(out=BC2[:, 0:4], in0=hb0[:, 0:4], in1=hb0[:, 4:8], op=ALU.add), reads=[b_hb0], writes=[b_BC2])
    for h in range(4):
        S.op("dve", lambda e, h=h: e.tensor_scalar(out=SP1[:, h, :], in0=STR[:, h, 0:512], scalar1=hfv[:, 1:2], scalar2=hb0[:, h:h + 1], op0=ALU.mult, op1=ALU.add),
             reads=[b_STR, b_hfv, b_hb0], accs=[b_SP1])
        S.op("dve", lambda e, h=h: e.tensor_scalar(out=SP2[:, h, :], in0=STR[:, h, 640:1152], scalar1=hfv[:, 0:1], scalar2=hb0[:, 4 + h:5 + h], op0=ALU.mult, op1=ALU.add),
             reads=[b_STR, b_hfv, b_hb0], accs=[b_SP2])
    for j in range(J):
        Sk, q0, Sq = jobs[j][:3]
        split = len(jobs[j]) > 3 and jobs[j][3]
        NKB = Sk // 128
        NKH = Sq // 128
        NQC = Sq // 512
        for h in range(4):
            hb = cnt["hd"] % NB2; cnt["hd"] += 1
            ld("sp", kT[hb][:, 0:Sk], cx["KA"][j].ap()[h, :, :], b_kT[hb], "kT%d" % hb, reads=[b_D[("KA", j)]])
            ld("sp", qT[hb][:, 0:Sq], cx["QA"][j].ap()[h, :, :], b_qT[hb], "qT%d" % hb, reads=[b_D[("QA", j)]])
            ld("sp", vh[hb][:, 0:NKB, :], cx["VA"][j].ap()[:, h * 128:(h + 1) * 128].rearrange("(a p) n -> p a n", p=128),
               b_vh[hb], "vh%d" % hb, reads=[b_D[("VA", j)]])
            for qc in range(Sq // 512):
                qa = q0 + qc * 512
                fi = cnt["fin"] % 2; cnt["fin"] += 1
                ob = (4 + 2 * fi, 5 + 2 * fi)
                if split or J == 1:
                    issue_casts(3, b_pT[cnt["pt"] % NPT])

                def issue_S(kb, hb=hb, qc=qc):
                    for m in range(2):
                        bank = (kb % 2) * 2 + m
                        S.op("pe", lambda e, kb=kb, m=m, bank=bank: e.matmul(PSB[bank][:, :], lhsT=kT[hb][m * 64:(m + 1) * 64, kb * 128:(kb + 1) * 128],
                                                                          rhs=qT[hb][m * 64:(m + 1) * 64, qc * 512:(qc + 1) * 512], start=True, stop=True),
                             reads=[b_kT[hb], b_qT[hb]], writes=[b_PSB[bank]])
                issue_S(0)
                for kb in range(NKB):
                    if kb + 1 < NKB:
                        issue_S(kb + 1)
                    run_pending(kb)
                    d = kb * 128 - qa
                    mode = "std"
                    if split and kb >= NKH:
                        if qc == NQC - 1 and kb == NKH:
                            mode = "sp1"
                        elif qc == 0 and kb == NKB - 1:
                            mode = "sp2"
                        else:
                            mode = "far2"
                    first = (kb == 0); last = (kb == NKB - 1)
                    pis = []
                    for m in range(2):
                        bank = (kb % 2) * 2 + m
                        pi = cnt["pt"] % NPT; cnt["pt"] += 1
                        pis.append(pi)
                        if mode in ("sp1", "sp2") or (mode == "std" and -128 <= d <= 512):
                            if mode == "std":
                                bt = STR[:, h, 512 - d:1024 - d]; bb = b_STR
                            else:
                                bt = (SP1 if mode == "sp1" else SP2)[:, h, :]; bb = b_SP1 if mode == "sp1" else b_SP2
                            si = cnt["sbi"] % 2; cnt["sbi"] += 1
                            S.op("dve", lambda e, bank=bank, si=si, bt=bt: e.scalar_tensor_tensor(
                                out=sbi[si][:], in0=PSB[bank][:, :], scalar=0.125, in1=bt, op0=ALU.mult, op1=ALU.add),
                                reads=[b_PSB[bank], bb], writes=[b_sbi[si]])
                            S.op("act", lambda e, si=si, pi=pi: e.activation(out=pT[pi][:], in_=sbi[si][:], func=AF.Exp),
                                 reads=[b_sbi[si]], writes=[b_pT[pi]])
                        else:
                            if mode == "far2":
                                bias_ap = BC2[:, h:h + 1]; bbuf = b_BC2
                            else:
                                sg_ = 1 if d > 0 else 0
                                bias_ap = cx["bcst"][:, sg_, h:h + 1]; bbuf = cx["b_bcst"]
                            S.op("act", lambda e, bank=bank, pi=pi, bias_ap=bias_ap: e.activation(out=pT[pi][:], in_=PSB[bank][:, :], func=AF.Exp,
                                                                                               scale=0.125, bias=bias_ap),
                                 reads=[b_PSB[bank], bbuf], writes=[b_pT[pi]])
                    for m in range(2):
                        pi = pis[m]
                        S.op("pe", lambda e, kb=kb, m=m, pi=pi, first=first, last=last, hb=hb, obk=ob[m]: e.matmul(PSB[obk][:, :], lhsT=vh[hb][:, kb, :], rhs=pT[pi][:],
                                                                                                          start=first, stop=last),
                             reads=[b_vh[hb], b_pT[pi]], writes=[b_PSB[ob[m]]] if first else (), accs=[b_PSB[ob[m]]] if not first else ())
                    for fnd in dacc_pend:
                        fnd()
                    dacc_pend.clear()
                    for m in range(2):
                        pi = pis[m]
                        deng = "dve" if m == 0 else "pool"
                        if first:
                            dacc_pend.append(lambda pi=pi, fi=fi, m=m, deng=deng: S.op(
                                deng, lambda e: e.tensor_copy(dacc[fi][m][:], pT[pi][:]), reads=[b_pT[pi]], writes=[b_dacc[fi][m]]))
                        else:
                            dacc_pend.append(lambda pi=pi, fi=fi, m=m, deng=deng: S.op(
                                deng, lambda e: e.tensor_tensor(out=dacc[fi][m][:], in0=dacc[fi][m][:], in1=pT[pi][:], op=ALU.add),
                                reads=[b_pT[pi]], accs=[b_dacc[fi][m]]))
                for fnd in dacc_pend:
                    fnd()
                dacc_pend.clear()
                if pending:
                    run_pending(0, force=True)
                oi = cnt["ots"] % 2; cnt["ots"] += 1
                c0 = tok0 + qc * 512

                def phA(ob=ob, fi=fi):
                    for m in range(2):
                        if m == 0:
                            S.op("act", lambda e, m=m: e.activation(out=ft[m][:], in_=PSB[ob[m]][:, :], func=AF.Identity), reads=[b_PSB[ob[m]]], writes=[b_ft[m]])
                        else:
                            S.op("dve", lambda e, m=m: e.tensor_copy(ft[m][:], PSB[ob[m]][:, :]), reads=[b_PSB[ob[m]]], writes=[b_ft[m]])
                        S.op("pe", lambda e, m=m: e.matmul(PSB[ob[m]][:, :], lhsT=cx["ones_f"][:, :], rhs=dacc[fi][m][:], start=True, stop=True),
                             reads=[cx["b_ones_f"], b_dacc[fi][m]], writes=[b_PSB[ob[m]]])

                def phB1(ob=ob):
                    for m in range(2):
                        S.op("dve", lambda e, m=m: e.reciprocal(out=ft[3][:], in_=PSB[ob[m]][:, :]), reads=[b_PSB[ob[m]]], writes=[b_ft[3]])
                        S.op("dve", lambda e, m=m: e.tensor_tensor(out=ft[m][:], in0=ft[m][:], in1=ft[3][:], op=ALU.mult), reads=[b_ft[3]], accs=[b_ft[m]])
                    S.op("dve", lambda e: e.scalar_tensor_tensor(out=ft[2][:], in0=ft[1][:], scalar=cx["NLAM"], in1=ft[0][:], op0=ALU.mult, op1=ALU.add),
                         reads=[b_ft[0], b_ft[1], cx["b_lsm"]], writes=[b_ft[2]])
                    S.op("dve", lambda e: e.tensor_tensor(out=sqb[:], in0=ft[2][:], in1=ft[2][:], op=ALU.mult), reads=[b_ft[2]], writes=[b_sqb])
                    S.op("pe", lambda e: e.matmul(PSB[ob[0]][:, :], lhsT=cx["ones_b"][:, :], rhs=sqb[:], start=True, stop=True),
                         reads=[cx["b_ones_b"], b_sqb], writes=[b_PSB[ob[0]]])

                def phB2(ob=ob, oi=oi, h=h, c0=c0):
                    S.op("act", lambda e: e.activation(out=ft[3][:], in_=PSB[ob[0]][:, :], func=AF.Sqrt, scale=1.0 / 128, bias=cx["epsb"][:, 0:1]),
                         reads=[b_PSB[ob[0]], cx["b_epsb"]], writes=[b_ft[3]])
                    S.op("dve", lambda e: e.reciprocal(out=ft[4][:], in_=ft[3][:]), reads=[b_ft[3]], writes=[b_ft[4]])
                    S.op("dve", lambda e: e.scalar_tensor_tensor(out=oTs[oi][:], in0=ft[2][:], scalar=cx["gsub"][:, 0:1], in1=ft[4][:],
                                                                 op0=ALU.mult, op1=ALU.mult),
                         reads=[b_ft[2], b_ft[4], cx["b_gsub"]], writes=[b_oTs[oi]])
                    S.op("act", lambda e: e.dma_start(out=cx["OT"].ap()[h, :, c0:c0 + 512], in_=oTs[oi][:]),
                         reads=[b_oTs[oi]], accs=[b_OT], dma_key="oTs%d_st" % oi)
                pending.append([phA, phB1, phB2])
        while pending:
            run_pending(0, force=True)
        for n in range(2):
            hb = cnt["hd"] % NB2; cnt["hd"] += 1
            ld("sp", kT[hb][:, 0:Sk], cx["KB"][j].ap()[n, :, :], b_kT[hb], "kT%d" % hb, reads=[b_D[("KB", j)]])
            vb1 = vh[hb][:, :, :].rearrange("p a n -> p (a n)")[:, 0:NKB * 65].rearrange("p (a n) -> p a n", n=65)
            ld("sp", vb1, cx["VB"][j].ap()[:, n, :].rearrange("(a p) n -> p a n", p=128), b_vh[hb], "vh%d" % hb, reads=[b_D[("VB", j)]])
            for cpair in range(2):
                c = 2 * n + cpair
                qb_ = cnt["hd"] % NB2 if False else (hb + cpair) % NB2
                ld("sp", qT[qb_][:, 0:Sq], cx["QB"][j].ap()[c, :, :], b_qT[qb_], "qT%d" % qb_, reads=[b_D[("QB", j)]])
                for qc in range(Sq // 512):
                    def issue_SB(kb, hb=hb, qb_=qb_, qc=qc):
                        for hh in range(2):
                            bank = (kb % 2) * 2 + hh
                            S.op("pe", lambda e, kb=kb, hh=hh, bank=bank: e.matmul(PSB[bank][:, :], lhsT=kT[hb][hh * 64:(hh + 1) * 64, kb * 128:(kb + 1) * 128],
                                                                                rhs=qT[qb_][hh * 64:(hh + 1) * 64, qc * 512:(qc + 1) * 512], start=True, stop=True),
                                 reads=[b_kT[hb], b_qT[qb_]], writes=[b_PSB[bank]])
                    if split or J == 1:
                        issue_casts(3, b_pT[cnt["pt"] % NPT])
                    issue_SB(0)
                    for kb in range(NKB):
                        if kb + 1 < NKB:
                            issue_SB(kb + 1)
                        first = (kb == 0); last = (kb == NKB - 1)
                        for hh in range(2):
                            bank = (kb % 2) * 2 + hh
                            pi = cnt["pt"] % NPT; cnt["pt"] += 1
                            S.op("act", lambda e, bank=bank, pi=pi: e.activation(out=pT[pi][:], in_=PSB[bank][:, :], func=AF.Exp, scale=0.125),
                                 reads=[b_PSB[bank]], writes=[b_pT[pi]])
                            S.op("pe", lambda e, kb=kb, hh=hh, pi=pi, first=first, last=last, vb1=vb1: e.matmul(PSB[4 + hh][0:65, :], lhsT=vb1[:, kb, :], rhs=pT[pi][:],
                                                                                                             start=first, stop=last),
                                 reads=[b_vh[hb], b_pT[pi]], writes=[b_PSB[4 + hh]] if first else (), accs=[b_PSB[4 + hh]] if not first else ())
                    for hh in range(2):
                        S.op("dve", lambda e, hh=hh: e.reciprocal(out=ft[hh][64:65, :], in_=PSB[4 + hh][64:65, :]),
                             reads=[b_PSB[4 + hh]], writes=[b_ft[hh]])
                        S.op("pe", lambda e, hh=hh: e.matmul(PSB[6 + hh][0:64, :], lhsT=cx["ones_f"][64:65, 0:64], rhs=ft[hh][64:65, :], start=True, stop=True),
                             reads=[cx["b_ones_f"], b_ft[hh]], writes=[b_PSB[6 + hh]])
                        S.op("act", lambda e, hh=hh: e.activation(out=ft[2 + hh][0:64, :], in_=PSB[6 + hh][0:64, :], func=AF.Identity),
                             reads=[b_PSB[6 + hh]], writes=[b_ft[2 + hh]])
                        oi = cnt["ots"] % 2; cnt["ots"] += 1
                        S.op("dve", lambda e, hh=hh, oi=oi: e.tensor_tensor(out=oTs[oi][0:64, :], in0=PSB[4 + hh][0:64, :], in1=ft[2 + hh][0:64, :], op=ALU.mult),
                             reads=[b_PSB[4 + hh], b_ft[2 + hh]], writes=[b_oTs[oi]])
                        c0 = tok0 + qc * 512
                        S.op("act", lambda e, oi=oi, c=c, hh=hh, c0=c0: e.dma_start(out=cx["OT"].ap()[4 + c, hh * 64:(hh + 1) * 64, c0:c0 + 512], in_=oTs[oi][0:64, :]),
                             reads=[b_oTs[oi]], accs=[b_OT], dma_key="oTs%d_st" % oi)
        tok0 += Sq
    issue_casts(10 ** 6, b_pT[0])
    sst.close(); cx["stk"].pop()
    S.barrier()


def stage_O(cx):
    nc, S, sb, jobs, J = cx["nc"], cx["S"], cx["sb"], cx["jobs"], cx["J"]
    PSB, b_PSB = cx["PSB"], cx["b_PSB"]
    ld = cx["ld"]; W = cx["W"]
    NT, NBLK = cx["NT"], cx["NBLK"]
    st = cx["st"]; cx["stk"].append(st)
    E8 = sb("E8", [128, NT, 8], F32); b_E8 = Buf("E8")
    R8 = sb("R8", [128, NT, 8], F32); b_R8 = Buf("R8")
    G8 = sb("G8", [128, NT, 8], F32); b_G8 = Buf("G8")
    SL8 = sb("SL8", [128, NT, 8], I32); b_SL8 = Buf("SL8")
    IDXW = sb("IDXW", [128, NBLK], I32); b_IDXW = Buf("IDXW")
    IDXD = sb("IDXD", [128, NBLK], I32); b_IDXD = Buf("IDXD")
    IDXD2 = sb("IDXD2", [128, NBLK], I32); b_IDXD2 = Buf("IDXD2")
    cx["IDXD2"] = IDXD2; cx["b_IDXD2"] = b_IDXD2
    cx["stk"].pop()
    cx.update(E8=E8, b_E8=b_E8, R8=R8, b_R8=b_R8, G8=G8, b_G8=b_G8, SL8=SL8, b_SL8=b_SL8,
              IDXW=IDXW, b_IDXW=b_IDXW, IDXD=IDXD, b_IDXD=b_IDXD)
    sst = ExitStack(); cx["stk"].append(sst)
    rd = lambda n: W[n].ap().rearrange("(kc p) n -> p kc n", p=128)
    wout = sb("wout", [128, 8, D], BF16); b_wout = Buf("wout")
    wr = sb("wr", [128, 8, NE], BF16); b_wr = Buf("wr")
    wsgu = sb("wsgu", [128, 8, 512], BF16); b_wsgu = Buf("wsgu")
    wsd = sb("wsd", [128, 2, D], BF16); b_wsd = Buf("wsd")
    ld("pool", wout[:], rd("w_out"), b_wout, "wout")
    ld("pool", wr[:], rd("w_router"), b_wr, "wr")
    ld("pool", wsgu[:, :, 0:256], rd("w_sh_gate"), b_wsgu, "wsgu")
    ld("pool", wsgu[:, :, 256:512], rd("w_sh_up"), b_wsgu, "wsgu", acc=True)
    ld("pool", wsd[:], W["w_sh_down"].ap().rearrange("(fc p) n -> p fc n", p=128), b_wsd, "wsd")
    rbias = sb("rbias", [128, NE], F32); b_rbias = Buf("rbias")
    ld("sp", rbias[:], bass.AP(W["router_bias"], 0, [[0, 128], [1, NE]]), b_rbias, "rbias")
    E1 = sb("E1", [128, NE], F32); b_E1 = Buf("E1")
    S.op("dve", lambda e: e.tensor_scalar(out=E1[:], in0=cx["iota_e"][:], scalar1=16384.0, scalar2=1.0, op0=ALU.mult, op1=ALU.add),
         reads=[cx["b_iota_e"]], writes=[b_E1])
    Mcum = sb("Mcum", [128, NE], BF16); b_Mcum = Buf("Mcum")
    S.op("dve", lambda e: e.memset(Mcum[:], 0.0), writes=[b_Mcum])
    MOD = [sb("mod%d" % v, [128, D], F32) for v in range(4)]; b_MOD = [Buf("mod%d" % v) for v in range(4)]
    oT = sb("oT", [128, 8, 512], BF16); b_oT = Buf("oT")
    xt = [sb("xo%d" % i, [128, D], F32) for i in range(2)]; b_xt = [Buf("xo%d" % i) for i in range(2)]
    sqj = sb("sqo", [128, D], BF16); b_sqj = Buf("sqo")
    st1 = sb("sto", [128, 8], F32); b_st1 = Buf("sto")
    tmp = sb("tmpo", [128, D], F32); b_tmp = Buf("tmpo")
    h2b = [sb("h2b%d" % i, [128, D], BF16) for i in range(2)]; b_h2b = [Buf("h2b%d" % i) for i in range(2)]
    h2T = sb("h2T", [128, 8, 128], BF16); b_h2T = Buf("h2T")
    sgt = sb("sgt", [128, 256], F32); b_sgt = Buf("sgt")
    ab = sb("ab", [128, 256], BF16); b_ab = Buf("ab")
    aT = sb("aT", [128, 2, 128], BF16); b_aT = Buf("aT")
    scr = sb("scr", [128, NE], F32); b_scr = Buf("scr")
    sel = sb("sel", [128, NE], F32); b_sel = Buf("sel")
    selm = sb("selm", [128, NE], F32); b_selm = Buf("selm")
    g8 = sb("g8", [128, 8, 8], F32); b_g8 = Buf("g8")
    gs = sb("gs", [128, 32], F32); b_gs = Buf("gs")
    Mb = sb("Mb", [128, NE], BF16); b_Mb = Buf("Mb")
    wg = sb("wg", [128, NE], F32); b_wg = Buf("wg")
    pvm = sb("pvm", [128, NE], F32); b_pvm = Buf("pvm")
    p8 = sb("p8", [128, 8], F32); b_p8 = Buf("p8")
    p8i = sb("p8i", [128, 16], I32); b_p8i = Buf("p8i")
    junk = sb("junko", [128, NE], F32); b_junk = Buf("junko")
    b_X1 = cx["b_X1"] = Buf("X1")
    b_H2 = cx["b_H2"] = Buf("H2")
    b_MODR = Buf("MODRr")
    tok0 = 0
    tg = 0
    for j in range(J):
        Sk, q0, Sq = jobs[j][:3]
        for v in range(4):
            ld("sp", MOD[v][:], bass.AP(cx["MODR"], (j * 4 + v) * D, [[0, 128], [1, D]]), b_MOD[v], "mod%d" % v)
        GT1B, G2B, SH2B, GT2B = MOD
        for ch in range(Sq // 512):
            c0 = tok0 + ch * 512
            ld("sp", oT[:], cx["OT"].ap()[:, :, c0:c0 + 512].rearrange("c p t -> p c t"), b_oT, "oT", reads=[cx["b_OT"]])
            for ti in range(4):
                g0 = c0 + ti * 128
                r0 = q0 + ch * 512 + ti * 128
                x_ = xt[tg % 2]; bx = b_xt[tg % 2]
                hb_ = h2b[tg % 2]; bh = b_h2b[tg % 2]
                ld("sp", x_[:], cx["xs"][j].ap()[r0:r0 + 128, :], bx, "xo%d" % (tg % 2))
                for half in range(2):
                    for c in range(8):
                        S.op("pe", lambda e, half=half, c=c, ti=ti: e.matmul(PSB[half][:, :], lhsT=oT[:, c, ti * 128:(ti + 1) * 128],
                                                                            rhs=wout[:, c, half * 512:(half + 1) * 512], start=(c == 0), stop=(c == 7)),
                             reads=[b_oT, b_wout], writes=[b_PSB[half]] if c == 0 else (), accs=[b_PSB[half]] if c > 0 else ())
                    sl = slice(half * 512, (half + 1) * 512)
                    S.op("dve", lambda e, half=half, sl=sl: e.tensor_tensor(out=tmp[:, sl], in0=PSB[half][:, :], in1=GT1B[:, sl], op=ALU.mult),
                         reads=[b_PSB[half], b_MOD[0]], writes=[b_tmp] if half == 0 else (), accs=[b_tmp] if half == 1 else ())
                    S.op("dve", lambda e, sl=sl, x_=x_: e.tensor_tensor(out=x_[:, sl], in0=x_[:, sl], in1=tmp[:, sl], op=ALU.add),
                         reads=[b_tmp], accs=[bx])
                S.op("act", lambda e, x_=x_: e.activation(out=sqj[:], in_=x_[:], func=AF.Square, accum_out=st1[:, 0:1]),
                     reads=[bx], writes=[b_sqj, b_st1])
                S.op("act", lambda e: e.activation(out=st1[:, 1:2], in_=st1[:, 0:1], func=AF.Sqrt, scale=1.0 / D, bias=cx["epsb"][:, 0:1]),
                     reads=[cx["b_epsb"]], accs=[b_st1])
                S.op("dve", lambda e: e.reciprocal(out=st1[:, 2:3], in_=st1[:, 1:2]), accs=[b_st1])
                S.op("dve", lambda e, x_=x_: e.scalar_tensor_tensor(out=tmp[:], in0=x_[:], scalar=st1[:, 2:3], in1=G2B[:], op0=ALU.mult, op1=ALU.mult),
                     reads=[bx, b_st1, b_MOD[1]], writes=[b_tmp])
                S.op("dve", lambda e, hb_=hb_: e.tensor_tensor(out=hb_[:], in0=tmp[:], in1=SH2B[:], op=ALU.add),
                     reads=[b_tmp, b_MOD[2]], writes=[bh])
                S.op("act", lambda e, hb_=hb_, g0=g0: e.dma_start(out=cx["H2"].ap()[g0:g0 + 128, :], in_=hb_[:]),
                     reads=[bh], accs=[b_H2], dma_key="h2b%d_st" % (tg % 2))
                pT_ = PSB[2][:, :].bitcast(BF16).rearrange("p (kc t) -> p kc t", kc=8)
                for kc in range(8):
                    S.op("pe", lambda e, kc=kc, hb_=hb_, pT_=pT_: e.transpose(out=pT_[:, kc, :], in_=hb_[:, kc * 128:(kc + 1) * 128], identity=cx["ident_b"][:]),
                         reads=[bh, cx["b_ident_b"]], writes=[b_PSB[2]] if kc == 0 else (), accs=[b_PSB[2]] if kc > 0 else ())
                S.op("act", lambda e, pT_=pT_: e.activation(out=h2T[:], in_=pT_, func=AF.Identity), reads=[b_PSB[2]], writes=[b_h2T])
                for kc in range(8):
                    S.op("pe", lambda e, kc=kc: e.matmul(PSB[3][:, 0:NE], lhsT=h2T[:, kc, :], rhs=wr[:, kc, :], start=(kc == 0), stop=(kc == 7)),
                         reads=[b_h2T, b_wr], writes=[b_PSB[3]] if kc == 0 else (), accs=[b_PSB[3]] if kc > 0 else ())
                for kc in range(8):
                    S.op("pe", lambda e, kc=kc: e.matmul(PSB[4][:, :], lhsT=h2T[:, kc, :], rhs=wsgu[:, kc, :], start=(kc == 0), stop=(kc == 7)),
                         reads=[b_h2T, b_wsgu], writes=[b_PSB[4]] if kc == 0 else (), accs=[b_PSB[4]] if kc > 0 else ())
                S.op("act", lambda e: e.activation(out=sgt[:], in_=PSB[4][:, 0:256], func=AF.Silu), reads=[b_PSB[4]], writes=[b_sgt])
                S.op("dve", lambda e: e.tensor_tensor(out=ab[:], in0=PSB[4][:, 256:512], in1=sgt[:], op=ALU.mult),
                     reads=[b_PSB[4], b_sgt], writes=[b_ab])
                pA = PSB[5][:, :].bitcast(BF16)[:, 0:256].rearrange("p (c t) -> p c t", c=2)
                for fc in range(2):
                    S.op("pe", lambda e, fc=fc, pA=pA: e.transpose(out=pA[:, fc, :], in_=ab[:, fc * 128:(fc + 1) * 128], identity=cx["ident_b"][:]),
                         reads=[b_ab, cx["b_ident_b"]], writes=[b_PSB[5]] if fc == 0 else (), accs=[b_PSB[5]] if fc > 0 else ())
                S.op("act", lambda e, pA=pA: e.activation(out=aT[:], in_=pA, func=AF.Identity), reads=[b_PSB[5]], writes=[b_aT])
                for half in range(2):
                    for fc in range(2):
                        S.op("pe", lambda e, half=half, fc=fc: e.matmul(PSB[6 + half][:, :], lhsT=aT[:, fc, :], rhs=wsd[:, fc, half * 512:(half + 1) * 512],
                                                                       start=(fc == 0), stop=(fc == 1)),
                             reads=[b_aT, b_wsd], writes=[b_PSB[6 + half]] if fc == 0 else (), accs=[b_PSB[6 + half]] if fc > 0 else ())
                    sl = slice(half * 512, (half + 1) * 512)
                    S.op("dve", lambda e, half=half, sl=sl: e.tensor_tensor(out=tmp[:, sl], in0=PSB[6 + half][:, :], in1=GT2B[:, sl], op=ALU.mult),
                         reads=[b_PSB[6 + half], b_MOD[3]], writes=[b_tmp] if half == 0 else (), accs=[b_tmp] if half == 1 else ())
                    S.op("dve", lambda e, sl=sl, x_=x_: e.tensor_tensor(out=x_[:, sl], in0=x_[:, sl], in1=tmp[:, sl], op=ALU.add),
                         reads=[b_tmp], accs=[bx])
                S.op("act", lambda e, x_=x_, g0=g0: e.dma_start(out=cx["X1"].ap()[g0:g0 + 128, :], in_=x_[:]),
                     reads=[bx], accs=[b_X1], dma_key="xo%d_st" % (tg % 2))
                S.op("act", lambda e: e.activation(out=scr[:], in_=PSB[3][:, 0:NE], func=AF.Sigmoid), reads=[b_PSB[3]], writes=[b_scr])
                S.op("dve", lambda e: e.tensor_tensor(out=sel[:], in0=scr[:], in1=rbias[:], op=ALU.add), reads=[b_scr, b_rbias], writes=[b_sel])
                for g in range(8):
                    S.op("dve", lambda e, g=g: e.max(out=g8[:, g, :], in_=sel[:, g * 32:(g + 1) * 32]), reads=[b_sel],
                         writes=[b_g8] if g == 0 else (), accs=[b_g8] if g > 0 else ())
                S.op("dve", lambda e: e.tensor_tensor(out=gs[:, 0:8], in0=g8[:, :, 0], in1=g8[:, :, 1], op=ALU.add), reads=[b_g8], writes=[b_gs])
                S.op("dve", lambda e: e.max(out=gs[:, 8:16], in_=gs[:, 0:8]), accs=[b_gs])
                S.op("dve", lambda e: e.tensor_scalar(out=gs[:, 16:24], in0=gs[:, 0:8], scalar1=gs[:, 11:12], scalar2=None, op0=ALU.is_ge), accs=[b_gs])
                S.op("dve", lambda e: e.scalar_tensor_tensor(out=selm[:].rearrange("p (g i) -> p g i", g=8), in0=sel[:].rearrange("p (g i) -> p g i", g=8),
                                                             scalar=10.0, in1=gs[:, 16:24].unsqueeze(2).to_broadcast([128, 8, 32]), op0=ALU.add, op1=ALU.mult),
                     reads=[b_sel, b_gs], writes=[b_selm])
                S.op("dve", lambda e: e.max(out=gs[:, 24:32], in_=selm[:]), reads=[b_selm], accs=[b_gs])
                S.op("dve", lambda e: e.tensor_scalar(out=Mb[:], in0=selm[:], scalar1=gs[:, 31:32], scalar2=None, op0=ALU.is_ge),
                     reads=[b_selm, b_gs], writes=[b_Mb])
                S.op("dve", lambda e: e.tensor_tensor(out=wg[:], in0=scr[:], in1=Mb[:], op=ALU.mult), reads=[b_scr, b_Mb], writes=[b_wg])
                S.op("dve", lambda e: e.tensor_reduce(out=p8[:, 0:1], in_=wg[:], axis=AX.X, op=ALU.add), reads=[b_wg], writes=[b_p8])
                S.op("dve", lambda e: e.reciprocal(out=p8[:, 1:2], in_=p8[:, 0:1]), accs=[b_p8])
                S.op("dve", lambda e: e.tensor_scalar(out=wg[:], in0=wg[:], scalar1=p8[:, 1:2], scalar2=2.5, op0=ALU.mult, op1=ALU.mult),
                     reads=[b_p8], accs=[b_wg])
                S.op("pe", lambda e: e.matmul(PSB[5][:, 0:NE], lhsT=cx["tri_b"][:, :], rhs=Mb[:], start=True, stop=False),
                     reads=[cx["b_tri"], b_Mb], writes=[b_PSB[5]])
                S.op("pe", lambda e: e.matmul(PSB[5][:, 0:NE], lhsT=cx["ones_b"][:, :], rhs=Mcum[:], start=False, stop=True),
                     reads=[cx["b_ones_b"], b_Mcum], accs=[b_PSB[5]])
                S.op("dve", lambda e: e.tensor_tensor(out=pvm[:], in0=PSB[5][:, 0:NE], in1=E1[:], op=ALU.add), reads=[b_PSB[5], b_E1], writes=[b_pvm])
                S.op("dve", lambda e: e.tensor_tensor(out=pvm[:], in0=pvm[:], in1=Mb[:], op=ALU.mult), reads=[b_Mb], accs=[b_pvm])
                S.op("dve", lambda e: e.tensor_tensor(out=Mcum[:], in0=Mcum[:], in1=Mb[:], op=ALU.add), reads=[b_Mb], writes=[b_Mcum])
                S.op("dve", lambda e: e.max(out=p8[:, 0:8], in_=pvm[:]), reads=[b_pvm], writes=[b_p8])
                S.op("dve", lambda e: e.tensor_copy(p8i[:, 0:8], p8[:, 0:8]), reads=[b_p8], writes=[b_p8i])
                S.op("dve", lambda e: e.tensor_scalar(out=p8i[:, 8:16], in0=p8i[:, 0:8], scalar1=14, scalar2=None, op0=ALU.arith_shift_right), accs=[b_p8i])
                S.op("dve", lambda e, tg=tg: e.tensor_copy(E8[:, tg, :], p8i[:, 8:16]), reads=[b_p8i], accs=[b_E8])
                S.op("dve", lambda e: e.tensor_scalar(out=p8i[:, 8:16], in0=p8i[:, 0:8], scalar1=16383, scalar2=None, op0=ALU.bitwise_and), accs=[b_p8i])
                S.op("dve", lambda e, tg=tg: e.tensor_copy(R8[:, tg, :], p8i[:, 8:16]), reads=[b_p8i], accs=[b_R8])
                for k in range(8):
                    S.op("dve", lambda e, k=k, tg=tg: e.scalar_tensor_tensor(out=junk[:], in0=pvm[:], scalar=p8[:, k:k + 1], in1=wg[:], op0=ALU.is_equal, op1=ALU.mult,
                                                                             accum_out=G8[:, tg, k:k + 1]),
                         reads=[b_pvm, b_p8, b_wg], writes=[b_junk], accs=[b_G8])
                tg += 1
        tok0 += Sq
    cc = sb("cc", [128, 16], F32); b_cc = Buf("cc")
    cci = sb("cci", [128, 8], I32); b_cci = Buf("cci")
    dg = sb("dg", [128, 128], F32); b_dg = Buf("dg")
    PSrow = sb("PSrow", [128, NE], F32); b_PSrow = Buf("PSrow")
    for ec in range(2):
        S.op("pe", lambda e, ec=ec: e.matmul(PSB[0][:, ec:ec + 1], lhsT=Mcum[:, ec * 128:(ec + 1) * 128], rhs=cx["ones_b"][:, 0:1], start=True, stop=True),
             reads=[b_Mcum, cx["b_ones_b"]], writes=[b_PSB[0]] if ec == 0 else (), accs=[b_PSB[0]] if ec else ())
    S.op("dve", lambda e: e.tensor_scalar(out=cc[:, 0:2], in0=PSB[0][:, 0:2], scalar1=127.0, scalar2=None, op0=ALU.add), reads=[b_PSB[0]], writes=[b_cc])
    S.op("dve", lambda e: e.tensor_copy(cci[:, 0:2], cc[:, 0:2]), reads=[b_cc], writes=[b_cci])
    S.op("dve", lambda e: e.tensor_scalar(out=cci[:, 2:4], in0=cci[:, 0:2], scalar1=7, scalar2=None, op0=ALU.arith_shift_right), accs=[b_cci])
    S.op("dve", lambda e: e.tensor_copy(cc[:, 2:4], cci[:, 2:4]), reads=[b_cci], accs=[b_cc])
    for ec in range(2):
        S.op("pe", lambda e, ec=ec: e.matmul(PSB[1][:, ec:ec + 1], lhsT=cx["tril_f"][:, :], rhs=cc[:, 2 + ec:3 + ec], start=True, stop=(ec == 0)),
             reads=[cx["b_tril"], b_cc], writes=[b_PSB[1]] if ec == 0 else (), accs=[b_PSB[1]] if ec else ())
        if ec == 1:
            S.op("pe", lambda e: e.matmul(PSB[1][:, 1:2], lhsT=cx["ones_f"][:, :], rhs=cc[:, 2:3], start=False, stop=True),
                 reads=[cx["b_ones_f"], b_cc], accs=[b_PSB[1]])
    S.op("dve", lambda e: e.tensor_copy(cc[:, 4:6], PSB[1][:, 0:2]), reads=[b_PSB[1]], accs=[b_cc])
    S.op("dve", lambda e: e.tensor_tensor(out=cc[:, 6:8], in0=cc[:, 4:6], in1=cc[:, 2:4], op=ALU.subtract), accs=[b_cc])
    S.op("dve", lambda e: e.tensor_scalar(out=cc[:, 8:10], in0=cc[:, 6:8], scalar1=128.0, scalar2=None, op0=ALU.mult), accs=[b_cc])
    for ec in range(2):
        S.op("dve", lambda e, ec=ec: e.tensor_scalar(out=dg[:], in0=cx["ident_f"][:], scalar1=cc[:, 8 + ec:9 + ec], scalar2=None, op0=ALU.mult),
             reads=[cx["b_ident_f"], b_cc], writes=[b_dg])
        S.op("pe", lambda e, ec=ec: e.matmul(PSB[2][:, ec * 128:(ec + 1) * 128], lhsT=cx["ones_f"][:, :], rhs=dg[:], start=True, stop=True),
             reads=[cx["b_ones_f"], b_dg], writes=[b_PSB[2]] if ec == 0 else (), accs=[b_PSB[2]] if ec else ())
    S.op("dve", lambda e: e.tensor_copy(PSrow[:], PSB[2][:, 0:NE]), reads=[b_PSB[2]], writes=[b_PSrow])
    for t in range(NT):
        for k in range(8):
            S.op("dve", lambda e, k=k, t=t: e.scalar_tensor_tensor(out=junk[:], in0=cx["iota_e"][:], scalar=E8[:, t, k:k + 1], in1=PSrow[:], op0=ALU.is_equal, op1=ALU.mult,
                                                                   accum_out=p8[:, k:k + 1]),
                 reads=[cx["b_iota_e"], b_E8, b_PSrow], writes=[b_junk], accs=[b_p8])
        S.op("dve", lambda e, t=t: e.tensor_tensor(out=p8[:, 0:8], in0=p8[:, 0:8], in1=R8[:, t, :], op=ALU.add), reads=[b_R8], accs=[b_p8])
        S.op("dve", lambda e, t=t: e.tensor_scalar(out=SL8[:, t, :], in0=p8[:, 0:8], scalar1=-1.0, scalar2=None, op0=ALU.add), reads=[b_p8], accs=[b_SL8])
    NB1 = min(NBLK, 512)
    ind = sb("ind", [128, NBLK], BF16); b_ind = Buf("ind")
    EB = sb("EB", [128, NBLK], F32); b_EB = Buf("EB")
    CH = sb("CH", [128, NBLK], F32); b_CH = Buf("CH")
    for ec in range(2):
        S.op("dve", lambda e, ec=ec: e.tensor_scalar(out=ind[:], in0=cx["iota_b"][:, 0:NBLK], scalar1=cc[:, 4 + ec:5 + ec], scalar2=None, op0=ALU.is_ge),
             reads=[cx["b_iota_b"], b_cc], writes=[b_ind])
        for a0 in range(0, NBLK, 512):
            n_ = min(512, NBLK - a0)
            bk = 3 + a0 // 512
            S.op("pe", lambda e, ec=ec, a0=a0, n_=n_, bk=bk: e.matmul(PSB[bk][:, 0:n_], lhsT=cx["ones_b"][:, :], rhs=ind[:, a0:a0 + n_], start=(ec == 0), stop=(ec == 1)),
                 reads=[cx["b_ones_b"], b_ind], writes=[b_PSB[bk]] if ec == 0 else (), accs=[b_PSB[bk]] if ec else ())
    for a0 in range(0, NBLK, 512):
        n_ = min(512, NBLK - a0)
        bk = 3 + a0 // 512
        S.op("dve", lambda e, a0=a0, n_=n_, bk=bk: e.tensor_scalar(out=EB[:, a0:a0 + n_], in0=PSB[bk][:, 0:n_], scalar1=255.0, scalar2=None, op0=ALU.min),
             reads=[b_PSB[bk]], writes=[b_EB] if a0 == 0 else (), accs=[b_EB] if a0 else ())
    S.op("dve", lambda e: e.memset(CH[:, 0:4], 1.0), writes=[b_CH])
    S.op("dve", lambda e: e.tensor_tensor(out=CH[:, 4:NBLK], in0=EB[:, 4:NBLK], in1=EB[:, 0:NBLK - 4], op=ALU.not_equal), reads=[b_EB], accs=[b_CH])
    S.op("dve", lambda e: e.memset(CH[0:1, :], 1.0), accs=[b_CH])
    BIGI = 1.0e6
    EBs = sb("EBs", [128, NBLK], F32); b_EBs = Buf("EBs")
    S.op("dve", lambda e: e.tensor_scalar(out=EBs[:], in0=EB[:], scalar1=128.0, scalar2=cx["iota_p"][:, 0:1], op0=ALU.mult, op1=ALU.add),
         reads=[b_EB, cx["b_iota_p"]], writes=[b_EBs])
    S.op("dve", lambda e: e.scalar_tensor_tensor(out=EBs[:], in0=EBs[:], scalar=-BIGI, in1=CH[:], op0=ALU.add, op1=ALU.mult),
         reads=[b_CH], accs=[b_EBs])
    S.op("dve", lambda e: e.tensor_scalar(out=IDXW[:], in0=EBs[:], scalar1=BIGI, scalar2=None, op0=ALU.add), reads=[b_EBs], writes=[b_IDXW])
    b_XS = cx["b_XS"] = Buf("XS")
    for t in range(NT):
        hb_ = h2b[t % 2]; bh = b_h2b[t % 2]
        ld("sp", hb_[:], cx["H2"].ap()[t * 128:(t + 1) * 128, :], bh, "h2b%d_ld" % (t % 2), reads=[b_H2])
        for k in range(8):
            S.op("pool", lambda e, hb_=hb_, t=t, k=k: e.indirect_dma_start(out=cx["XS"].ap(), out_offset=bass.IndirectOffsetOnAxis(ap=SL8[:, t, k:k + 1], axis=0),
                                                                          in_=hb_[:], in_offset=None),
                 reads=[bh, b_SL8], accs=[b_XS], dma_key="h2b%d_sc" % (t % 2))
    sst.close(); cx["stk"].pop()
    S.barrier()


_REGS = {}


def breg(e, nc, val):
    k = (id(nc), val)
    if k not in _REGS:
        _REGS[k] = e.to_reg(val)
    return _REGS[k]


def stage_E(cx):
    nc, S, sb = cx["nc"], cx["S"], cx["sb"]
    PSB, b_PSB = cx["PSB"], cx["b_PSB"]
    W = cx["W"]; NBLK = cx["NBLK"]
    IDXW = cx["IDXW"]
    sst = ExitStack(); cx["stk"].append(sst)
    RW = 4
    wall = [sb("wall%d" % i, [128, 6144], BF16) for i in range(RW)]; b_wall = [Buf("wall%d" % i) for i in range(RW)]
    NXB = 3
    xb = [sb("xb%d" % i, [128, D], BF16) for i in range(NXB)]; b_xb = [Buf("xb%d" % i) for i in range(NXB)]
    xT = [sb("xTe%d" % i, [128, 8, 128], BF16) for i in range(2)]; b_xT = [Buf("xTe%d" % i) for i in range(2)]
    sg = [sb("sge%d" % i, [128, 256], F32) for i in range(2)]; b_sg = [Buf("sge%d" % i) for i in range(2)]
    aT = [sb("aTe%d" % i, [128, 2, 128], BF16) for i in range(2)]; b_aT = [Buf("aTe%d" % i) for i in range(2)]
    yb = [sb("yb%d" % i, [128, D], BF16) for i in range(2)]; b_yb = [Buf("yb%d" % i) for i in range(2)]
    b_YS = cx["b_YS"] = Buf("YS")
    NROW_W = NE * 128 - 1

    def load_w(b):
        i = b % RW
        S.op("pool", lambda e, b=b, i=i: e.indirect_dma_start(out=wall[i][:, 0:4096], out_offset=None, in_=cx["WBGU"].ap(),
                                                             in_offset=bass.IndirectOffsetOnAxis(ap=IDXW[:, b:b + 1], axis=0),
                                                             bounds_check=breg(e, nc, NROW_W), oob_is_err=False),
             reads=[cx["b_IDXW"], cx["b_WB"]], writes=[b_wall[i]], dma_key="wall%d" % i)
        S.op("pool", lambda e, b=b, i=i: e.indirect_dma_start(out=wall[i][:, 4096:6144], out_offset=None, in_=cx["WBD"].ap(),
                                                             in_offset=bass.IndirectOffsetOnAxis(ap=IDXW[:, b:b + 1], axis=0),
                                                             bounds_check=breg(e, nc, NROW_W), oob_is_err=False),
             reads=[cx["b_IDXW"], cx["b_WB"]], accs=[b_wall[i]], dma_key="wall%d" % i)

    def load_x(b):
        cx["ld"]("sp", xb[b % NXB][:], cx["XS"].ap()[b * 128:(b + 1) * 128, :], b_xb[b % NXB], "xb%d" % (b % NXB), reads=[cx["b_XS"]])

    def do_T(b):
        i = b % 2
        x_ = xb[b % NXB]; bx = b_xb[b % NXB]
        pX = PSB[i][:, :].bitcast(BF16).rearrange("p (kc t) -> p kc t", kc=8)
        xv = x_[:, :].rearrange("p (q kc) -> p kc q", kc=8)
        for kc in range(8):
            S.op("pe", lambda e, kc=kc, pX=pX, xv=xv: e.transpose(out=pX[:, kc, :], in_=xv[:, kc, :], identity=cx["ident_b"][:]),
                 reads=[bx, cx["b_ident_b"]], writes=[b_PSB[i]] if kc == 0 else (), accs=[b_PSB[i]] if kc > 0 else ())
        S.op("act", lambda e, pX=pX, i=i: e.activation(out=xT[i][:, 0:4, :], in_=pX[:, 0:4, :], func=AF.Identity), reads=[b_PSB[i]], writes=[b_xT[i]])
        S.op("dve", lambda e, pX=pX, i=i: e.tensor_copy(xT[i][:, 4:8, :], pX[:, 4:8, :]), reads=[b_PSB[i]], accs=[b_xT[i]])

    def do_GU(b):
        i = b % 2
        bank = 2 + i
        first = True
        wr_ = b % RW
        wgu = wall[wr_][:, 0:4096].rearrange("p (w kc f) -> p w kc f", w=2, kc=8)
        for which in range(2):
            wt = wgu[:, which]
            bw = b_wall[wr_]
            for fc in range(2):
                for kc in range(8):
                    S.op("pe", lambda e, wt=wt, fc=fc, kc=kc, which=which, bank=bank, i=i: e.matmul(
                        PSB[bank][:, which * 256 + fc * 128: which * 256 + (fc + 1) * 128], lhsT=wt[:, kc, fc:256:2], rhs=xT[i][:, kc, :],
                        start=(kc == 0), stop=(kc == 7)),
                        reads=[bw, b_xT[i]], writes=[b_PSB[bank]] if first else (), accs=[b_PSB[bank]] if not first else ())
                    first = False
        S.op("act", lambda e, bank=bank, i=i: e.activation(out=sg[i][:], in_=PSB[bank][:, 0:256], func=AF.Silu), reads=[b_PSB[bank]], writes=[b_sg[i]])
        S.op("dve", lambda e, bank=bank, i=i: e.tensor_tensor(out=aT[i][:].rearrange("p c t -> p (c t)"), in0=PSB[bank][:, 256:512], in1=sg[i][:], op=ALU.mult),
             reads=[b_PSB[bank], b_sg[i]], writes=[b_aT[i]])

    def do_D(b):
        i = b % 2
        y_ = yb[i]; by = b_yb[i]
        for half in range(2):
            bank = 4 + 2 * i + half
            for fc in range(2):
                wdv_ = wall[b % RW][:, 4096:6144].rearrange("p (fc d) -> p fc d", fc=2)
                S.op("pe", lambda e, half=half, fc=fc, bank=bank, i=i, wdv_=wdv_: e.matmul(PSB[bank][:, :], lhsT=aT[i][:, fc, :], rhs=wdv_[:, fc, half * 512:(half + 1) * 512],
                                                                                        start=(fc == 0), stop=(fc == 1)),
                     reads=[b_aT[i], b_wall[b % RW]], writes=[b_PSB[bank]] if fc == 0 else (), accs=[b_PSB[bank]] if fc else ())
            if half == 0:
                S.op("act", lambda e, y_=y_, bank=bank: e.activation(out=y_[:, 0:512], in_=PSB[bank][:, :], func=AF.Identity),
                     reads=[b_PSB[bank]], writes=[by])
            else:
                S.op("dve", lambda e, y_=y_, bank=bank: e.tensor_copy(y_[:, 512:1024], PSB[bank][:, :]), reads=[b_PSB[bank]], accs=[by])
        S.op("act", lambda e, y_=y_, b=b: e.dma_start(out=cx["YS"].ap()[b * 128:(b + 1) * 128, :], in_=y_[:]),
             reads=[by], accs=[b_YS], dma_key="yb%d_st" % i)

    for b0 in range(min(RW, NBLK)):
        load_w(b0)
    load_x(0); load_x(1)
    do_T(0)
    for b in range(NBLK):
        if b + 2 < NBLK:
            load_x(b + 2)
        if b + 1 < NBLK:
            do_T(b + 1)
        do_GU(b)
        if b >= 1:
            do_D(b - 1)
            if b - 1 + RW < NBLK:
                load_w(b - 1 + RW)
    do_D(NBLK - 1)
    sst.close(); cx["stk"].pop()
    S.barrier()


def stage_C(cx):
    nc, S, sb, jobs, J = cx["nc"], cx["S"], cx["sb"], cx["jobs"], cx["J"]
    NT = cx["NT"]
    SL8, G8 = cx["SL8"], cx["G8"]
    sst = ExitStack(); cx["stk"].append(sst)
    gfrow = sb("gfrow", [128, D], F32); b_gfrow = Buf("gfrow")
    cx["ld"]("sp", gfrow[:], bass.AP(cx["W"]["g_final"], 0, [[0, 128], [1, D]]), b_gfrow, "gfrow")
    gt2 = sb("gt2c", [128, D], F32); b_gt2 = Buf("gt2c")
    x1 = [sb("x1c%d" % i, [128, D], F32) for i in range(2)]; b_x1 = [Buf("x1c%d" % i) for i in range(2)]
    yg = [sb("yg%d" % i, [128, D], BF16) for i in range(8)]; b_yg = [Buf("yg%d" % i) for i in range(8)]
    acc = sb("accc", [128, D], F32); b_acc = Buf("accc")
    sq = sb("sqc", [128, D], BF16); b_sq = Buf("sqc")
    stc = sb("stc", [128, 8], F32); b_stc = Buf("stc")
    outs = []
    t = 0
    for j in range(J):
        Sk, q0, Sq = jobs[j][:3]
        cx["ld"]("sp", gt2[:], bass.AP(cx["MODR"], (j * 4 + 3) * D, [[0, 128], [1, D]]), b_gt2, "gt2c")
        for tl in range(Sq // 128):
            x_ = x1[t % 2]; bx = b_x1[t % 2]
            cx["ld"]("sp", x_[:], cx["X1"].ap()[t * 128:(t + 1) * 128, :], bx, "x1c%d" % (t % 2), reads=[cx["b_X1"]])
            for k in range(8):
                S.op("pool", lambda e, t=t, k=k: e.indirect_dma_start(out=yg[k][:], out_offset=None, in_=cx["YS"].ap(),
                                                                     in_offset=bass.IndirectOffsetOnAxis(ap=SL8[:, t, k:k + 1], axis=0)),
                     reads=[cx["b_SL8"], cx["b_YS"]], writes=[b_yg[k]], dma_key="yg%d" % k)
            S.op("dve", lambda e, t=t: e.tensor_scalar(out=acc[:], in0=yg[0][:], scalar1=G8[:, t, 0:1], scalar2=None, op0=ALU.mult),
                 reads=[b_yg[0], cx["b_G8"]], writes=[b_acc])
            for k in range(1, 8):
                S.op("dve", lambda e, t=t, k=k: e.scalar_tensor_tensor(out=acc[:], in0=yg[k][:], scalar=G8[:, t, k:k + 1], in1=acc[:], op0=ALU.mult, op1=ALU.add),
                     reads=[b_yg[k], cx["b_G8"]], accs=[b_acc])
            S.op("dve", lambda e: e.tensor_tensor(out=acc[:], in0=acc[:], in1=gt2[:], op=ALU.mult), reads=[b_gt2], accs=[b_acc])
            S.op("dve", lambda e, x_=x_: e.tensor_tensor(out=x_[:], in0=x_[:], in1=acc[:], op=ALU.add), reads=[b_acc], accs=[bx])
            S.op("act", lambda e, x_=x_: e.activation(out=sq[:], in_=x_[:], func=AF.Square, accum_out=stc[:, 0:1]), reads=[bx], writes=[b_sq, b_stc])
            S.op("act", lambda e: e.activation(out=stc[:, 1:2], in_=stc[:, 0:1], func=AF.Sqrt, scale=1.0 / D, bias=cx["epsb"][:, 0:1]),
                 reads=[cx["b_epsb"]], accs=[b_stc])
            S.op("dve", lambda e: e.reciprocal(out=stc[:, 2:3], in_=stc[:, 1:2]), accs=[b_stc])
            S.op("dve", lambda e, x_=x_: e.scalar_tensor_tensor(out=x_[:], in0=x_[:], scalar=stc[:, 2:3], in1=gfrow[:], op0=ALU.mult, op1=ALU.mult),
                 reads=[b_stc, b_gfrow], accs=[bx])
            o = S.op("act", lambda e, x_=x_, t=t: e.dma_start(out=cx["y_out"].ap()[t * 128:(t + 1) * 128, :], in_=x_[:]),
                     reads=[bx], dma_key="x1c%d_st" % (t % 2))
            outs.append(o)
            t += 1
    sst.close(); cx["stk"].pop()
    return outs


def build_all(jobs, dbg=False):
    cx = build(jobs, dbg=dbg)
    stage_P(cx)
    stage_T(cx)
    stage_O(cx)
    stage_E(cx)
    outs = stage_C(cx)
    cx["S"].emit(outs)
    return cx


def make_in_map(jobs, xseqs, cvecs, poss, hf, weights, consts):
    J = len(jobs)
    im = {}
    for j in range(J):
        im["xs%d" % j] = np.ascontiguousarray(xseqs[j], dtype=np.float32)
        im["rope%d" % j] = rope_table(poss[j])
    cT = np.stack([np.asarray(cv, np.float32).reshape(8, 128).T for cv in cvecs], axis=-1)
    im["cT"] = np.ascontiguousarray(cT)
    im.update(weights)
    for n, v in consts.items():
        im["c_" + n] = v
    im["c_hfv"] = np.broadcast_to(np.array([[float(hf), 1.0 - float(hf)]], np.float32), (128, 2)).copy()
    return im


def kernel(**inputs):
    jobs = [(2048, 0, 2048, False), (2048, 0, 2048, False), (8192, 0, 4096, True)]
    cx = build_all(jobs)
    weights = {}
    for n in WEIGHT_NAMES:
        a = np.asarray(inputs[n], dtype=np.float32)
        if n not in ("rel_bias", "g_final"):
            a = a[0]
        weights[n] = np.ascontiguousarray(a)
    consts = host_constants()
    xp = np.asarray(inputs["x_prompt"], np.float32)
    xsm = np.asarray(inputs["x_sample"], np.float32)
    cp = np.asarray(inputs["c_prompt"], np.float32)
    cs = np.asarray(inputs["c_sample"], np.float32)
    in_maps = []
    for c in range(8):
        sbi, hf = c // 2, c % 2
        seq = xsm[sbi]
        pos2 = np.arange(8192)
        if hf:
            seq = np.concatenate([seq[4096:], seq[:4096]], axis=0)
            pos2 = np.concatenate([pos2[4096:], pos2[:4096]])
        in_maps.append(make_in_map(jobs, [xp[2 * c], xp[2 * c + 1], seq], [cp[2 * c], cp[2 * c + 1], cs[sbi]],
                                   [np.arange(2048), np.arange(2048), pos2], hf, weights, consts))
    res = run_bass_kernel_spmd(cx["nc"], in_maps, core_ids=list(range(8)))
    y_prompt = np.empty((16, 2048, D), np.float32)
    y_sample = np.empty((4, 8192, D), np.float32)
    for c in range(8):
        y = np.asarray(res.results[c]["y"], dtype=np.float32)
        y_prompt[2 * c] = y[0:2048]
        y_prompt[2 * c + 1] = y[2048:4096]
        y_sample[c // 2, (c % 2) * 4096:(c % 2 + 1) * 4096] = y[4096:8192]
    return (y_prompt, y_sample)
```

```python
import math
from contextlib import ExitStack
import numpy as np
import concourse.bass as bass
import concourse.mybir as mybir
from concourse.bass_utils import run_bass_kernel_spmd

F32 = mybir.dt.float32
BF16 = mybir.dt.bfloat16
I32 = mybir.dt.int32
AF = mybir.ActivationFunctionType
ALU = mybir.AluOpType
AX = mybir.AxisListType

D = 1024
DP = 2304
NE = 256
EPS = 1e-6
ENGS = ("pe", "act", "dve", "pool", "sp")


class Buf:
    __slots__ = ("name", "writers", "readers")

    def __init__(self, name):
        self.name = name
        self.writers = []
        self.readers = []


class Op:
    __slots__ = ("eng", "fn", "deps", "is_dma", "sem", "val", "signals")

    def __init__(self, eng, fn, is_dma):
        self.eng = eng
        self.fn = fn
        self.deps = []
        self.is_dma = is_dma
        self.sem = None
        self.val = 0
        self.signals = False


class Sched:
    def __init__(self, nc, stack):
        self.nc = nc
        self.stack = stack
        self.ops = {e: [] for e in ENGS}
        self.esem = {e: stack.enter_context(nc.semaphore("es_" + e)) for e in ENGS}
        self.dma_sems = {}
        self.pending = {e: [] for e in ENGS}
        self.last_dma = {}

    def barrier(self):
        deps = []
        for e in ENGS:
            for o in reversed(self.ops[e]):
                if not o.is_dma:
                    deps.append(o)
                    break
        deps.extend(self.last_dma.values())
        for e in ENGS:
            self.pending[e] = list(deps)

    def _dma_sem(self, key):
        if key not in self.dma_sems:
            s = self.stack.enter_context(self.nc.semaphore("ds%d" % len(self.dma_sems)))
            self.dma_sems[key] = [s, 0]
        return self.dma_sems[key]

    def op(self, eng, fn, reads=(), writes=(), accs=(), dma_key=None):
        is_dma = dma_key is not None
        o = Op(eng, fn, is_dma)
        deps = []
        for b in reads:
            deps.extend(b.writers)
        for b in writes:
            deps.extend(b.writers)
            deps.extend(b.readers)
        for b in accs:
            deps.extend(b.readers)
            for w in b.writers:
                if w.is_dma and is_dma:
                    continue
                deps.append(w)
        if self.pending[eng]:
            deps.extend(self.pending[eng])
            self.pending[eng] = []
        seen = set()
        for d in deps:
            if id(d) in seen or d is o:
                continue
            seen.add(id(d))
            if (not d.is_dma) and (not is_dma) and d.eng == "pe" and eng == "pe":
                continue
            o.deps.append(d)
            d.signals = True
        for b in reads:
            b.readers.append(o)
        for b in writes:
            b.writers = [o]
            b.readers = []
        for b in accs:
            b.writers.append(o)
            b.readers = []
        if is_dma:
            s = self._dma_sem(dma_key)
            s[1] += 16
            o.sem = s[0]
            o.val = s[1]
            o.signals = True
            self.last_dma[dma_key] = o
        self.ops[eng].append(o)
        return o

    def emit(self, final_ops):
        nc = self.nc
        for e in ENGS:
            c = 0
            for o in self.ops[e]:
                if not o.is_dma and o.signals:
                    c += 1
                    o.sem = self.esem[e]
                    o.val = c
        sched = self

        def run(engname, eh):
            waited = {}
            for o in sched.ops[engname]:
                for d in o.deps:
                    k = id(d.sem)
                    if waited.get(k, 0) >= d.val:
                        continue
                    eh.wait_ge(d.sem, d.val)
                    waited[k] = d.val
                ins = o.fn(eh)
                if o.is_dma:
                    ins.then_inc(o.sem, 16)
                elif o.signals:
                    ins.then_inc(o.sem, 1)
            if engname == "sp":
                for d in final_ops:
                    if waited.get(id(d.sem), 0) < d.val:
                        eh.wait_ge(d.sem, d.val)
                        waited[id(d.sem)] = d.val

        allsems = list(self.esem.values()) + [v[0] for v in self.dma_sems.values()]
        with nc.Block() as blk0:
            @blk0.sync
            def _(e):
                for s_ in allsems:
                    e.sem_clear(s_)

        with nc.Block() as block:
            @block.tensor
            def _(e):
                run("pe", e)

            @block.scalar
            def _(e):
                run("act", e)

            @block.vector
            def _(e):
                run("dve", e)

            @block.gpsimd
            def _(e):
                run("pool", e)

            @block.sync
            def _(e):
                run("sp", e)


def t5_bucket_np(rel):
    nb = 16
    max_exact = 8
    ret = np.where(rel > 0, nb, 0)
    n = np.abs(rel)
    nf = np.maximum(n, 1).astype(np.float32)
    large = max_exact + (np.log(nf / np.float32(max_exact)) / np.float32(math.log(128 / max_exact))
                         * np.float32(nb - max_exact)).astype(np.int32)
    large = np.minimum(large, nb - 1)
    return ret + np.where(n < max_exact, n, large)


def host_constants():
    c = {}
    c["ident"] = np.eye(128, dtype=np.float32)
    c["jrev"] = np.eye(128, dtype=np.float32)[::-1].copy()
    tri = np.zeros((128, 128), np.float32)
    for a in range(128):
        tri[a, a + 1:] = 1.0
    c["tri"] = tri
    tril = np.zeros((128, 128), np.float32)
    for a in range(128):
        tril[a, a:] = 1.0
    c["tril"] = tril
    i = np.arange(1280)
    bk = t5_bucket_np((639 - i).astype(np.int32))
    oh = np.zeros((32, 1280), np.float32)
    oh[bk, i] = 1.0
    c["ohr"] = oh
    c["iota_e"] = np.broadcast_to(np.arange(256, dtype=np.float32)[None, :], (128, 256)).copy()
    c["iota_b"] = np.broadcast_to(np.arange(1024, dtype=np.float32)[None, :], (128, 1024)).copy()
    c["iota_p"] = np.arange(128, dtype=np.float32).reshape(128, 1).copy()
    return c


def rope_table(pos):
    t = np.asarray(pos)
    c = {}
    half = 32
    inv = (10000.0 ** (-np.arange(0, half, 2, dtype=np.float32) / half)).astype(np.float32)
    row = (t // 64).astype(np.float32)
    col = (t % 64).astype(np.float32)
    ar = row[:, None] * inv[None, :]
    ac = col[:, None] * inv[None, :]
    return np.stack([np.cos(ar), np.sin(ar), np.cos(ac), np.sin(ac)], axis=1).astype(np.float32)


WEIGHT_NAMES = ["rel_bias", "w_ada", "b_ada", "g_norm1", "w_in", "lambda_q1", "lambda_k1", "lambda_q2",
                "lambda_k2", "g_subln", "g_qnorm", "g_knorm", "w_out", "g_norm2", "w_router",
                "router_bias", "w_exp_gate", "w_exp_up", "w_exp_down", "w_sh_gate", "w_sh_up",
                "w_sh_down", "g_final"]


def build(jobs, dbg=False):
    J = len(jobs)
    T = sum(j_[2] for j_ in jobs)
    NT = T // 128
    NBLK = T * 8 // 128 + NE
    SMAX = max(j[0] for j in jobs)
    nc = bass.Bass("TRN2", target_bir_lowering=False)
    st = ExitStack()
    S = Sched(nc, st)

    def din(name, shape, dt=F32):
        return nc.dram_tensor(name, list(shape), dt, kind="ExternalInput")

    def dscr(name, shape, dt):
        return nc.dram_tensor(name, list(shape), dt, kind="ExternalOutput" if dbg else "Internal")

    xs = [din("xs%d" % j, [jobs[j][0], D]) for j in range(J)]
    cT_d = din("cT", [128, 8, J])
    W = {}
    wshapes = dict(rel_bias=[32, 4], w_ada=[D, 6 * D], b_ada=[6 * D], g_norm1=[D], w_in=[D, DP],
                   lambda_q1=[64], lambda_k1=[64], lambda_q2=[64], lambda_k2=[64], g_subln=[128],
                   g_qnorm=[64], g_knorm=[64], w_out=[D, D], g_norm2=[D], w_router=[D, NE],
                   router_bias=[NE], w_exp_gate=[NE, D, 256], w_exp_up=[NE, D, 256],
                   w_exp_down=[NE, 256, D], w_sh_gate=[D, 256], w_sh_up=[D, 256], w_sh_down=[256, D],
                   g_final=[D])
    for n in WEIGHT_NAMES:
        W[n] = din(n, wshapes[n])
    cshapes = dict(ident=[128, 128], jrev=[128, 128], tri=[128, 128], tril=[128, 128], ohr=[32, 1280],
                   iota_e=[128, 256], iota_b=[128, 1024], iota_p=[128, 1], hfv=[128, 2])
    C = {n: din("c_" + n, s) for n, s in cshapes.items()}
    ROPE = [din("rope%d" % j, [jobs[j][0], 4, 16]) for j in range(J)]
    y_out = nc.dram_tensor("y", [T, D], F32, kind="ExternalOutput")

    QA = [dscr("QA%d" % j, [4, 128, jobs[j][2]], BF16) for j in range(J)]
    KA = [dscr("KA%d" % j, [4, 128, jobs[j][0]], BF16) for j in range(J)]
    VA = [dscr("VA%d" % j, [jobs[j][0], 512], BF16) for j in range(J)]
    QB = [dscr("QB%d" % j, [4, 128, jobs[j][2]], BF16) for j in range(J)]
    KB = [dscr("KB%d" % j, [2, 128, jobs[j][0]], BF16) for j in range(J)]
    VB = [dscr("VB%d" % j, [jobs[j][0], 2, 65], BF16) for j in range(J)]
    OT = dscr("OT", [8, 128, T], BF16)
    X1 = dscr("X1", [T, D], F32)
    H2 = dscr("H2", [T, D], BF16)
    XS = dscr("XSLOT", [NBLK * 128, D], BF16)
    YS = dscr("YSLOT", [NBLK * 128, D], BF16)
    GRD = dscr("GRD", [4, 1280], F32)
    WBGU = nc.dram_tensor("WBGU", [NE * 128, 4096], BF16, kind="Internal")
    WBD = nc.dram_tensor("WBD", [NE * 128, 2048], BF16, kind="Internal")

    sb_bytes = [0]

    stk = [st]

    def sb(name, shape, dt):
        return stk[-1].enter_context(nc.sbuf_tensor(name, list(shape), dt))

    def ps(name, shape, dt=F32):
        return st.enter_context(nc.psum_tensor(name, list(shape), dt))

    MODR = dscr("MODR", [J, 4, D], F32)
    b_MODR = Buf("MODR")
    ident_f = sb("ident_f", [128, 128], F32); b_ident_f = Buf("ident_f")
    ident_b = sb("ident_b", [128, 128], BF16); b_ident_b = Buf("ident_b")
    ones_b = sb("ones_b", [128, 128], BF16); b_ones_b = Buf("ones_b")
    ones_f = sb("ones_f", [128, 128], F32); b_ones_f = Buf("ones_f")
    tri_b = sb("tri_b", [128, 128], BF16); b_tri = Buf("tri_b")
    tril_f = sb("tril_f", [128, 128], F32); b_tril = Buf("tril_f")
    iota_e = sb("iota_e", [128, 256], F32); b_iota_e = Buf("iota_e")
    iota_b = sb("iota_b", [128, 1024], F32); b_iota_b = Buf("iota_b")
    iota_p = sb("iota_p", [128, 1], F32); b_iota_p = Buf("iota_p")
    epsb = sb("epsb", [128, 1], F32); b_epsb = Buf("epsb")
    G1T = sb("G1T", [128, 8, J], F32); b_G1T = Buf("G1T")
    SH1T = sb("SH1T", [128, 8, J], F32); b_SH1T = Buf("SH1T")
    lsm = sb("lsm", [128, 8], F32); b_lsm = Buf("lsm")
    gsub = sb("gsub", [128, 1], F32); b_gsub = Buf("gsub")
    gq = sb("gq", [128, 64], F32); b_gq = Buf("gq")
    gk = sb("gk", [128, 64], F32); b_gk = Buf("gk")
    bcst = sb("bcst", [128, 2, 4], F32); b_bcst = Buf("bcst")

    PSALL = ps("psall", [128, 8, 512], F32)
    PSB = [PSALL[:, i, :] for i in range(8)]
    b_PSB = [Buf("psb%d" % i) for i in range(8)]

    def ld(eng, out_ap, in_ap, wbuf, key, reads=(), acc=False, slow=False):
        fn = (lambda e: e.dma_start(out=out_ap, in_=in_ap, allow_slow_non_contiguous=True)) if slow else \
             (lambda e: e.dma_start(out=out_ap, in_=in_ap))
        if acc:
            return S.op(eng, fn, reads=reads, accs=[wbuf], dma_key=key)
        return S.op(eng, fn, reads=reads, writes=[wbuf], dma_key=key)

    ld("sp", ident_f[:], C["ident"].ap(), b_ident_f, "ident_f")
    ld("sp", tril_f[:], C["tril"].ap(), b_tril, "tril_f")
    ld("sp", iota_e[:], C["iota_e"].ap(), b_iota_e, "iota_e")
    ld("sp", iota_b[:], C["iota_b"].ap(), b_iota_b, "iota_b")
    ld("sp", iota_p[:], C["iota_p"].ap(), b_iota_p, "iota_p")
    ld("pool", ident_b[:], C["ident"].ap(), b_ident_b, "ident_b")
    ld("pool", tri_b[:], C["tri"].ap(), b_tri, "tri_b")
    S.op("dve", lambda e: e.memset(ones_b[:], 1.0), writes=[b_ones_b])
    S.op("dve", lambda e: e.memset(ones_f[:], 1.0), writes=[b_ones_f])
    S.op("dve", lambda e: e.memset(epsb[:], EPS), writes=[b_epsb])

    pst = ExitStack()
    stk.append(pst)
    big = sb("big", [128, 4096], F32); b_big = Buf("big")
    cT = sb("cT_sb", [128, 8, J], F32); b_cT = Buf("cT")
    scT = sb("scT", [128, 8, J], F32); b_scT = Buf("scT")
    ld("sp", cT[:], cT_d.ap(), b_cT, "cT")
    S.op("act", lambda e: e.activation(out=scT[:], in_=cT[:], func=AF.Silu), reads=[b_cT], writes=[b_scT])
    badaT = sb("badaT", [128, 16], F32); b_badaT = Buf("badaT")
    g1T = sb("g1T", [128, 8], F32); b_g1T = Buf("g1T")
    ld("sp", badaT[:], W["b_ada"].ap()[0:2048].rearrange("(c p) -> p c", p=128), b_badaT, "badaT", slow=True)
    ld("sp", g1T[:], W["g_norm1"].ap().rearrange("(c p) -> p c", p=128), b_g1T, "g1T", slow=True)
    scbc = sb("scbc", [128, 8, 128], F32); b_scbc = Buf("scbc")
    wada_v = W["w_ada"].ap().rearrange("(kc p) n -> p kc n", p=128)
    wst = big[:, 0:4096].rearrange("p (kc n) -> p kc n", kc=8)
    for cc in range(4):
        ld("sp", wst, wada_v[:, :, cc * 512:(cc + 1) * 512], b_big, "big_ld")
        for sub in range(4):
            col = cc * 4 + sub
            pb = b_PSB[col % 2]
            pt = PSB[col % 2]
            for kc in range(8):
                S.op("pe", lambda e, kc=kc, sub=sub, pt=pt: e.matmul(pt[:, 0:J], lhsT=wst[:, kc, sub * 128:(sub + 1) * 128],
                                                                   rhs=scT[:, kc, :], start=(kc == 0), stop=(kc == 7)),
                     reads=[b_big, b_scT], writes=[pb] if kc == 0 else (), accs=[pb] if kc > 0 else ())
            if col < 8:
                S.op("dve", lambda e, col=col, pt=pt: e.tensor_scalar(out=SH1T[:, col, :], in0=pt[:, 0:J], scalar1=badaT[:, col:col + 1],
                                                                    scalar2=None, op0=ALU.add),
                     reads=[pb, b_badaT], accs=[b_SH1T])
            else:
                c8 = col - 8
                S.op("dve", lambda e, col=col, c8=c8, pt=pt: e.tensor_scalar(out=G1T[:, c8, :], in0=pt[:, 0:J], scalar1=badaT[:, col:col + 1],
                                                                           scalar2=1.0, op0=ALU.add, op1=ALU.add),
                     reads=[pb, b_badaT], accs=[b_G1T])
                S.op("dve", lambda e, c8=c8: e.tensor_scalar(out=G1T[:, c8, :], in0=G1T[:, c8, :], scalar1=g1T[:, c8:c8 + 1],
                                                             scalar2=None, op0=ALU.mult),
                     reads=[b_g1T], accs=[b_G1T])
    brow = sb("brow", [128, D], F32); b_brow = Buf("brow")
    g2row = sb("g2row", [128, D], F32); b_g2row = Buf("g2row")
    mtmp = sb("mtmp", [128, D], F32); b_mtmp = Buf("mtmp")
    ld("sp", g2row[:], bass.AP(W["g_norm2"], 0, [[0, 128], [1, D]]), b_g2row, "g2row")
    vmap = {2: 0, 3: 2, 4: 1, 5: 3}
    for v6 in (2, 3, 4, 5):
        ld("sp", brow[:], bass.AP(W["b_ada"], v6 * D, [[0, 128], [1, D]]), b_brow, "brow")
        for j in range(J):
            for hc in range(2):
                ld("sp", wst, wada_v[:, :, v6 * D + hc * 512: v6 * D + (hc + 1) * 512], b_big, "big_ld")
                S.op("dve", lambda e, j=j: e.tensor_copy(scbc[:], scT[:, :, j:j + 1].to_broadcast([128, 8, 128])),
                     reads=[b_scT], writes=[b_scbc])
                pb = b_PSB[2 + hc]
                pt = PSB[2 + hc]
                for kc in range(8):
                    S.op("pe", lambda e, kc=kc, pt=pt: e.matmul(pt[:, :], lhsT=scbc[:, kc, :], rhs=wst[:, kc, :],
                                                              start=(kc == 0), stop=(kc == 7)),
                         reads=[b_big, b_scbc], writes=[pb] if kc == 0 else (), accs=[pb] if kc > 0 else ())
                sl = slice(hc * 512, (hc + 1) * 512)
                S.op("dve", lambda e, pt=pt, sl=sl: e.tensor_tensor(out=mtmp[:, sl], in0=pt[:, :], in1=brow[:, sl], op=ALU.add),
                     reads=[pb, b_brow], writes=[b_mtmp] if hc == 0 else (), accs=[b_mtmp] if hc == 1 else ())
            if v6 == 4:
                S.op("dve", lambda e: e.scalar_tensor_tensor(out=mtmp[:], in0=mtmp[:], scalar=1.0, in1=g2row[:],
                                                             op0=ALU.add, op1=ALU.mult),
                     reads=[b_g2row], accs=[b_mtmp])
            S.op("sp", lambda e, j=j, v6=v6: e.dma_start(out=MODR.ap()[j, vmap[v6]:vmap[v6] + 1, :], in_=mtmp[0:1, :]),
                 reads=[b_mtmp], accs=[b_MODR], dma_key="mtmp_st")
    lamv = sb("lamv", [128, 4, 64], F32); b_lamv = Buf("lamv")
    for i, n in enumerate(["lambda_q1", "lambda_k1", "lambda_q2", "lambda_k2"]):
        ld("sp", lamv[:, i, :], bass.AP(W[n], 0, [[0, 128], [1, 64]]), b_lamv, "lamv", acc=(i > 0))
    ljunk = sb("ljunk", [128, 64], F32); b_ljunk = Buf("ljunk")
    for i in range(2):
        S.op("dve", lambda e, i=i: e.tensor_tensor(out=ljunk[:], in0=lamv[:, 2 * i, :], in1=lamv[:, 2 * i + 1, :], op=ALU.mult),
             reads=[b_lamv], writes=[b_ljunk])
        S.op("dve", lambda e, i=i: e.tensor_reduce(out=lsm[:, i:i + 1], in_=ljunk[:], axis=AX.X, op=ALU.add),
             reads=[b_ljunk], accs=[b_lsm])
    S.op("act", lambda e: e.activation(out=lsm[:, 2:4], in_=lsm[:, 0:2], func=AF.Exp), reads=[b_lsm], accs=[b_lsm])
    S.op("dve", lambda e: e.tensor_tensor(out=lsm[:, 4:5], in0=lsm[:, 3:4], in1=lsm[:, 2:3], op=ALU.subtract),
         reads=[b_lsm], accs=[b_lsm])
    S.op("dve", lambda e: e.tensor_scalar(out=lsm[:, 5:6], in0=lsm[:, 4:5], scalar1=-0.2, scalar2=None, op0=ALU.add),
         reads=[b_lsm], accs=[b_lsm])
    NLAM = lsm[:, 5:6]
    ld("sp", gsub[:], W["g_subln"].ap().rearrange("(p o) -> p o", o=1), b_gsub, "gsub")
    S.op("dve", lambda e: e.tensor_scalar(out=gsub[:], in0=gsub[:], scalar1=0.8, scalar2=None, op0=ALU.mult),
         reads=[], writes=[b_gsub])
    ld("sp", gq[:], bass.AP(W["g_qnorm"], 0, [[0, 128], [1, 64]]), b_gq, "gq")
    ld("sp", gk[:], bass.AP(W["g_knorm"], 0, [[0, 128], [1, 64]]), b_gk, "gk")
    ld("sp", bcst[:, 0, :], bass.AP(W["rel_bias"], 15 * 4, [[0, 128], [1, 4]]), b_bcst, "bcst")
    ld("sp", bcst[:, 1, :], bass.AP(W["rel_bias"], 31 * 4, [[0, 128], [1, 4]]), b_bcst, "bcst", acc=True)
    rb = sb("rb", [32, 4], F32); b_rb = Buf("rb")
    ohr = sb("ohr", [32, 1280], F32); b_ohr = Buf("ohr")
    ld("sp", rb[:], W["rel_bias"].ap(), b_rb, "rb")
    ld("sp", ohr[:], C["ohr"].ap(), b_ohr, "ohr")
    grs = sb("grs", [4, 1280], F32); b_grs = Buf("grs")
    for i, (a_, n_) in enumerate([(0, 512), (512, 512), (1024, 256)]):
        S.op("pe", lambda e, a_=a_, n_=n_, i=i: e.matmul(PSB[4 + i][0:4, 0:n_], lhsT=rb[:, :], rhs=ohr[:, a_:a_ + n_], start=True, stop=True),
             reads=[b_rb, b_ohr], writes=[b_PSB[4 + i]])
        S.op("dve", lambda e, a_=a_, n_=n_, i=i: e.tensor_copy(grs[:, a_:a_ + n_], PSB[4 + i][0:4, 0:n_]),
             reads=[b_PSB[4 + i]], accs=[b_grs])
    b_GRD = Buf("GRD")
    S.op("sp", lambda e: e.dma_start(out=GRD.ap(), in_=grs[:]), reads=[b_grs], writes=[b_GRD], dma_key="grs_st")
    pst.close()
    stk.pop()
    S.barrier()

    ctx = dict(PSALL=PSALL, WBGU=WBGU, WBD=WBD, ROPE=ROPE, nc=nc, S=S, st=st, sb=sb, stk=stk, jobs=jobs, J=J, T=T, NT=NT, NBLK=NBLK, xs=xs, W=W, C=C, y_out=y_out,
               QA=QA, KA=KA, VA=VA, QB=QB, KB=KB, VB=VB, OT=OT, X1=X1, H2=H2, XS=XS, YS=YS, GRD=GRD, MODR=MODR,
               PSB=PSB, b_PSB=b_PSB, ident_f=ident_f, b_ident_f=b_ident_f, ident_b=ident_b, b_ident_b=b_ident_b,
               ones_b=ones_b, b_ones_b=b_ones_b, ones_f=ones_f, b_ones_f=b_ones_f, tri_b=tri_b, b_tri=b_tri,
               tril_f=tril_f, b_tril=b_tril, iota_e=iota_e, b_iota_e=b_iota_e, iota_b=iota_b, b_iota_b=b_iota_b,
               iota_p=iota_p, b_iota_p=b_iota_p, epsb=epsb, b_epsb=b_epsb,
               G1T=G1T, b_G1T=b_G1T, SH1T=SH1T, b_SH1T=b_SH1T,
               NLAM=NLAM, b_lsm=b_lsm, gsub=gsub, b_gsub=b_gsub, gq=gq, b_gq=b_gq, gk=gk, b_gk=b_gk,
               bcst=bcst, b_bcst=b_bcst, ld=ld, dbg=dbg)
    return ctx


def stage_P(cx):
    nc, S, sb, jobs, J = cx["nc"], cx["S"], cx["sb"], cx["jobs"], cx["J"]
    PSB, b_PSB = cx["PSB"], cx["b_PSB"]
    sst = ExitStack(); cx["stk"].append(sst)
    win = sb("win", [128, 8, DP], BF16); b_win = Buf("win")
    for half in range(2):
        cx["ld"]("pool", win[:, :, half * 1152:(half + 1) * 1152],
                 cx["W"]["w_in"].ap().rearrange("(kc p) n -> p kc n", p=128)[:, :, half * 1152:(half + 1) * 1152],
                 b_win, "win", acc=(half == 1))
    xt = [sb("xt%d" % i, [128, D], F32) for i in range(2)]; b_xt = [Buf("xt%d" % i) for i in range(2)]
    sq = sb("sqj", [128, D], BF16); b_sq = Buf("sqj")
    st1 = sb("st1", [128, 8], F32); b_st1 = Buf("st1")
    xn = sb("xn", [128, D], BF16); b_xn = Buf("xn")
    hT = sb("hT", [128, 8, 512], BF16); b_hT = Buf("hT")
    fmst = sb("fmst", [128, 8, 512], BF16); b_fmst = Buf("fmst")
    vast = sb("vast", [128, 4, 512], BF16); b_vast = Buf("vast")
    qbf = sb("qbf", [128, 512], F32); b_qbf = Buf("qbf")
    kvf = sb("kvf", [128, 256], F32); b_kvf = Buf("kvf")
    rt = sb("ropet", [128, 4, 16], F32); b_rt = Buf("ropet")
    tq = [sb("tq%d" % i, [128, 512], F32) for i in range(3)]; b_tq = [Buf("tq%d" % i) for i in range(3)]
    nst = sb("nst", [128, 16], F32); b_nst = Buf("nst")
    qbb = sb("qbb", [128, 512], BF16); b_qbb = Buf("qbb")
    kbb = sb("kbb", [128, 2, 2, 64], BF16); b_kbb = Buf("kbb")
    qbst = sb("qbst", [128, 4, 512], BF16); b_qbst = Buf("qbst")
    kbst = sb("kbst", [128, 2, 512], BF16); b_kbst = Buf("kbst")
    vbst = sb("vbst", [128, 4, 2, 65], BF16); b_vbst = Buf("vbst")
    S.op("dve", lambda e: e.memset(vbst[:], 1.0), writes=[b_vbst])
    b_D = cx["b_D"] = {}
    gq, gk = cx["gq"], cx["gk"]
    for j in range(J):
        Sk, q0, Sq = jobs[j][:3]
        for n in ("QA", "KA", "VA", "QB", "KB", "VB"):
            b_D[(n, j)] = Buf("%s%d" % (n, j))
        G1 = cx["G1T"][:, :, j:j + 1]
        SH1 = cx["SH1T"][:, :, j:j + 1]
        for ch in range(Sk // 512):
            t0 = ch * 512
            own = (t0 >= q0) and (t0 < q0 + Sq)
            for ti in range(4):
                r0 = t0 + ti * 128
                x_ = xt[ti % 2]; bx = b_xt[ti % 2]
                cx["ld"]("sp", x_[:], cx["xs"][j].ap()[r0:r0 + 128, :], bx, "xt%d" % (ti % 2))
                S.op("act", lambda e, x_=x_: e.activation(out=sq[:], in_=x_[:], func=AF.Square, accum_out=st1[:, 0:1]),
                     reads=[bx], writes=[b_sq, b_st1])
                S.op("act", lambda e: e.activation(out=st1[:, 1:2], in_=st1[:, 0:1], func=AF.Sqrt, scale=1.0 / D, bias=cx["epsb"][:, 0:1]),
                     reads=[cx["b_epsb"]], accs=[b_st1])
                S.op("dve", lambda e: e.reciprocal(out=st1[:, 2:3], in_=st1[:, 1:2]), accs=[b_st1])
                S.op("dve", lambda e, x_=x_: e.tensor_scalar(out=xn[:], in0=x_[:], scalar1=st1[:, 2:3], scalar2=None, op0=ALU.mult),
                     reads=[bx, b_st1], writes=[b_xn])
                pT = PSB[0][:, :].bitcast(BF16).rearrange("p (kc t) -> p kc t", kc=8)
                for kc in range(8):
                    S.op("pe", lambda e, kc=kc, pT=pT: e.transpose(out=pT[:, kc, :], in_=xn[:, kc * 128:(kc + 1) * 128], identity=cx["ident_b"][:]),
                         reads=[b_xn, cx["b_ident_b"]], writes=[b_PSB[0]] if kc == 0 else (), accs=[b_PSB[0]] if kc > 0 else ())
                hs = hT[:, :, ti * 128:(ti + 1) * 128]
                S.op("dve", lambda e, hs=hs, pT=pT, G1=G1: e.tensor_tensor(out=hs, in0=pT, in1=G1.to_broadcast([128, 8, 128]), op=ALU.mult),
                     reads=[b_PSB[0], cx["b_G1T"]], writes=[b_hT] if ti == 0 else (), accs=[b_hT] if ti > 0 else ())
                S.op("dve", lambda e, hs=hs, SH1=SH1: e.tensor_tensor(out=hs, in0=hs, in1=SH1.to_broadcast([128, 8, 128]), op=ALU.add),
                     reads=[cx["b_SH1T"]], accs=[b_hT])
            chunks = ([(h, h) for h in range(4)] if own else []) + [(4 + h, 4 + h) for h in range(4)]
            for i, (slot, wc) in enumerate(chunks):
                pb = b_PSB[1 + (i % 2)]; pt = PSB[1 + (i % 2)]
                for kc in range(8):
                    S.op("pe", lambda e, kc=kc, wc=wc, pt=pt: e.matmul(pt[:, :], lhsT=win[:, kc, wc * 128:(wc + 1) * 128], rhs=hT[:, kc, :],
                                                                     start=(kc == 0), stop=(kc == 7)),
                         reads=[b_win, b_hT], writes=[pb] if kc == 0 else (), accs=[pb] if kc > 0 else ())
                S.op("act", lambda e, slot=slot, pt=pt: e.activation(out=fmst[:, slot, :], in_=pt[:, :], func=AF.Identity),
                     reads=[pb], writes=[b_fmst] if i == 0 else (), accs=[b_fmst] if i > 0 else ())
            if own:
                S.op("act", lambda e, j=j, t0=t0, q0=q0: e.dma_start(out=cx["QA"][j].ap()[:, :, t0 - q0:t0 - q0 + 512].rearrange("h p t -> p h t"),
                                                                  in_=fmst[:, 0:4, :]),
                     reads=[b_fmst], accs=[b_D[("QA", j)]], dma_key="fmst_st")
            S.op("act", lambda e, j=j, t0=t0: e.dma_start(out=cx["KA"][j].ap()[:, :, t0:t0 + 512].rearrange("h p t -> p h t"), in_=fmst[:, 4:8, :]),
                 reads=[b_fmst], accs=[b_D[("KA", j)]], dma_key="fmst_st")
            for ti in range(4):
                r0 = t0 + ti * 128
                for kc in range(8):
                    S.op("pe", lambda e, kc=kc, ti=ti: e.matmul(PSB[3][:, :], lhsT=hT[:, kc, ti * 128:(ti + 1) * 128], rhs=win[:, kc, 1024:1536],
                                                               start=(kc == 0), stop=(kc == 7)),
                         reads=[b_win, b_hT], writes=[b_PSB[3]] if kc == 0 else (), accs=[b_PSB[3]] if kc > 0 else ())
                S.op("act", lambda e, ti=ti: e.activation(out=vast[:, ti, :], in_=PSB[3][:, :], func=AF.Identity),
                     reads=[b_PSB[3]], writes=[b_vast] if ti == 0 else (), accs=[b_vast] if ti > 0 else ())
                for kc in range(8):
                    S.op("pe", lambda e, kc=kc, ti=ti: e.matmul(PSB[4][:, 0:256], lhsT=hT[:, kc, ti * 128:(ti + 1) * 128], rhs=win[:, kc, 2048:2304],
                                                               start=(kc == 0), stop=(kc == 7)),
                         reads=[b_win, b_hT], writes=[b_PSB[4]] if kc == 0 else (), accs=[b_PSB[4]] if kc > 0 else ())
                S.op("dve", lambda e: e.tensor_copy(kvf[:], PSB[4][:, 0:256]), reads=[b_PSB[4]], writes=[b_kvf])
                S.op("dve", lambda e, ti=ti: e.tensor_copy(vbst[:, ti, :, 0:64], kvf[:, 128:256].rearrange("p (h d) -> p h d", h=2)),
                     reads=[b_kvf], accs=[b_vbst])
                cx["ld"]("sp", rt[:], cx["ROPE"][j].ap()[r0:r0 + 128, :, :], b_rt, "ropet")

                def norm_rope(src, H, gtile, b_g, dst_ap, b_dst, b_src):
                    HW = H * 64
                    s3 = src[:, 0:HW].rearrange("p (h d) -> p h d", h=H)
                    a3 = tq[0][:, 0:HW].rearrange("p (h d) -> p h d", h=H)
                    S.op("dve", lambda e: e.tensor_tensor(out=tq[0][:, 0:HW], in0=src[:, 0:HW], in1=src[:, 0:HW], op=ALU.mult),
                         reads=[b_src], writes=[b_tq[0]])
                    S.op("dve", lambda e: e.tensor_reduce(out=nst[:, 0:H], in_=a3, axis=AX.X, op=ALU.add),
                         reads=[b_tq[0]], writes=[b_nst])
                    S.op("act", lambda e: e.activation(out=nst[:, 8:8 + H], in_=nst[:, 0:H], func=AF.Sqrt, scale=1.0 / 64, bias=cx["epsb"][:, 0:1]),
                         reads=[cx["b_epsb"]], accs=[b_nst])
                    S.op("dve", lambda e: e.reciprocal(out=nst[:, 0:H], in_=nst[:, 8:8 + H]), accs=[b_nst])
                    S.op("dve", lambda e: e.tensor_tensor(out=a3, in0=s3, in1=nst[:, 0:H].unsqueeze(2).to_broadcast([128, H, 64]), op=ALU.mult),
                         reads=[b_src, b_nst], writes=[b_tq[0]])
                    S.op("dve", lambda e: e.tensor_tensor(out=a3, in0=a3, in1=gtile[:, :].unsqueeze(1).to_broadcast([128, H, 64]), op=ALU.mult),
                         reads=[b_g], accs=[b_tq[0]])
                    xv = tq[0][:, 0:HW].rearrange("p (h f two d) -> p h f two d", h=H, f=2, two=2)
                    t1 = tq[1][:, 0:HW // 2].rearrange("p (h f d) -> p h f d", h=H, f=2)
                    t2 = tq[2][:, 0:HW // 2].rearrange("p (h f d) -> p h f d", h=H, f=2)
                    cosb = rt[:, 0:4:2, :].unsqueeze(1).to_broadcast([128, H, 2, 16])
                    sinb = rt[:, 1:4:2, :].unsqueeze(1).to_broadcast([128, H, 2, 16])
                    dv = dst_ap.rearrange("p h (f two d) -> p h f two d", f=2, two=2)
                    x1 = xv[:, :, :, 0, :]; x2 = xv[:, :, :, 1, :]
                    S.op("dve", lambda e: e.tensor_tensor(out=t1, in0=x1, in1=cosb, op=ALU.mult), reads=[b_tq[0], b_rt], writes=[b_tq[1]])
                    S.op("dve", lambda e: e.tensor_tensor(out=t2, in0=x2, in1=sinb, op=ALU.mult), reads=[b_tq[0], b_rt], writes=[b_tq[2]])
                    S.op("dve", lambda e: e.tensor_tensor(out=dv[:, :, :, 0, :], in0=t1, in1=t2, op=ALU.subtract),
                         reads=[b_tq[1], b_tq[2]], accs=[b_dst])
                    S.op("dve", lambda e: e.tensor_tensor(out=t1, in0=x1, in1=sinb, op=ALU.mult), reads=[b_tq[0], b_rt], writes=[b_tq[1]])
                    S.op("dve", lambda e: e.tensor_tensor(out=t2, in0=x2, in1=cosb, op=ALU.mult), reads=[b_tq[0], b_rt], writes=[b_tq[2]])
                    S.op("dve", lambda e: e.tensor_tensor(out=dv[:, :, :, 1, :], in0=t1, in1=t2, op=ALU.add),
                         reads=[b_tq[1], b_tq[2]], accs=[b_dst])

                norm_rope(kvf, 2, gk, cx["b_gk"], kbb[:, :, 0, :], b_kbb, b_kvf)
                S.op("dve", lambda e: e.tensor_copy(kbb[:, :, 1, :], kbb[:, :, 0, :]), accs=[b_kbb])
                pK = PSB[5][:, :].bitcast(BF16)[:, 0:256].rearrange("p (h t) -> p h t", h=2)
                for h in range(2):
                    S.op("pe", lambda e, h=h, pK=pK: e.transpose(out=pK[:, h, :], in_=kbb[:, h, :, :].rearrange("p a d -> p (a d)"), identity=cx["ident_b"][:]),
                         reads=[b_kbb, cx["b_ident_b"]], writes=[b_PSB[5]] if h == 0 else (), accs=[b_PSB[5]] if h > 0 else ())
                S.op("dve", lambda e, ti=ti, pK=pK: e.tensor_copy(kbst[:, :, ti * 128:(ti + 1) * 128], pK), reads=[b_PSB[5]],
                     writes=[b_kbst] if ti == 0 else (), accs=[b_kbst] if ti > 0 else ())
                if own:
                    for kc in range(8):
                        S.op("pe", lambda e, kc=kc, ti=ti: e.matmul(PSB[6][:, :], lhsT=hT[:, kc, ti * 128:(ti + 1) * 128], rhs=win[:, kc, 1536:2048],
                                                                   start=(kc == 0), stop=(kc == 7)),
                             reads=[b_win, b_hT], writes=[b_PSB[6]] if kc == 0 else (), accs=[b_PSB[6]] if kc > 0 else ())
                    S.op("dve", lambda e: e.tensor_copy(qbf[:], PSB[6][:, :]), reads=[b_PSB[6]], writes=[b_qbf])
                    norm_rope(qbf, 8, gq, cx["b_gq"], qbb[:, :].rearrange("p (h d) -> p h d", h=8), b_qbb, b_qbf)
                    pQ = PSB[7][:, :].bitcast(BF16)[:, 0:512].rearrange("p (c t) -> p c t", c=4)
                    for c in range(4):
                        S.op("pe", lambda e, c=c, pQ=pQ: e.transpose(out=pQ[:, c, :], in_=qbb[:, c * 128:(c + 1) * 128], identity=cx["ident_b"][:]),
                             reads=[b_qbb, cx["b_ident_b"]], writes=[b_PSB[7]] if c == 0 else (), accs=[b_PSB[7]] if c > 0 else ())
                    S.op("dve", lambda e, ti=ti, pQ=pQ: e.tensor_copy(qbst[:, :, ti * 128:(ti + 1) * 128], pQ), reads=[b_PSB[7]],
                         writes=[b_qbst] if ti == 0 else (), accs=[b_qbst] if ti > 0 else ())
            S.op("act", lambda e, j=j, t0=t0: e.dma_start(out=cx["VA"][j].ap()[t0:t0 + 512, :].rearrange("(a p) n -> p a n", p=128), in_=vast[:]),
                 reads=[b_vast], accs=[b_D[("VA", j)]], dma_key="vast_st")
            S.op("act", lambda e, j=j, t0=t0: e.dma_start(out=cx["VB"][j].ap()[t0:t0 + 512, :, :].rearrange("(a p) h d -> p a h d", p=128), in_=vbst[:]),
                 reads=[b_vbst], accs=[b_D[("VB", j)]], dma_key="vbst_st")
            S.op("act", lambda e, j=j, t0=t0: e.dma_start(out=cx["KB"][j].ap()[:, :, t0:t0 + 512].rearrange("h p t -> p h t"), in_=kbst[:]),
                 reads=[b_kbst], accs=[b_D[("KB", j)]], dma_key="kbst_st")
            if own:
                S.op("act", lambda e, j=j, t0=t0, q0=q0: e.dma_start(out=cx["QB"][j].ap()[:, :, t0 - q0:t0 - q0 + 512].rearrange("c p t -> p c t"), in_=qbst[:]),
                     reads=[b_qbst], accs=[b_D[("QB", j)]], dma_key="qbst_st")
    sst.close(); cx["stk"].pop()
    S.barrier()


def stage_T(cx):
    nc, S, sb, jobs, J = cx["nc"], cx["S"], cx["sb"], cx["jobs"], cx["J"]
    PSB, b_PSB = cx["PSB"], cx["b_PSB"]
    b_D = cx["b_D"]
    ld = cx["ld"]
    sst = ExitStack(); cx["stk"].append(sst)
    SKM = max(j_[0] for j_ in jobs); SQM = max(j_[2] for j_ in jobs)
    jrev = sb("jrev", [128, 128], F32); b_jrev = Buf("jrev")
    ld("sp", jrev[:], cx["C"]["jrev"].ap(), b_jrev, "jrev")
    hk = sb("hk", [128, 1152], F32); b_hk = Buf("hk")
    STR = sb("STR", [128, 4, 1152], F32); b_STR = Buf("STR")
    for h in range(4):
        ld("sp", hk[:], bass.AP(cx["GRD"], h * 1280, [[1, 128], [1, 1152]]), b_hk, "hk")
        for i, (a_, n_) in enumerate([(0, 512), (512, 512), (1024, 128)]):
            S.op("pe", lambda e, a_=a_, n_=n_, i=i: e.matmul(PSB[i][:, 0:n_], lhsT=jrev[:, :], rhs=hk[:, a_:a_ + n_], start=True, stop=True),
                 reads=[b_jrev, b_hk], writes=[b_PSB[i]])
            S.op("dve", lambda e, a_=a_, n_=n_, i=i, h=h: e.tensor_copy(STR[:, h, a_:a_ + n_], PSB[i][:, 0:n_]),
                 reads=[b_PSB[i]], accs=[b_STR])
    W = cx["W"]
    b_WB = cx["b_WB"] = Buf("WB")
    srcs = (W["w_exp_gate"].ap().rearrange("e (p kc) f -> (e p) (kc f)", kc=8),
            W["w_exp_up"].ap().rearrange("e (p kc) f -> (e p) (kc f)", kc=8),
            W["w_exp_down"].ap().rearrange("e (p fc) d -> (e p) (fc d)", fc=2))
    RCH = 512
    cast_jobs = []
    for r0_ in range(0, NE * 128, RCH):
        for wi in range(3):
            cast_jobs.append((r0_, wi))

    def issue_casts(n, gate_buf):
        for _ in range(n):
            if not cast_jobs:
                return
            r0_, wi = cast_jobs.pop(0)
            S.op("pool", lambda e, r0_=r0_, wi=wi: e.dma_start(out=(cx["WBGU"].ap()[r0_:r0_ + RCH, wi * 2048:(wi + 1) * 2048] if wi < 2 else cx["WBD"].ap()[r0_:r0_ + RCH, :]), in_=srcs[wi][r0_:r0_ + RCH, :]),
                 reads=[gate_buf], accs=[b_WB], dma_key="wb_cast")
    NB2 = 2
    kT = [sb("kT%d" % i, [128, SKM], BF16) for i in range(NB2)]; b_kT = [Buf("kT%d" % i) for i in range(NB2)]
    vh = [sb("vh%d" % i, [128, SKM // 128, 128], BF16) for i in range(NB2)]; b_vh = [Buf("vh%d" % i) for i in range(NB2)]
    qT = [sb("qT%d" % i, [128, SQM], BF16) for i in range(NB2)]; b_qT = [Buf("qT%d" % i) for i in range(NB2)]
    NPT = 3
    PSALL = cx["PSALL"]
    pT = [sb("pT%d" % i, [128, 2, 512], BF16) for i in range(NPT)]; b_pT = [Buf("pT%d" % i) for i in range(NPT)]
    sbi = [sb("sbi%d" % i, [128, 2, 512], F32) for i in range(2)]; b_sbi = [Buf("sbi%d" % i) for i in range(2)]
    ft = [sb("ft%d" % i, [128, 512], F32) for i in range(5)]; b_ft = [Buf("ft%d" % i) for i in range(5)]
    sqb = sb("sqb", [128, 512], BF16); b_sqb = Buf("sqb")
    oTs = [sb("oTs%d" % i, [128, 512], BF16) for i in range(2)]; b_oTs = [Buf("oTs%d" % i) for i in range(2)]
    b_OT = cx["b_OT"] = Buf("OT")
    cnt = dict(pt=0, sbi=0, hd=0, ots=0, fin=0)
    tok0 = 0
    dacc = [sb("dacc%d" % i, [128, 2, 512], F32) for i in range(2)]
    b_dacc = [Buf("dacc%d" % i) for i in range(2)]
    dacc_pend = []
    pending = []

    def run_pending(kb, force=False):
        if not pending:
            return
        ph = pending[0]
        trig = (1, 2, 8)
        while ph and (force or kb >= trig[3 - len(ph)]):
            ph.pop(0)()
        if not ph:
            pending.pop(0)

    hfv = sb("hfv", [128, 2], F32); b_hfv = Buf("hfv")
    ld("sp", hfv[:], cx["C"]["hfv"].ap(), b_hfv, "hfv")
    BC2 = sb("BC2", [128, 8], F32); b_BC2 = Buf("BC2")
    SP1 = sb("SP1", [128, 4, 512], F32); b_SP1 = Buf("SP1")
    SP2 = sb("SP2", [128, 4, 512], F32); b_SP2 = Buf("SP2")
    bc = cx["bcst"]
    hb0 = sb("hb0", [128, 8], F32); b_hb0 = Buf("hb0")
    S.op("dve", lambda e: e.tensor_scalar(out=hb0[:, 0:4], in0=bc[:, 0, :], scalar1=hfv[:, 0:1], scalar2=None, op0=ALU.mult),
         reads=[cx["b_bcst"], b_hfv], writes=[b_hb0])
    S.op("dve", lambda e: e.tensor_scalar(out=hb0[:, 4:8], in0=bc[:, 1, :], scalar1=hfv[:, 1:2], scalar2=None, op0=ALU.mult),
         reads=[cx["b_bcst"], b_hfv], accs=[b_hb0])
    S.op("dve", lambda e: e.tensor_tensor(out=BC2[:, 0:4], in0=hb0[:, 0:4], in1=hb0[:, 4:8], op=ALU.add), reads=[b_hb0], writes=[b_BC2])
    for h in range(4):
        S.op("dve", lambda e, h=h: e.tensor_scalar(out=SP1[:, h, :], in0=STR[:, h, 0:512], scalar1=hfv[:, 1:2], scalar2=hb0[:, h:h + 1], op0=ALU.mult, op1=ALU.add),
             reads=[b_STR, b_hfv, b_hb0], accs=[b_SP1])
        S.op("dve", lambda e, h=h: e.tensor_scalar(out=SP2[:, h, :], in0=STR[:, h, 640:1152], scalar1=hfv[:, 0:1], scalar2=hb0[:, 4 + h:5 + h], op0=ALU.mult, op1=ALU.add),
             reads=[b_STR, b_hfv, b_hb0], accs=[b_SP2])
    for j in range(J):
        Sk, q0, Sq = jobs[j][:3]
        split = len(jobs[j]) > 3 and jobs[j][3]
        NKB = Sk // 128
        NKH = Sq // 128
        NQC = Sq // 512
        for h in range(4):
            hb = cnt["hd"] % NB2; cnt["hd"] += 1
            ld("sp", kT[hb][:, 0:Sk], cx["KA"][j].ap()[h, :, :], b_kT[hb], "kT%d" % hb, reads=[b_D[("KA", j)]])
            ld("sp", qT[hb][:, 0:Sq], cx["QA"][j].ap()[h, :, :], b_qT[hb], "qT%d" % hb, reads=[b_D[("QA", j)]])
            ld("sp", vh[hb][:, 0:NKB, :], cx["VA"][j].ap()[:, h * 128:(h + 1) * 128].rearrange("(a p) n -> p a n", p=128),
               b_vh[hb], "vh%d" % hb, reads=[b_D[("VA", j)]])
            for qc in range(Sq // 512):
                qa = q0 + qc * 512
                fi = cnt["fin"] % 2; cnt["fin"] += 1
                ob = (4 + 2 * fi, 5 + 2 * fi)
                if split or J == 1:
                    issue_casts(3, b_pT[cnt["pt"] % NPT])

                def issue_S(kb, hb=hb, qc=qc):
                    for m in range(2):
                        bank = (kb % 2) * 2 + m
                        S.op("pe", lambda e, kb=kb, m=m, bank=bank: e.matmul(PSB[bank][:, :], lhsT=kT[hb][m * 64:(m + 1) * 64, kb * 128:(kb + 1) * 128],
                                                                          rhs=qT[hb][m * 64:(m + 1) * 64, qc * 512:(qc + 1) * 512], start=True, stop=True),
                             reads=[b_kT[hb], b_qT[hb]], writes=[b_PSB[bank]])
                issue_S(0)
                for kb in range(NKB):
                    if kb + 1 < NKB:
                        issue_S(kb + 1)
                    run_pending(kb)
                    d = kb * 128 - qa
                    mode = "std"
                    if split and kb >= NKH:
                        if qc == NQC - 1 and kb == NKH:
                            mode = "sp1"
                        elif qc == 0 and kb == NKB - 1:
                            mode = "sp2"
                        else:
                            mode = "far2"
                    first = (kb == 0); last = (kb == NKB - 1)
                    sb_ = (kb % 2) * 2
                    pi = cnt["pt"] % NPT; cnt["pt"] += 1
                    if mode in ("sp1", "sp2") or (mode == "std" and -128 <= d <= 512):
                        if mode == "std":
                            bt = STR[:, h, 512 - d:1024 - d]; bb = b_STR
                        else:
                            bt = (SP1 if mode == "sp1" else SP2)[:, h, :]; bb = b_SP1 if mode == "sp1" else b_SP2
                        si = cnt["sbi"] % 2; cnt["sbi"] += 1
                        for m in range(2):
                            S.op("dve", lambda e, m=m, sb_=sb_, si=si, bt=bt: e.scalar_tensor_tensor(
                                out=sbi[si][:, m, :], in0=PSB[sb_ + m][:, :], scalar=0.125, in1=bt, op0=ALU.mult, op1=ALU.add),
                                reads=[b_PSB[sb_ + m], bb], writes=[b_sbi[si]] if m == 0 else (), accs=[b_sbi[si]] if m else ())
                        S.op("act", lambda e, si=si, pi=pi: e.activation(out=pT[pi][:], in_=sbi[si][:], func=AF.Exp),
                             reads=[b_sbi[si]], writes=[b_pT[pi]])
                    else:
                        if mode == "far2":
                            bias_ap = BC2[:, h:h + 1]; bbuf = b_BC2
                        else:
                            sg_ = 1 if d > 0 else 0
                            bias_ap = cx["bcst"][:, sg_, h:h + 1]; bbuf = cx["b_bcst"]
                        S.op("act", lambda e, sb_=sb_, pi=pi, bias_ap=bias_ap: e.activation(out=pT[pi][:], in_=PSALL[:, sb_:sb_ + 2, :], func=AF.Exp,
                                                                                         scale=0.125, bias=bias_ap),
                             reads=[b_PSB[sb_], b_PSB[sb_ + 1], bbuf], writes=[b_pT[pi]])
                    for m in range(2):
                        S.op("pe", lambda e, kb=kb, m=m, pi=pi, first=first, last=last, hb=hb, obk=ob[m]: e.matmul(PSB[obk][:, :], lhsT=vh[hb][:, kb, :], rhs=pT[pi][:, m, :],
                                                                                                          start=first, stop=last),
                             reads=[b_vh[hb], b_pT[pi]], writes=[b_PSB[ob[m]]] if first else (), accs=[b_PSB[ob[m]]] if not first else ())
                    for fnd in dacc_pend:
                        fnd()
                    dacc_pend.clear()
                    if first:
                        dacc_pend.append(lambda pi=pi, fi=fi: S.op(
                            "dve", lambda e: e.tensor_copy(dacc[fi][:], pT[pi][:]), reads=[b_pT[pi]], writes=[b_dacc[fi]]))
                    else:
                        dacc_pend.append(lambda pi=pi, fi=fi: S.op(
                            "dve", lambda e: e.tensor_tensor(out=dacc[fi][:], in0=dacc[fi][:], in1=pT[pi][:], op=ALU.add),
                            reads=[b_pT[pi]], accs=[b_dacc[fi]]))
                for fnd in dacc_pend:
                    fnd()
                dacc_pend.clear()
                if pending:
                    run_pending(0, force=True)
                oi = cnt["ots"] % 2; cnt["ots"] += 1
                c0 = tok0 + qc * 512

                def phA(ob=ob, fi=fi):
                    for m in range(2):
                        if m == 0:
                            S.op("act", lambda e, m=m: e.activation(out=ft[m][:], in_=PSB[ob[m]][:, :], func=AF.Identity), reads=[b_PSB[ob[m]]], writes=[b_ft[m]])
                        else:
                            S.op("dve", lambda e, m=m: e.tensor_copy(ft[m][:], PSB[ob[m]][:, :]), reads=[b_PSB[ob[m]]], writes=[b_ft[m]])
                        S.op("pe", lambda e, m=m: e.matmul(PSB[ob[m]][:, :], lhsT=cx["ones_f"][:, :], rhs=dacc[fi][:, m, :], start=True, stop=True),
                             reads=[cx["b_ones_f"], b_dacc[fi]], writes=[b_PSB[ob[m]]])

                def phB1(ob=ob):
                    for m in range(2):
                        S.op("dve", lambda e, m=m: e.reciprocal(out=ft[3][:], in_=PSB[ob[m]][:, :]), reads=[b_PSB[ob[m]]], writes=[b_ft[3]])
                        S.op("dve", lambda e, m=m: e.tensor_tensor(out=ft[m][:], in0=ft[m][:], in1=ft[3][:], op=ALU.mult), reads=[b_ft[3]], accs=[b_ft[m]])
                    S.op("dve", lambda e: e.scalar_tensor_tensor(out=ft[2][:], in0=ft[1][:], scalar=cx["NLAM"], in1=ft[0][:], op0=ALU.mult, op1=ALU.add),
                         reads=[b_ft[0], b_ft[1], cx["b_lsm"]], writes=[b_ft[2]])
                    S.op("dve", lambda e: e.tensor_tensor(out=sqb[:], in0=ft[2][:], in1=ft[2][:], op=ALU.mult), reads=[b_ft[2]], writes=[b_sqb])
                    S.op("pe", lambda e: e.matmul(PSB[ob[0]][:, :], lhsT=cx["ones_b"][:, :], rhs=sqb[:], start=True, stop=True),
                         reads=[cx["b_ones_b"], b_sqb], writes=[b_PSB[ob[0]]])

                def phB2(ob=ob, oi=oi, h=h, c0=c0):
                    S.op("act", lambda e: e.activation(out=ft[3][:], in_=PSB[ob[0]][:, :], func=AF.Sqrt, scale=1.0 / 128, bias=cx["epsb"][:, 0:1]),
                         reads=[b_PSB[ob[0]], cx["b_epsb"]], writes=[b_ft[3]])
                    S.op("dve", lambda e: e.reciprocal(out=ft[4][:], in_=ft[3][:]), reads=[b_ft[3]], writes=[b_ft[4]])
                    S.op("dve", lambda e: e.scalar_tensor_tensor(out=oTs[oi][:], in0=ft[2][:], scalar=cx["gsub"][:, 0:1], in1=ft[4][:],
                                                                 op0=ALU.mult, op1=ALU.mult),
                         reads=[b_ft[2], b_ft[4], cx["b_gsub"]], writes=[b_oTs[oi]])
                    S.op("act", lambda e: e.dma_start(out=cx["OT"].ap()[h, :, c0:c0 + 512], in_=oTs[oi][:]),
                         reads=[b_oTs[oi]], accs=[b_OT], dma_key="oTs%d_st" % oi)
                pending.append([phA, phB1, phB2])
        while pending:
            run_pending(0, force=True)
        for n in range(2):
            hb = cnt["hd"] % NB2; cnt["hd"] += 1
            ld("sp", kT[hb][:, 0:Sk], cx["KB"][j].ap()[n, :, :], b_kT[hb], "kT%d" % hb, reads=[b_D[("KB", j)]])
            vb1 = vh[hb][:, :, :].rearrange("p a n -> p (a n)")[:, 0:NKB * 65].rearrange("p (a n) -> p a n", n=65)
            ld("sp", vb1, cx["VB"][j].ap()[:, n, :].rearrange("(a p) n -> p a n", p=128), b_vh[hb], "vh%d" % hb, reads=[b_D[("VB", j)]])
            for cpair in range(2):
                c = 2 * n + cpair
                qb_ = cnt["hd"] % NB2 if False else (hb + cpair) % NB2
                ld("sp", qT[qb_][:, 0:Sq], cx["QB"][j].ap()[c, :, :], b_qT[qb_], "qT%d" % qb_, reads=[b_D[("QB", j)]])
                for qc in range(Sq // 512):
                    def issue_SB(kb, hb=hb, qb_=qb_, qc=qc):
                        for hh in range(2):
                            bank = (kb % 2) * 2 + hh
                            S.op("pe", lambda e, kb=kb, hh=hh, bank=bank: e.matmul(PSB[bank][:, :], lhsT=kT[hb][hh * 64:(hh + 1) * 64, kb * 128:(kb + 1) * 128],
                                                                                rhs=qT[qb_][hh * 64:(hh + 1) * 64, qc * 512:(qc + 1) * 512], start=True, stop=True),
                                 reads=[b_kT[hb], b_qT[qb_]], writes=[b_PSB[bank]])
                    if split or J == 1:
                        issue_casts(3, b_pT[cnt["pt"] % NPT])
                    issue_SB(0)
                    for kb in range(NKB):
                        if kb + 1 < NKB:
                            issue_SB(kb + 1)
                        first = (kb == 0); last = (kb == NKB - 1)
                        sb_ = (kb % 2) * 2
                        pi = cnt["pt"] % NPT; cnt["pt"] += 1
                        S.op("act", lambda e, sb_=sb_, pi=pi: e.activation(out=pT[pi][:], in_=PSALL[:, sb_:sb_ + 2, :], func=AF.Exp, scale=0.125),
                             reads=[b_PSB[sb_], b_PSB[sb_ + 1]], writes=[b_pT[pi]])
                        for hh in range(2):
                            S.op("pe", lambda e, kb=kb, hh=hh, pi=pi, first=first, last=last, vb1=vb1: e.matmul(PSB[4 + hh][0:65, :], lhsT=vb1[:, kb, :], rhs=pT[pi][:, hh, :],
                                                                                                             start=first, stop=last),
                                 reads=[b_vh[hb], b_pT[pi]], writes=[b_PSB[4 + hh]] if first else (), accs=[b_PSB[4 + hh]] if not first else ())
                    for hh in range(2):
                        S.op("dve", lambda e, hh=hh: e.reciprocal(out=ft[hh][64:65, :], in_=PSB[4 + hh][64:65, :]),
                             reads=[b_PSB[4 + hh]], writes=[b_ft[hh]])
                        S.op("pe", lambda e, hh=hh: e.matmul(PSB[6 + hh][0:64, :], lhsT=cx["ones_f"][64:65, 0:64], rhs=ft[hh][64:65, :], start=True, stop=True),
                             reads=[cx["b_ones_f"], b_ft[hh]], writes=[b_PSB[6 + hh]])
                        S.op("act", lambda e, hh=hh: e.activation(out=ft[2 + hh][0:64, :], in_=PSB[6 + hh][0:64, :], func=AF.Identity),
                             reads=[b_PSB[6 + hh]], writes=[b_ft[2 + hh]])
                        oi = cnt["ots"] % 2; cnt["ots"] += 1
                        S.op("dve", lambda e, hh=hh, oi=oi: e.tensor_tensor(out=oTs[oi][0:64, :], in0=PSB[4 + hh][0:64, :], in1=ft[2 + hh][0:64, :], op=ALU.mult),
                             reads=[b_PSB[4 + hh], b_ft[2 + hh]], writes=[b_oTs[oi]])
                        c0 = tok0 + qc * 512
                        S.op("act", lambda e, oi=oi, c=c, hh=hh, c0=c0: e.dma_start(out=cx["OT"].ap()[4 + c, hh * 64:(hh + 1) * 64, c0:c0 + 512], in_=oTs[oi][0:64, :]),
                             reads=[b_oTs[oi]], accs=[b_OT], dma_key="oTs%d_st" % oi)
        tok0 += Sq
    issue_casts(10 ** 6, b_pT[0])
    sst.close(); cx["stk"].pop()
    S.barrier()


def stage_O(cx):
    nc, S, sb, jobs, J = cx["nc"], cx["S"], cx["sb"], cx["jobs"], cx["J"]
    PSB, b_PSB = cx["PSB"], cx["b_PSB"]
    ld = cx["ld"]; W = cx["W"]
    NT, NBLK = cx["NT"], cx["NBLK"]
    st = cx["st"]; cx["stk"].append(st)
    E8 = sb("E8", [128, NT, 8], F32); b_E8 = Buf("E8")
    R8 = sb("R8", [128, NT, 8], F32); b_R8 = Buf("R8")
    G8 = sb("G8", [128, NT, 8], F32); b_G8 = Buf("G8")
    SL8 = sb("SL8", [128, NT, 8], I32); b_SL8 = Buf("SL8")
    IDXW = sb("IDXW", [128, NBLK], I32); b_IDXW = Buf("IDXW")
    IDXD = sb("IDXD", [128, NBLK], I32); b_IDXD = Buf("IDXD")
    IDXD2 = sb("IDXD2", [128, NBLK], I32); b_IDXD2 = Buf("IDXD2")
    cx["IDXD2"] = IDXD2; cx["b_IDXD2"] = b_IDXD2
    cx["stk"].pop()
    cx.update(E8=E8, b_E8=b_E8, R8=R8, b_R8=b_R8, G8=G8, b_G8=b_G8, SL8=SL8, b_SL8=b_SL8,
              IDXW=IDXW, b_IDXW=b_IDXW, IDXD=IDXD, b_IDXD=b_IDXD)
    sst = ExitStack(); cx["stk"].append(sst)
    rd = lambda n: W[n].ap().rearrange("(kc p) n -> p kc n", p=128)
    wout = sb("wout", [128, 8, D], BF16); b_wout = Buf("wout")
    wr = sb("wr", [128, 8, NE], BF16); b_wr = Buf("wr")
    wsgu = sb("wsgu", [128, 8, 512], BF16); b_wsgu = Buf("wsgu")
    wsd = sb("wsd", [128, 2, D], BF16); b_wsd = Buf("wsd")
    ld("pool", wout[:], rd("w_out"), b_wout, "wout")
    ld("pool", wr[:], rd("w_router"), b_wr, "wr")
    ld("pool", wsgu[:, :, 0:256], rd("w_sh_gate"), b_wsgu, "wsgu")
    ld("pool", wsgu[:, :, 256:512], rd("w_sh_up"), b_wsgu, "wsgu", acc=True)
    ld("pool", wsd[:], W["w_sh_down"].ap().rearrange("(fc p) n -> p fc n", p=128), b_wsd, "wsd")
    rbias = sb("rbias", [128, NE], F32); b_rbias = Buf("rbias")
    ld("sp", rbias[:], bass.AP(W["router_bias"], 0, [[0, 128], [1, NE]]), b_rbias, "rbias")
    E1 = sb("E1", [128, NE], F32); b_E1 = Buf("E1")
    S.op("dve", lambda e: e.tensor_scalar(out=E1[:], in0=cx["iota_e"][:], scalar1=16384.0, scalar2=1.0, op0=ALU.mult, op1=ALU.add),
         reads=[cx["b_iota_e"]], writes=[b_E1])
    Mcum = sb("Mcum", [128, NE], BF16); b_Mcum = Buf("Mcum")
    S.op("dve", lambda e: e.memset(Mcum[:], 0.0), writes=[b_Mcum])
    MOD = [sb("mod%d" % v, [128, D], F32) for v in range(4)]; b_MOD = [Buf("mod%d" % v) for v in range(4)]
    oT = sb("oT", [128, 8, 512], BF16); b_oT = Buf("oT")
    xt = [sb("xo%d" % i, [128, D], F32) for i in range(2)]; b_xt = [Buf("xo%d" % i) for i in range(2)]
    sqj = sb("sqo", [128, D], BF16); b_sqj = Buf("sqo")
    st1 = sb("sto", [128, 8], F32); b_st1 = Buf("sto")
    tmp = sb("tmpo", [128, D], F32); b_tmp = Buf("tmpo")
    h2b = [sb("h2b%d" % i, [128, D], BF16) for i in range(2)]; b_h2b = [Buf("h2b%d" % i) for i in range(2)]
    h2T = sb("h2T", [128, 8, 128], BF16); b_h2T = Buf("h2T")
    sgt = sb("sgt", [128, 256], F32); b_sgt = Buf("sgt")
    ab = sb("ab", [128, 256], BF16); b_ab = Buf("ab")
    aT = sb("aT", [128, 2, 128], BF16); b_aT = Buf("aT")
    scr = sb("scr", [128, NE], F32); b_scr = Buf("scr")
    sel = sb("sel", [128, NE], F32); b_sel = Buf("sel")
    selm = sb("selm", [128, NE], F32); b_selm = Buf("selm")
    g8 = sb("g8", [128, 8, 8], F32); b_g8 = Buf("g8")
    gs = sb("gs", [128, 32], F32); b_gs = Buf("gs")
    Mb = sb("Mb", [128, NE], BF16); b_Mb = Buf("Mb")
    wg = sb("wg", [128, NE], F32); b_wg = Buf("wg")
    pvm = sb("pvm", [128, NE], F32); b_pvm = Buf("pvm")
    p8 = sb("p8", [128, 8], F32); b_p8 = Buf("p8")
    p8i = sb("p8i", [128, 16], I32); b_p8i = Buf("p8i")
    junk = sb("junko", [128, NE], F32); b_junk = Buf("junko")
    b_X1 = cx["b_X1"] = Buf("X1")
    b_H2 = cx["b_H2"] = Buf("H2")
    b_MODR = Buf("MODRr")
    tok0 = 0
    tg = 0
    for j in range(J):
        Sk, q0, Sq = jobs[j][:3]
        for v in range(4):
            ld("sp", MOD[v][:], bass.AP(cx["MODR"], (j * 4 + v) * D, [[0, 128], [1, D]]), b_MOD[v], "mod%d" % v)
        GT1B, G2B, SH2B, GT2B = MOD
        for ch in range(Sq // 512):
            c0 = tok0 + ch * 512
            ld("sp", oT[:], cx["OT"].ap()[:, :, c0:c0 + 512].rearrange("c p t -> p c t"), b_oT, "oT", reads=[cx["b_OT"]])
            for ti in range(4):
                g0 = c0 + ti * 128
                r0 = q0 + ch * 512 + ti * 128
                x_ = xt[tg % 2]; bx = b_xt[tg % 2]
                hb_ = h2b[tg % 2]; bh = b_h2b[tg % 2]
                ld("sp", x_[:], cx["xs"][j].ap()[r0:r0 + 128, :], bx, "xo%d" % (tg % 2))
                for half in range(2):
                    for c in range(8):
                        S.op("pe", lambda e, half=half, c=c, ti=ti: e.matmul(PSB[half][:, :], lhsT=oT[:, c, ti * 128:(ti + 1) * 128],
                                                                            rhs=wout[:, c, half * 512:(half + 1) * 512], start=(c == 0), stop=(c == 7)),
                             reads=[b_oT, b_wout], writes=[b_PSB[half]] if c == 0 else (), accs=[b_PSB[half]] if c > 0 else ())
                    sl = slice(half * 512, (half + 1) * 512)
                    S.op("dve", lambda e, half=half, sl=sl: e.tensor_tensor(out=tmp[:, sl], in0=PSB[half][:, :], in1=GT1B[:, sl], op=ALU.mult),
                         reads=[b_PSB[half], b_MOD[0]], writes=[b_tmp] if half == 0 else (), accs=[b_tmp] if half == 1 else ())
                    S.op("dve", lambda e, sl=sl, x_=x_: e.tensor_tensor(out=x_[:, sl], in0=x_[:, sl], in1=tmp[:, sl], op=ALU.add),
                         reads=[b_tmp], accs=[bx])
                S.op("act", lambda e, x_=x_: e.activation(out=sqj[:], in_=x_[:], func=AF.Square, accum_out=st1[:, 0:1]),
                     reads=[bx], writes=[b_sqj, b_st1])
                S.op("act", lambda e: e.activation(out=st1[:, 1:2], in_=st1[:, 0:1], func=AF.Sqrt, scale=1.0 / D, bias=cx["epsb"][:, 0:1]),
                     reads=[cx["b_epsb"]], accs=[b_st1])
                S.op("dve", lambda e: e.reciprocal(out=st1[:, 2:3], in_=st1[:, 1:2]), accs=[b_st1])
                S.op("dve", lambda e, x_=x_: e.scalar_tensor_tensor(out=tmp[:], in0=x_[:], scalar=st1[:, 2:3], in1=G2B[:], op0=ALU.mult, op1=ALU.mult),
                     reads=[bx, b_st1, b_MOD[1]], writes=[b_tmp])
                S.op("dve", lambda e, hb_=hb_: e.tensor_tensor(out=hb_[:], in0=tmp[:], in1=SH2B[:], op=ALU.add),
                     reads=[b_tmp, b_MOD[2]], writes=[bh])
                S.op("act", lambda e, hb_=hb_, g0=g0: e.dma_start(out=cx["H2"].ap()[g0:g0 + 128, :], in_=hb_[:]),
                     reads=[bh], accs=[b_H2], dma_key="h2b%d_st" % (tg % 2))
                pT_ = PSB[2][:, :].bitcast(BF16).rearrange("p (kc t) -> p kc t", kc=8)
                for kc in range(8):
                    S.op("pe", lambda e, kc=kc, hb_=hb_, pT_=pT_: e.transpose(out=pT_[:, kc, :], in_=hb_[:, kc * 128:(kc + 1) * 128], identity=cx["ident_b"][:]),
                         reads=[bh, cx["b_ident_b"]], writes=[b_PSB[2]] if kc == 0 else (), accs=[b_PSB[2]] if kc > 0 else ())
                S.op("act", lambda e, pT_=pT_: e.activation(out=h2T[:], in_=pT_, func=AF.Identity), reads=[b_PSB[2]], writes=[b_h2T])
                for kc in range(8):
                    S.op("pe", lambda e, kc=kc: e.matmul(PSB[3][:, 0:NE], lhsT=h2T[:, kc, :], rhs=wr[:, kc, :], start=(kc == 0), stop=(kc == 7)),
                         reads=[b_h2T, b_wr], writes=[b_PSB[3]] if kc == 0 else (), accs=[b_PSB[3]] if kc > 0 else ())
                for kc in range(8):
                    S.op("pe", lambda e, kc=kc: e.matmul(PSB[4][:, :], lhsT=h2T[:, kc, :], rhs=wsgu[:, kc, :], start=(kc == 0), stop=(kc == 7)),
                         reads=[b_h2T, b_wsgu], writes=[b_PSB[4]] if kc == 0 else (), accs=[b_PSB[4]] if kc > 0 else ())
                S.op("act", lambda e: e.activation(out=sgt[:], in_=PSB[4][:, 0:256], func=AF.Silu), reads=[b_PSB[4]], writes=[b_sgt])
                S.op("dve", lambda e: e.tensor_tensor(out=ab[:], in0=PSB[4][:, 256:512], in1=sgt[:], op=ALU.mult),
                     reads=[b_PSB[4], b_sgt], writes=[b_ab])
                pA = PSB[5][:, :].bitcast(BF16)[:, 0:256].rearrange("p (c t) -> p c t", c=2)
                for fc in range(2):
                    S.op("pe", lambda e, fc=fc, pA=pA: e.transpose(out=pA[:, fc, :], in_=ab[:, fc * 128:(fc + 1) * 128], identity=cx["ident_b"][:]),
                         reads=[b_ab, cx["b_ident_b"]], writes=[b_PSB[5]] if fc == 0 else (), accs=[b_PSB[5]] if fc > 0 else ())
                S.op("act", lambda e, pA=pA: e.activation(out=aT[:], in_=pA, func=AF.Identity), reads=[b_PSB[5]], writes=[b_aT])
                for half in range(2):
                    for fc in range(2):
                        S.op("pe", lambda e, half=half, fc=fc: e.matmul(PSB[6 + half][:, :], lhsT=aT[:, fc, :], rhs=wsd[:, fc, half * 512:(half + 1) * 512],
                                                                       start=(fc == 0), stop=(fc == 1)),
                             reads=[b_aT, b_wsd], writes=[b_PSB[6 + half]] if fc == 0 else (), accs=[b_PSB[6 + half]] if fc > 0 else ())
                    sl = slice(half * 512, (half + 1) * 512)
                    S.op("dve", lambda e, half=half, sl=sl: e.tensor_tensor(out=tmp[:, sl], in0=PSB[6 + half][:, :], in1=GT2B[:, sl], op=ALU.mult),
                         reads=[b_PSB[6 + half], b_MOD[3]], writes=[b_tmp] if half == 0 else (), accs=[b_tmp] if half == 1 else ())
                    S.op("dve", lambda e, sl=sl, x_=x_: e.tensor_tensor(out=x_[:, sl], in0=x_[:, sl], in1=tmp[:, sl], op=ALU.add),
                         reads=[b_tmp], accs=[bx])
                S.op("act", lambda e, x_=x_, g0=g0: e.dma_start(out=cx["X1"].ap()[g0:g0 + 128, :], in_=x_[:]),
                     reads=[bx], accs=[b_X1], dma_key="xo%d_st" % (tg % 2))
                S.op("act", lambda e: e.activation(out=scr[:], in_=PSB[3][:, 0:NE], func=AF.Sigmoid), reads=[b_PSB[3]], writes=[b_scr])
                S.op("dve", lambda e: e.tensor_tensor(out=sel[:], in0=scr[:], in1=rbias[:], op=ALU.add), reads=[b_scr, b_rbias], writes=[b_sel])
                for g in range(8):
                    S.op("dve", lambda e, g=g: e.max(out=g8[:, g, :], in_=sel[:, g * 32:(g + 1) * 32]), reads=[b_sel],
                         writes=[b_g8] if g == 0 else (), accs=[b_g8] if g > 0 else ())
                S.op("dve", lambda e: e.tensor_tensor(out=gs[:, 0:8], in0=g8[:, :, 0], in1=g8[:, :, 1], op=ALU.add), reads=[b_g8], writes=[b_gs])
                S.op("dve", lambda e: e.max(out=gs[:, 8:16], in_=gs[:, 0:8]), accs=[b_gs])
                S.op("dve", lambda e: e.tensor_scalar(out=gs[:, 16:24], in0=gs[:, 0:8], scalar1=gs[:, 11:12], scalar2=None, op0=ALU.is_ge), accs=[b_gs])
                S.op("dve", lambda e: e.scalar_tensor_tensor(out=selm[:].rearrange("p (g i) -> p g i", g=8), in0=sel[:].rearrange("p (g i) -> p g i", g=8),
                                                             scalar=10.0, in1=gs[:, 16:24].unsqueeze(2).to_broadcast([128, 8, 32]), op0=ALU.add, op1=ALU.mult),
                     reads=[b_sel, b_gs], writes=[b_selm])
                S.op("dve", lambda e: e.max(out=gs[:, 24:32], in_=selm[:]), reads=[b_selm], accs=[b_gs])
                S.op("dve", lambda e: e.tensor_scalar(out=Mb[:], in0=selm[:], scalar1=gs[:, 31:32], scalar2=None, op0=ALU.is_ge),
                     reads=[b_selm, b_gs], writes=[b_Mb])
                S.op("dve", lambda e: e.tensor_tensor(out=wg[:], in0=scr[:], in1=Mb[:], op=ALU.mult), reads=[b_scr, b_Mb], writes=[b_wg])
                S.op("dve", lambda e: e.tensor_reduce(out=p8[:, 0:1], in_=wg[:], axis=AX.X, op=ALU.add), reads=[b_wg], writes=[b_p8])
                S.op("dve", lambda e: e.reciprocal(out=p8[:, 1:2], in_=p8[:, 0:1]), accs=[b_p8])
                S.op("dve", lambda e: e.tensor_scalar(out=wg[:], in0=wg[:], scalar1=p8[:, 1:2], scalar2=2.5, op0=ALU.mult, op1=ALU.mult),
                     reads=[b_p8], accs=[b_wg])
                S.op("pe", lambda e: e.matmul(PSB[5][:, 0:NE], lhsT=cx["tri_b"][:, :], rhs=Mb[:], start=True, stop=False),
                     reads=[cx["b_tri"], b_Mb], writes=[b_PSB[5]])
                S.op("pe", lambda e: e.matmul(PSB[5][:, 0:NE], lhsT=cx["ones_b"][:, :], rhs=Mcum[:], start=False, stop=True),
                     reads=[cx["b_ones_b"], b_Mcum], accs=[b_PSB[5]])
                S.op("dve", lambda e: e.tensor_tensor(out=pvm[:], in0=PSB[5][:, 0:NE], in1=E1[:], op=ALU.add), reads=[b_PSB[5], b_E1], writes=[b_pvm])
                S.op("dve", lambda e: e.tensor_tensor(out=pvm[:], in0=pvm[:], in1=Mb[:], op=ALU.mult), reads=[b_Mb], accs=[b_pvm])
                S.op("dve", lambda e: e.tensor_tensor(out=Mcum[:], in0=Mcum[:], in1=Mb[:], op=ALU.add), reads=[b_Mb], writes=[b_Mcum])
                S.op("dve", lambda e: e.max(out=p8[:, 0:8], in_=pvm[:]), reads=[b_pvm], writes=[b_p8])
                S.op("dve", lambda e: e.tensor_copy(p8i[:, 0:8], p8[:, 0:8]), reads=[b_p8], writes=[b_p8i])
                S.op("dve", lambda e: e.tensor_scalar(out=p8i[:, 8:16], in0=p8i[:, 0:8], scalar1=14, scalar2=None, op0=ALU.arith_shift_right), accs=[b_p8i])
                S.op("dve", lambda e, tg=tg: e.tensor_copy(E8[:, tg, :], p8i[:, 8:16]), reads=[b_p8i], accs=[b_E8])
                S.op("dve", lambda e: e.tensor_scalar(out=p8i[:, 8:16], in0=p8i[:, 0:8], scalar1=16383, scalar2=None, op0=ALU.bitwise_and), accs=[b_p8i])
                S.op("dve", lambda e, tg=tg: e.tensor_copy(R8[:, tg, :], p8i[:, 8:16]), reads=[b_p8i], accs=[b_R8])
                for k in range(8):
                    S.op("dve", lambda e, k=k, tg=tg: e.scalar_tensor_tensor(out=junk[:], in0=pvm[:], scalar=p8[:, k:k + 1], in1=wg[:], op0=ALU.is_equal, op1=ALU.mult,
                                                                             accum_out=G8[:, tg, k:k + 1]),
                         reads=[b_pvm, b_p8, b_wg], writes=[b_junk], accs=[b_G8])
                tg += 1
        tok0 += Sq
    cc = sb("cc", [128, 16], F32); b_cc = Buf("cc")
    cci = sb("cci", [128, 8], I32); b_cci = Buf("cci")
    dg = sb("dg", [128, 128], F32); b_dg = Buf("dg")
    PSrow = sb("PSrow", [128, NE], F32); b_PSrow = Buf("PSrow")
    for ec in range(2):
        S.op("pe", lambda e, ec=ec: e.matmul(PSB[0][:, ec:ec + 1], lhsT=Mcum[:, ec * 128:(ec + 1) * 128], rhs=cx["ones_b"][:, 0:1], start=True, stop=True),
             reads=[b_Mcum, cx["b_ones_b"]], writes=[b_PSB[0]] if ec == 0 else (), accs=[b_PSB[0]] if ec else ())
    S.op("dve", lambda e: e.tensor_scalar(out=cc[:, 0:2], in0=PSB[0][:, 0:2], scalar1=127.0, scalar2=None, op0=ALU.add), reads=[b_PSB[0]], writes=[b_cc])
    S.op("dve", lambda e: e.tensor_copy(cci[:, 0:2], cc[:, 0:2]), reads=[b_cc], writes=[b_cci])
    S.op("dve", lambda e: e.tensor_scalar(out=cci[:, 2:4], in0=cci[:, 0:2], scalar1=7, scalar2=None, op0=ALU.arith_shift_right), accs=[b_cci])
    S.op("dve", lambda e: e.tensor_copy(cc[:, 2:4], cci[:, 2:4]), reads=[b_cci], accs=[b_cc])
    for ec in range(2):
        S.op("pe", lambda e, ec=ec: e.matmul(PSB[1][:, ec:ec + 1], lhsT=cx["tril_f"][:, :], rhs=cc[:, 2 + ec:3 + ec], start=True, stop=(ec == 0)),
             reads=[cx["b_tril"], b_cc], writes=[b_PSB[1]] if ec == 0 else (), accs=[b_PSB[1]] if ec else ())
        if ec == 1:
            S.op("pe", lambda e: e.matmul(PSB[1][:, 1:2], lhsT=cx["ones_f"][:, :], rhs=cc[:, 2:3], start=False, stop=True),
                 reads=[cx["b_ones_f"], b_cc], accs=[b_PSB[1]])
    S.op("dve", lambda e: e.tensor_copy(cc[:, 4:6], PSB[1][:, 0:2]), reads=[b_PSB[1]], accs=[b_cc])
    S.op("dve", lambda e: e.tensor_tensor(out=cc[:, 6:8], in0=cc[:, 4:6], in1=cc[:, 2:4], op=ALU.subtract), accs=[b_cc])
    S.op("dve", lambda e: e.tensor_scalar(out=cc[:, 8:10], in0=cc[:, 6:8], scalar1=128.0, scalar2=None, op0=ALU.mult), accs=[b_cc])
    for ec in range(2):
        S.op("dve", lambda e, ec=ec: e.tensor_scalar(out=dg[:], in0=cx["ident_f"][:], scalar1=cc[:, 8 + ec:9 + ec], scalar2=None, op0=ALU.mult),
             reads=[cx["b_ident_f"], b_cc], writes=[b_dg])
        S.op("pe", lambda e, ec=ec: e.matmul(PSB[2][:, ec * 128:(ec + 1) * 128], lhsT=cx["ones_f"][:, :], rhs=dg[:], start=True, stop=True),
             reads=[cx["b_ones_f"], b_dg], writes=[b_PSB[2]] if ec == 0 else (), accs=[b_PSB[2]] if ec else ())
    S.op("dve", lambda e: e.tensor_copy(PSrow[:], PSB[2][:, 0:NE]), reads=[b_PSB[2]], writes=[b_PSrow])
    for t in range(NT):
        for k in range(8):
            S.op("dve", lambda e, k=k, t=t: e.scalar_tensor_tensor(out=junk[:], in0=cx["iota_e"][:], scalar=E8[:, t, k:k + 1], in1=PSrow[:], op0=ALU.is_equal, op1=ALU.mult,
                                                                   accum_out=p8[:, k:k + 1]),
                 reads=[cx["b_iota_e"], b_E8, b_PSrow], writes=[b_junk], accs=[b_p8])
        S.op("dve", lambda e, t=t: e.tensor_tensor(out=p8[:, 0:8], in0=p8[:, 0:8], in1=R8[:, t, :], op=ALU.add), reads=[b_R8], accs=[b_p8])
        S.op("dve", lambda e, t=t: e.tensor_scalar(out=SL8[:, t, :], in0=p8[:, 0:8], scalar1=-1.0, scalar2=None, op0=ALU.add), reads=[b_p8], accs=[b_SL8])
    NB1 = min(NBLK, 512)
    ind = sb("ind", [128, NBLK], BF16); b_ind = Buf("ind")
    EB = sb("EB", [128, NBLK], F32); b_EB = Buf("EB")
    CH = sb("CH", [128, NBLK], F32); b_CH = Buf("CH")
    for ec in range(2):
        S.op("dve", lambda e, ec=ec: e.tensor_scalar(out=ind[:], in0=cx["iota_b"][:, 0:NBLK], scalar1=cc[:, 4 + ec:5 + ec], scalar2=None, op0=ALU.is_ge),
             reads=[cx["b_iota_b"], b_cc], writes=[b_ind])
        for a0 in range(0, NBLK, 512):
            n_ = min(512, NBLK - a0)
            bk = 3 + a0 // 512
            S.op("pe", lambda e, ec=ec, a0=a0, n_=n_, bk=bk: e.matmul(PSB[bk][:, 0:n_], lhsT=cx["ones_b"][:, :], rhs=ind[:, a0:a0 + n_], start=(ec == 0), stop=(ec == 1)),
                 reads=[cx["b_ones_b"], b_ind], writes=[b_PSB[bk]] if ec == 0 else (), accs=[b_PSB[bk]] if ec else ())
    for a0 in range(0, NBLK, 512):
        n_ = min(512, NBLK - a0)
        bk = 3 + a0 // 512
        S.op("dve", lambda e, a0=a0, n_=n_, bk=bk: e.tensor_scalar(out=EB[:, a0:a0 + n_], in0=PSB[bk][:, 0:n_], scalar1=255.0, scalar2=None, op0=ALU.min),
             reads=[b_PSB[bk]], writes=[b_EB] if a0 == 0 else (), accs=[b_EB] if a0 else ())
    S.op("dve", lambda e: e.memset(CH[:, 0:4], 1.0), writes=[b_CH])
    S.op("dve", lambda e: e.tensor_tensor(out=CH[:, 4:NBLK], in0=EB[:, 4:NBLK], in1=EB[:, 0:NBLK - 4], op=ALU.not_equal), reads=[b_EB], accs=[b_CH])
    S.op("dve", lambda e: e.memset(CH[0:1, :], 1.0), accs=[b_CH])
    BIGI = 1.0e6
    EBs = sb("EBs", [128, NBLK], F32); b_EBs = Buf("EBs")
    S.op("dve", lambda e: e.tensor_scalar(out=EBs[:], in0=EB[:], scalar1=128.0, scalar2=cx["iota_p"][:, 0:1], op0=ALU.mult, op1=ALU.add),
         reads=[b_EB, cx["b_iota_p"]], writes=[b_EBs])
    S.op("dve", lambda e: e.scalar_tensor_tensor(out=EBs[:], in0=EBs[:], scalar=-BIGI, in1=CH[:], op0=ALU.add, op1=ALU.mult),
         reads=[b_CH], accs=[b_EBs])
    S.op("dve", lambda e: e.tensor_scalar(out=IDXW[:], in0=EBs[:], scalar1=BIGI, scalar2=None, op0=ALU.add), reads=[b_EBs], writes=[b_IDXW])
    b_XS = cx["b_XS"] = Buf("XS")
    for t in range(NT):
        hb_ = h2b[t % 2]; bh = b_h2b[t % 2]
        ld("sp", hb_[:], cx["H2"].ap()[t * 128:(t + 1) * 128, :], bh, "h2b%d_ld" % (t % 2), reads=[b_H2])
        for k in range(8):
            S.op("pool", lambda e, hb_=hb_, t=t, k=k: e.indirect_dma_start(out=cx["XS"].ap(), out_offset=bass.IndirectOffsetOnAxis(ap=SL8[:, t, k:k + 1], axis=0),
                                                                          in_=hb_[:], in_offset=None),
                 reads=[bh, b_SL8], accs=[b_XS], dma_key="h2b%d_sc" % (t % 2))
    sst.close(); cx["stk"].pop()
    S.barrier()


_REGS = {}


def breg(e, nc, val):
    k = (id(nc), val)
    if k not in _REGS:
        _REGS[k] = e.to_reg(val)
    return _REGS[k]


def stage_E(cx):
    nc, S, sb = cx["nc"], cx["S"], cx["sb"]
    PSB, b_PSB = cx["PSB"], cx["b_PSB"]
    W = cx["W"]; NBLK = cx["NBLK"]
    IDXW = cx["IDXW"]
    sst = ExitStack(); cx["stk"].append(sst)
    RW = 4
    wall = [sb("wall%d" % i, [128, 6144], BF16) for i in range(RW)]; b_wall = [Buf("wall%d" % i) for i in range(RW)]
    NXB = 3
    xb = [sb("xb%d" % i, [128, D], BF16) for i in range(NXB)]; b_xb = [Buf("xb%d" % i) for i in range(NXB)]
    xT = [sb("xTe%d" % i, [128, 8, 128], BF16) for i in range(2)]; b_xT = [Buf("xTe%d" % i) for i in range(2)]
    sg = [sb("sge%d" % i, [128, 256], F32) for i in range(2)]; b_sg = [Buf("sge%d" % i) for i in range(2)]
    aT = [sb("aTe%d" % i, [128, 2, 128], BF16) for i in range(2)]; b_aT = [Buf("aTe%d" % i) for i in range(2)]
    yb = [sb("yb%d" % i, [128, D], BF16) for i in range(2)]; b_yb = [Buf("yb%d" % i) for i in range(2)]
    b_YS = cx["b_YS"] = Buf("YS")
    NROW_W = NE * 128 - 1

    def load_w(b):
        i = b % RW
        S.op("pool", lambda e, b=b, i=i: e.indirect_dma_start(out=wall[i][:, 0:4096], out_offset=None, in_=cx["WBGU"].ap(),
                                                             in_offset=bass.IndirectOffsetOnAxis(ap=IDXW[:, b:b + 1], axis=0),
                                                             bounds_check=breg(e, nc, NROW_W), oob_is_err=False),
             reads=[cx["b_IDXW"], cx["b_WB"]], writes=[b_wall[i]], dma_key="wall%d" % i)
        S.op("pool", lambda e, b=b, i=i: e.indirect_dma_start(out=wall[i][:, 4096:6144], out_offset=None, in_=cx["WBD"].ap(),
                                                             in_offset=bass.IndirectOffsetOnAxis(ap=IDXW[:, b:b + 1], axis=0),
                                                             bounds_check=breg(e, nc, NROW_W), oob_is_err=False),
             reads=[cx["b_IDXW"], cx["b_WB"]], accs=[b_wall[i]], dma_key="wall%d" % i)

    def load_x(b):
        cx["ld"]("sp", xb[b % NXB][:], cx["XS"].ap()[b * 128:(b + 1) * 128, :], b_xb[b % NXB], "xb%d" % (b % NXB), reads=[cx["b_XS"]])

    def do_T(b):
        i = b % 2
        x_ = xb[b % NXB]; bx = b_xb[b % NXB]
        pX = PSB[i][:, :].bitcast(BF16).rearrange("p (kc t) -> p kc t", kc=8)
        xv = x_[:, :].rearrange("p (q kc) -> p kc q", kc=8)
        for kc in range(8):
            S.op("pe", lambda e, kc=kc, pX=pX, xv=xv: e.transpose(out=pX[:, kc, :], in_=xv[:, kc, :], identity=cx["ident_b"][:]),
                 reads=[bx, cx["b_ident_b"]], writes=[b_PSB[i]] if kc == 0 else (), accs=[b_PSB[i]] if kc > 0 else ())
        S.op("act", lambda e, pX=pX, i=i: e.activation(out=xT[i][:, 0:4, :], in_=pX[:, 0:4, :], func=AF.Identity), reads=[b_PSB[i]], writes=[b_xT[i]])
        S.op("dve", lambda e, pX=pX, i=i: e.tensor_copy(xT[i][:, 4:8, :], pX[:, 4:8, :]), reads=[b_PSB[i]], accs=[b_xT[i]])

    def do_GU(b):
        i = b % 2
        bank = 2 + i
        first = True
        wr_ = b % RW
        wgu = wall[wr_][:, 0:4096].rearrange("p (w kc f) -> p w kc f", w=2, kc=8)
        for which in range(2):
            wt = wgu[:, which]
            bw = b_wall[wr_]
            for fc in range(2):
                for kc in range(8):
                    S.op("pe", lambda e, wt=wt, fc=fc, kc=kc, which=which, bank=bank, i=i: e.matmul(
                        PSB[bank][:, which * 256 + fc * 128: which * 256 + (fc + 1) * 128], lhsT=wt[:, kc, fc:256:2], rhs=xT[i][:, kc, :],
                        start=(kc == 0), stop=(kc == 7)),
                        reads=[bw, b_xT[i]], writes=[b_PSB[bank]] if first else (), accs=[b_PSB[bank]] if not first else ())
                    first = False
        S.op("act", lambda e, bank=bank, i=i: e.activation(out=sg[i][:], in_=PSB[bank][:, 0:256], func=AF.Silu), reads=[b_PSB[bank]], writes=[b_sg[i]])
        S.op("dve", lambda e, bank=bank, i=i: e.tensor_tensor(out=aT[i][:].rearrange("p c t -> p (c t)"), in0=PSB[bank][:, 256:512], in1=sg[i][:], op=ALU.mult),
             reads=[b_PSB[bank], b_sg[i]], writes=[b_aT[i]])

    def do_D(b):
        i = b % 2
        y_ = yb[i]; by = b_yb[i]
        for half in range(2):
            bank = 4 + 2 * i + half
            for fc in range(2):
                wdv_ = wall[b % RW][:, 4096:6144].rearrange("p (fc d) -> p fc d", fc=2)
                S.op("pe", lambda e, half=half, fc=fc, bank=bank, i=i, wdv_=wdv_: e.matmul(PSB[bank][:, :], lhsT=aT[i][:, fc, :], rhs=wdv_[:, fc, half * 512:(half + 1) * 512],
                                                                                        start=(fc == 0), stop=(fc == 1)),
                     reads=[b_aT[i], b_wall[b % RW]], writes=[b_PSB[bank]] if fc == 0 else (), accs=[b_PSB[bank]] if fc else ())
            if half == 0:
                S.op("act", lambda e, y_=y_, bank=bank: e.activation(out=y_[:, 0:512], in_=PSB[bank][:, :], func=AF.Identity),
                     reads=[b_PSB[bank]], writes=[by])
            else:
                S.op("dve", lambda e, y_=y_, bank=bank: e.tensor_copy(y_[:, 512:1024], PSB[bank][:, :]), reads=[b_PSB[bank]], accs=[by])
        S.op("act", lambda e, y_=y_, b=b: e.dma_start(out=cx["YS"].ap()[b * 128:(b + 1) * 128, :], in_=y_[:]),
             reads=[by], accs=[b_YS], dma_key="yb%d_st" % i)

    for b0 in range(min(RW, NBLK)):
        load_w(b0)
    load_x(0); load_x(1)
    do_T(0)
    for b in range(NBLK):
        if b + 2 < NBLK:
            load_x(b + 2)
        if b + 1 < NBLK:
            do_T(b + 1)
        do_GU(b)
        if b >= 1:
            do_D(b - 1)
            if b - 1 + RW < NBLK:
                load_w(b - 1 + RW)
    do_D(NBLK - 1)
    sst.close(); cx["stk"].pop()
    S.barrier()


def stage_C(cx):
    nc, S, sb, jobs, J = cx["nc"], cx["S"], cx["sb"], cx["jobs"], cx["J"]
    NT = cx["NT"]
    SL8, G8 = cx["SL8"], cx["G8"]
    sst = ExitStack(); cx["stk"].append(sst)
    gfrow = sb("gfrow", [128, D], F32); b_gfrow = Buf("gfrow")
    cx["ld"]("sp", gfrow[:], bass.AP(cx["W"]["g_final"], 0, [[0, 128], [1, D]]), b_gfrow, "gfrow")
    gt2 = sb("gt2c", [128, D], F32); b_gt2 = Buf("gt2c")
    x1 = [sb("x1c%d" % i, [128, D], F32) for i in range(2)]; b_x1 = [Buf("x1c%d" % i) for i in range(2)]
    yg = [sb("yg%d" % i, [128, D], BF16) for i in range(8)]; b_yg = [Buf("yg%d" % i) for i in range(8)]
    acc = sb("accc", [128, D], F32); b_acc = Buf("accc")
    sq = sb("sqc", [128, D], BF16); b_sq = Buf("sqc")
    stc = sb("stc", [128, 8], F32); b_stc = Buf("stc")
    outs = []
    t = 0
    for j in range(J):
        Sk, q0, Sq = jobs[j][:3]
        cx["ld"]("sp", gt2[:], bass.AP(cx["MODR"], (j * 4 + 3) * D, [[0, 128], [1, D]]), b_gt2, "gt2c")
        for tl in range(Sq // 128):
            x_ = x1[t % 2]; bx = b_x1[t % 2]
            cx["ld"]("sp", x_[:], cx["X1"].ap()[t * 128:(t + 1) * 128, :], bx, "x1c%d" % (t % 2), reads=[cx["b_X1"]])
            for k in range(8):
                S.op("pool", lambda e, t=t, k=k: e.indirect_dma_start(out=yg[k][:], out_offset=None, in_=cx["YS"].ap(),
                                                                     in_offset=bass.IndirectOffsetOnAxis(ap=SL8[:, t, k:k + 1], axis=0)),
                     reads=[cx["b_SL8"], cx["b_YS"]], writes=[b_yg[k]], dma_key="yg%d" % k)
            S.op("dve", lambda e, t=t: e.tensor_scalar(out=acc[:], in0=yg[0][:], scalar1=G8[:, t, 0:1], scalar2=None, op0=ALU.mult),
                 reads=[b_yg[0], cx["b_G8"]], writes=[b_acc])
            for k in range(1, 8):
                S.op("dve", lambda e, t=t, k=k: e.scalar_tensor_tensor(out=acc[:], in0=yg[k][:], scalar=G8[:, t, k:k + 1], in1=acc[:], op0=ALU.mult, op1=ALU.add),
                     reads=[b_yg[k], cx["b_G8"]], accs=[b_acc])
            S.op("dve", lambda e: e.tensor_tensor(out=acc[:], in0=acc[:], in1=gt2[:], op=ALU.mult), reads=[b_gt2], accs=[b_acc])
            S.op("dve", lambda e, x_=x_: e.tensor_tensor(out=x_[:], in0=x_[:], in1=acc[:], op=ALU.add), reads=[b_acc], accs=[bx])
            S.op("act", lambda e, x_=x_: e.activation(out=sq[:], in_=x_[:], func=AF.Square, accum_out=stc[:, 0:1]), reads=[bx], writes=[b_sq, b_stc])
            S.op("act", lambda e: e.activation(out=stc[:, 1:2], in_=stc[:, 0:1], func=AF.Sqrt, scale=1.0 / D, bias=cx["epsb"][:, 0:1]),
                 reads=[cx["b_epsb"]], accs=[b_stc])
            S.op("dve", lambda e: e.reciprocal(out=stc[:, 2:3], in_=stc[:, 1:2]), accs=[b_stc])
            S.op("dve", lambda e, x_=x_: e.scalar_tensor_tensor(out=x_[:], in0=x_[:], scalar=stc[:, 2:3], in1=gfrow[:], op0=ALU.mult, op1=ALU.mult),
                 reads=[b_stc, b_gfrow], accs=[bx])
            o = S.op("act", lambda e, x_=x_, t=t: e.dma_start(out=cx["y_out"].ap()[t * 128:(t + 1) * 128, :], in_=x_[:]),
                     reads=[bx], dma_key="x1c%d_st" % (t % 2))
            outs.append(o)
            t += 1
    sst.close(); cx["stk"].pop()
    return outs


def build_all(jobs, dbg=False):
    cx = build(jobs, dbg=dbg)
    stage_P(cx)
    stage_T(cx)
    stage_O(cx)
    stage_E(cx)
    outs = stage_C(cx)
    cx["S"].emit(outs)
    return cx


def make_in_map(jobs, xseqs, cvecs, poss, hf, weights, consts):
    J = len(jobs)
    im = {}
    for j in range(J):
        im["xs%d" % j] = np.ascontiguousarray(xseqs[j], dtype=np.float32)
        im["rope%d" % j] = rope_table(poss[j])
    cT = np.stack([np.asarray(cv, np.float32).reshape(8, 128).T for cv in cvecs], axis=-1)
    im["cT"] = np.ascontiguousarray(cT)
    im.update(weights)
    for n, v in consts.items():
        im["c_" + n] = v
    im["c_hfv"] = np.broadcast_to(np.array([[float(hf), 1.0 - float(hf)]], np.float32), (128, 2)).copy()
    return im


def kernel(**inputs):
    jobs = [(2048, 0, 2048, False), (2048, 0, 2048, False), (8192, 0, 4096, True)]
    cx = build_all(jobs)
    weights = {}
    for n in WEIGHT_NAMES:
        a = np.asarray(inputs[n], dtype=np.float32)
        if n not in ("rel_bias", "g_final"):
            a = a[0]
        weights[n] = np.ascontiguousarray(a)
    consts = host_constants()
    xp = np.asarray(inputs["x_prompt"], np.float32)
    xsm = np.asarray(inputs["x_sample"], np.float32)
    cp = np.asarray(inputs["c_prompt"], np.float32)
    cs = np.asarray(inputs["c_sample"], np.float32)
    in_maps = []
    for c in range(8):
        sbi, hf = c // 2, c % 2
        seq = xsm[sbi]
        pos2 = np.arange(8192)
        if hf:
            seq = np.concatenate([seq[4096:], seq[:4096]], axis=0)
            pos2 = np.concatenate([pos2[4096:], pos2[:4096]])
        in_maps.append(make_in_map(jobs, [xp[2 * c], xp[2 * c + 1], seq], [cp[2 * c], cp[2 * c + 1], cs[sbi]],
                                   [np.arange(2048), np.arange(2048), pos2], hf, weights, consts))
    res = run_bass_kernel_spmd(cx["nc"], in_maps, core_ids=list(range(8)))
    y_prompt = np.empty((16, 2048, D), np.float32)
    y_sample = np.empty((4, 8192, D), np.float32)
    for c in range(8):
        y = np.asarray(res.results[c]["y"], dtype=np.float32)
        y_prompt[2 * c] = y[0:2048]
        y_prompt[2 * c + 1] = y[2048:4096]
        y_sample[c // 2, (c % 2) * 4096:(c % 2 + 1) * 4096] = y[4096:8192]
    return (y_prompt, y_sample)
```

```python
import math
from contextlib import ExitStack
import numpy as np
import concourse.bass as bass
import concourse.mybir as mybir
from concourse.bass_utils import run_bass_kernel_spmd

F32 = mybir.dt.float32
BF16 = mybir.dt.bfloat16
I32 = mybir.dt.int32
AF = mybir.ActivationFunctionType
ALU = mybir.AluOpType
AX = mybir.AxisListType

D = 1024
DP = 2304
NE = 256
EPS = 1e-6
ENGS = ("pe", "act", "dve", "pool", "sp")


class Buf:
    __slots__ = ("name", "writers", "readers")

    def __init__(self, name):
        self.name = name
        self.writers = []
        self.readers = []


class Op:
    __slots__ = ("eng", "fn", "deps", "is_dma", "sem", "val", "signals")

    def __init__(self, eng, fn, is_dma):
        self.eng = eng
        self.fn = fn
        self.deps = []
        self.is_dma = is_dma
        self.sem = None
        self.val = 0
        self.signals = False


class Sched:
    def __init__(self, nc, stack):
        self.nc = nc
        self.stack = stack
        self.ops = {e: [] for e in ENGS}
        self.esem = {e: stack.enter_context(nc.semaphore("es_" + e)) for e in ENGS}
        self.dma_sems = {}
        self.pending = {e: [] for e in ENGS}
        self.last_dma = {}

    def barrier(self):
        deps = []
        for e in ENGS:
            for o in reversed(self.ops[e]):
                if not o.is_dma:
                    deps.append(o)
                    break
        deps.extend(self.last_dma.values())
        for e in ENGS:
            self.pending[e] = list(deps)

    def _dma_sem(self, key):
        if key not in self.dma_sems:
            s = self.stack.enter_context(self.nc.semaphore("ds%d" % len(self.dma_sems)))
            self.dma_sems[key] = [s, 0]
        return self.dma_sems[key]

    def op(self, eng, fn, reads=(), writes=(), accs=(), dma_key=None):
        is_dma = dma_key is not None
        o = Op(eng, fn, is_dma)
        deps = []
        for b in reads:
            deps.extend(b.writers)
        for b in writes:
            deps.extend(b.writers)
            deps.extend(b.readers)
        for b in accs:
            deps.extend(b.readers)
            for w in b.writers:
                if w.is_dma and is_dma:
                    continue
                deps.append(w)
        if self.pending[eng]:
            deps.extend(self.pending[eng])
            self.pending[eng] = []
        seen = set()
        for d in deps:
            if id(d) in seen or d is o:
                continue
            seen.add(id(d))
            if (not d.is_dma) and (not is_dma) and d.eng == "pe" and eng == "pe":
                continue
            o.deps.append(d)
            d.signals = True
        for b in reads:
            b.readers.append(o)
        for b in writes:
            b.writers = [o]
            b.readers = []
        for b in accs:
            b.writers.append(o)
            b.readers = []
        if is_dma:
            s = self._dma_sem(dma_key)
            s[1] += 16
            o.sem = s[0]
            o.val = s[1]
            o.signals = True
            self.last_dma[dma_key] = o
        self.ops[eng].append(o)
        return o

    def emit(self, final_ops):
        nc = self.nc
        for e in ENGS:
            c = 0
            for o in self.ops[e]:
                if not o.is_dma and o.signals:
                    c += 1
                    o.sem = self.esem[e]
                    o.val = c
        sched = self

        def run(engname, eh):
            waited = {}
            for o in sched.ops[engname]:
                for d in o.deps:
                    k = id(d.sem)
                    if waited.get(k, 0) >= d.val:
                        continue
                    eh.wait_ge(d.sem, d.val)
                    waited[k] = d.val
                ins = o.fn(eh)
                if o.is_dma:
                    ins.then_inc(o.sem, 16)
                elif o.signals:
                    ins.then_inc(o.sem, 1)
            if engname == "sp":
                for d in final_ops:
                    if waited.get(id(d.sem), 0) < d.val:
                        eh.wait_ge(d.sem, d.val)
                        waited[id(d.sem)] = d.val

        allsems = list(self.esem.values()) + [v[0] for v in self.dma_sems.values()]
        with nc.Block() as blk0:
            @blk0.sync
            def _(e):
                for s_ in allsems:
                    e.sem_clear(s_)

        with nc.Block() as block:
            @block.tensor
            def _(e):
                run("pe", e)

            @block.scalar
            def _(e):
                run("act", e)

            @block.vector
            def _(e):
                run("dve", e)

            @block.gpsimd
            def _(e):
                run("pool", e)

            @block.sync
            def _(e):
                run("sp", e)


def t5_bucket_np(rel):
    nb = 16
    max_exact = 8
    ret = np.where(rel > 0, nb, 0)
    n = np.abs(rel)
    nf = np.maximum(n, 1).astype(np.float32)
    large = max_exact + (np.log(nf / np.float32(max_exact)) / np.float32(math.log(128 / max_exact))
                         * np.float32(nb - max_exact)).astype(np.int32)
    large = np.minimum(large, nb - 1)
    return ret + np.where(n < max_exact, n, large)


def host_constants():
    c = {}
    c["ident"] = np.eye(128, dtype=np.float32)
    c["jrev"] = np.eye(128, dtype=np.float32)[::-1].copy()
    tri = np.zeros((128, 128), np.float32)
    for a in range(128):
        tri[a, a + 1:] = 1.0
    c["tri"] = tri
    tril = np.zeros((128, 128), np.float32)
    for a in range(128):
        tril[a, a:] = 1.0
    c["tril"] = tril
    i = np.arange(1280)
    bk = t5_bucket_np((639 - i).astype(np.int32))
    oh = np.zeros((32, 1280), np.float32)
    oh[bk, i] = 1.0
    c["ohr"] = oh
    c["iota_e"] = np.broadcast_to(np.arange(256, dtype=np.float32)[None, :], (128, 256)).copy()
    c["iota_b"] = np.broadcast_to(np.arange(1024, dtype=np.float32)[None, :], (128, 1024)).copy()
    c["iota_p"] = np.arange(128, dtype=np.float32).reshape(128, 1).copy()
    return c


def rope_table(pos):
    t = np.asarray(pos)
    c = {}
    half = 32
    inv = (10000.0 ** (-np.arange(0, half, 2, dtype=np.float32) / half)).astype(np.float32)
    row = (t // 64).astype(np.float32)
    col = (t % 64).astype(np.float32)
    ar = row[:, None] * inv[None, :]
    ac = col[:, None] * inv[None, :]
    return np.stack([np.cos(ar), np.sin(ar), np.cos(ac), np.sin(ac)], axis=1).astype(np.float32)


WEIGHT_NAMES = ["rel_bias", "w_ada", "b_ada", "g_norm1", "w_in", "lambda_q1", "lambda_k1", "lambda_q2",
                "lambda_k2", "g_subln", "g_qnorm", "g_knorm", "w_out", "g_norm2", "w_router",
                "router_bias", "w_exp_gate", "w_exp_up", "w_exp_down", "w_sh_gate", "w_sh_up",
                "w_sh_down", "g_final"]


def build(jobs, dbg=False):
    J = len(jobs)
    T = sum(j_[2] for j_ in jobs)
    NT = T // 128
    NBLK = T * 8 // 128 + NE
    SMAX = max(j[0] for j in jobs)
    nc = bass.Bass("TRN2", target_bir_lowering=False)
    st = ExitStack()
    S = Sched(nc, st)

    def din(name, shape, dt=F32):
        return nc.dram_tensor(name, list(shape), dt, kind="ExternalInput")

    def dscr(name, shape, dt):
        return nc.dram_tensor(name, list(shape), dt, kind="ExternalOutput" if dbg else "Internal")

    xs = [din("xs%d" % j, [jobs[j][0], D]) for j in range(J)]
    cT_d = din("cT", [128, 8, J])
    W = {}
    wshapes = dict(rel_bias=[32, 4], w_ada=[D, 6 * D], b_ada=[6 * D], g_norm1=[D], w_in=[D, DP],
                   lambda_q1=[64], lambda_k1=[64], lambda_q2=[64], lambda_k2=[64], g_subln=[128],
                   g_qnorm=[64], g_knorm=[64], w_out=[D, D], g_norm2=[D], w_router=[D, NE],
                   router_bias=[NE], w_exp_gate=[NE, D, 256], w_exp_up=[NE, D, 256],
                   w_exp_down=[NE, 256, D], w_sh_gate=[D, 256], w_sh_up=[D, 256], w_sh_down=[256, D],
                   g_final=[D])
    for n in WEIGHT_NAMES:
        W[n] = din(n, wshapes[n])
    cshapes = dict(ident=[128, 128], jrev=[128, 128], tri=[128, 128], tril=[128, 128], ohr=[32, 1280],
                   iota_e=[128, 256], iota_b=[128, 1024], iota_p=[128, 1], hfv=[128, 2])
    C = {n: din("c_" + n, s) for n, s in cshapes.items()}
    ROPE = [din("rope%d" % j, [jobs[j][0], 4, 16]) for j in range(J)]
    y_out = nc.dram_tensor("y", [T, D], F32, kind="ExternalOutput")

    QA = [dscr("QA%d" % j, [4, 128, jobs[j][2]], BF16) for j in range(J)]
    KA = [dscr("KA%d" % j, [4, 128, jobs[j][0]], BF16) for j in range(J)]
    VA = [dscr("VA%d" % j, [jobs[j][0], 512], BF16) for j in range(J)]
    QB = [dscr("QB%d" % j, [4, 128, jobs[j][2]], BF16) for j in range(J)]
    KB = [dscr("KB%d" % j, [2, 128, jobs[j][0]], BF16) for j in range(J)]
    VB = [dscr("VB%d" % j, [jobs[j][0], 2, 65], BF16) for j in range(J)]
    OT = dscr("OT", [8, 128, T], BF16)
    X1 = dscr("X1", [T, D], F32)
    H2 = dscr("H2", [T, D], BF16)
    XS = dscr("XSLOT", [NBLK * 128, D], BF16)
    YS = dscr("YSLOT", [NBLK * 128, D], BF16)
    GRD = dscr("GRD", [4, 1280], F32)
    WBGU = nc.dram_tensor("WBGU", [NE * 128, 4096], BF16, kind="Internal")
    WBD = nc.dram_tensor("WBD", [NE * 128, 2048], BF16, kind="Internal")

    sb_bytes = [0]

    stk = [st]

    def sb(name, shape, dt):
        return stk[-1].enter_context(nc.sbuf_tensor(name, list(shape), dt))

    def ps(name, shape, dt=F32):
        return st.enter_context(nc.psum_tensor(name, list(shape), dt))

    MODR = dscr("MODR", [J, 4, D], F32)
    b_MODR = Buf("MODR")
    ident_f = sb("ident_f", [128, 128], F32); b_ident_f = Buf("ident_f")
    ident_b = sb("ident_b", [128, 128], BF16); b_ident_b = Buf("ident_b")
    ones_b = sb("ones_b", [128, 128], BF16); b_ones_b = Buf("ones_b")
    ones_f = sb("ones_f", [128, 128], F32); b_ones_f = Buf("ones_f")
    tri_b = sb("tri_b", [128, 128], BF16); b_tri = Buf("tri_b")
    tril_f = sb("tril_f", [128, 128], F32); b_tril = Buf("tril_f")
    iota_e = sb("iota_e", [128, 256], F32); b_iota_e = Buf("iota_e")
    iota_b = sb("iota_b", [128, 1024], F32); b_iota_b = Buf("iota_b")
    iota_p = sb("iota_p", [128, 1], F32); b_iota_p = Buf("iota_p")
    epsb = sb("epsb", [128, 1], F32); b_epsb = Buf("epsb")
    G1T = sb("G1T", [128, 8, J], F32); b_G1T = Buf("G1T")
    SH1T = sb("SH1T", [128, 8, J], F32); b_SH1T = Buf("SH1T")
    lsm = sb("lsm", [128, 8], F32); b_lsm = Buf("lsm")
    gsub = sb("gsub", [128, 1], F32); b_gsub = Buf("gsub")
    gq = sb("gq", [128, 64], F32); b_gq = Buf("gq")
    gk = sb("gk", [128, 64], F32); b_gk = Buf("gk")
    bcst = sb("bcst", [128, 2, 4], F32); b_bcst = Buf("bcst")

    PSB = [ps("psb%d" % i, [128, 512], F32) for i in range(8)]
    b_PSB = [Buf("psb%d" % i) for i in range(8)]

    def ld(eng, out_ap, in_ap, wbuf, key, reads=(), acc=False, slow=False):
        fn = (lambda e: e.dma_start(out=out_ap, in_=in_ap, allow_slow_non_contiguous=True)) if slow else \
             (lambda e: e.dma_start(out=out_ap, in_=in_ap))
        if acc:
            return S.op(eng, fn, reads=reads, accs=[wbuf], dma_key=key)
        return S.op(eng, fn, reads=reads, writes=[wbuf], dma_key=key)

    ld("sp", ident_f[:], C["ident"].ap(), b_ident_f, "ident_f")
    ld("sp", tril_f[:], C["tril"].ap(), b_tril, "tril_f")
    ld("sp", iota_e[:], C["iota_e"].ap(), b_iota_e, "iota_e")
    ld("sp", iota_b[:], C["iota_b"].ap(), b_iota_b, "iota_b")
    ld("sp", iota_p[:], C["iota_p"].ap(), b_iota_p, "iota_p")
    ld("pool", ident_b[:], C["ident"].ap(), b_ident_b, "ident_b")
    ld("pool", tri_b[:], C["tri"].ap(), b_tri, "tri_b")
    S.op("dve", lambda e: e.memset(ones_b[:], 1.0), writes=[b_ones_b])
    S.op("dve", lambda e: e.memset(ones_f[:], 1.0), writes=[b_ones_f])
    S.op("dve", lambda e: e.memset(epsb[:], EPS), writes=[b_epsb])

    pst = ExitStack()
    stk.append(pst)
    big = sb("big", [128, 4096], F32); b_big = Buf("big")
    cT = sb("cT_sb", [128, 8, J], F32); b_cT = Buf("cT")
    scT = sb("scT", [128, 8, J], F32); b_scT = Buf("scT")
    ld("sp", cT[:], cT_d.ap(), b_cT, "cT")
    S.op("act", lambda e: e.activation(out=scT[:], in_=cT[:], func=AF.Silu), reads=[b_cT], writes=[b_scT])
    badaT = sb("badaT", [128, 16], F32); b_badaT = Buf("badaT")
    g1T = sb("g1T", [128, 8], F32); b_g1T = Buf("g1T")
    ld("sp", badaT[:], W["b_ada"].ap()[0:2048].rearrange("(c p) -> p c", p=128), b_badaT, "badaT", slow=True)
    ld("sp", g1T[:], W["g_norm1"].ap().rearrange("(c p) -> p c", p=128), b_g1T, "g1T", slow=True)
    scbc = sb("scbc", [128, 8, 128], F32); b_scbc = Buf("scbc")
    wada_v = W["w_ada"].ap().rearrange("(kc p) n -> p kc n", p=128)
    wst = big[:, 0:4096].rearrange("p (kc n) -> p kc n", kc=8)
    for cc in range(4):
        ld("sp", wst, wada_v[:, :, cc * 512:(cc + 1) * 512], b_big, "big_ld")
        for sub in range(4):
            col = cc * 4 + sub
            pb = b_PSB[col % 2]
            pt = PSB[col % 2]
            for kc in range(8):
                S.op("pe", lambda e, kc=kc, sub=sub, pt=pt: e.matmul(pt[:, 0:J], lhsT=wst[:, kc, sub * 128:(sub + 1) * 128],
                                                                   rhs=scT[:, kc, :], start=(kc == 0), stop=(kc == 7)),
                     reads=[b_big, b_scT], writes=[pb] if kc == 0 else (), accs=[pb] if kc > 0 else ())
            if col < 8:
                S.op("dve", lambda e, col=col, pt=pt: e.tensor_scalar(out=SH1T[:, col, :], in0=pt[:, 0:J], scalar1=badaT[:, col:col + 1],
                                                                    scalar2=None, op0=ALU.add),
                     reads=[pb, b_badaT], accs=[b_SH1T])
            else:
                c8 = col - 8
                S.op("dve", lambda e, col=col, c8=c8, pt=pt: e.tensor_scalar(out=G1T[:, c8, :], in0=pt[:, 0:J], scalar1=badaT[:, col:col + 1],
                                                                           scalar2=1.0, op0=ALU.add, op1=ALU.add),
                     reads=[pb, b_badaT], accs=[b_G1T])
                S.op("dve", lambda e, c8=c8: e.tensor_scalar(out=G1T[:, c8, :], in0=G1T[:, c8, :], scalar1=g1T[:, c8:c8 + 1],
                                                             scalar2=None, op0=ALU.mult),
                     reads=[b_g1T], accs=[b_G1T])
    brow = sb("brow", [128, D], F32); b_brow = Buf("brow")
    g2row = sb("g2row", [128, D], F32); b_g2row = Buf("g2row")
    mtmp = sb("mtmp", [128, D], F32); b_mtmp = Buf("mtmp")
    ld("sp", g2row[:], bass.AP(W["g_norm2"], 0, [[0, 128], [1, D]]), b_g2row, "g2row")
    vmap = {2: 0, 3: 2, 4: 1, 5: 3}
    for v6 in (2, 3, 4, 5):
        ld("sp", brow[:], bass.AP(W["b_ada"], v6 * D, [[0, 128], [1, D]]), b_brow, "brow")
        for j in range(J):
            for hc in range(2):
                ld("sp", wst, wada_v[:, :, v6 * D + hc * 512: v6 * D + (hc + 1) * 512], b_big, "big_ld")
                S.op("dve", lambda e, j=j: e.tensor_copy(scbc[:], scT[:, :, j:j + 1].to_broadcast([128, 8, 128])),
                     reads=[b_scT], writes=[b_scbc])
                pb = b_PSB[2 + hc]
                pt = PSB[2 + hc]
                for kc in range(8):
                    S.op("pe", lambda e, kc=kc, pt=pt: e.matmul(pt[:, :], lhsT=scbc[:, kc, :], rhs=wst[:, kc, :],
                                                              start=(kc == 0), stop=(kc == 7)),
                         reads=[b_big, b_scbc], writes=[pb] if kc == 0 else (), accs=[pb] if kc > 0 else ())
                sl = slice(hc * 512, (hc + 1) * 512)
                S.op("dve", lambda e, pt=pt, sl=sl: e.tensor_tensor(out=mtmp[:, sl], in0=pt[:, :], in1=brow[:, sl], op=ALU.add),
                     reads=[pb, b_brow], writes=[b_mtmp] if hc == 0 else (), accs=[b_mtmp] if hc == 1 else ())
            if v6 == 4:
                S.op("dve", lambda e: e.scalar_tensor_tensor(out=mtmp[:], in0=mtmp[:], scalar=1.0, in1=g2row[:],
                                                             op0=ALU.add, op1=ALU.mult),
                     reads=[b_g2row], accs=[b_mtmp])
            S.op("sp", lambda e, j=j, v6=v6: e.dma_start(out=MODR.ap()[j, vmap[v6]:vmap[v6] + 1, :], in_=mtmp[0:1, :]),
                 reads=[b_mtmp], accs=[b_MODR], dma_key="mtmp_st")
    lamv = sb("lamv", [128, 4, 64], F32); b_lamv = Buf("lamv")
    for i, n in enumerate(["lambda_q1", "lambda_k1", "lambda_q2", "lambda_k2"]):
        ld("sp", lamv[:, i, :], bass.AP(W[n], 0, [[0, 128], [1, 64]]), b_lamv, "lamv", acc=(i > 0))
    ljunk = sb("ljunk", [128, 64], F32); b_ljunk = Buf("ljunk")
    for i in range(2):
        S.op("dve", lambda e, i=i: e.tensor_tensor(out=ljunk[:], in0=lamv[:, 2 * i, :], in1=lamv[:, 2 * i + 1, :], op=ALU.mult),
             reads=[b_lamv], writes=[b_ljunk])
        S.op("dve", lambda e, i=i: e.tensor_reduce(out=lsm[:, i:i + 1], in_=ljunk[:], axis=AX.X, op=ALU.add),
             reads=[b_ljunk], accs=[b_lsm])
    S.op("act", lambda e: e.activation(out=lsm[:, 2:4], in_=lsm[:, 0:2], func=AF.Exp), reads=[b_lsm], accs=[b_lsm])
    S.op("dve", lambda e: e.tensor_tensor(out=lsm[:, 4:5], in0=lsm[:, 3:4], in1=lsm[:, 2:3], op=ALU.subtract),
         reads=[b_lsm], accs=[b_lsm])
    S.op("dve", lambda e: e.tensor_scalar(out=lsm[:, 5:6], in0=lsm[:, 4:5], scalar1=-0.2, scalar2=None, op0=ALU.add),
         reads=[b_lsm], accs=[b_lsm])
    NLAM = lsm[:, 5:6]
    ld("sp", gsub[:], W["g_subln"].ap().rearrange("(p o) -> p o", o=1), b_gsub, "gsub")
    S.op("dve", lambda e: e.tensor_scalar(out=gsub[:], in0=gsub[:], scalar1=0.8, scalar2=None, op0=ALU.mult),
         reads=[], writes=[b_gsub])
    ld("sp", gq[:], bass.AP(W["g_qnorm"], 0, [[0, 128], [1, 64]]), b_gq, "gq")
    ld("sp", gk[:], bass.AP(W["g_knorm"], 0, [[0, 128], [1, 64]]), b_gk, "gk")
    ld("sp", bcst[:, 0, :], bass.AP(W["rel_bias"], 15 * 4, [[0, 128], [1, 4]]), b_bcst, "bcst")
    ld("sp", bcst[:, 1, :], bass.AP(W["rel_bias"], 31 * 4, [[0, 128], [1, 4]]), b_bcst, "bcst", acc=True)
    rb = sb("rb", [32, 4], F32); b_rb = Buf("rb")
    ohr = sb("ohr", [32, 1280], F32); b_ohr = Buf("ohr")
    ld("sp", rb[:], W["rel_bias"].ap(), b_rb, "rb")
    ld("sp", ohr[:], C["ohr"].ap(), b_ohr, "ohr")
    grs = sb("grs", [4, 1280], F32); b_grs = Buf("grs")
    for i, (a_, n_) in enumerate([(0, 512), (512, 512), (1024, 256)]):
        S.op("pe", lambda e, a_=a_, n_=n_, i=i: e.matmul(PSB[4 + i][0:4, 0:n_], lhsT=rb[:, :], rhs=ohr[:, a_:a_ + n_], start=True, stop=True),
             reads=[b_rb, b_ohr], writes=[b_PSB[4 + i]])
        S.op("dve", lambda e, a_=a_, n_=n_, i=i: e.tensor_copy(grs[:, a_:a_ + n_], PSB[4 + i][0:4, 0:n_]),
             reads=[b_PSB[4 + i]], accs=[b_grs])
    b_GRD = Buf("GRD")
    S.op("sp", lambda e: e.dma_start(out=GRD.ap(), in_=grs[:]), reads=[b_grs], writes=[b_GRD], dma_key="grs_st")
    pst.close()
    stk.pop()
    S.barrier()

    ctx = dict(WBGU=WBGU, WBD=WBD, ROPE=ROPE, nc=nc, S=S, st=st, sb=sb, stk=stk, jobs=jobs, J=J, T=T, NT=NT, NBLK=NBLK, xs=xs, W=W, C=C, y_out=y_out,
               QA=QA, KA=KA, VA=VA, QB=QB, KB=KB, VB=VB, OT=OT, X1=X1, H2=H2, XS=XS, YS=YS, GRD=GRD, MODR=MODR,
               PSB=PSB, b_PSB=b_PSB, ident_f=ident_f, b_ident_f=b_ident_f, ident_b=ident_b, b_ident_b=b_ident_b,
               ones_b=ones_b, b_ones_b=b_ones_b, ones_f=ones_f, b_ones_f=b_ones_f, tri_b=tri_b, b_tri=b_tri,
               tril_f=tril_f, b_tril=b_tril, iota_e=iota_e, b_iota_e=b_iota_e, iota_b=iota_b, b_iota_b=b_iota_b,
               iota_p=iota_p, b_iota_p=b_iota_p, epsb=epsb, b_epsb=b_epsb,
               G1T=G1T, b_G1T=b_G1T, SH1T=SH1T, b_SH1T=b_SH1T,
               NLAM=NLAM, b_lsm=b_lsm, gsub=gsub, b_gsub=b_gsub, gq=gq, b_gq=b_gq, gk=gk, b_gk=b_gk,
               bcst=bcst, b_bcst=b_bcst, ld=ld, dbg=dbg)
    return ctx


def stage_P(cx):
    nc, S, sb, jobs, J = cx["nc"], cx["S"], cx["sb"], cx["jobs"], cx["J"]
    PSB, b_PSB = cx["PSB"], cx["b_PSB"]
    sst = ExitStack(); cx["stk"].append(sst)
    win = sb("win", [128, 8, DP], BF16); b_win = Buf("win")
    for half in range(2):
        cx["ld"]("pool", win[:, :, half * 1152:(half + 1) * 1152],
                 cx["W"]["w_in"].ap().rearrange("(kc p) n -> p kc n", p=128)[:, :, half * 1152:(half + 1) * 1152],
                 b_win, "win", acc=(half == 1))
    xt = [sb("xt%d" % i, [128, D], F32) for i in range(2)]; b_xt = [Buf("xt%d" % i) for i in range(2)]
    sq = sb("sqj", [128, D], BF16); b_sq = Buf("sqj")
    st1 = sb("st1", [128, 8], F32); b_st1 = Buf("st1")
    xn = sb("xn", [128, D], BF16); b_xn = Buf("xn")
    hT = sb("hT", [128, 8, 512], BF16); b_hT = Buf("hT")
    fmst = sb("fmst", [128, 8, 512], BF16); b_fmst = Buf("fmst")
    vast = sb("vast", [128, 4, 512], BF16); b_vast = Buf("vast")
    qbf = sb("qbf", [128, 512], F32); b_qbf = Buf("qbf")
    kvf = sb("kvf", [128, 256], F32); b_kvf = Buf("kvf")
    rt = sb("ropet", [128, 4, 16], F32); b_rt = Buf("ropet")
    tq = [sb("tq%d" % i, [128, 512], F32) for i in range(3)]; b_tq = [Buf("tq%d" % i) for i in range(3)]
    nst = sb("nst", [128, 16], F32); b_nst = Buf("nst")
    qbb = sb("qbb", [128, 512], BF16); b_qbb = Buf("qbb")
    kbb = sb("kbb", [128, 2, 2, 64], BF16); b_kbb = Buf("kbb")
    qbst = sb("qbst", [128, 4, 512], BF16); b_qbst = Buf("qbst")
    kbst = sb("kbst", [128, 2, 512], BF16); b_kbst = Buf("kbst")
    vbst = sb("vbst", [128, 4, 2, 65], BF16); b_vbst = Buf("vbst")
    S.op("dve", lambda e: e.memset(vbst[:], 1.0), writes=[b_vbst])
    b_D = cx["b_D"] = {}
    gq, gk = cx["gq"], cx["gk"]
    for j in range(J):
        Sk, q0, Sq = jobs[j][:3]
        for n in ("QA", "KA", "VA", "QB", "KB", "VB"):
            b_D[(n, j)] = Buf("%s%d" % (n, j))
        G1 = cx["G1T"][:, :, j:j + 1]
        SH1 = cx["SH1T"][:, :, j:j + 1]
        for ch in range(Sk // 512):
            t0 = ch * 512
            own = (t0 >= q0) and (t0 < q0 + Sq)
            for ti in range(4):
                r0 = t0 + ti * 128
                x_ = xt[ti % 2]; bx = b_xt[ti % 2]
                cx["ld"]("sp", x_[:], cx["xs"][j].ap()[r0:r0 + 128, :], bx, "xt%d" % (ti % 2))
                S.op("act", lambda e, x_=x_: e.activation(out=sq[:], in_=x_[:], func=AF.Square, accum_out=st1[:, 0:1]),
                     reads=[bx], writes=[b_sq, b_st1])
                S.op("act", lambda e: e.activation(out=st1[:, 1:2], in_=st1[:, 0:1], func=AF.Sqrt, scale=1.0 / D, bias=cx["epsb"][:, 0:1]),
                     reads=[cx["b_epsb"]], accs=[b_st1])
                S.op("dve", lambda e: e.reciprocal(out=st1[:, 2:3], in_=st1[:, 1:2]), accs=[b_st1])
                S.op("dve", lambda e, x_=x_: e.tensor_scalar(out=xn[:], in0=x_[:], scalar1=st1[:, 2:3], scalar2=None, op0=ALU.mult),
                     reads=[bx, b_st1], writes=[b_xn])
                pT = PSB[0][:, :].bitcast(BF16).rearrange("p (kc t) -> p kc t", kc=8)
                for kc in range(8):
                    S.op("pe", lambda e, kc=kc, pT=pT: e.transpose(out=pT[:, kc, :], in_=xn[:, kc * 128:(kc + 1) * 128], identity=cx["ident_b"][:]),
                         reads=[b_xn, cx["b_ident_b"]], writes=[b_PSB[0]] if kc == 0 else (), accs=[b_PSB[0]] if kc > 0 else ())
                hs = hT[:, :, ti * 128:(ti + 1) * 128]
                S.op("dve", lambda e, hs=hs, pT=pT, G1=G1: e.tensor_tensor(out=hs, in0=pT, in1=G1.to_broadcast([128, 8, 128]), op=ALU.mult),
                     reads=[b_PSB[0], cx["b_G1T"]], writes=[b_hT] if ti == 0 else (), accs=[b_hT] if ti > 0 else ())
                S.op("dve", lambda e, hs=hs, SH1=SH1: e.tensor_tensor(out=hs, in0=hs, in1=SH1.to_broadcast([128, 8, 128]), op=ALU.add),
                     reads=[cx["b_SH1T"]], accs=[b_hT])
            chunks = ([(h, h) for h in range(4)] if own else []) + [(4 + h, 4 + h) for h in range(4)]
            for i, (slot, wc) in enumerate(chunks):
                pb = b_PSB[1 + (i % 2)]; pt = PSB[1 + (i % 2)]
                for kc in range(8):
                    S.op("pe", lambda e, kc=kc, wc=wc, pt=pt: e.matmul(pt[:, :], lhsT=win[:, kc, wc * 128:(wc + 1) * 128], rhs=hT[:, kc, :],
                                                                     start=(kc == 0), stop=(kc == 7)),
                         reads=[b_win, b_hT], writes=[pb] if kc == 0 else (), accs=[pb] if kc > 0 else ())
                S.op("act", lambda e, slot=slot, pt=pt: e.activation(out=fmst[:, slot, :], in_=pt[:, :], func=AF.Identity),
                     reads=[pb], writes=[b_fmst] if i == 0 else (), accs=[b_fmst] if i > 0 else ())
            if own:
                S.op("act", lambda e, j=j, t0=t0, q0=q0: e.dma_start(out=cx["QA"][j].ap()[:, :, t0 - q0:t0 - q0 + 512].rearrange("h p t -> p h t"),
                                                                  in_=fmst[:, 0:4, :]),
                     reads=[b_fmst], accs=[b_D[("QA", j)]], dma_key="fmst_st")
            S.op("act", lambda e, j=j, t0=t0: e.dma_start(out=cx["KA"][j].ap()[:, :, t0:t0 + 512].rearrange("h p t -> p h t"), in_=fmst[:, 4:8, :]),
                 reads=[b_fmst], accs=[b_D[("KA", j)]], dma_key="fmst_st")
            for ti in range(4):
                r0 = t0 + ti * 128
                for kc in range(8):
                    S.op("pe", lambda e, kc=kc, ti=ti: e.matmul(PSB[3][:, :], lhsT=hT[:, kc, ti * 128:(ti + 1) * 128], rhs=win[:, kc, 1024:1536],
                                                               start=(kc == 0), stop=(kc == 7)),
                         reads=[b_win, b_hT], writes=[b_PSB[3]] if kc == 0 else (), accs=[b_PSB[3]] if kc > 0 else ())
                S.op("act", lambda e, ti=ti: e.activation(out=vast[:, ti, :], in_=PSB[3][:, :], func=AF.Identity),
                     reads=[b_PSB[3]], writes=[b_vast] if ti == 0 else (), accs=[b_vast] if ti > 0 else ())
                for kc in range(8):
                    S.op("pe", lambda e, kc=kc, ti=ti: e.matmul(PSB[4][:, 0:256], lhsT=hT[:, kc, ti * 128:(ti + 1) * 128], rhs=win[:, kc, 2048:2304],
                                                               start=(kc == 0), stop=(kc == 7)),
                         reads=[b_win, b_hT], writes=[b_PSB[4]] if kc == 0 else (), accs=[b_PSB[4]] if kc > 0 else ())
                S.op("dve", lambda e: e.tensor_copy(kvf[:], PSB[4][:, 0:256]), reads=[b_PSB[4]], writes=[b_kvf])
                S.op("dve", lambda e, ti=ti: e.tensor_copy(vbst[:, ti, :, 0:64], kvf[:, 128:256].rearrange("p (h d) -> p h d", h=2)),
                     reads=[b_kvf], accs=[b_vbst])
                cx["ld"]("sp", rt[:], cx["ROPE"][j].ap()[r0:r0 + 128, :, :], b_rt, "ropet")

                def norm_rope(src, H, gtile, b_g, dst_ap, b_dst, b_src):
                    HW = H * 64
                    s3 = src[:, 0:HW].rearrange("p (h d) -> p h d", h=H)
                    a3 = tq[0][:, 0:HW].rearrange("p (h d) -> p h d", h=H)
                    S.op("dve", lambda e: e.tensor_tensor(out=tq[0][:, 0:HW], in0=src[:, 0:HW], in1=src[:, 0:HW], op=ALU.mult),
                         reads=[b_src], writes=[b_tq[0]])
                    S.op("dve", lambda e: e.tensor_reduce(out=nst[:, 0:H], in_=a3, axis=AX.X, op=ALU.add),
                         reads=[b_tq[0]], writes=[b_nst])
                    S.op("act", lambda e: e.activation(out=nst[:, 8:8 + H], in_=nst[:, 0:H], func=AF.Sqrt, scale=1.0 / 64, bias=cx["epsb"][:, 0:1]),
                         reads=[cx["b_epsb"]], accs=[b_nst])
                    S.op("dve", lambda e: e.reciprocal(out=nst[:, 0:H], in_=nst[:, 8:8 + H]), accs=[b_nst])
                    S.op("dve", lambda e: e.tensor_tensor(out=a3, in0=s3, in1=nst[:, 0:H].unsqueeze(2).to_broadcast([128, H, 64]), op=ALU.mult),
                         reads=[b_src, b_nst], writes=[b_tq[0]])
                    S.op("dve", lambda e: e.tensor_tensor(out=a3, in0=a3, in1=gtile[:, :].unsqueeze(1).to_broadcast([128, H, 64]), op=ALU.mult),
                         reads=[b_g], accs=[b_tq[0]])
                    xv = tq[0][:, 0:HW].rearrange("p (h f two d) -> p h f two d", h=H, f=2, two=2)
                    t1 = tq[1][:, 0:HW // 2].rearrange("p (h f d) -> p h f d", h=H, f=2)
                    t2 = tq[2][:, 0:HW // 2].rearrange("p (h f d) -> p h f d", h=H, f=2)
                    cosb = rt[:, 0:4:2, :].unsqueeze(1).to_broadcast([128, H, 2, 16])
                    sinb = rt[:, 1:4:2, :].unsqueeze(1).to_broadcast([128, H, 2, 16])
                    dv = dst_ap.rearrange("p h (f two d) -> p h f two d", f=2, two=2)
                    x1 = xv[:, :, :, 0, :]; x2 = xv[:, :, :, 1, :]
                    S.op("dve", lambda e: e.tensor_tensor(out=t1, in0=x1, in1=cosb, op=ALU.mult), reads=[b_tq[0], b_rt], writes=[b_tq[1]])
                    S.op("dve", lambda e: e.tensor_tensor(out=t2, in0=x2, in1=sinb, op=ALU.mult), reads=[b_tq[0], b_rt], writes=[b_tq[2]])
                    S.op("dve", lambda e: e.tensor_tensor(out=dv[:, :, :, 0, :], in0=t1, in1=t2, op=ALU.subtract),
                         reads=[b_tq[1], b_tq[2]], accs=[b_dst])
                    S.op("dve", lambda e: e.tensor_tensor(out=t1, in0=x1, in1=sinb, op=ALU.mult), reads=[b_tq[0], b_rt], writes=[b_tq[1]])
                    S.op("dve", lambda e: e.tensor_tensor(out=t2, in0=x2, in1=cosb, op=ALU.mult), reads=[b_tq[0], b_rt], writes=[b_tq[2]])
                    S.op("dve", lambda e: e.tensor_tensor(out=dv[:, :, :, 1, :], in0=t1, in1=t2, op=ALU.add),
                         reads=[b_tq[1], b_tq[2]], accs=[b_dst])

                norm_rope(kvf, 2, gk, cx["b_gk"], kbb[:, :, 0, :], b_kbb, b_kvf)
                S.op("dve", lambda e: e.tensor_copy(kbb[:, :, 1, :], kbb[:, :, 0, :]), accs=[b_kbb])
                pK = PSB[5][:, :].bitcast(BF16)[:, 0:256].rearrange("p (h t) -> p h t", h=2)
                for h in range(2):
                    S.op("pe", lambda e, h=h, pK=pK: e.transpose(out=pK[:, h, :], in_=kbb[:, h, :, :].rearrange("p a d -> p (a d)"), identity=cx["ident_b"][:]),
                         reads=[b_kbb, cx["b_ident_b"]], writes=[b_PSB[5]] if h == 0 else (), accs=[b_PSB[5]] if h > 0 else ())
                S.op("dve", lambda e, ti=ti, pK=pK: e.tensor_copy(kbst[:, :, ti * 128:(ti + 1) * 128], pK), reads=[b_PSB[5]],
                     writes=[b_kbst] if ti == 0 else (), accs=[b_kbst] if ti > 0 else ())
                if own:
                    for kc in range(8):
                        S.op("pe", lambda e, kc=kc, ti=ti: e.matmul(PSB[6][:, :], lhsT=hT[:, kc, ti * 128:(ti + 1) * 128], rhs=win[:, kc, 1536:2048],
                                                                   start=(kc == 0), stop=(kc == 7)),
                             reads=[b_win, b_hT], writes=[b_PSB[6]] if kc == 0 else (), accs=[b_PSB[6]] if kc > 0 else ())
                    S.op("dve", lambda e: e.tensor_copy(qbf[:], PSB[6][:, :]), reads=[b_PSB[6]], writes=[b_qbf])
                    norm_rope(qbf, 8, gq, cx["b_gq"], qbb[:, :].rearrange("p (h d) -> p h d", h=8), b_qbb, b_qbf)
                    pQ = PSB[7][:, :].bitcast(BF16)[:, 0:512].rearrange("p (c t) -> p c t", c=4)
                    for c in range(4):
                        S.op("pe", lambda e, c=c, pQ=pQ: e.transpose(out=pQ[:, c, :], in_=qbb[:, c * 128:(c + 1) * 128], identity=cx["ident_b"][:]),
                             reads=[b_qbb, cx["b_ident_b"]], writes=[b_PSB[7]] if c == 0 else (), accs=[b_PSB[7]] if c > 0 else ())
                    S.op("dve", lambda e, ti=ti, pQ=pQ: e.tensor_copy(qbst[:, :, ti * 128:(ti + 1) * 128], pQ), reads=[b_PSB[7]],
                         writes=[b_qbst] if ti == 0 else (), accs=[b_qbst] if ti > 0 else ())
            S.op("act", lambda e, j=j, t0=t0: e.dma_start(out=cx["VA"][j].ap()[t0:t0 + 512, :].rearrange("(a p) n -> p a n", p=128), in_=vast[:]),
                 reads=[b_vast], accs=[b_D[("VA", j)]], dma_key="vast_st")
            S.op("act", lambda e, j=j, t0=t0: e.dma_start(out=cx["VB"][j].ap()[t0:t0 + 512, :, :].rearrange("(a p) h d -> p a h d", p=128), in_=vbst[:]),
                 reads=[b_vbst], accs=[b_D[("VB", j)]], dma_key="vbst_st")
            S.op("act", lambda e, j=j, t0=t0: e.dma_start(out=cx["KB"][j].ap()[:, :, t0:t0 + 512].rearrange("h p t -> p h t"), in_=kbst[:]),
                 reads=[b_kbst], accs=[b_D[("KB", j)]], dma_key="kbst_st")
            if own:
                S.op("act", lambda e, j=j, t0=t0, q0=q0: e.dma_start(out=cx["QB"][j].ap()[:, :, t0 - q0:t0 - q0 + 512].rearrange("c p t -> p c t"), in_=qbst[:]),
                     reads=[b_qbst], accs=[b_D[("QB", j)]], dma_key="qbst_st")
    sst.close(); cx["stk"].pop()
    S.barrier()


def stage_T(cx):
    nc, S, sb, jobs, J = cx["nc"], cx["S"], cx["sb"], cx["jobs"], cx["J"]
    PSB, b_PSB = cx["PSB"], cx["b_PSB"]
    b_D = cx["b_D"]
    ld = cx["ld"]
    sst = ExitStack(); cx["stk"].append(sst)
    SKM = max(j_[0] for j_ in jobs); SQM = max(j_[2] for j_ in jobs)
    jrev = sb("jrev", [128, 128], F32); b_jrev = Buf("jrev")
    ld("sp", jrev[:], cx["C"]["jrev"].ap(), b_jrev, "jrev")
    hk = sb("hk", [128, 1152], F32); b_hk = Buf("hk")
    STR = sb("STR", [128, 4, 1152], F32); b_STR = Buf("STR")
    for h in range(4):
        ld("sp", hk[:], bass.AP(cx["GRD"], h * 1280, [[1, 128], [1, 1152]]), b_hk, "hk")
        for i, (a_, n_) in enumerate([(0, 512), (512, 512), (1024, 128)]):
            S.op("pe", lambda e, a_=a_, n_=n_, i=i: e.matmul(PSB[i][:, 0:n_], lhsT=jrev[:, :], rhs=hk[:, a_:a_ + n_], start=True, stop=True),
                 reads=[b_jrev, b_hk], writes=[b_PSB[i]])
            S.op("dve", lambda e, a_=a_, n_=n_, i=i, h=h: e.tensor_copy(STR[:, h, a_:a_ + n_], PSB[i][:, 0:n_]),
                 reads=[b_PSB[i]], accs=[b_STR])
    W = cx["W"]
    b_WB = cx["b_WB"] = Buf("WB")
    srcs = (W["w_exp_gate"].ap().rearrange("e (p kc) f -> (e p) (kc f)", kc=8),
            W["w_exp_up"].ap().rearrange("e (p kc) f -> (e p) (kc f)", kc=8),
            W["w_exp_down"].ap().rearrange("e (p fc) d -> (e p) (fc d)", fc=2))
    RCH = 512
    cast_jobs = []
    for r0_ in range(0, NE * 128, RCH):
        for wi in range(3):
            cast_jobs.append((r0_, wi))

    def issue_casts(n, gate_buf):
        for _ in range(n):
            if not cast_jobs:
                return
            r0_, wi = cast_jobs.pop(0)
            S.op("pool", lambda e, r0_=r0_, wi=wi: e.dma_start(out=(cx["WBGU"].ap()[r0_:r0_ + RCH, wi * 2048:(wi + 1) * 2048] if wi < 2 else cx["WBD"].ap()[r0_:r0_ + RCH, :]), in_=srcs[wi][r0_:r0_ + RCH, :]),
                 reads=[gate_buf], accs=[b_WB], dma_key="wb_cast")
    NB2 = 2
    kT = [sb("kT%d" % i, [128, SKM], BF16) for i in range(NB2)]; b_kT = [Buf("kT%d" % i) for i in range(NB2)]
    vh = [sb("vh%d" % i, [128, SKM // 128, 128], BF16) for i in range(NB2)]; b_vh = [Buf("vh%d" % i) for i in range(NB2)]
    qT = [sb("qT%d" % i, [128, SQM], BF16) for i in range(NB2)]; b_qT = [Buf("qT%d" % i) for i in range(NB2)]
    NPT = 6
    pT = [sb("pT%d" % i, [128, 512], BF16) for i in range(NPT)]; b_pT = [Buf("pT%d" % i) for i in range(NPT)]
    sbi = [sb("sbi%d" % i, [128, 512], F32) for i in range(2)]; b_sbi = [Buf("sbi%d" % i) for i in range(2)]
    ft = [sb("ft%d" % i, [128, 512], F32) for i in range(5)]; b_ft = [Buf("ft%d" % i) for i in range(5)]
    sqb = sb("sqb", [128, 512], BF16); b_sqb = Buf("sqb")
    oTs = [sb("oTs%d" % i, [128, 512], BF16) for i in range(2)]; b_oTs = [Buf("oTs%d" % i) for i in range(2)]
    b_OT = cx["b_OT"] = Buf("OT")
    cnt = dict(pt=0, sbi=0, hd=0, ots=0, fin=0)
    tok0 = 0
    dacc = [[sb("dacc%d_%d" % (i, m), [128, 512], F32) for m in range(2)] for i in range(2)]
    b_dacc = [[Buf("dacc%d_%d" % (i, m)) for m in range(2)] for i in range(2)]
    dacc_pend = []
    pending = []

    def run_pending(kb, force=False):
        if not pending:
            return
        ph = pending[0]
        trig = (1, 2, 8)
        while ph and (force or kb >= trig[3 - len(ph)]):
            ph.pop(0)()
        if not ph:
            pending.pop(0)

    hfv = sb("hfv", [128, 2], F32); b_hfv = Buf("hfv")
    ld("sp", hfv[:], cx["C"]["hfv"].ap(), b_hfv, "hfv")
    BC2 = sb("BC2", [128, 8], F32); b_BC2 = Buf("BC2")
    SP1 = sb("SP1", [128, 4, 512], F32); b_SP1 = Buf("SP1")
    SP2 = sb("SP2", [128, 4, 512], F32); b_SP2 = Buf("SP2")
    bc = cx["bcst"]
    hb0 = sb("hb0", [128, 8], F32); b_hb0 = Buf("hb0")
    S.op("dve", lambda e: e.tensor_scalar(out=hb0[:, 0:4], in0=bc[:, 0, :], scalar1=hfv[:, 0:1], scalar2=None, op0=ALU.mult),
         reads=[cx["b_bcst"], b_hfv], writes=[b_hb0])
    S.op("dve", lambda e: e.tensor_scalar(out=hb0[:, 4:8], in0=bc[:, 1, :], scalar1=hfv[:, 1:2], scalar2=None, op0=ALU.mult),
         reads=[cx["b_bcst"], b_hfv], accs=[b_hb0])
    S.op("dve", lambda e: e.tensor_tensor(out=BC2[:, 0:4], in0=hb0[:, 0:4], in1=hb0[:, 4:8], op=ALU.add), reads=[b_hb0], writes=[b_BC2])
    for h in range(4):
        S.op("dve", lambda e, h=h: e.tensor_scalar(out=SP1[:, h, :], in0=STR[:, h, 0:512], scalar1=hfv[:, 1:2], scalar2=hb0[:, h:h + 1], op0=ALU.mult, op1=ALU.add),
             reads=[b_STR, b_hfv, b_hb0], accs=[b_SP1])
        S.op("dve", lambda e, h=h: e.tensor_scalar(out=SP2[:, h, :], in0=STR[:, h, 640:1152], scalar1=hfv[:, 0:1], scalar2=hb0[:, 4 + h:5 + h], op0=ALU.mult, op1=ALU.add),
             reads=[b_STR, b_hfv, b_hb0], accs=[b_SP2])
    for j in range(J):
        Sk, q0, Sq = jobs[j][:3]
        split = len(jobs[j]) > 3 and jobs[j][3]
        NKB = Sk // 128
        NKH = Sq // 128
        NQC = Sq // 512
        for h in range(4):
            hb = cnt["hd"] % NB2; cnt["hd"] += 1
            ld("sp", kT[hb][:, 0:Sk], cx["KA"][j].ap()[h, :, :], b_kT[hb], "kT%d" % hb, reads=[b_D[("KA", j)]])
            ld("sp", qT[hb][:, 0:Sq], cx["QA"][j].ap()[h, :, :], b_qT[hb], "qT%d" % hb, reads=[b_D[("QA", j)]])
            ld("sp", vh[hb][:, 0:NKB, :], cx["VA"][j].ap()[:, h * 128:(h + 1) * 128].rearrange("(a p) n -> p a n", p=128),
               b_vh[hb], "vh%d" % hb, reads=[b_D[("VA", j)]])
            for qc in range(Sq // 512):
                qa = q0 + qc * 512
                fi = cnt["fin"] % 2; cnt["fin"] += 1
                ob = (4 + 2 * fi, 5 + 2 * fi)
                if split or J == 1:
                    issue_casts(3, b_pT[cnt["pt"] % NPT])

                def issue_S(kb, hb=hb, qc=qc):
                    for m in range(2):
                        bank = (kb % 2) * 2 + m
                        S.op("pe", lambda e, kb=kb, m=m, bank=bank: e.matmul(PSB[bank][:, :], lhsT=kT[hb][m * 64:(m + 1) * 64, kb * 128:(kb + 1) * 128],
                                                                          rhs=qT[hb][m * 64:(m + 1) * 64, qc * 512:(qc + 1) * 512], start=True, stop=True),
                             reads=[b_kT[hb], b_qT[hb]], writes=[b_PSB[bank]])
                issue_S(0)
                for kb in range(NKB):
                    if kb + 1 < NKB:
                        issue_S(kb + 1)
                    run_pending(kb)
                    d = kb * 128 - qa
                    mode = "std"
                    if split and kb >= NKH:
                        if qc == NQC - 1 and kb == NKH:
                            mode = "sp1"
                        elif qc == 0 and kb == NKB - 1:
                            mode = "sp2"
                        else:
                            mode = "far2"
                    first = (kb == 0); last = (kb == NKB - 1)
                    pis = []
                    for m in range(2):
                        bank = (kb % 2) * 2 + m
                        pi = cnt["pt"] % NPT; cnt["pt"] += 1
                        pis.append(pi)
                        if mode in ("sp1", "sp2") or (mode == "std" and -128 <= d <= 512):
                            if mode == "std":
                                bt = STR[:, h, 512 - d:1024 - d]; bb = b_STR
                            else:
                                bt = (SP1 if mode == "sp1" else SP2)[:, h, :]; bb = b_SP1 if mode == "sp1" else b_SP2
                            si = cnt["sbi"] % 2; cnt["sbi"] += 1
                            S.op("dve", lambda e, bank=bank, si=si, bt=bt: e.scalar_tensor_tensor(
                                out=sbi[si][:], in0=PSB[bank][:, :], scalar=0.125, in1=bt, op0=ALU.mult, op1=ALU.add),
                                reads=[b_PSB[bank], bb], writes=[b_sbi[si]])
                            S.op("act", lambda e, si=si, pi=pi: e.activation(out=pT[pi][:], in_=sbi[si][:], func=AF.Exp),
                                 reads=[b_sbi[si]], writes=[b_pT[pi]])
                        else:
                            if mode == "far2":
                                bias_ap = BC2[:, h:h + 1]; bbuf = b_BC2
                            else:
                                sg_ = 1 if d > 0 else 0
                                bias_ap = cx["bcst"][:, sg_, h:h + 1]; bbuf = cx["b_bcst"]
                            S.op("act", lambda e, bank=bank, pi=pi, bias_ap=bias_ap: e.activation(out=pT[pi][:], in_=PSB[bank][:, :], func=AF.Exp,
                                                                                               scale=0.125, bias=bias_ap),
                                 reads=[b_PSB[bank], bbuf], writes=[b_pT[pi]])
                    for m in range(2):
                        pi = pis[m]
                        S.op("pe", lambda e, kb=kb, m=m, pi=pi, first=first, last=last, hb=hb, obk=ob[m]: e.matmul(PSB[obk][:, :], lhsT=vh[hb][:, kb, :], rhs=pT[pi][:],
                                                                                                          start=first, stop=last),
                             reads=[b_vh[hb], b_pT[pi]], writes=[b_PSB[ob[m]]] if first else (), accs=[b_PSB[ob[m]]] if not first else ())
                    for fnd in dacc_pend:
                        fnd()
                    dacc_pend.clear()
                    for m in range(2):
                        pi = pis[m]
                        if first:
                            dacc_pend.append(lambda pi=pi, fi=fi, m=m: S.op(
                                "dve", lambda e: e.tensor_copy(dacc[fi][m][:], pT[pi][:]), reads=[b_pT[pi]], writes=[b_dacc[fi][m]]))
                        else:
                            dacc_pend.append(lambda pi=pi, fi=fi, m=m: S.op(
                                "dve", lambda e: e.tensor_tensor(out=dacc[fi][m][:], in0=dacc[fi][m][:], in1=pT[pi][:], op=ALU.add),
                                reads=[b_pT[pi]], accs=[b_dacc[fi][m]]))
                for fnd in dacc_pend:
                    fnd()
                dacc_pend.clear()
                if pending:
                    run_pending(0, force=True)
                oi = cnt["ots"] % 2; cnt["ots"] += 1
                c0 = tok0 + qc * 512

                def phA(ob=ob, fi=fi):
                    for m in range(2):
                        if m == 0:
                            S.op("act", lambda e, m=m: e.activation(out=ft[m][:], in_=PSB[ob[m]][:, :], func=AF.Identity), reads=[b_PSB[ob[m]]], writes=[b_ft[m]])
                        else:
                            S.op("dve", lambda e, m=m: e.tensor_copy(ft[m][:], PSB[ob[m]][:, :]), reads=[b_PSB[ob[m]]], writes=[b_ft[m]])
                        S.op("pe", lambda e, m=m: e.matmul(PSB[ob[m]][:, :], lhsT=cx["ones_f"][:, :], rhs=dacc[fi][m][:], start=True, stop=True),
                             reads=[cx["b_ones_f"], b_dacc[fi][m]], writes=[b_PSB[ob[m]]])

                def phB1(ob=ob):
                    for m in range(2):
                        S.op("act", lambda e, m=m: e.activation(out=ft[3][:], in_=PSB[ob[m]][:, :], func=AF.Ln), reads=[b_PSB[ob[m]]], writes=[b_ft[3]])
                        S.op("act", lambda e: e.activation(out=ft[3][:], in_=ft[3][:], func=AF.Exp, scale=-1.0), accs=[b_ft[3]])
                        S.op("dve", lambda e, m=m: e.tensor_tensor(out=ft[m][:], in0=ft[m][:], in1=ft[3][:], op=ALU.mult), reads=[b_ft[3]], accs=[b_ft[m]])
                    S.op("dve", lambda e: e.scalar_tensor_tensor(out=ft[2][:], in0=ft[1][:], scalar=cx["NLAM"], in1=ft[0][:], op0=ALU.mult, op1=ALU.add),
                         reads=[b_ft[0], b_ft[1], cx["b_lsm"]], writes=[b_ft[2]])
                    S.op("dve", lambda e: e.tensor_tensor(out=sqb[:], in0=ft[2][:], in1=ft[2][:], op=ALU.mult), reads=[b_ft[2]], writes=[b_sqb])
                    S.op("pe", lambda e: e.matmul(PSB[ob[0]][:, :], lhsT=cx["ones_b"][:, :], rhs=sqb[:], start=True, stop=True),
                         reads=[cx["b_ones_b"], b_sqb], writes=[b_PSB[ob[0]]])

                def phB2(ob=ob, oi=oi, h=h, c0=c0):
                    S.op("act", lambda e: e.activation(out=ft[3][:], in_=PSB[ob[0]][:, :], func=AF.Ln, scale=1.0 / 128, bias=cx["epsb"][:, 0:1]),
                         reads=[b_PSB[ob[0]], cx["b_epsb"]], writes=[b_ft[3]])
                    S.op("act", lambda e: e.activation(out=ft[4][:], in_=ft[3][:], func=AF.Exp, scale=-0.5), reads=[b_ft[3]], writes=[b_ft[4]])
                    S.op("dve", lambda e: e.scalar_tensor_tensor(out=oTs[oi][:], in0=ft[2][:], scalar=cx["gsub"][:, 0:1], in1=ft[4][:],
                                                                 op0=ALU.mult, op1=ALU.mult),
                         reads=[b_ft[2], b_ft[4], cx["b_gsub"]], writes=[b_oTs[oi]])
                    S.op("act", lambda e: e.dma_start(out=cx["OT"].ap()[h, :, c0:c0 + 512], in_=oTs[oi][:]),
                         reads=[b_oTs[oi]], accs=[b_OT], dma_key="oTs%d_st" % oi)
                pending.append([phA, phB1, phB2])
        while pending:
            run_pending(0, force=True)
        for n in range(2):
            hb = cnt["hd"] % NB2; cnt["hd"] += 1
            ld("sp", kT[hb][:, 0:Sk], cx["KB"][j].ap()[n, :, :], b_kT[hb], "kT%d" % hb, reads=[b_D[("KB", j)]])
            vb1 = vh[hb][:, :, :].rearrange("p a n -> p (a n)")[:, 0:NKB * 65].rearrange("p (a n) -> p a n", n=65)
            ld("sp", vb1, cx["VB"][j].ap()[:, n, :].rearrange("(a p) n -> p a n", p=128), b_vh[hb], "vh%d" % hb, reads=[b_D[("VB", j)]])
            for cpair in range(2):
                c = 2 * n + cpair
                qb_ = cnt["hd"] % NB2 if False else (hb + cpair) % NB2
                ld("sp", qT[qb_][:, 0:Sq], cx["QB"][j].ap()[c, :, :], b_qT[qb_], "qT%d" % qb_, reads=[b_D[("QB", j)]])
                for qc in range(Sq // 512):
                    def issue_SB(kb, hb=hb, qb_=qb_, qc=qc):
                        for hh in range(2):
                            bank = (kb % 2) * 2 + hh
                            S.op("pe", lambda e, kb=kb, hh=hh, bank=bank: e.matmul(PSB[bank][:, :], lhsT=kT[hb][hh * 64:(hh + 1) * 64, kb * 128:(kb + 1) * 128],
                                                                                rhs=qT[qb_][hh * 64:(hh + 1) * 64, qc * 512:(qc + 1) * 512], start=True, stop=True),
                                 reads=[b_kT[hb], b_qT[qb_]], writes=[b_PSB[bank]])
                    if split or J == 1:
                        issue_casts(3, b_pT[cnt["pt"] % NPT])
                    issue_SB(0)
                    for kb in range(NKB):
                        if kb + 1 < NKB:
                            issue_SB(kb + 1)
                        first = (kb == 0); last = (kb == NKB - 1)
                        for hh in range(2):
                            bank = (kb % 2) * 2 + hh
                            pi = cnt["pt"] % NPT; cnt["pt"] += 1
                            S.op("act", lambda e, bank=bank, pi=pi: e.activation(out=pT[pi][:], in_=PSB[bank][:, :], func=AF.Exp, scale=0.125),
                                 reads=[b_PSB[bank]], writes=[b_pT[pi]])
                            S.op("pe", lambda e, kb=kb, hh=hh, pi=pi, first=first, last=last, vb1=vb1: e.matmul(PSB[4 + hh][0:65, :], lhsT=vb1[:, kb, :], rhs=pT[pi][:],
                                                                                                             start=first, stop=last),
                                 reads=[b_vh[hb], b_pT[pi]], writes=[b_PSB[4 + hh]] if first else (), accs=[b_PSB[4 + hh]] if not first else ())
                    for hh in range(2):
                        S.op("act", lambda e, hh=hh: e.activation(out=ft[hh][64:65, :], in_=PSB[4 + hh][64:65, :], func=AF.Ln),
                             reads=[b_PSB[4 + hh]], writes=[b_ft[hh]])
                        S.op("act", lambda e, hh=hh: e.activation(out=ft[hh][64:65, :], in_=ft[hh][64:65, :], func=AF.Exp, scale=-1.0), accs=[b_ft[hh]])
                        S.op("pe", lambda e, hh=hh: e.matmul(PSB[6 + hh][0:64, :], lhsT=cx["ones_f"][64:65, 0:64], rhs=ft[hh][64:65, :], start=True, stop=True),
                             reads=[cx["b_ones_f"], b_ft[hh]], writes=[b_PSB[6 + hh]])
                        S.op("act", lambda e, hh=hh: e.activation(out=ft[2 + hh][0:64, :], in_=PSB[6 + hh][0:64, :], func=AF.Identity),
                             reads=[b_PSB[6 + hh]], writes=[b_ft[2 + hh]])
                        oi = cnt["ots"] % 2; cnt["ots"] += 1
                        S.op("dve", lambda e, hh=hh, oi=oi: e.tensor_tensor(out=oTs[oi][0:64, :], in0=PSB[4 + hh][0:64, :], in1=ft[2 + hh][0:64, :], op=ALU.mult),
                             reads=[b_PSB[4 + hh], b_ft[2 + hh]], writes=[b_oTs[oi]])
                        c0 = tok0 + qc * 512
                        S.op("act", lambda e, oi=oi, c=c, hh=hh, c0=c0: e.dma_start(out=cx["OT"].ap()[4 + c, hh * 64:(hh + 1) * 64, c0:c0 + 512], in_=oTs[oi][0:64, :]),
                             reads=[b_oTs[oi]], accs=[b_OT], dma_key="oTs%d_st" % oi)
        tok0 += Sq
    issue_casts(10 ** 6, b_pT[0])
    sst.close(); cx["stk"].pop()
    S.barrier()


def stage_O(cx):
    nc, S, sb, jobs, J = cx["nc"], cx["S"], cx["sb"], cx["jobs"], cx["J"]
    PSB, b_PSB = cx["PSB"], cx["b_PSB"]
    ld = cx["ld"]; W = cx["W"]
    NT, NBLK = cx["NT"], cx["NBLK"]
    st = cx["st"]; cx["stk"].append(st)
    E8 = sb("E8", [128, NT, 8], F32); b_E8 = Buf("E8")
    R8 = sb("R8", [128, NT, 8], F32); b_R8 = Buf("R8")
    G8 = sb("G8", [128, NT, 8], F32); b_G8 = Buf("G8")
    SL8 = sb("SL8", [128, NT, 8], I32); b_SL8 = Buf("SL8")
    IDXW = sb("IDXW", [128, NBLK], I32); b_IDXW = Buf("IDXW")
    IDXD = sb("IDXD", [128, NBLK], I32); b_IDXD = Buf("IDXD")
    IDXD2 = sb("IDXD2", [128, NBLK], I32); b_IDXD2 = Buf("IDXD2")
    cx["IDXD2"] = IDXD2; cx["b_IDXD2"] = b_IDXD2
    cx["stk"].pop()
    cx.update(E8=E8, b_E8=b_E8, R8=R8, b_R8=b_R8, G8=G8, b_G8=b_G8, SL8=SL8, b_SL8=b_SL8,
              IDXW=IDXW, b_IDXW=b_IDXW, IDXD=IDXD, b_IDXD=b_IDXD)
    sst = ExitStack(); cx["stk"].append(sst)
    rd = lambda n: W[n].ap().rearrange("(kc p) n -> p kc n", p=128)
    wout = sb("wout", [128, 8, D], BF16); b_wout = Buf("wout")
    wr = sb("wr", [128, 8, NE], BF16); b_wr = Buf("wr")
    wsgu = sb("wsgu", [128, 8, 512], BF16); b_wsgu = Buf("wsgu")
    wsd = sb("wsd", [128, 2, D], BF16); b_wsd = Buf("wsd")
    ld("pool", wout[:], rd("w_out"), b_wout, "wout")
    ld("pool", wr[:], rd("w_router"), b_wr, "wr")
    ld("pool", wsgu[:, :, 0:256], rd("w_sh_gate"), b_wsgu, "wsgu")
    ld("pool", wsgu[:, :, 256:512], rd("w_sh_up"), b_wsgu, "wsgu", acc=True)
    ld("pool", wsd[:], W["w_sh_down"].ap().rearrange("(fc p) n -> p fc n", p=128), b_wsd, "wsd")
    rbias = sb("rbias", [128, NE], F32); b_rbias = Buf("rbias")
    ld("sp", rbias[:], bass.AP(W["router_bias"], 0, [[0, 128], [1, NE]]), b_rbias, "rbias")
    E1 = sb("E1", [128, NE], F32); b_E1 = Buf("E1")
    S.op("dve", lambda e: e.tensor_scalar(out=E1[:], in0=cx["iota_e"][:], scalar1=16384.0, scalar2=1.0, op0=ALU.mult, op1=ALU.add),
         reads=[cx["b_iota_e"]], writes=[b_E1])
    Mcum = sb("Mcum", [128, NE], BF16); b_Mcum = Buf("Mcum")
    S.op("dve", lambda e: e.memset(Mcum[:], 0.0), writes=[b_Mcum])
    MOD = [sb("mod%d" % v, [128, D], F32) for v in range(4)]; b_MOD = [Buf("mod%d" % v) for v in range(4)]
    oT = sb("oT", [128, 8, 512], BF16); b_oT = Buf("oT")
    xt = [sb("xo%d" % i, [128, D], F32) for i in range(2)]; b_xt = [Buf("xo%d" % i) for i in range(2)]
    sqj = sb("sqo", [128, D], BF16); b_sqj = Buf("sqo")
    st1 = sb("sto", [128, 8], F32); b_st1 = Buf("sto")
    tmp = sb("tmpo", [128, D], F32); b_tmp = Buf("tmpo")
    h2b = [sb("h2b%d" % i, [128, D], BF16) for i in range(2)]; b_h2b = [Buf("h2b%d" % i) for i in range(2)]
    h2T = sb("h2T", [128, 8, 128], BF16); b_h2T = Buf("h2T")
    sgt = sb("sgt", [128, 256], F32); b_sgt = Buf("sgt")
    ab = sb("ab", [128, 256], BF16); b_ab = Buf("ab")
    aT = sb("aT", [128, 2, 128], BF16); b_aT = Buf("aT")
    scr = sb("scr", [128, NE], F32); b_scr = Buf("scr")
    sel = sb("sel", [128, NE], F32); b_sel = Buf("sel")
    selm = sb("selm", [128, NE], F32); b_selm = Buf("selm")
    g8 = sb("g8", [128, 8, 8], F32); b_g8 = Buf("g8")
    gs = sb("gs", [128, 32], F32); b_gs = Buf("gs")
    Mb = sb("Mb", [128, NE], BF16); b_Mb = Buf("Mb")
    wg = sb("wg", [128, NE], F32); b_wg = Buf("wg")
    pvm = sb("pvm", [128, NE], F32); b_pvm = Buf("pvm")
    p8 = sb("p8", [128, 8], F32); b_p8 = Buf("p8")
    p8i = sb("p8i", [128, 16], I32); b_p8i = Buf("p8i")
    junk = sb("junko", [128, NE], F32); b_junk = Buf("junko")
    b_X1 = cx["b_X1"] = Buf("X1")
    b_H2 = cx["b_H2"] = Buf("H2")
    b_MODR = Buf("MODRr")
    tok0 = 0
    tg = 0
    for j in range(J):
        Sk, q0, Sq = jobs[j][:3]
        for v in range(4):
            ld("sp", MOD[v][:], bass.AP(cx["MODR"], (j * 4 + v) * D, [[0, 128], [1, D]]), b_MOD[v], "mod%d" % v)
        GT1B, G2B, SH2B, GT2B = MOD
        for ch in range(Sq // 512):
            c0 = tok0 + ch * 512
            ld("sp", oT[:], cx["OT"].ap()[:, :, c0:c0 + 512].rearrange("c p t -> p c t"), b_oT, "oT", reads=[cx["b_OT"]])
            for ti in range(4):
                g0 = c0 + ti * 128
                r0 = q0 + ch * 512 + ti * 128
                x_ = xt[tg % 2]; bx = b_xt[tg % 2]
                hb_ = h2b[tg % 2]; bh = b_h2b[tg % 2]
                ld("sp", x_[:], cx["xs"][j].ap()[r0:r0 + 128, :], bx, "xo%d" % (tg % 2))
                for half in range(2):
                    for c in range(8):
                        S.op("pe", lambda e, half=half, c=c, ti=ti: e.matmul(PSB[half][:, :], lhsT=oT[:, c, ti * 128:(ti + 1) * 128],
                                                                            rhs=wout[:, c, half * 512:(half + 1) * 512], start=(c == 0), stop=(c == 7)),
                             reads=[b_oT, b_wout], writes=[b_PSB[half]] if c == 0 else (), accs=[b_PSB[half]] if c > 0 else ())
                    sl = slice(half * 512, (half + 1) * 512)
                    S.op("dve", lambda e, half=half, sl=sl: e.tensor_tensor(out=tmp[:, sl], in0=PSB[half][:, :], in1=GT1B[:, sl], op=ALU.mult),
                         reads=[b_PSB[half], b_MOD[0]], writes=[b_tmp] if half == 0 else (), accs=[b_tmp] if half == 1 else ())
                    S.op("dve", lambda e, sl=sl, x_=x_: e.tensor_tensor(out=x_[:, sl], in0=x_[:, sl], in1=tmp[:, sl], op=ALU.add),
                         reads=[b_tmp], accs=[bx])
                S.op("act", lambda e, x_=x_: e.activation(out=sqj[:], in_=x_[:], func=AF.Square, accum_out=st1[:, 0:1]),
                     reads=[bx], writes=[b_sqj, b_st1])
                S.op("act", lambda e: e.activation(out=st1[:, 1:2], in_=st1[:, 0:1], func=AF.Sqrt, scale=1.0 / D, bias=cx["epsb"][:, 0:1]),
                     reads=[cx["b_epsb"]], accs=[b_st1])
                S.op("dve", lambda e: e.reciprocal(out=st1[:, 2:3], in_=st1[:, 1:2]), accs=[b_st1])
                S.op("dve", lambda e, x_=x_: e.scalar_tensor_tensor(out=tmp[:], in0=x_[:], scalar=st1[:, 2:3], in1=G2B[:], op0=ALU.mult, op1=ALU.mult),
                     reads=[bx, b_st1, b_MOD[1]], writes=[b_tmp])
                S.op("dve", lambda e, hb_=hb_: e.tensor_tensor(out=hb_[:], in0=tmp[:], in1=SH2B[:], op=ALU.add),
                     reads=[b_tmp, b_MOD[2]], writes=[bh])
                S.op("act", lambda e, hb_=hb_, g0=g0: e.dma_start(out=cx["H2"].ap()[g0:g0 + 128, :], in_=hb_[:]),
                     reads=[bh], accs=[b_H2], dma_key="h2b%d_st" % (tg % 2))
                pT_ = PSB[2][:, :].bitcast(BF16).rearrange("p (kc t) -> p kc t", kc=8)
                for kc in range(8):
                    S.op("pe", lambda e, kc=kc, hb_=hb_, pT_=pT_: e.transpose(out=pT_[:, kc, :], in_=hb_[:, kc * 128:(kc + 1) * 128], identity=cx["ident_b"][:]),
                         reads=[bh, cx["b_ident_b"]], writes=[b_PSB[2]] if kc == 0 else (), accs=[b_PSB[2]] if kc > 0 else ())
                S.op("act", lambda e, pT_=pT_: e.activation(out=h2T[:], in_=pT_, func=AF.Identity), reads=[b_PSB[2]], writes=[b_h2T])
                for kc in range(8):
                    S.op("pe", lambda e, kc=kc: e.matmul(PSB[3][:, 0:NE], lhsT=h2T[:, kc, :], rhs=wr[:, kc, :], start=(kc == 0), stop=(kc == 7)),
                         reads=[b_h2T, b_wr], writes=[b_PSB[3]] if kc == 0 else (), accs=[b_PSB[3]] if kc > 0 else ())
                for kc in range(8):
                    S.op("pe", lambda e, kc=kc: e.matmul(PSB[4][:, :], lhsT=h2T[:, kc, :], rhs=wsgu[:, kc, :], start=(kc == 0), stop=(kc == 7)),
                         reads=[b_h2T, b_wsgu], writes=[b_PSB[4]] if kc == 0 else (), accs=[b_PSB[4]] if kc > 0 else ())
                S.op("act", lambda e: e.activation(out=sgt[:], in_=PSB[4][:, 0:256], func=AF.Silu), reads=[b_PSB[4]], writes=[b_sgt])
                S.op("dve", lambda e: e.tensor_tensor(out=ab[:], in0=PSB[4][:, 256:512], in1=sgt[:], op=ALU.mult),
                     reads=[b_PSB[4], b_sgt], writes=[b_ab])
                pA = PSB[5][:, :].bitcast(BF16)[:, 0:256].rearrange("p (c t) -> p c t", c=2)
                for fc in range(2):
                    S.op("pe", lambda e, fc=fc, pA=pA: e.transpose(out=pA[:, fc, :], in_=ab[:, fc * 128:(fc + 1) * 128], identity=cx["ident_b"][:]),
                         reads=[b_ab, cx["b_ident_b"]], writes=[b_PSB[5]] if fc == 0 else (), accs=[b_PSB[5]] if fc > 0 else ())
                S.op("act", lambda e, pA=pA: e.activation(out=aT[:], in_=pA, func=AF.Identity), reads=[b_PSB[5]], writes=[b_aT])
                for half in range(2):
                    for fc in range(2):
                        S.op("pe", lambda e, half=half, fc=fc: e.matmul(PSB[6 + half][:, :], lhsT=aT[:, fc, :], rhs=wsd[:, fc, half * 512:(half + 1) * 512],
                                                                       start=(fc == 0), stop=(fc == 1)),
                             reads=[b_aT, b_wsd], writes=[b_PSB[6 + half]] if fc == 0 else (), accs=[b_PSB[6 + half]] if fc > 0 else ())
                    sl = slice(half * 512, (half + 1) * 512)
                    S.op("dve", lambda e, half=half, sl=sl: e.tensor_tensor(out=tmp[:, sl], in0=PSB[6 + half][:, :], in1=GT2B[:, sl], op=ALU.mult),
                         reads=[b_PSB[6 + half], b_MOD[3]], writes=[b_tmp] if half == 0 else (), accs=[b_tmp] if half == 1 else ())
                    S.op("dve", lambda e, sl=sl, x_=x_: e.tensor_tensor(out=x_[:, sl], in0=x_[:, sl], in1=tmp[:, sl], op=ALU.add),
                         reads=[b_tmp], accs=[bx])
                S.op("act", lambda e, x_=x_, g0=g0: e.dma_start(out=cx["X1"].ap()[g0:g0 + 128, :], in_=x_[:]),
                     reads=[bx], accs=[b_X1], dma_key="xo%d_st" % (tg % 2))
                S.op("act", lambda e: e.activation(out=scr[:], in_=PSB[3][:, 0:NE], func=AF.Sigmoid), reads=[b_PSB[3]], writes=[b_scr])
                S.op("dve", lambda e: e.tensor_tensor(out=sel[:], in0=scr[:], in1=rbias[:], op=ALU.add), reads=[b_scr, b_rbias], writes=[b_sel])
                for g in range(8):
                    S.op("dve", lambda e, g=g: e.max(out=g8[:, g, :], in_=sel[:, g * 32:(g + 1) * 32]), reads=[b_sel],
                         writes=[b_g8] if g == 0 else (), accs=[b_g8] if g > 0 else ())
                S.op("dve", lambda e: e.tensor_tensor(out=gs[:, 0:8], in0=g8[:, :, 0], in1=g8[:, :, 1], op=ALU.add), reads=[b_g8], writes=[b_gs])
                S.op("dve", lambda e: e.max(out=gs[:, 8:16], in_=gs[:, 0:8]), accs=[b_gs])
                S.op("dve", lambda e: e.tensor_scalar(out=gs[:, 16:24], in0=gs[:, 0:8], scalar1=gs[:, 11:12], scalar2=None, op0=ALU.is_ge), accs=[b_gs])
                S.op("dve", lambda e: e.scalar_tensor_tensor(out=selm[:].rearrange("p (g i) -> p g i", g=8), in0=sel[:].rearrange("p (g i) -> p g i", g=8),
                                                             scalar=10.0, in1=gs[:, 16:24].unsqueeze(2).to_broadcast([128, 8, 32]), op0=ALU.add, op1=ALU.mult),
                     reads=[b_sel, b_gs], writes=[b_selm])
                S.op("dve", lambda e: e.max(out=gs[:, 24:32], in_=selm[:]), reads=[b_selm], accs=[b_gs])
                S.op("dve", lambda e: e.tensor_scalar(out=Mb[:], in0=selm[:], scalar1=gs[:, 31:32], scalar2=None, op0=ALU.is_ge),
                     reads=[b_selm, b_gs], writes=[b_Mb])
                S.op("dve", lambda e: e.tensor_tensor(out=wg[:], in0=scr[:], in1=Mb[:], op=ALU.mult), reads=[b_scr, b_Mb], writes=[b_wg])
                S.op("dve", lambda e: e.tensor_reduce(out=p8[:, 0:1], in_=wg[:], axis=AX.X, op=ALU.add), reads=[b_wg], writes=[b_p8])
                S.op("dve", lambda e: e.reciprocal(out=p8[:, 1:2], in_=p8[:, 0:1]), accs=[b_p8])
                S.op("dve", lambda e: e.tensor_scalar(out=wg[:], in0=wg[:], scalar1=p8[:, 1:2], scalar2=2.5, op0=ALU.mult, op1=ALU.mult),
                     reads=[b_p8], accs=[b_wg])
                S.op("pe", lambda e: e.matmul(PSB[5][:, 0:NE], lhsT=cx["tri_b"][:, :], rhs=Mb[:], start=True, stop=False),
                     reads=[cx["b_tri"], b_Mb], writes=[b_PSB[5]])
                S.op("pe", lambda e: e.matmul(PSB[5][:, 0:NE], lhsT=cx["ones_b"][:, :], rhs=Mcum[:], start=False, stop=True),
                     reads=[cx["b_ones_b"], b_Mcum], accs=[b_PSB[5]])
                S.op("dve", lambda e: e.tensor_tensor(out=pvm[:], in0=PSB[5][:, 0:NE], in1=E1[:], op=ALU.add), reads=[b_PSB[5], b_E1], writes=[b_pvm])
                S.op("dve", lambda e: e.tensor_tensor(out=pvm[:], in0=pvm[:], in1=Mb[:], op=ALU.mult), reads=[b_Mb], accs=[b_pvm])
                S.op("dve", lambda e: e.tensor_tensor(out=Mcum[:], in0=Mcum[:], in1=Mb[:], op=ALU.add), reads=[b_Mb], writes=[b_Mcum])
                S.op("dve", lambda e: e.max(out=p8[:, 0:8], in_=pvm[:]), reads=[b_pvm], writes=[b_p8])
                S.op("dve", lambda e: e.tensor_copy(p8i[:, 0:8], p8[:, 0:8]), reads=[b_p8], writes=[b_p8i])
                S.op("dve", lambda e: e.tensor_scalar(out=p8i[:, 8:16], in0=p8i[:, 0:8], scalar1=14, scalar2=None, op0=ALU.arith_shift_right), accs=[b_p8i])
                S.op("dve", lambda e, tg=tg: e.tensor_copy(E8[:, tg, :], p8i[:, 8:16]), reads=[b_p8i], accs=[b_E8])
                S.op("dve", lambda e: e.tensor_scalar(out=p8i[:, 8:16], in0=p8i[:, 0:8], scalar1=16383, scalar2=None, op0=ALU.bitwise_and), accs=[b_p8i])
                S.op("dve", lambda e, tg=tg: e.tensor_copy(R8[:, tg, :], p8i[:, 8:16]), reads=[b_p8i], accs=[b_R8])
                for k in range(8):
                    S.op("dve", lambda e, k=k, tg=tg: e.scalar_tensor_tensor(out=junk[:], in0=pvm[:], scalar=p8[:, k:k + 1], in1=wg[:], op0=ALU.is_equal, op1=ALU.mult,
                                                                             accum_out=G8[:, tg, k:k + 1]),
                         reads=[b_pvm, b_p8, b_wg], writes=[b_junk], accs=[b_G8])
                tg += 1
        tok0 += Sq
    cc = sb("cc", [128, 16], F32); b_cc = Buf("cc")
    cci = sb("cci", [128, 8], I32); b_cci = Buf("cci")
    dg = sb("dg", [128, 128], F32); b_dg = Buf("dg")
    PSrow = sb("PSrow", [128, NE], F32); b_PSrow = Buf("PSrow")
    for ec in range(2):
        S.op("pe", lambda e, ec=ec: e.matmul(PSB[0][:, ec:ec + 1], lhsT=Mcum[:, ec * 128:(ec + 1) * 128], rhs=cx["ones_b"][:, 0:1], start=True, stop=True),
             reads=[b_Mcum, cx["b_ones_b"]], writes=[b_PSB[0]] if ec == 0 else (), accs=[b_PSB[0]] if ec else ())
    S.op("dve", lambda e: e.tensor_scalar(out=cc[:, 0:2], in0=PSB[0][:, 0:2], scalar1=127.0, scalar2=None, op0=ALU.add), reads=[b_PSB[0]], writes=[b_cc])
    S.op("dve", lambda e: e.tensor_copy(cci[:, 0:2], cc[:, 0:2]), reads=[b_cc], writes=[b_cci])
    S.op("dve", lambda e: e.tensor_scalar(out=cci[:, 2:4], in0=cci[:, 0:2], scalar1=7, scalar2=None, op0=ALU.arith_shift_right), accs=[b_cci])
    S.op("dve", lambda e: e.tensor_copy(cc[:, 2:4], cci[:, 2:4]), reads=[b_cci], accs=[b_cc])
    for ec in range(2):
        S.op("pe", lambda e, ec=ec: e.matmul(PSB[1][:, ec:ec + 1], lhsT=cx["tril_f"][:, :], rhs=cc[:, 2 + ec:3 + ec], start=True, stop=(ec == 0)),
             reads=[cx["b_tril"], b_cc], writes=[b_PSB[1]] if ec == 0 else (), accs=[b_PSB[1]] if ec else ())
        if ec == 1:
            S.op("pe", lambda e: e.matmul(PSB[1][:, 1:2], lhsT=cx["ones_f"][:, :], rhs=cc[:, 2:3], start=False, stop=True),
                 reads=[cx["b_ones_f"], b_cc], accs=[b_PSB[1]])
    S.op("dve", lambda e: e.tensor_copy(cc[:, 4:6], PSB[1][:, 0:2]), reads=[b_PSB[1]], accs=[b_cc])
    S.op("dve", lambda e: e.tensor_tensor(out=cc[:, 6:8], in0=cc[:, 4:6], in1=cc[:, 2:4], op=ALU.subtract), accs=[b_cc])
    S.op("dve", lambda e: e.tensor_scalar(out=cc[:, 8:10], in0=cc[:, 6:8], scalar1=128.0, scalar2=None, op0=ALU.mult), accs=[b_cc])
    for ec in range(2):
        S.op("dve", lambda e, ec=ec: e.tensor_scalar(out=dg[:], in0=cx["ident_f"][:], scalar1=cc[:, 8 + ec:9 + ec], scalar2=None, op0=ALU.mult),
             reads=[cx["b_ident_f"], b_cc], writes=[b_dg])
        S.op("pe", lambda e, ec=ec: e.matmul(PSB[2][:, ec * 128:(ec + 1) * 128], lhsT=cx["ones_f"][:, :], rhs=dg[:], start=True, stop=True),
             reads=[cx["b_ones_f"], b_dg], writes=[b_PSB[2]] if ec == 0 else (), accs=[b_PSB[2]] if ec else ())
    S.op("dve", lambda e: e.tensor_copy(PSrow[:], PSB[2][:, 0:NE]), reads=[b_PSB[2]], writes=[b_PSrow])
    for t in range(NT):
        for k in range(8):
            S.op("dve", lambda e, k=k, t=t: e.scalar_tensor_tensor(out=junk[:], in0=cx["iota_e"][:], scalar=E8[:, t, k:k + 1], in1=PSrow[:], op0=ALU.is_equal, op1=ALU.mult,
                                                                   accum_out=p8[:, k:k + 1]),
                 reads=[cx["b_iota_e"], b_E8, b_PSrow], writes=[b_junk], accs=[b_p8])
        S.op("dve", lambda e, t=t: e.tensor_tensor(out=p8[:, 0:8], in0=p8[:, 0:8], in1=R8[:, t, :], op=ALU.add), reads=[b_R8], accs=[b_p8])
        S.op("dve", lambda e, t=t: e.tensor_scalar(out=SL8[:, t, :], in0=p8[:, 0:8], scalar1=-1.0, scalar2=None, op0=ALU.add), reads=[b_p8], accs=[b_SL8])
    NB1 = min(NBLK, 512)
    ind = sb("ind", [128, NBLK], BF16); b_ind = Buf("ind")
    EB = sb("EB", [128, NBLK], F32); b_EB = Buf("EB")
    CH = sb("CH", [128, NBLK], F32); b_CH = Buf("CH")
    for ec in range(2):
        S.op("dve", lambda e, ec=ec: e.tensor_scalar(out=ind[:], in0=cx["iota_b"][:, 0:NBLK], scalar1=cc[:, 4 + ec:5 + ec], scalar2=None, op0=ALU.is_ge),
             reads=[cx["b_iota_b"], b_cc], writes=[b_ind])
        for a0 in range(0, NBLK, 512):
            n_ = min(512, NBLK - a0)
            bk = 3 + a0 // 512
            S.op("pe", lambda e, ec=ec, a0=a0, n_=n_, bk=bk: e.matmul(PSB[bk][:, 0:n_], lhsT=cx["ones_b"][:, :], rhs=ind[:, a0:a0 + n_], start=(ec == 0), stop=(ec == 1)),
                 reads=[cx["b_ones_b"], b_ind], writes=[b_PSB[bk]] if ec == 0 else (), accs=[b_PSB[bk]] if ec else ())
    for a0 in range(0, NBLK, 512):
        n_ = min(512, NBLK - a0)
        bk = 3 + a0 // 512
        S.op("dve", lambda e, a0=a0, n_=n_, bk=bk: e.tensor_scalar(out=EB[:, a0:a0 + n_], in0=PSB[bk][:, 0:n_], scalar1=255.0, scalar2=None, op0=ALU.min),
             reads=[b_PSB[bk]], writes=[b_EB] if a0 == 0 else (), accs=[b_EB] if a0 else ())
    S.op("dve", lambda e: e.memset(CH[:, 0:4], 1.0), writes=[b_CH])
    S.op("dve", lambda e: e.tensor_tensor(out=CH[:, 4:NBLK], in0=EB[:, 4:NBLK], in1=EB[:, 0:NBLK - 4], op=ALU.not_equal), reads=[b_EB], accs=[b_CH])
    S.op("dve", lambda e: e.memset(CH[0:1, :], 1.0), accs=[b_CH])
    BIGI = 1.0e6
    EBs = sb("EBs", [128, NBLK], F32); b_EBs = Buf("EBs")
    S.op("dve", lambda e: e.tensor_scalar(out=EBs[:], in0=EB[:], scalar1=128.0, scalar2=cx["iota_p"][:, 0:1], op0=ALU.mult, op1=ALU.add),
         reads=[b_EB, cx["b_iota_p"]], writes=[b_EBs])
    S.op("dve", lambda e: e.scalar_tensor_tensor(out=EBs[:], in0=EBs[:], scalar=-BIGI, in1=CH[:], op0=ALU.add, op1=ALU.mult),
         reads=[b_CH], accs=[b_EBs])
    S.op("dve", lambda e: e.tensor_scalar(out=IDXW[:], in0=EBs[:], scalar1=BIGI, scalar2=None, op0=ALU.add), reads=[b_EBs], writes=[b_IDXW])
    b_XS = cx["b_XS"] = Buf("XS")
    for t in range(NT):
        hb_ = h2b[t % 2]; bh = b_h2b[t % 2]
        ld("sp", hb_[:], cx["H2"].ap()[t * 128:(t + 1) * 128, :], bh, "h2b%d_ld" % (t % 2), reads=[b_H2])
        for k in range(8):
            S.op("pool", lambda e, hb_=hb_, t=t, k=k: e.indirect_dma_start(out=cx["XS"].ap(), out_offset=bass.IndirectOffsetOnAxis(ap=SL8[:, t, k:k + 1], axis=0),
                                                                          in_=hb_[:], in_offset=None),
                 reads=[bh, b_SL8], accs=[b_XS], dma_key="h2b%d_sc" % (t % 2))
    sst.close(); cx["stk"].pop()
    S.barrier()


_REGS = {}


def breg(e, nc, val):
    k = (id(nc), val)
    if k not in _REGS:
        _REGS[k] = e.to_reg(val)
    return _REGS[k]


def stage_E(cx):
    nc, S, sb = cx["nc"], cx["S"], cx["sb"]
    PSB, b_PSB = cx["PSB"], cx["b_PSB"]
    W = cx["W"]; NBLK = cx["NBLK"]
    IDXW = cx["IDXW"]
    sst = ExitStack(); cx["stk"].append(sst)
    RW = 4
    wall = [sb("wall%d" % i, [128, 6144], BF16) for i in range(RW)]; b_wall = [Buf("wall%d" % i) for i in range(RW)]
    NXB = 3
    xb = [sb("xb%d" % i, [128, D], BF16) for i in range(NXB)]; b_xb = [Buf("xb%d" % i) for i in range(NXB)]
    xT = [sb("xTe%d" % i, [128, 8, 128], BF16) for i in range(2)]; b_xT = [Buf("xTe%d" % i) for i in range(2)]
    sg = [sb("sge%d" % i, [128, 256], F32) for i in range(2)]; b_sg = [Buf("sge%d" % i) for i in range(2)]
    aT = [sb("aTe%d" % i, [128, 2, 128], BF16) for i in range(2)]; b_aT = [Buf("aTe%d" % i) for i in range(2)]
    yb = [sb("yb%d" % i, [128, D], BF16) for i in range(2)]; b_yb = [Buf("yb%d" % i) for i in range(2)]
    b_YS = cx["b_YS"] = Buf("YS")
    NROW_W = NE * 128 - 1

    def load_w(b):
        i = b % RW
        S.op("pool", lambda e, b=b, i=i: e.indirect_dma_start(out=wall[i][:, 0:4096], out_offset=None, in_=cx["WBGU"].ap(),
                                                             in_offset=bass.IndirectOffsetOnAxis(ap=IDXW[:, b:b + 1], axis=0),
                                                             bounds_check=breg(e, nc, NROW_W), oob_is_err=False),
             reads=[cx["b_IDXW"], cx["b_WB"]], writes=[b_wall[i]], dma_key="wall%d" % i)
        S.op("pool", lambda e, b=b, i=i: e.indirect_dma_start(out=wall[i][:, 4096:6144], out_offset=None, in_=cx["WBD"].ap(),
                                                             in_offset=bass.IndirectOffsetOnAxis(ap=IDXW[:, b:b + 1], axis=0),
                                                             bounds_check=breg(e, nc, NROW_W), oob_is_err=False),
             reads=[cx["b_IDXW"], cx["b_WB"]], accs=[b_wall[i]], dma_key="wall%d" % i)

    def load_x(b):
        cx["ld"]("sp", xb[b % NXB][:], cx["XS"].ap()[b * 128:(b + 1) * 128, :], b_xb[b % NXB], "xb%d" % (b % NXB), reads=[cx["b_XS"]])

    def do_T(b):
        i = b % 2
        x_ = xb[b % NXB]; bx = b_xb[b % NXB]
        pX = PSB[i][:, :].bitcast(BF16).rearrange("p (kc t) -> p kc t", kc=8)
        xv = x_[:, :].rearrange("p (q kc) -> p kc q", kc=8)
        for kc in range(8):
            S.op("pe", lambda e, kc=kc, pX=pX, xv=xv: e.transpose(out=pX[:, kc, :], in_=xv[:, kc, :], identity=cx["ident_b"][:]),
                 reads=[bx, cx["b_ident_b"]], writes=[b_PSB[i]] if kc == 0 else (), accs=[b_PSB[i]] if kc > 0 else ())
        S.op("act", lambda e, pX=pX, i=i: e.activation(out=xT[i][:, 0:4, :], in_=pX[:, 0:4, :], func=AF.Identity), reads=[b_PSB[i]], writes=[b_xT[i]])
        S.op("dve", lambda e, pX=pX, i=i: e.tensor_copy(xT[i][:, 4:8, :], pX[:, 4:8, :]), reads=[b_PSB[i]], accs=[b_xT[i]])

    def do_GU(b):
        i = b % 2
        bank = 2 + i
        first = True
        wr_ = b % RW
        wgu = wall[wr_][:, 0:4096].rearrange("p (w kc f) -> p w kc f", w=2, kc=8)
        for which in range(2):
            wt = wgu[:, which]
            bw = b_wall[wr_]
            for fc in range(2):
                for kc in range(8):
                    S.op("pe", lambda e, wt=wt, fc=fc, kc=kc, which=which, bank=bank, i=i: e.matmul(
                        PSB[bank][:, which * 256 + fc * 128: which * 256 + (fc + 1) * 128], lhsT=wt[:, kc, fc:256:2], rhs=xT[i][:, kc, :],
                        start=(kc == 0), stop=(kc == 7)),
                        reads=[bw, b_xT[i]], writes=[b_PSB[bank]] if first else (), accs=[b_PSB[bank]] if not first else ())
                    first = False
        S.op("act", lambda e, bank=bank, i=i: e.activation(out=sg[i][:], in_=PSB[bank][:, 0:256], func=AF.Silu), reads=[b_PSB[bank]], writes=[b_sg[i]])
        S.op("dve", lambda e, bank=bank, i=i: e.tensor_tensor(out=aT[i][:].rearrange("p c t -> p (c t)"), in0=PSB[bank][:, 256:512], in1=sg[i][:], op=ALU.mult),
             reads=[b_PSB[bank], b_sg[i]], writes=[b_aT[i]])

    def do_D(b):
        i = b % 2
        y_ = yb[i]; by = b_yb[i]
        for half in range(2):
            bank = 4 + 2 * i + half
            for fc in range(2):
                wdv_ = wall[b % RW][:, 4096:6144].rearrange("p (fc d) -> p fc d", fc=2)
                S.op("pe", lambda e, half=half, fc=fc, bank=bank, i=i, wdv_=wdv_: e.matmul(PSB[bank][:, :], lhsT=aT[i][:, fc, :], rhs=wdv_[:, fc, half * 512:(half + 1) * 512],
                                                                                        start=(fc == 0), stop=(fc == 1)),
                     reads=[b_aT[i], b_wall[b % RW]], writes=[b_PSB[bank]] if fc == 0 else (), accs=[b_PSB[bank]] if fc else ())
            if half == 0:
                S.op("act", lambda e, y_=y_, bank=bank: e.activation(out=y_[:, 0:512], in_=PSB[bank][:, :], func=AF.Identity),
                     reads=[b_PSB[bank]], writes=[by])
            else:
                S.op("dve", lambda e, y_=y_, bank=bank: e.tensor_copy(y_[:, 512:1024], PSB[bank][:, :]), reads=[b_PSB[bank]], accs=[by])
        S.op("act", lambda e, y_=y_, b=b: e.dma_start(out=cx["YS"].ap()[b * 128:(b + 1) * 128, :], in_=y_[:]),
             reads=[by], accs=[b_YS], dma_key="yb%d_st" % i)

    for b0 in range(min(RW, NBLK)):
        load_w(b0)
    load_x(0); load_x(1)
    do_T(0)
    for b in range(NBLK):
        if b + 2 < NBLK:
            load_x(b + 2)
        if b + 1 < NBLK:
            do_T(b + 1)
        do_GU(b)
        if b >= 1:
            do_D(b - 1)
            if b - 1 + RW < NBLK:
                load_w(b - 1 + RW)
    do_D(NBLK - 1)
    sst.close(); cx["stk"].pop()
    S.barrier()


def stage_C(cx):
    nc, S, sb, jobs, J = cx["nc"], cx["S"], cx["sb"], cx["jobs"], cx["J"]
    NT = cx["NT"]
    SL8, G8 = cx["SL8"], cx["G8"]
    sst = ExitStack(); cx["stk"].append(sst)
    gfrow = sb("gfrow", [128, D], F32); b_gfrow = Buf("gfrow")
    cx["ld"]("sp", gfrow[:], bass.AP(cx["W"]["g_final"], 0, [[0, 128], [1, D]]), b_gfrow, "gfrow")
    gt2 = sb("gt2c", [128, D], F32); b_gt2 = Buf("gt2c")
    x1 = [sb("x1c%d" % i, [128, D], F32) for i in range(2)]; b_x1 = [Buf("x1c%d" % i) for i in range(2)]
    yg = [sb("yg%d" % i, [128, D], BF16) for i in range(8)]; b_yg = [Buf("yg%d" % i) for i in range(8)]
    acc = sb("accc", [128, D], F32); b_acc = Buf("accc")
    sq = sb("sqc", [128, D], BF16); b_sq = Buf("sqc")
    stc = sb("stc", [128, 8], F32); b_stc = Buf("stc")
    outs = []
    t = 0
    for j in range(J):
        Sk, q0, Sq = jobs[j][:3]
        cx["ld"]("sp", gt2[:], bass.AP(cx["MODR"], (j * 4 + 3) * D, [[0, 128], [1, D]]), b_gt2, "gt2c")
        for tl in range(Sq // 128):
            x_ = x1[t % 2]; bx = b_x1[t % 2]
            cx["ld"]("sp", x_[:], cx["X1"].ap()[t * 128:(t + 1) * 128, :], bx, "x1c%d" % (t % 2), reads=[cx["b_X1"]])
            for k in range(8):
                S.op("pool", lambda e, t=t, k=k: e.indirect_dma_start(out=yg[k][:], out_offset=None, in_=cx["YS"].ap(),
                                                                     in_offset=bass.IndirectOffsetOnAxis(ap=SL8[:, t, k:k + 1], axis=0)),
                     reads=[cx["b_SL8"], cx["b_YS"]], writes=[b_yg[k]], dma_key="yg%d" % k)
            S.op("dve", lambda e, t=t: e.tensor_scalar(out=acc[:], in0=yg[0][:], scalar1=G8[:, t, 0:1], scalar2=None, op0=ALU.mult),
                 reads=[b_yg[0], cx["b_G8"]], writes=[b_acc])
            for k in range(1, 8):
                S.op("dve", lambda e, t=t, k=k: e.scalar_tensor_tensor(out=acc[:], in0=yg[k][:], scalar=G8[:, t, k:k + 1], in1=acc[:], op0=ALU.mult, op1=ALU.add),
                     reads=[b_yg[k], cx["b_G8"]], accs=[b_acc])
            S.op("dve", lambda e: e.tensor_tensor(out=acc[:], in0=acc[:], in1=gt2[:], op=ALU.mult), reads=[b_gt2], accs=[b_acc])
            S.op("dve", lambda e, x_=x_: e.tensor_tensor(out=x_[:], in0=x_[:], in1=acc[:], op=ALU.add), reads=[b_acc], accs=[bx])
            S.op("act", lambda e, x_=x_: e.activation(out=sq[:], in_=x_[:], func=AF.Square, accum_out=stc[:, 0:1]), reads=[bx], writes=[b_sq, b_stc])
            S.op("act", lambda e: e.activation(out=stc[:, 1:2], in_=stc[:, 0:1], func=AF.Sqrt, scale=1.0 / D, bias=cx["epsb"][:, 0:1]),
                 reads=[cx["b_epsb"]], accs=[b_stc])
            S.op("dve", lambda e: e.reciprocal(out=stc[:, 2:3], in_=stc[:, 1:2]), accs=[b_stc])
            S.op("dve", lambda e, x_=x_: e.scalar_tensor_tensor(out=x_[:], in0=x_[:], scalar=stc[:, 2:3], in1=gfrow[:], op0=ALU.mult, op1=ALU.mult),
                 reads=[b_stc, b_gfrow], accs=[bx])
            o = S.op("act", lambda e, x_=x_, t=t: e.dma_start(out=cx["y_out"].ap()[t * 128:(t + 1) * 128, :], in_=x_[:]),
                     reads=[bx], dma_key="x1c%d_st" % (t % 2))
            outs.append(o)
            t += 1
    sst.close(); cx["stk"].pop()
    return outs


def build_all(jobs, dbg=False):
    cx = build(jobs, dbg=dbg)
    stage_P(cx)
    stage_T(cx)
    stage_O(cx)
    stage_E(cx)
    outs = stage_C(cx)
    cx["S"].emit(outs)
    return cx


def make_in_map(jobs, xseqs, cvecs, poss, hf, weights, consts):
    J = len(jobs)
    im = {}
    for j in range(J):
        im["xs%d" % j] = np.ascontiguousarray(xseqs[j], dtype=np.float32)
        im["rope%d" % j] = rope_table(poss[j])
    cT = np.stack([np.asarray(cv, np.float32).reshape(8, 128).T for cv in cvecs], axis=-1)
    im["cT"] = np.ascontiguousarray(cT)
    im.update(weights)
    for n, v in consts.items():
        im["c_" + n] = v
    im["c_hfv"] = np.broadcast_to(np.array([[float(hf), 1.0 - float(hf)]], np.float32), (128, 2)).copy()
    return im


def kernel(**inputs):
    jobs = [(2048, 0, 2048, False), (2048, 0, 2048, False), (8192, 0, 4096, True)]
    cx = build_all(jobs)
    weights = {}
    for n in WEIGHT_NAMES:
        a = np.asarray(inputs[n], dtype=np.float32)
        if n not in ("rel_bias", "g_final"):
            a = a[0]
        weights[n] = np.ascontiguousarray(a)
    consts = host_constants()
    xp = np.asarray(inputs["x_prompt"], np.float32)
    xsm = np.asarray(inputs["x_sample"], np.float32)
    cp = np.asarray(inputs["c_prompt"], np.float32)
    cs = np.asarray(inputs["c_sample"], np.float32)
    in_maps = []
    for c in range(8):
        sbi, hf = c // 2, c % 2
        seq = xsm[sbi]
        pos2 = np.arange(8192)
        if hf:
            seq = np.concatenate([seq[4096:], seq[:4096]], axis=0)
            pos2 = np.concatenate([pos2[4096:], pos2[:4096]])
        in_maps.append(make_in_map(jobs, [xp[2 * c], xp[2 * c + 1], seq], [cp[2 * c], cp[2 * c + 1], cs[sbi]],
                                   [np.arange(2048), np.arange(2048), pos2], hf, weights, consts))
    res = run_bass_kernel_spmd(cx["nc"], in_maps, core_ids=list(range(8)))
    y_prompt = np.empty((16, 2048, D), np.float32)
    y_sample = np.empty((4, 8192, D), np.float32)
    for c in range(8):
        y = np.asarray(res.results[c]["y"], dtype=np.float32)
        y_prompt[2 * c] = y[0:2048]
        y_prompt[2 * c + 1] = y[2048:4096]
        y_sample[c // 2, (c % 2) * 4096:(c % 2 + 1) * 4096] = y[4096:8192]
    return (y_prompt, y_sample)
```

```python
import math
from contextlib import ExitStack
import numpy as np
import concourse.bass as bass
import concourse.mybir as mybir
from concourse.bass_utils import run_bass_kernel_spmd

F32 = mybir.dt.float32
BF16 = mybir.dt.bfloat16
I32 = mybir.dt.int32
AF = mybir.ActivationFunctionType
ALU = mybir.AluOpType
AX = mybir.AxisListType

D = 1024
DP = 2304
NE = 256
EPS = 1e-6
ENGS = ("pe", "act", "dve", "pool", "sp")


class Buf:
    __slots__ = ("name", "writers", "readers")

    def __init__(self, name):
        self.name = name
        self.writers = []
        self.readers = []


class Op:
    __slots__ = ("eng", "fn", "deps", "is_dma", "sem", "val", "signals")

    def __init__(self, eng, fn, is_dma):
        self.eng = eng
        self.fn = fn
        self.deps = []
        self.is_dma = is_dma
        self.sem = None
        self.val = 0
        self.signals = False


class Sched:
    def __init__(self, nc, stack):
        self.nc = nc
        self.stack = stack
        self.ops = {e: [] for e in ENGS}
        self.esem = {e: stack.enter_context(nc.semaphore("es_" + e)) for e in ENGS}
        self.dma_sems = {}
        self.pending = {e: [] for e in ENGS}
        self.last_dma = {}

    def barrier(self):
        deps = []
        for e in ENGS:
            for o in reversed(self.ops[e]):
                if not o.is_dma:
                    deps.append(o)
                    break
        deps.extend(self.last_dma.values())
        for e in ENGS:
            self.pending[e] = list(deps)

    def _dma_sem(self, key):
        if key not in self.dma_sems:
            s = self.stack.enter_context(self.nc.semaphore("ds%d" % len(self.dma_sems)))
            self.dma_sems[key] = [s, 0]
        return self.dma_sems[key]

    def op(self, eng, fn, reads=(), writes=(), accs=(), dma_key=None):
        is_dma = dma_key is not None
        o = Op(eng, fn, is_dma)
        deps = []
        for b in reads:
            deps.extend(b.writers)
        for b in writes:
            deps.extend(b.writers)
            deps.extend(b.readers)
        for b in accs:
            deps.extend(b.readers)
            for w in b.writers:
                if w.is_dma and is_dma:
                    continue
                deps.append(w)
        if self.pending[eng]:
            deps.extend(self.pending[eng])
            self.pending[eng] = []
        seen = set()
        for d in deps:
            if id(d) in seen or d is o:
                continue
            seen.add(id(d))
            if (not d.is_dma) and (not is_dma) and d.eng == "pe" and eng == "pe":
                continue
            o.deps.append(d)
            d.signals = True
        for b in reads:
            b.readers.append(o)
        for b in writes:
            b.writers = [o]
            b.readers = []
        for b in accs:
            b.writers.append(o)
            b.readers = []
        if is_dma:
            s = self._dma_sem(dma_key)
            s[1] += 16
            o.sem = s[0]
            o.val = s[1]
            o.signals = True
            self.last_dma[dma_key] = o
        self.ops[eng].append(o)
        return o

    def emit(self, final_ops):
        nc = self.nc
        for e in ENGS:
            c = 0
            for o in self.ops[e]:
                if not o.is_dma and o.signals:
                    c += 1
                    o.sem = self.esem[e]
                    o.val = c
        sched = self

        def run(engname, eh):
            waited = {}
            for o in sched.ops[engname]:
                for d in o.deps:
                    k = id(d.sem)
                    if waited.get(k, 0) >= d.val:
                        continue
                    eh.wait_ge(d.sem, d.val)
                    waited[k] = d.val
                ins = o.fn(eh)
                if o.is_dma:
                    ins.then_inc(o.sem, 16)
                elif o.signals:
                    ins.then_inc(o.sem, 1)
            if engname == "sp":
                for d in final_ops:
                    if waited.get(id(d.sem), 0) < d.val:
                        eh.wait_ge(d.sem, d.val)
                        waited[id(d.sem)] = d.val

        allsems = list(self.esem.values()) + [v[0] for v in self.dma_sems.values()]
        with nc.Block() as blk0:
            @blk0.sync
            def _(e):
                for s_ in allsems:
                    e.sem_clear(s_)

        with nc.Block() as block:
            @block.tensor
            def _(e):
                run("pe", e)

            @block.scalar
            def _(e):
                run("act", e)

            @block.vector
            def _(e):
                run("dve", e)

            @block.gpsimd
            def _(e):
                run("pool", e)

            @block.sync
            def _(e):
                run("sp", e)


def t5_bucket_np(rel):
    nb = 16
    max_exact = 8
    ret = np.where(rel > 0, nb, 0)
    n = np.abs(rel)
    nf = np.maximum(n, 1).astype(np.float32)
    large = max_exact + (np.log(nf / np.float32(max_exact)) / np.float32(math.log(128 / max_exact))
                         * np.float32(nb - max_exact)).astype(np.int32)
    large = np.minimum(large, nb - 1)
    return ret + np.where(n < max_exact, n, large)


def host_constants():
    c = {}
    c["ident"] = np.eye(128, dtype=np.float32)
    c["jrev"] = np.eye(128, dtype=np.float32)[::-1].copy()
    tri = np.zeros((128, 128), np.float32)
    for a in range(128):
        tri[a, a + 1:] = 1.0
    c["tri"] = tri
    tril = np.zeros((128, 128), np.float32)
    for a in range(128):
        tril[a, a:] = 1.0
    c["tril"] = tril
    i = np.arange(1280)
    bk = t5_bucket_np((639 - i).astype(np.int32))
    oh = np.zeros((32, 1280), np.float32)
    oh[bk, i] = 1.0
    c["ohr"] = oh
    c["iota_e"] = np.broadcast_to(np.arange(256, dtype=np.float32)[None, :], (128, 256)).copy()
    c["iota_b"] = np.broadcast_to(np.arange(1024, dtype=np.float32)[None, :], (128, 1024)).copy()
    c["iota_p"] = np.arange(128, dtype=np.float32).reshape(128, 1).copy()
    return c


def rope_table(pos):
    t = np.asarray(pos)
    c = {}
    half = 32
    inv = (10000.0 ** (-np.arange(0, half, 2, dtype=np.float32) / half)).astype(np.float32)
    row = (t // 64).astype(np.float32)
    col = (t % 64).astype(np.float32)
    ar = row[:, None] * inv[None, :]
    ac = col[:, None] * inv[None, :]
    return np.stack([np.cos(ar), np.sin(ar), np.cos(ac), np.sin(ac)], axis=1).astype(np.float32)


WEIGHT_NAMES = ["rel_bias", "w_ada", "b_ada", "g_norm1", "w_in", "lambda_q1", "lambda_k1", "lambda_q2",
                "lambda_k2", "g_subln", "g_qnorm", "g_knorm", "w_out", "g_norm2", "w_router",
                "router_bias", "w_exp_gate", "w_exp_up", "w_exp_down", "w_sh_gate", "w_sh_up",
                "w_sh_down", "g_final"]


def build(jobs, dbg=False):
    J = len(jobs)
    T = sum(j_[2] for j_ in jobs)
    NT = T // 128
    NBLK = T * 8 // 128 + NE
    SMAX = max(j[0] for j in jobs)
    nc = bass.Bass("TRN2", target_bir_lowering=False)
    st = ExitStack()
    S = Sched(nc, st)

    def din(name, shape, dt=F32):
        return nc.dram_tensor(name, list(shape), dt, kind="ExternalInput")

    def dscr(name, shape, dt):
        return nc.dram_tensor(name, list(shape), dt, kind="ExternalOutput" if dbg else "Internal")

    xs = [din("xs%d" % j, [jobs[j][0], D]) for j in range(J)]
    cT_d = din("cT", [128, 8, J])
    W = {}
    wshapes = dict(rel_bias=[32, 4], w_ada=[D, 6 * D], b_ada=[6 * D], g_norm1=[D], w_in=[D, DP],
                   lambda_q1=[64], lambda_k1=[64], lambda_q2=[64], lambda_k2=[64], g_subln=[128],
                   g_qnorm=[64], g_knorm=[64], w_out=[D, D], g_norm2=[D], w_router=[D, NE],
                   router_bias=[NE], w_exp_gate=[NE, D, 256], w_exp_up=[NE, D, 256],
                   w_exp_down=[NE, 256, D], w_sh_gate=[D, 256], w_sh_up=[D, 256], w_sh_down=[256, D],
                   g_final=[D])
    for n in WEIGHT_NAMES:
        W[n] = din(n, wshapes[n])
    cshapes = dict(ident=[128, 128], jrev=[128, 128], tri=[128, 128], tril=[128, 128], ohr=[32, 1280],
                   iota_e=[128, 256], iota_b=[128, 1024], iota_p=[128, 1], hfv=[128, 2])
    C = {n: din("c_" + n, s) for n, s in cshapes.items()}
    ROPE = [din("rope%d" % j, [jobs[j][0], 4, 16]) for j in range(J)]
    y_out = nc.dram_tensor("y", [T, D], F32, kind="ExternalOutput")

    QA = [dscr("QA%d" % j, [4, 128, jobs[j][2]], BF16) for j in range(J)]
    KA = [dscr("KA%d" % j, [4, 128, jobs[j][0]], BF16) for j in range(J)]
    VA = [dscr("VA%d" % j, [jobs[j][0], 512], BF16) for j in range(J)]
    QB = [dscr("QB%d" % j, [4, 128, jobs[j][2]], BF16) for j in range(J)]
    KB = [dscr("KB%d" % j, [2, 128, jobs[j][0]], BF16) for j in range(J)]
    VB = [dscr("VB%d" % j, [jobs[j][0], 2, 65], BF16) for j in range(J)]
    OT = dscr("OT", [8, 128, T], BF16)
    X1 = dscr("X1", [T, D], F32)
    H2 = dscr("H2", [T, D], BF16)
    XS = dscr("XSLOT", [NBLK * 128, D], BF16)
    YS = dscr("YSLOT", [NBLK * 128, D], BF16)
    GRD = dscr("GRD", [4, 1280], F32)
    WBGU = nc.dram_tensor("WBGU", [NE * 128, 4096], BF16, kind="Internal")
    WBD = nc.dram_tensor("WBD", [NE * 128, 2048], BF16, kind="Internal")

    sb_bytes = [0]

    stk = [st]

    def sb(name, shape, dt):
        return stk[-1].enter_context(nc.sbuf_tensor(name, list(shape), dt))

    def ps(name, shape, dt=F32):
        return st.enter_context(nc.psum_tensor(name, list(shape), dt))

    MODR = dscr("MODR", [J, 4, D], F32)
    b_MODR = Buf("MODR")
    ident_f = sb("ident_f", [128, 128], F32); b_ident_f = Buf("ident_f")
    ident_b = sb("ident_b", [128, 128], BF16); b_ident_b = Buf("ident_b")
    ones_b = sb("ones_b", [128, 128], BF16); b_ones_b = Buf("ones_b")
    ones_f = sb("ones_f", [128, 128], F32); b_ones_f = Buf("ones_f")
    tri_b = sb("tri_b", [128, 128], BF16); b_tri = Buf("tri_b")
    tril_f = sb("tril_f", [128, 128], F32); b_tril = Buf("tril_f")
    iota_e = sb("iota_e", [128, 256], F32); b_iota_e = Buf("iota_e")
    iota_b = sb("iota_b", [128, 1024], F32); b_iota_b = Buf("iota_b")
    iota_p = sb("iota_p", [128, 1], F32); b_iota_p = Buf("iota_p")
    epsb = sb("epsb", [128, 1], F32); b_epsb = Buf("epsb")
    G1T = sb("G1T", [128, 8, J], F32); b_G1T = Buf("G1T")
    SH1T = sb("SH1T", [128, 8, J], F32); b_SH1T = Buf("SH1T")
    lsm = sb("lsm", [128, 8], F32); b_lsm = Buf("lsm")
    gsub = sb("gsub", [128, 1], F32); b_gsub = Buf("gsub")
    gq = sb("gq", [128, 64], F32); b_gq = Buf("gq")
    gk = sb("gk", [128, 64], F32); b_gk = Buf("gk")
    bcst = sb("bcst", [128, 2, 4], F32); b_bcst = Buf("bcst")

    PSB = [ps("psb%d" % i, [128, 512], F32) for i in range(8)]
    b_PSB = [Buf("psb%d" % i) for i in range(8)]

    def ld(eng, out_ap, in_ap, wbuf, key, reads=(), acc=False, slow=False):
        fn = (lambda e: e.dma_start(out=out_ap, in_=in_ap, allow_slow_non_contiguous=True)) if slow else \
             (lambda e: e.dma_start(out=out_ap, in_=in_ap))
        if acc:
            return S.op(eng, fn, reads=reads, accs=[wbuf], dma_key=key)
        return S.op(eng, fn, reads=reads, writes=[wbuf], dma_key=key)

    ld("sp", ident_f[:], C["ident"].ap(), b_ident_f, "ident_f")
    ld("sp", tril_f[:], C["tril"].ap(), b_tril, "tril_f")
    ld("sp", iota_e[:], C["iota_e"].ap(), b_iota_e, "iota_e")
    ld("sp", iota_b[:], C["iota_b"].ap(), b_iota_b, "iota_b")
    ld("sp", iota_p[:], C["iota_p"].ap(), b_iota_p, "iota_p")
    ld("pool", ident_b[:], C["ident"].ap(), b_ident_b, "ident_b")
    ld("pool", tri_b[:], C["tri"].ap(), b_tri, "tri_b")
    S.op("dve", lambda e: e.memset(ones_b[:], 1.0), writes=[b_ones_b])
    S.op("dve", lambda e: e.memset(ones_f[:], 1.0), writes=[b_ones_f])
    S.op("dve", lambda e: e.memset(epsb[:], EPS), writes=[b_epsb])

    pst = ExitStack()
    stk.append(pst)
    big = sb("big", [128, 4096], F32); b_big = Buf("big")
    cT = sb("cT_sb", [128, 8, J], F32); b_cT = Buf("cT")
    scT = sb("scT", [128, 8, J], F32); b_scT = Buf("scT")
    ld("sp", cT[:], cT_d.ap(), b_cT, "cT")
    S.op("act", lambda e: e.activation(out=scT[:], in_=cT[:], func=AF.Silu), reads=[b_cT], writes=[b_scT])
    badaT = sb("badaT", [128, 16], F32); b_badaT = Buf("badaT")
    g1T = sb("g1T", [128, 8], F32); b_g1T = Buf("g1T")
    ld("sp", badaT[:], W["b_ada"].ap()[0:2048].rearrange("(c p) -> p c", p=128), b_badaT, "badaT", slow=True)
    ld("sp", g1T[:], W["g_norm1"].ap().rearrange("(c p) -> p c", p=128), b_g1T, "g1T", slow=True)
    scbc = sb("scbc", [128, 8, 128], F32); b_scbc = Buf("scbc")
    wada_v = W["w_ada"].ap().rearrange("(kc p) n -> p kc n", p=128)
    wst = big[:, 0:4096].rearrange("p (kc n) -> p kc n", kc=8)
    for cc in range(4):
        ld("sp", wst, wada_v[:, :, cc * 512:(cc + 1) * 512], b_big, "big_ld")
        for sub in range(4):
            col = cc * 4 + sub
            pb = b_PSB[col % 2]
            pt = PSB[col % 2]
            for kc in range(8):
                S.op("pe", lambda e, kc=kc, sub=sub, pt=pt: e.matmul(pt[:, 0:J], lhsT=wst[:, kc, sub * 128:(sub + 1) * 128],
                                                                   rhs=scT[:, kc, :], start=(kc == 0), stop=(kc == 7)),
                     reads=[b_big, b_scT], writes=[pb] if kc == 0 else (), accs=[pb] if kc > 0 else ())
            if col < 8:
                S.op("dve", lambda e, col=col, pt=pt: e.tensor_scalar(out=SH1T[:, col, :], in0=pt[:, 0:J], scalar1=badaT[:, col:col + 1],
                                                                    scalar2=None, op0=ALU.add),
                     reads=[pb, b_badaT], accs=[b_SH1T])
            else:
                c8 = col - 8
                S.op("dve", lambda e, col=col, c8=c8, pt=pt: e.tensor_scalar(out=G1T[:, c8, :], in0=pt[:, 0:J], scalar1=badaT[:, col:col + 1],
                                                                           scalar2=1.0, op0=ALU.add, op1=ALU.add),
                     reads=[pb, b_badaT], accs=[b_G1T])
                S.op("dve", lambda e, c8=c8: e.tensor_scalar(out=G1T[:, c8, :], in0=G1T[:, c8, :], scalar1=g1T[:, c8:c8 + 1],
                                                             scalar2=None, op0=ALU.mult),
                     reads=[b_g1T], accs=[b_G1T])
    brow = sb("brow", [128, D], F32); b_brow = Buf("brow")
    g2row = sb("g2row", [128, D], F32); b_g2row = Buf("g2row")
    mtmp = sb("mtmp", [128, D], F32); b_mtmp = Buf("mtmp")
    ld("sp", g2row[:], bass.AP(W["g_norm2"], 0, [[0, 128], [1, D]]), b_g2row, "g2row")
    vmap = {2: 0, 3: 2, 4: 1, 5: 3}
    for v6 in (2, 3, 4, 5):
        ld("sp", brow[:], bass.AP(W["b_ada"], v6 * D, [[0, 128], [1, D]]), b_brow, "brow")
        for j in range(J):
            for hc in range(2):
                ld("sp", wst, wada_v[:, :, v6 * D + hc * 512: v6 * D + (hc + 1) * 512], b_big, "big_ld")
                S.op("dve", lambda e, j=j: e.tensor_copy(scbc[:], scT[:, :, j:j + 1].to_broadcast([128, 8, 128])),
                     reads=[b_scT], writes=[b_scbc])
                pb = b_PSB[2 + hc]
                pt = PSB[2 + hc]
                for kc in range(8):
                    S.op("pe", lambda e, kc=kc, pt=pt: e.matmul(pt[:, :], lhsT=scbc[:, kc, :], rhs=wst[:, kc, :],
                                                              start=(kc == 0), stop=(kc == 7)),
                         reads=[b_big, b_scbc], writes=[pb] if kc == 0 else (), accs=[pb] if kc > 0 else ())
                sl = slice(hc * 512, (hc + 1) * 512)
                S.op("dve", lambda e, pt=pt, sl=sl: e.tensor_tensor(out=mtmp[:, sl], in0=pt[:, :], in1=brow[:, sl], op=ALU.add),
                     reads=[pb, b_brow], writes=[b_mtmp] if hc == 0 else (), accs=[b_mtmp] if hc == 1 else ())
            if v6 == 4:
                S.op("dve", lambda e: e.scalar_tensor_tensor(out=mtmp[:], in0=mtmp[:], scalar=1.0, in1=g2row[:],
                                                             op0=ALU.add, op1=ALU.mult),
                     reads=[b_g2row], accs=[b_mtmp])
            S.op("sp", lambda e, j=j, v6=v6: e.dma_start(out=MODR.ap()[j, vmap[v6]:vmap[v6] + 1, :], in_=mtmp[0:1, :]),
                 reads=[b_mtmp], accs=[b_MODR], dma_key="mtmp_st")
    lamv = sb("lamv", [128, 4, 64], F32); b_lamv = Buf("lamv")
    for i, n in enumerate(["lambda_q1", "lambda_k1", "lambda_q2", "lambda_k2"]):
        ld("sp", lamv[:, i, :], bass.AP(W[n], 0, [[0, 128], [1, 64]]), b_lamv, "lamv", acc=(i > 0))
    ljunk = sb("ljunk", [128, 64], F32); b_ljunk = Buf("ljunk")
    for i in range(2):
        S.op("dve", lambda e, i=i: e.tensor_tensor(out=ljunk[:], in0=lamv[:, 2 * i, :], in1=lamv[:, 2 * i + 1, :], op=ALU.mult),
             reads=[b_lamv], writes=[b_ljunk])
        S.op("dve", lambda e, i=i: e.tensor_reduce(out=lsm[:, i:i + 1], in_=ljunk[:], axis=AX.X, op=ALU.add),
             reads=[b_ljunk], accs=[b_lsm])
    S.op("act", lambda e: e.activation(out=lsm[:, 2:4], in_=lsm[:, 0:2], func=AF.Exp), reads=[b_lsm], accs=[b_lsm])
    S.op("dve", lambda e: e.tensor_tensor(out=lsm[:, 4:5], in0=lsm[:, 3:4], in1=lsm[:, 2:3], op=ALU.subtract),
         reads=[b_lsm], accs=[b_lsm])
    S.op("dve", lambda e: e.tensor_scalar(out=lsm[:, 5:6], in0=lsm[:, 4:5], scalar1=-0.2, scalar2=None, op0=ALU.add),
         reads=[b_lsm], accs=[b_lsm])
    NLAM = lsm[:, 5:6]
    ld("sp", gsub[:], W["g_subln"].ap().rearrange("(p o) -> p o", o=1), b_gsub, "gsub")
    S.op("dve", lambda e: e.tensor_scalar(out=gsub[:], in0=gsub[:], scalar1=0.8, scalar2=None, op0=ALU.mult),
         reads=[], writes=[b_gsub])
    ld("sp", gq[:], bass.AP(W["g_qnorm"], 0, [[0, 128], [1, 64]]), b_gq, "gq")
    ld("sp", gk[:], bass.AP(W["g_knorm"], 0, [[0, 128], [1, 64]]), b_gk, "gk")
    ld("sp", bcst[:, 0, :], bass.AP(W["rel_bias"], 15 * 4, [[0, 128], [1, 4]]), b_bcst, "bcst")
    ld("sp", bcst[:, 1, :], bass.AP(W["rel_bias"], 31 * 4, [[0, 128], [1, 4]]), b_bcst, "bcst", acc=True)
    rb = sb("rb", [32, 4], F32); b_rb = Buf("rb")
    ohr = sb("ohr", [32, 1280], F32); b_ohr = Buf("ohr")
    ld("sp", rb[:], W["rel_bias"].ap(), b_rb, "rb")
    ld("sp", ohr[:], C["ohr"].ap(), b_ohr, "ohr")
    grs = sb("grs", [4, 1280], F32); b_grs = Buf("grs")
    for i, (a_, n_) in enumerate([(0, 512), (512, 512), (1024, 256)]):
        S.op("pe", lambda e, a_=a_, n_=n_, i=i: e.matmul(PSB[4 + i][0:4, 0:n_], lhsT=rb[:, :], rhs=ohr[:, a_:a_ + n_], start=True, stop=True),
             reads=[b_rb, b_ohr], writes=[b_PSB[4 + i]])
        S.op("dve", lambda e, a_=a_, n_=n_, i=i: e.tensor_copy(grs[:, a_:a_ + n_], PSB[4 + i][0:4, 0:n_]),
             reads=[b_PSB[4 + i]], accs=[b_grs])
    b_GRD = Buf("GRD")
    S.op("sp", lambda e: e.dma_start(out=GRD.ap(), in_=grs[:]), reads=[b_grs], writes=[b_GRD], dma_key="grs_st")
    pst.close()
    stk.pop()
    S.barrier()

    ctx = dict(WBGU=WBGU, WBD=WBD, ROPE=ROPE, nc=nc, S=S, st=st, sb=sb, stk=stk, jobs=jobs, J=J, T=T, NT=NT, NBLK=NBLK, xs=xs, W=W, C=C, y_out=y_out,
               QA=QA, KA=KA, VA=VA, QB=QB, KB=KB, VB=VB, OT=OT, X1=X1, H2=H2, XS=XS, YS=YS, GRD=GRD, MODR=MODR,
               PSB=PSB, b_PSB=b_PSB, ident_f=ident_f, b_ident_f=b_ident_f, ident_b=ident_b, b_ident_b=b_ident_b,
               ones_b=ones_b, b_ones_b=b_ones_b, ones_f=ones_f, b_ones_f=b_ones_f, tri_b=tri_b, b_tri=b_tri,
               tril_f=tril_f, b_tril=b_tril, iota_e=iota_e, b_iota_e=b_iota_e, iota_b=iota_b, b_iota_b=b_iota_b,
               iota_p=iota_p, b_iota_p=b_iota_p, epsb=epsb, b_epsb=b_epsb,
               G1T=G1T, b_G1T=b_G1T, SH1T=SH1T, b_SH1T=b_SH1T,
               NLAM=NLAM, b_lsm=b_lsm, gsub=gsub, b_gsub=b_gsub, gq=gq, b_gq=b_gq, gk=gk, b_gk=b_gk,
               bcst=bcst, b_bcst=b_bcst, ld=ld, dbg=dbg)
    return ctx


def stage_P(cx):
    nc, S, sb, jobs, J = cx["nc"], cx["S"], cx["sb"], cx["jobs"], cx["J"]
    PSB, b_PSB = cx["PSB"], cx["b_PSB"]
    sst = ExitStack(); cx["stk"].append(sst)
    win = sb("win", [128, 8, DP], BF16); b_win = Buf("win")
    for half in range(2):
        cx["ld"]("pool", win[:, :, half * 1152:(half + 1) * 1152],
                 cx["W"]["w_in"].ap().rearrange("(kc p) n -> p kc n", p=128)[:, :, half * 1152:(half + 1) * 1152],
                 b_win, "win", acc=(half == 1))
    xt = [sb("xt%d" % i, [128, D], F32) for i in range(2)]; b_xt = [Buf("xt%d" % i) for i in range(2)]
    sq = sb("sqj", [128, D], BF16); b_sq = Buf("sqj")
    st1 = sb("st1", [128, 8], F32); b_st1 = Buf("st1")
    xn = sb("xn", [128, D], BF16); b_xn = Buf("xn")
    hT = sb("hT", [128, 8, 512], BF16); b_hT = Buf("hT")
    fmst = sb("fmst", [128, 8, 512], BF16); b_fmst = Buf("fmst")
    vast = sb("vast", [128, 4, 512], BF16); b_vast = Buf("vast")
    qbf = sb("qbf", [128, 512], F32); b_qbf = Buf("qbf")
    kvf = sb("kvf", [128, 256], F32); b_kvf = Buf("kvf")
    rt = sb("ropet", [128, 4, 16], F32); b_rt = Buf("ropet")
    tq = [sb("tq%d" % i, [128, 512], F32) for i in range(3)]; b_tq = [Buf("tq%d" % i) for i in range(3)]
    nst = sb("nst", [128, 16], F32); b_nst = Buf("nst")
    qbb = sb("qbb", [128, 512], BF16); b_qbb = Buf("qbb")
    kbb = sb("kbb", [128, 2, 2, 64], BF16); b_kbb = Buf("kbb")
    qbst = sb("qbst", [128, 4, 512], BF16); b_qbst = Buf("qbst")
    kbst = sb("kbst", [128, 2, 512], BF16); b_kbst = Buf("kbst")
    vbst = sb("vbst", [128, 4, 2, 65], BF16); b_vbst = Buf("vbst")
    S.op("dve", lambda e: e.memset(vbst[:], 1.0), writes=[b_vbst])
    b_D = cx["b_D"] = {}
    gq, gk = cx["gq"], cx["gk"]
    for j in range(J):
        Sk, q0, Sq = jobs[j][:3]
        for n in ("QA", "KA", "VA", "QB", "KB", "VB"):
            b_D[(n, j)] = Buf("%s%d" % (n, j))
        G1 = cx["G1T"][:, :, j:j + 1]
        SH1 = cx["SH1T"][:, :, j:j + 1]
        for ch in range(Sk // 512):
            t0 = ch * 512
            own = (t0 >= q0) and (t0 < q0 + Sq)
            for ti in range(4):
                r0 = t0 + ti * 128
                x_ = xt[ti % 2]; bx = b_xt[ti % 2]
                cx["ld"]("sp", x_[:], cx["xs"][j].ap()[r0:r0 + 128, :], bx, "xt%d" % (ti % 2))
                S.op("act", lambda e, x_=x_: e.activation(out=sq[:], in_=x_[:], func=AF.Square, accum_out=st1[:, 0:1]),
                     reads=[bx], writes=[b_sq, b_st1])
                S.op("act", lambda e: e.activation(out=st1[:, 1:2], in_=st1[:, 0:1], func=AF.Sqrt, scale=1.0 / D, bias=cx["epsb"][:, 0:1]),
                     reads=[cx["b_epsb"]], accs=[b_st1])
                S.op("dve", lambda e: e.reciprocal(out=st1[:, 2:3], in_=st1[:, 1:2]), accs=[b_st1])
                S.op("dve", lambda e, x_=x_: e.tensor_scalar(out=xn[:], in0=x_[:], scalar1=st1[:, 2:3], scalar2=None, op0=ALU.mult),
                     reads=[bx, b_st1], writes=[b_xn])
                pT = PSB[0][:, :].bitcast(BF16).rearrange("p (kc t) -> p kc t", kc=8)
                for kc in range(8):
                    S.op("pe", lambda e, kc=kc, pT=pT: e.transpose(out=pT[:, kc, :], in_=xn[:, kc * 128:(kc + 1) * 128], identity=cx["ident_b"][:]),
                         reads=[b_xn, cx["b_ident_b"]], writes=[b_PSB[0]] if kc == 0 else (), accs=[b_PSB[0]] if kc > 0 else ())
                hs = hT[:, :, ti * 128:(ti + 1) * 128]
                S.op("dve", lambda e, hs=hs, pT=pT, G1=G1: e.tensor_tensor(out=hs, in0=pT, in1=G1.to_broadcast([128, 8, 128]), op=ALU.mult),
                     reads=[b_PSB[0], cx["b_G1T"]], writes=[b_hT] if ti == 0 else (), accs=[b_hT] if ti > 0 else ())
                S.op("dve", lambda e, hs=hs, SH1=SH1: e.tensor_tensor(out=hs, in0=hs, in1=SH1.to_broadcast([128, 8, 128]), op=ALU.add),
                     reads=[cx["b_SH1T"]], accs=[b_hT])
            chunks = ([(h, h) for h in range(4)] if own else []) + [(4 + h, 4 + h) for h in range(4)]
            for i, (slot, wc) in enumerate(chunks):
                pb = b_PSB[1 + (i % 2)]; pt = PSB[1 + (i % 2)]
                for kc in range(8):
                    S.op("pe", lambda e, kc=kc, wc=wc, pt=pt: e.matmul(pt[:, :], lhsT=win[:, kc, wc * 128:(wc + 1) * 128], rhs=hT[:, kc, :],
                                                                     start=(kc == 0), stop=(kc == 7)),
                         reads=[b_win, b_hT], writes=[pb] if kc == 0 else (), accs=[pb] if kc > 0 else ())
                S.op("act", lambda e, slot=slot, pt=pt: e.activation(out=fmst[:, slot, :], in_=pt[:, :], func=AF.Identity),
                     reads=[pb], writes=[b_fmst] if i == 0 else (), accs=[b_fmst] if i > 0 else ())
            if own:
                S.op("act", lambda e, j=j, t0=t0, q0=q0: e.dma_start(out=cx["QA"][j].ap()[:, :, t0 - q0:t0 - q0 + 512].rearrange("h p t -> p h t"),
                                                                  in_=fmst[:, 0:4, :]),
                     reads=[b_fmst], accs=[b_D[("QA", j)]], dma_key="fmst_st")
            S.op("act", lambda e, j=j, t0=t0: e.dma_start(out=cx["KA"][j].ap()[:, :, t0:t0 + 512].rearrange("h p t -> p h t"), in_=fmst[:, 4:8, :]),
                 reads=[b_fmst], accs=[b_D[("KA", j)]], dma_key="fmst_st")
            for ti in range(4):
                r0 = t0 + ti * 128
                for kc in range(8):
                    S.op("pe", lambda e, kc=kc, ti=ti: e.matmul(PSB[3][:, :], lhsT=hT[:, kc, ti * 128:(ti + 1) * 128], rhs=win[:, kc, 1024:1536],
                                                               start=(kc == 0), stop=(kc == 7)),
                         reads=[b_win, b_hT], writes=[b_PSB[3]] if kc == 0 else (), accs=[b_PSB[3]] if kc > 0 else ())
                S.op("act", lambda e, ti=ti: e.activation(out=vast[:, ti, :], in_=PSB[3][:, :], func=AF.Identity),
                     reads=[b_PSB[3]], writes=[b_vast] if ti == 0 else (), accs=[b_vast] if ti > 0 else ())
                for kc in range(8):
                    S.op("pe", lambda e, kc=kc, ti=ti: e.matmul(PSB[4][:, 0:256], lhsT=hT[:, kc, ti * 128:(ti + 1) * 128], rhs=win[:, kc, 2048:2304],
                                                               start=(kc == 0), stop=(kc == 7)),
                         reads=[b_win, b_hT], writes=[b_PSB[4]] if kc == 0 else (), accs=[b_PSB[4]] if kc > 0 else ())
                S.op("act", lambda e: e.activation(out=kvf[:], in_=PSB[4][:, 0:256], func=AF.Identity), reads=[b_PSB[4]], writes=[b_kvf])
                S.op("dve", lambda e, ti=ti: e.tensor_copy(vbst[:, ti, :, 0:64], kvf[:, 128:256].rearrange("p (h d) -> p h d", h=2)),
                     reads=[b_kvf], accs=[b_vbst])
                cx["ld"]("sp", rt[:], cx["ROPE"][j].ap()[r0:r0 + 128, :, :], b_rt, "ropet")

                def norm_rope(src, H, gtile, b_g, dst_ap, b_dst, b_src):
                    HW = H * 64
                    s3 = src[:, 0:HW].rearrange("p (h d) -> p h d", h=H)
                    a3 = tq[0][:, 0:HW].rearrange("p (h d) -> p h d", h=H)
                    S.op("dve", lambda e: e.tensor_tensor(out=tq[0][:, 0:HW], in0=src[:, 0:HW], in1=src[:, 0:HW], op=ALU.mult),
                         reads=[b_src], writes=[b_tq[0]])
                    S.op("dve", lambda e: e.tensor_reduce(out=nst[:, 0:H], in_=a3, axis=AX.X, op=ALU.add),
                         reads=[b_tq[0]], writes=[b_nst])
                    S.op("act", lambda e: e.activation(out=nst[:, 8:8 + H], in_=nst[:, 0:H], func=AF.Sqrt, scale=1.0 / 64, bias=cx["epsb"][:, 0:1]),
                         reads=[cx["b_epsb"]], accs=[b_nst])
                    S.op("dve", lambda e: e.reciprocal(out=nst[:, 0:H], in_=nst[:, 8:8 + H]), accs=[b_nst])
                    S.op("dve", lambda e: e.tensor_tensor(out=a3, in0=s3, in1=nst[:, 0:H].unsqueeze(2).to_broadcast([128, H, 64]), op=ALU.mult),
                         reads=[b_src, b_nst], writes=[b_tq[0]])
                    S.op("dve", lambda e: e.tensor_tensor(out=a3, in0=a3, in1=gtile[:, :].unsqueeze(1).to_broadcast([128, H, 64]), op=ALU.mult),
                         reads=[b_g], accs=[b_tq[0]])
                    xv = tq[0][:, 0:HW].rearrange("p (h f two d) -> p h f two d", h=H, f=2, two=2)
                    t1 = tq[1][:, 0:HW // 2].rearrange("p (h f d) -> p h f d", h=H, f=2)
                    t2 = tq[2][:, 0:HW // 2].rearrange("p (h f d) -> p h f d", h=H, f=2)
                    cosb = rt[:, 0:4:2, :].unsqueeze(1).to_broadcast([128, H, 2, 16])
                    sinb = rt[:, 1:4:2, :].unsqueeze(1).to_broadcast([128, H, 2, 16])
                    dv = dst_ap.rearrange("p h (f two d) -> p h f two d", f=2, two=2)
                    x1 = xv[:, :, :, 0, :]; x2 = xv[:, :, :, 1, :]
                    S.op("dve", lambda e: e.tensor_tensor(out=t1, in0=x1, in1=cosb, op=ALU.mult), reads=[b_tq[0], b_rt], writes=[b_tq[1]])
                    S.op("dve", lambda e: e.tensor_tensor(out=t2, in0=x2, in1=sinb, op=ALU.mult), reads=[b_tq[0], b_rt], writes=[b_tq[2]])
                    S.op("dve", lambda e: e.tensor_tensor(out=dv[:, :, :, 0, :], in0=t1, in1=t2, op=ALU.subtract),
                         reads=[b_tq[1], b_tq[2]], accs=[b_dst])
                    S.op("dve", lambda e: e.tensor_tensor(out=t1, in0=x1, in1=sinb, op=ALU.mult), reads=[b_tq[0], b_rt], writes=[b_tq[1]])
                    S.op("dve", lambda e: e.tensor_tensor(out=t2, in0=x2, in1=cosb, op=ALU.mult), reads=[b_tq[0], b_rt], writes=[b_tq[2]])
                    S.op("dve", lambda e: e.tensor_tensor(out=dv[:, :, :, 1, :], in0=t1, in1=t2, op=ALU.add),
                         reads=[b_tq[1], b_tq[2]], accs=[b_dst])

                norm_rope(kvf, 2, gk, cx["b_gk"], kbb[:, :, 0, :], b_kbb, b_kvf)
                S.op("dve", lambda e: e.tensor_copy(kbb[:, :, 1, :], kbb[:, :, 0, :]), accs=[b_kbb])
                pK = PSB[5][:, :].bitcast(BF16)[:, 0:256].rearrange("p (h t) -> p h t", h=2)
                for h in range(2):
                    S.op("pe", lambda e, h=h, pK=pK: e.transpose(out=pK[:, h, :], in_=kbb[:, h, :, :].rearrange("p a d -> p (a d)"), identity=cx["ident_b"][:]),
                         reads=[b_kbb, cx["b_ident_b"]], writes=[b_PSB[5]] if h == 0 else (), accs=[b_PSB[5]] if h > 0 else ())
                S.op("act", lambda e, ti=ti, pK=pK: e.activation(out=kbst[:, :, ti * 128:(ti + 1) * 128], in_=pK, func=AF.Identity), reads=[b_PSB[5]],
                     writes=[b_kbst] if ti == 0 else (), accs=[b_kbst] if ti > 0 else ())
                if own:
                    for kc in range(8):
                        S.op("pe", lambda e, kc=kc, ti=ti: e.matmul(PSB[6][:, :], lhsT=hT[:, kc, ti * 128:(ti + 1) * 128], rhs=win[:, kc, 1536:2048],
                                                                   start=(kc == 0), stop=(kc == 7)),
                             reads=[b_win, b_hT], writes=[b_PSB[6]] if kc == 0 else (), accs=[b_PSB[6]] if kc > 0 else ())
                    S.op("act", lambda e: e.activation(out=qbf[:], in_=PSB[6][:, :], func=AF.Identity), reads=[b_PSB[6]], writes=[b_qbf])
                    norm_rope(qbf, 8, gq, cx["b_gq"], qbb[:, :].rearrange("p (h d) -> p h d", h=8), b_qbb, b_qbf)
                    pQ = PSB[7][:, :].bitcast(BF16)[:, 0:512].rearrange("p (c t) -> p c t", c=4)
                    for c in range(4):
                        S.op("pe", lambda e, c=c, pQ=pQ: e.transpose(out=pQ[:, c, :], in_=qbb[:, c * 128:(c + 1) * 128], identity=cx["ident_b"][:]),
                             reads=[b_qbb, cx["b_ident_b"]], writes=[b_PSB[7]] if c == 0 else (), accs=[b_PSB[7]] if c > 0 else ())
                    S.op("act", lambda e, ti=ti, pQ=pQ: e.activation(out=qbst[:, :, ti * 128:(ti + 1) * 128], in_=pQ, func=AF.Identity), reads=[b_PSB[7]],
                         writes=[b_qbst] if ti == 0 else (), accs=[b_qbst] if ti > 0 else ())
            S.op("act", lambda e, j=j, t0=t0: e.dma_start(out=cx["VA"][j].ap()[t0:t0 + 512, :].rearrange("(a p) n -> p a n", p=128), in_=vast[:]),
                 reads=[b_vast], accs=[b_D[("VA", j)]], dma_key="vast_st")
            S.op("act", lambda e, j=j, t0=t0: e.dma_start(out=cx["VB"][j].ap()[t0:t0 + 512, :, :].rearrange("(a p) h d -> p a h d", p=128), in_=vbst[:]),
                 reads=[b_vbst], accs=[b_D[("VB", j)]], dma_key="vbst_st")
            S.op("act", lambda e, j=j, t0=t0: e.dma_start(out=cx["KB"][j].ap()[:, :, t0:t0 + 512].rearrange("h p t -> p h t"), in_=kbst[:]),
                 reads=[b_kbst], accs=[b_D[("KB", j)]], dma_key="kbst_st")
            if own:
                S.op("act", lambda e, j=j, t0=t0, q0=q0: e.dma_start(out=cx["QB"][j].ap()[:, :, t0 - q0:t0 - q0 + 512].rearrange("c p t -> p c t"), in_=qbst[:]),
                     reads=[b_qbst], accs=[b_D[("QB", j)]], dma_key="qbst_st")
    sst.close(); cx["stk"].pop()
    S.barrier()


def stage_T(cx):
    nc, S, sb, jobs, J = cx["nc"], cx["S"], cx["sb"], cx["jobs"], cx["J"]
    PSB, b_PSB = cx["PSB"], cx["b_PSB"]
    b_D = cx["b_D"]
    ld = cx["ld"]
    sst = ExitStack(); cx["stk"].append(sst)
    SKM = max(j_[0] for j_ in jobs); SQM = max(j_[2] for j_ in jobs)
    jrev = sb("jrev", [128, 128], F32); b_jrev = Buf("jrev")
    ld("sp", jrev[:], cx["C"]["jrev"].ap(), b_jrev, "jrev")
    hk = sb("hk", [128, 1152], F32); b_hk = Buf("hk")
    STR = sb("STR", [128, 4, 1152], F32); b_STR = Buf("STR")
    for h in range(4):
        ld("sp", hk[:], bass.AP(cx["GRD"], h * 1280, [[1, 128], [1, 1152]]), b_hk, "hk")
        for i, (a_, n_) in enumerate([(0, 512), (512, 512), (1024, 128)]):
            S.op("pe", lambda e, a_=a_, n_=n_, i=i: e.matmul(PSB[i][:, 0:n_], lhsT=jrev[:, :], rhs=hk[:, a_:a_ + n_], start=True, stop=True),
                 reads=[b_jrev, b_hk], writes=[b_PSB[i]])
            S.op("dve", lambda e, a_=a_, n_=n_, i=i, h=h: e.tensor_copy(STR[:, h, a_:a_ + n_], PSB[i][:, 0:n_]),
                 reads=[b_PSB[i]], accs=[b_STR])
    W = cx["W"]
    b_WB = cx["b_WB"] = Buf("WB")
    srcs = (W["w_exp_gate"].ap().rearrange("e (p kc) f -> (e p) (kc f)", kc=8),
            W["w_exp_up"].ap().rearrange("e (p kc) f -> (e p) (kc f)", kc=8),
            W["w_exp_down"].ap().rearrange("e (p fc) d -> (e p) (fc d)", fc=2))
    RCH = 512
    cast_jobs = []
    for r0_ in range(0, NE * 128, RCH):
        for wi in range(3):
            cast_jobs.append((r0_, wi))

    def issue_casts(n, gate_buf):
        for _ in range(n):
            if not cast_jobs:
                return
            r0_, wi = cast_jobs.pop(0)
            S.op("pool", lambda e, r0_=r0_, wi=wi: e.dma_start(out=(cx["WBGU"].ap()[r0_:r0_ + RCH, wi * 2048:(wi + 1) * 2048] if wi < 2 else cx["WBD"].ap()[r0_:r0_ + RCH, :]), in_=srcs[wi][r0_:r0_ + RCH, :]),
                 reads=[gate_buf], accs=[b_WB], dma_key="wb_cast")
    NB2 = 2
    kT = [sb("kT%d" % i, [128, SKM], BF16) for i in range(NB2)]; b_kT = [Buf("kT%d" % i) for i in range(NB2)]
    vh = [sb("vh%d" % i, [128, SKM // 128, 128], BF16) for i in range(NB2)]; b_vh = [Buf("vh%d" % i) for i in range(NB2)]
    qT = [sb("qT%d" % i, [128, SQM], BF16) for i in range(NB2)]; b_qT = [Buf("qT%d" % i) for i in range(NB2)]
    NPT = 6
    pT = [sb("pT%d" % i, [128, 512], BF16) for i in range(NPT)]; b_pT = [Buf("pT%d" % i) for i in range(NPT)]
    sbi = [sb("sbi%d" % i, [128, 512], F32) for i in range(2)]; b_sbi = [Buf("sbi%d" % i) for i in range(2)]
    ft = [sb("ft%d" % i, [128, 512], F32) for i in range(5)]; b_ft = [Buf("ft%d" % i) for i in range(5)]
    sqb = sb("sqb", [128, 512], BF16); b_sqb = Buf("sqb")
    oTs = [sb("oTs%d" % i, [128, 512], BF16) for i in range(2)]; b_oTs = [Buf("oTs%d" % i) for i in range(2)]
    b_OT = cx["b_OT"] = Buf("OT")
    cnt = dict(pt=0, sbi=0, hd=0, ots=0, fin=0)
    tok0 = 0
    dacc = [[sb("dacc%d_%d" % (i, m), [128, 512], F32) for m in range(2)] for i in range(2)]
    b_dacc = [[Buf("dacc%d_%d" % (i, m)) for m in range(2)] for i in range(2)]
    dacc_pend = []
    pending = []

    def run_pending(kb, force=False):
        if not pending:
            return
        ph = pending[0]
        trig = (1, 2, 8)
        while ph and (force or kb >= trig[3 - len(ph)]):
            ph.pop(0)()
        if not ph:
            pending.pop(0)

    hfv = sb("hfv", [128, 2], F32); b_hfv = Buf("hfv")
    ld("sp", hfv[:], cx["C"]["hfv"].ap(), b_hfv, "hfv")
    BC2 = sb("BC2", [128, 8], F32); b_BC2 = Buf("BC2")
    SP1 = sb("SP1", [128, 4, 512], F32); b_SP1 = Buf("SP1")
    SP2 = sb("SP2", [128, 4, 512], F32); b_SP2 = Buf("SP2")
    bc = cx["bcst"]
    hb0 = sb("hb0", [128, 8], F32); b_hb0 = Buf("hb0")
    S.op("dve", lambda e: e.tensor_scalar(out=hb0[:, 0:4], in0=bc[:, 0, :], scalar1=hfv[:, 0:1], scalar2=None, op0=ALU.mult),
         reads=[cx["b_bcst"], b_hfv], writes=[b_hb0])
    S.op("dve", lambda e: e.tensor_scalar(out=hb0[:, 4:8], in0=bc[:, 1, :], scalar1=hfv[:, 1:2], scalar2=None, op0=ALU.mult),
         reads=[cx["b_bcst"], b_hfv], accs=[b_hb0])
    S.op("dve", lambda e: e.tensor_tensor(out=BC2[:, 0:4], in0=hb0[:, 0:4], in1=hb0[:, 4:8], op=ALU.add), reads=[b_hb0], writes=[b_BC2])
    for h in range(4):
        S.op("dve", lambda e, h=h: e.tensor_scalar(out=SP1[:, h, :], in0=STR[:, h, 0:512], scalar1=hfv[:, 1:2], scalar2=hb0[:, h:h + 1], op0=ALU.mult, op1=ALU.add),
             reads=[b_STR, b_hfv, b_hb0], accs=[b_SP1])
        S.op("dve", lambda e, h=h: e.tensor_scalar(out=SP2[:, h, :], in0=STR[:, h, 640:1152], scalar1=hfv[:, 0:1], scalar2=hb0[:, 4 + h:5 + h], op0=ALU.mult, op1=ALU.add),
             reads=[b_STR, b_hfv, b_hb0], accs=[b_SP2])
    for j in range(J):
        Sk, q0, Sq = jobs[j][:3]
        split = len(jobs[j]) > 3 and jobs[j][3]
        NKB = Sk // 128
        NKH = Sq // 128
        NQC = Sq // 512
        for h in range(4):
            hb = cnt["hd"] % NB2; cnt["hd"] += 1
            ld("sp", kT[hb][:, 0:Sk], cx["KA"][j].ap()[h, :, :], b_kT[hb], "kT%d" % hb, reads=[b_D[("KA", j)]])
            ld("sp", qT[hb][:, 0:Sq], cx["QA"][j].ap()[h, :, :], b_qT[hb], "qT%d" % hb, reads=[b_D[("QA", j)]])
            ld("sp", vh[hb][:, 0:NKB, :], cx["VA"][j].ap()[:, h * 128:(h + 1) * 128].rearrange("(a p) n -> p a n", p=128),
               b_vh[hb], "vh%d" % hb, reads=[b_D[("VA", j)]])
            for qc in range(Sq // 512):
                qa = q0 + qc * 512
                fi = cnt["fin"] % 2; cnt["fin"] += 1
                ob = (4 + 2 * fi, 5 + 2 * fi)
                if split or J == 1:
                    issue_casts(3, b_pT[cnt["pt"] % NPT])

                def issue_S(kb, hb=hb, qc=qc):
                    for m in range(2):
                        bank = (kb % 2) * 2 + m
                        S.op("pe", lambda e, kb=kb, m=m, bank=bank: e.matmul(PSB[bank][:, :], lhsT=kT[hb][m * 64:(m + 1) * 64, kb * 128:(kb + 1) * 128],
                                                                          rhs=qT[hb][m * 64:(m + 1) * 64, qc * 512:(qc + 1) * 512], start=True, stop=True),
                             reads=[b_kT[hb], b_qT[hb]], writes=[b_PSB[bank]])
                issue_S(0)
                for kb in range(NKB):
                    if kb + 1 < NKB:
                        issue_S(kb + 1)
                    run_pending(kb)
                    d = kb * 128 - qa
                    mode = "std"
                    if split and kb >= NKH:
                        if qc == NQC - 1 and kb == NKH:
                            mode = "sp1"
                        elif qc == 0 and kb == NKB - 1:
                            mode = "sp2"
                        else:
                            mode = "far2"
                    first = (kb == 0); last = (kb == NKB - 1)
                    pis = []
                    for m in range(2):
                        bank = (kb % 2) * 2 + m
                        pi = cnt["pt"] % NPT; cnt["pt"] += 1
                        pis.append(pi)
                        if mode in ("sp1", "sp2") or (mode == "std" and -128 <= d <= 512):
                            if mode == "std":
                                bt = STR[:, h, 512 - d:1024 - d]; bb = b_STR
                            else:
                                bt = (SP1 if mode == "sp1" else SP2)[:, h, :]; bb = b_SP1 if mode == "sp1" else b_SP2
                            si = cnt["sbi"] % 2; cnt["sbi"] += 1
                            S.op("dve", lambda e, bank=bank, si=si, bt=bt: e.scalar_tensor_tensor(
                                out=sbi[si][:], in0=PSB[bank][:, :], scalar=0.125, in1=bt, op0=ALU.mult, op1=ALU.add),
                                reads=[b_PSB[bank], bb], writes=[b_sbi[si]])
                            S.op("act", lambda e, si=si, pi=pi: e.activation(out=pT[pi][:], in_=sbi[si][:], func=AF.Exp),
                                 reads=[b_sbi[si]], writes=[b_pT[pi]])
                        else:
                            if mode == "far2":
                                bias_ap = BC2[:, h:h + 1]; bbuf = b_BC2
                            else:
                                sg_ = 1 if d > 0 else 0
                                bias_ap = cx["bcst"][:, sg_, h:h + 1]; bbuf = cx["b_bcst"]
                            S.op("act", lambda e, bank=bank, pi=pi, bias_ap=bias_ap: e.activation(out=pT[pi][:], in_=PSB[bank][:, :], func=AF.Exp,
                                                                                               scale=0.125, bias=bias_ap),
                                 reads=[b_PSB[bank], bbuf], writes=[b_pT[pi]])
                    for m in range(2):
                        pi = pis[m]
                        S.op("pe", lambda e, kb=kb, m=m, pi=pi, first=first, last=last, hb=hb, obk=ob[m]: e.matmul(PSB[obk][:, :], lhsT=vh[hb][:, kb, :], rhs=pT[pi][:],
                                                                                                          start=first, stop=last),
                             reads=[b_vh[hb], b_pT[pi]], writes=[b_PSB[ob[m]]] if first else (), accs=[b_PSB[ob[m]]] if not first else ())
                    for fnd in dacc_pend:
                        fnd()
                    dacc_pend.clear()
                    for m in range(2):
                        pi = pis[m]
                        if first:
                            dacc_pend.append(lambda pi=pi, fi=fi, m=m: S.op(
                                "dve", lambda e: e.tensor_copy(dacc[fi][m][:], pT[pi][:]), reads=[b_pT[pi]], writes=[b_dacc[fi][m]]))
                        else:
                            dacc_pend.append(lambda pi=pi, fi=fi, m=m: S.op(
                                "dve", lambda e: e.tensor_tensor(out=dacc[fi][m][:], in0=dacc[fi][m][:], in1=pT[pi][:], op=ALU.add),
                                reads=[b_pT[pi]], accs=[b_dacc[fi][m]]))
                for fnd in dacc_pend:
                    fnd()
                dacc_pend.clear()
                if pending:
                    run_pending(0, force=True)
                oi = cnt["ots"] % 2; cnt["ots"] += 1
                c0 = tok0 + qc * 512

                def phA(ob=ob, fi=fi):
                    for m in range(2):
                        if m == 0:
                            S.op("act", lambda e, m=m: e.activation(out=ft[m][:], in_=PSB[ob[m]][:, :], func=AF.Identity), reads=[b_PSB[ob[m]]], writes=[b_ft[m]])
                        else:
                            S.op("dve", lambda e, m=m: e.tensor_copy(ft[m][:], PSB[ob[m]][:, :]), reads=[b_PSB[ob[m]]], writes=[b_ft[m]])
                        S.op("pe", lambda e, m=m: e.matmul(PSB[ob[m]][:, :], lhsT=cx["ones_f"][:, :], rhs=dacc[fi][m][:], start=True, stop=True),
                             reads=[cx["b_ones_f"], b_dacc[fi][m]], writes=[b_PSB[ob[m]]])

                def phB1(ob=ob):
                    for m in range(2):
                        S.op("act", lambda e, m=m: e.activation(out=ft[3][:], in_=PSB[ob[m]][:, :], func=AF.Ln), reads=[b_PSB[ob[m]]], writes=[b_ft[3]])
                        S.op("act", lambda e: e.activation(out=ft[3][:], in_=ft[3][:], func=AF.Exp, scale=-1.0), accs=[b_ft[3]])
                        S.op("dve", lambda e, m=m: e.tensor_tensor(out=ft[m][:], in0=ft[m][:], in1=ft[3][:], op=ALU.mult), reads=[b_ft[3]], accs=[b_ft[m]])
                    S.op("dve", lambda e: e.scalar_tensor_tensor(out=ft[2][:], in0=ft[1][:], scalar=cx["NLAM"], in1=ft[0][:], op0=ALU.mult, op1=ALU.add),
                         reads=[b_ft[0], b_ft[1], cx["b_lsm"]], writes=[b_ft[2]])
                    S.op("dve", lambda e: e.tensor_tensor(out=sqb[:], in0=ft[2][:], in1=ft[2][:], op=ALU.mult), reads=[b_ft[2]], writes=[b_sqb])
                    S.op("pe", lambda e: e.matmul(PSB[ob[0]][:, :], lhsT=cx["ones_b"][:, :], rhs=sqb[:], start=True, stop=True),
                         reads=[cx["b_ones_b"], b_sqb], writes=[b_PSB[ob[0]]])

                def phB2(ob=ob, oi=oi, h=h, c0=c0):
                    S.op("act", lambda e: e.activation(out=ft[3][:], in_=PSB[ob[0]][:, :], func=AF.Ln, scale=1.0 / 128, bias=cx["epsb"][:, 0:1]),
                         reads=[b_PSB[ob[0]], cx["b_epsb"]], writes=[b_ft[3]])
                    S.op("act", lambda e: e.activation(out=ft[4][:], in_=ft[3][:], func=AF.Exp, scale=-0.5), reads=[b_ft[3]], writes=[b_ft[4]])
                    S.op("dve", lambda e: e.scalar_tensor_tensor(out=oTs[oi][:], in0=ft[2][:], scalar=cx["gsub"][:, 0:1], in1=ft[4][:],
                                                                 op0=ALU.mult, op1=ALU.mult),
                         reads=[b_ft[2], b_ft[4], cx["b_gsub"]], writes=[b_oTs[oi]])
                    S.op("act", lambda e: e.dma_start(out=cx["OT"].ap()[h, :, c0:c0 + 512], in_=oTs[oi][:]),
                         reads=[b_oTs[oi]], accs=[b_OT], dma_key="oTs%d_st" % oi)
                pending.append([phA, phB1, phB2])
        while pending:
            run_pending(0, force=True)
        for n in range(2):
            hb = cnt["hd"] % NB2; cnt["hd"] += 1
            ld("sp", kT[hb][:, 0:Sk], cx["KB"][j].ap()[n, :, :], b_kT[hb], "kT%d" % hb, reads=[b_D[("KB", j)]])
            vb1 = vh[hb][:, :, :].rearrange("p a n -> p (a n)")[:, 0:NKB * 65].rearrange("p (a n) -> p a n", n=65)
            ld("sp", vb1, cx["VB"][j].ap()[:, n, :].rearrange("(a p) n -> p a n", p=128), b_vh[hb], "vh%d" % hb, reads=[b_D[("VB", j)]])
            for cpair in range(2):
                c = 2 * n + cpair
                qb_ = cnt["hd"] % NB2 if False else (hb + cpair) % NB2
                ld("sp", qT[qb_][:, 0:Sq], cx["QB"][j].ap()[c, :, :], b_qT[qb_], "qT%d" % qb_, reads=[b_D[("QB", j)]])
                for qc in range(Sq // 512):
                    def issue_SB(kb, hb=hb, qb_=qb_, qc=qc):
                        for hh in range(2):
                            bank = (kb % 2) * 2 + hh
                            S.op("pe", lambda e, kb=kb, hh=hh, bank=bank: e.matmul(PSB[bank][:, :], lhsT=kT[hb][hh * 64:(hh + 1) * 64, kb * 128:(kb + 1) * 128],
                                                                                rhs=qT[qb_][hh * 64:(hh + 1) * 64, qc * 512:(qc + 1) * 512], start=True, stop=True),
                                 reads=[b_kT[hb], b_qT[qb_]], writes=[b_PSB[bank]])
                    if split or J == 1:
                        issue_casts(3, b_pT[cnt["pt"] % NPT])
                    issue_SB(0)
                    for kb in range(NKB):
                        if kb + 1 < NKB:
                            issue_SB(kb + 1)
                        first = (kb == 0); last = (kb == NKB - 1)
                        for hh in range(2):
                            bank = (kb % 2) * 2 + hh
                            pi = cnt["pt"] % NPT; cnt["pt"] += 1
                            S.op("act", lambda e, bank=bank, pi=pi: e.activation(out=pT[pi][:], in_=PSB[bank][:, :], func=AF.Exp, scale=0.125),
                                 reads=[b_PSB[bank]], writes=[b_pT[pi]])
                            S.op("pe", lambda e, kb=kb, hh=hh, pi=pi, first=first, last=last, vb1=vb1: e.matmul(PSB[4 + hh][0:65, :], lhsT=vb1[:, kb, :], rhs=pT[pi][:],
                                                                                                             start=first, stop=last),
                                 reads=[b_vh[hb], b_pT[pi]], writes=[b_PSB[4 + hh]] if first else (), accs=[b_PSB[4 + hh]] if not first else ())
                    for hh in range(2):
                        S.op("act", lambda e, hh=hh: e.activation(out=ft[hh][64:65, :], in_=PSB[4 + hh][64:65, :], func=AF.Ln),
                             reads=[b_PSB[4 + hh]], writes=[b_ft[hh]])
                        S.op("act", lambda e, hh=hh: e.activation(out=ft[hh][64:65, :], in_=ft[hh][64:65, :], func=AF.Exp, scale=-1.0), accs=[b_ft[hh]])
                        S.op("pe", lambda e, hh=hh: e.matmul(PSB[6 + hh][0:64, :], lhsT=cx["ones_f"][64:65, 0:64], rhs=ft[hh][64:65, :], start=True, stop=True),
                             reads=[cx["b_ones_f"], b_ft[hh]], writes=[b_PSB[6 + hh]])
                        S.op("act", lambda e, hh=hh: e.activation(out=ft[2 + hh][0:64, :], in_=PSB[6 + hh][0:64, :], func=AF.Identity),
                             reads=[b_PSB[6 + hh]], writes=[b_ft[2 + hh]])
                        oi = cnt["ots"] % 2; cnt["ots"] += 1
                        S.op("dve", lambda e, hh=hh, oi=oi: e.tensor_tensor(out=oTs[oi][0:64, :], in0=PSB[4 + hh][0:64, :], in1=ft[2 + hh][0:64, :], op=ALU.mult),
                             reads=[b_PSB[4 + hh], b_ft[2 + hh]], writes=[b_oTs[oi]])
                        c0 = tok0 + qc * 512
                        S.op("act", lambda e, oi=oi, c=c, hh=hh, c0=c0: e.dma_start(out=cx["OT"].ap()[4 + c, hh * 64:(hh + 1) * 64, c0:c0 + 512], in_=oTs[oi][0:64, :]),
                             reads=[b_oTs[oi]], accs=[b_OT], dma_key="oTs%d_st" % oi)
        tok0 += Sq
    issue_casts(10 ** 6, b_pT[0])
    sst.close(); cx["stk"].pop()
    S.barrier()


def stage_O(cx):
    nc, S, sb, jobs, J = cx["nc"], cx["S"], cx["sb"], cx["jobs"], cx["J"]
    PSB, b_PSB = cx["PSB"], cx["b_PSB"]
    ld = cx["ld"]; W = cx["W"]
    NT, NBLK = cx["NT"], cx["NBLK"]
    st = cx["st"]; cx["stk"].append(st)
    E8 = sb("E8", [128, NT, 8], F32); b_E8 = Buf("E8")
    R8 = sb("R8", [128, NT, 8], F32); b_R8 = Buf("R8")
    G8 = sb("G8", [128, NT, 8], F32); b_G8 = Buf("G8")
    SL8 = sb("SL8", [128, NT, 8], I32); b_SL8 = Buf("SL8")
    IDXW = sb("IDXW", [128, NBLK], I32); b_IDXW = Buf("IDXW")
    IDXD = sb("IDXD", [128, NBLK], I32); b_IDXD = Buf("IDXD")
    IDXD2 = sb("IDXD2", [128, NBLK], I32); b_IDXD2 = Buf("IDXD2")
    cx["IDXD2"] = IDXD2; cx["b_IDXD2"] = b_IDXD2
    cx["stk"].pop()
    cx.update(E8=E8, b_E8=b_E8, R8=R8, b_R8=b_R8, G8=G8, b_G8=b_G8, SL8=SL8, b_SL8=b_SL8,
              IDXW=IDXW, b_IDXW=b_IDXW, IDXD=IDXD, b_IDXD=b_IDXD)
    sst = ExitStack(); cx["stk"].append(sst)
    rd = lambda n: W[n].ap().rearrange("(kc p) n -> p kc n", p=128)
    wout = sb("wout", [128, 8, D], BF16); b_wout = Buf("wout")
    wr = sb("wr", [128, 8, NE], BF16); b_wr = Buf("wr")
    wsgu = sb("wsgu", [128, 8, 512], BF16); b_wsgu = Buf("wsgu")
    wsd = sb("wsd", [128, 2, D], BF16); b_wsd = Buf("wsd")
    ld("pool", wout[:], rd("w_out"), b_wout, "wout")
    ld("pool", wr[:], rd("w_router"), b_wr, "wr")
    ld("pool", wsgu[:, :, 0:256], rd("w_sh_gate"), b_wsgu, "wsgu")
    ld("pool", wsgu[:, :, 256:512], rd("w_sh_up"), b_wsgu, "wsgu", acc=True)
    ld("pool", wsd[:], W["w_sh_down"].ap().rearrange("(fc p) n -> p fc n", p=128), b_wsd, "wsd")
    rbias = sb("rbias", [128, NE], F32); b_rbias = Buf("rbias")
    ld("sp", rbias[:], bass.AP(W["router_bias"], 0, [[0, 128], [1, NE]]), b_rbias, "rbias")
    E1 = sb("E1", [128, NE], F32); b_E1 = Buf("E1")
    S.op("dve", lambda e: e.tensor_scalar(out=E1[:], in0=cx["iota_e"][:], scalar1=16384.0, scalar2=1.0, op0=ALU.mult, op1=ALU.add),
         reads=[cx["b_iota_e"]], writes=[b_E1])
    Mcum = sb("Mcum", [128, NE], BF16); b_Mcum = Buf("Mcum")
    S.op("dve", lambda e: e.memset(Mcum[:], 0.0), writes=[b_Mcum])
    MOD = [sb("mod%d" % v, [128, D], F32) for v in range(4)]; b_MOD = [Buf("mod%d" % v) for v in range(4)]
    oT = sb("oT", [128, 8, 512], BF16); b_oT = Buf("oT")
    xt = [sb("xo%d" % i, [128, D], F32) for i in range(2)]; b_xt = [Buf("xo%d" % i) for i in range(2)]
    sqj = sb("sqo", [128, D], BF16); b_sqj = Buf("sqo")
    st1 = sb("sto", [128, 8], F32); b_st1 = Buf("sto")
    tmp = sb("tmpo", [128, D], F32); b_tmp = Buf("tmpo")
    h2b = [sb("h2b%d" % i, [128, D], BF16) for i in range(2)]; b_h2b = [Buf("h2b%d" % i) for i in range(2)]
    h2T = sb("h2T", [128, 8, 128], BF16); b_h2T = Buf("h2T")
    sgt = sb("sgt", [128, 256], F32); b_sgt = Buf("sgt")
    ab = sb("ab", [128, 256], BF16); b_ab = Buf("ab")
    aT = sb("aT", [128, 2, 128], BF16); b_aT = Buf("aT")
    scr = sb("scr", [128, NE], F32); b_scr = Buf("scr")
    sel = sb("sel", [128, NE], F32); b_sel = Buf("sel")
    selm = sb("selm", [128, NE], F32); b_selm = Buf("selm")
    g8 = sb("g8", [128, 8, 8], F32); b_g8 = Buf("g8")
    gs = sb("gs", [128, 32], F32); b_gs = Buf("gs")
    Mb = sb("Mb", [128, NE], BF16); b_Mb = Buf("Mb")
    wg = sb("wg", [128, NE], F32); b_wg = Buf("wg")
    pvm = sb("pvm", [128, NE], F32); b_pvm = Buf("pvm")
    p8 = sb("p8", [128, 8], F32); b_p8 = Buf("p8")
    p8i = sb("p8i", [128, 16], I32); b_p8i = Buf("p8i")
    junk = sb("junko", [128, NE], F32); b_junk = Buf("junko")
    b_X1 = cx["b_X1"] = Buf("X1")
    b_H2 = cx["b_H2"] = Buf("H2")
    b_MODR = Buf("MODRr")
    tok0 = 0
    tg = 0
    for j in range(J):
        Sk, q0, Sq = jobs[j][:3]
        for v in range(4):
            ld("sp", MOD[v][:], bass.AP(cx["MODR"], (j * 4 + v) * D, [[0, 128], [1, D]]), b_MOD[v], "mod%d" % v)
        GT1B, G2B, SH2B, GT2B = MOD
        for ch in range(Sq // 512):
            c0 = tok0 + ch * 512
            ld("sp", oT[:], cx["OT"].ap()[:, :, c0:c0 + 512].rearrange("c p t -> p c t"), b_oT, "oT", reads=[cx["b_OT"]])
            for ti in range(4):
                g0 = c0 + ti * 128
                r0 = q0 + ch * 512 + ti * 128
                x_ = xt[tg % 2]; bx = b_xt[tg % 2]
                hb_ = h2b[tg % 2]; bh = b_h2b[tg % 2]
                ld("sp", x_[:], cx["xs"][j].ap()[r0:r0 + 128, :], bx, "xo%d" % (tg % 2))
                for half in range(2):
                    for c in range(8):
                        S.op("pe", lambda e, half=half, c=c, ti=ti: e.matmul(PSB[half][:, :], lhsT=oT[:, c, ti * 128:(ti + 1) * 128],
                                                                            rhs=wout[:, c, half * 512:(half + 1) * 512], start=(c == 0), stop=(c == 7)),
                             reads=[b_oT, b_wout], writes=[b_PSB[half]] if c == 0 else (), accs=[b_PSB[half]] if c > 0 else ())
                    sl = slice(half * 512, (half + 1) * 512)
                    S.op("dve", lambda e, half=half, sl=sl: e.tensor_tensor(out=tmp[:, sl], in0=PSB[half][:, :], in1=GT1B[:, sl], op=ALU.mult),
                         reads=[b_PSB[half], b_MOD[0]], writes=[b_tmp] if half == 0 else (), accs=[b_tmp] if half == 1 else ())
                    S.op("dve", lambda e, sl=sl, x_=x_: e.tensor_tensor(out=x_[:, sl], in0=x_[:, sl], in1=tmp[:, sl], op=ALU.add),
                         reads=[b_tmp], accs=[bx])
                S.op("act", lambda e, x_=x_: e.activation(out=sqj[:], in_=x_[:], func=AF.Square, accum_out=st1[:, 0:1]),
                     reads=[bx], writes=[b_sqj, b_st1])
                S.op("act", lambda e: e.activation(out=st1[:, 1:2], in_=st1[:, 0:1], func=AF.Sqrt, scale=1.0 / D, bias=cx["epsb"][:, 0:1]),
                     reads=[cx["b_epsb"]], accs=[b_st1])
                S.op("dve", lambda e: e.reciprocal(out=st1[:, 2:3], in_=st1[:, 1:2]), accs=[b_st1])
                S.op("dve", lambda e, x_=x_: e.scalar_tensor_tensor(out=tmp[:], in0=x_[:], scalar=st1[:, 2:3], in1=G2B[:], op0=ALU.mult, op1=ALU.mult),
                     reads=[bx, b_st1, b_MOD[1]], writes=[b_tmp])
                S.op("dve", lambda e, hb_=hb_: e.tensor_tensor(out=hb_[:], in0=tmp[:], in1=SH2B[:], op=ALU.add),
                     reads=[b_tmp, b_MOD[2]], writes=[bh])
                S.op("act", lambda e, hb_=hb_, g0=g0: e.dma_start(out=cx["H2"].ap()[g0:g0 + 128, :], in_=hb_[:]),
                     reads=[bh], accs=[b_H2], dma_key="h2b%d_st" % (tg % 2))
                pT_ = PSB[2][:, :].bitcast(BF16).rearrange("p (kc t) -> p kc t", kc=8)
                for kc in range(8):
                    S.op("pe", lambda e, kc=kc, hb_=hb_, pT_=pT_: e.transpose(out=pT_[:, kc, :], in_=hb_[:, kc * 128:(kc + 1) * 128], identity=cx["ident_b"][:]),
                         reads=[bh, cx["b_ident_b"]], writes=[b_PSB[2]] if kc == 0 else (), accs=[b_PSB[2]] if kc > 0 else ())
                S.op("act", lambda e, pT_=pT_: e.activation(out=h2T[:], in_=pT_, func=AF.Identity), reads=[b_PSB[2]], writes=[b_h2T])
                for kc in range(8):
                    S.op("pe", lambda e, kc=kc: e.matmul(PSB[3][:, 0:NE], lhsT=h2T[:, kc, :], rhs=wr[:, kc, :], start=(kc == 0), stop=(kc == 7)),
                         reads=[b_h2T, b_wr], writes=[b_PSB[3]] if kc == 0 else (), accs=[b_PSB[3]] if kc > 0 else ())
                for kc in range(8):
                    S.op("pe", lambda e, kc=kc: e.matmul(PSB[4][:, :], lhsT=h2T[:, kc, :], rhs=wsgu[:, kc, :], start=(kc == 0), stop=(kc == 7)),
                         reads=[b_h2T, b_wsgu], writes=[b_PSB[4]] if kc == 0 else (), accs=[b_PSB[4]] if kc > 0 else ())
                S.op("act", lambda e: e.activation(out=sgt[:], in_=PSB[4][:, 0:256], func=AF.Silu), reads=[b_PSB[4]], writes=[b_sgt])
                S.op("dve", lambda e: e.tensor_tensor(out=ab[:], in0=PSB[4][:, 256:512], in1=sgt[:], op=ALU.mult),
                     reads=[b_PSB[4], b_sgt], writes=[b_ab])
                pA = PSB[5][:, :].bitcast(BF16)[:, 0:256].rearrange("p (c t) -> p c t", c=2)
                for fc in range(2):
                    S.op("pe", lambda e, fc=fc, pA=pA: e.transpose(out=pA[:, fc, :], in_=ab[:, fc * 128:(fc + 1) * 128], identity=cx["ident_b"][:]),
                         reads=[b_ab, cx["b_ident_b"]], writes=[b_PSB[5]] if fc == 0 else (), accs=[b_PSB[5]] if fc > 0 else ())
                S.op("act", lambda e, pA=pA: e.activation(out=aT[:], in_=pA, func=AF.Identity), reads=[b_PSB[5]], writes=[b_aT])
                for half in range(2):
                    for fc in range(2):
                        S.op("pe", lambda e, half=half, fc=fc: e.matmul(PSB[6 + half][:, :], lhsT=aT[:, fc, :], rhs=wsd[:, fc, half * 512:(half + 1) * 512],
                                                                       start=(fc == 0), stop=(fc == 1)),
                             reads=[b_aT, b_wsd], writes=[b_PSB[6 + half]] if fc == 0 else (), accs=[b_PSB[6 + half]] if fc > 0 else ())
                    sl = slice(half * 512, (half + 1) * 512)
                    S.op("dve", lambda e, half=half, sl=sl: e.tensor_tensor(out=tmp[:, sl], in0=PSB[6 + half][:, :], in1=GT2B[:, sl], op=ALU.mult),
                         reads=[b_PSB[6 + half], b_MOD[3]], writes=[b_tmp] if half == 0 else (), accs=[b_tmp] if half == 1 else ())
                    S.op("dve", lambda e, sl=sl, x_=x_: e.tensor_tensor(out=x_[:, sl], in0=x_[:, sl], in1=tmp[:, sl], op=ALU.add),
                         reads=[b_tmp], accs=[bx])
                S.op("act", lambda e, x_=x_, g0=g0: e.dma_start(out=cx["X1"].ap()[g0:g0 + 128, :], in_=x_[:]),
                     reads=[bx], accs=[b_X1], dma_key="xo%d_st" % (tg % 2))
                S.op("act", lambda e: e.activation(out=scr[:], in_=PSB[3][:, 0:NE], func=AF.Sigmoid), reads=[b_PSB[3]], writes=[b_scr])
                S.op("dve", lambda e: e.tensor_tensor(out=sel[:], in0=scr[:], in1=rbias[:], op=ALU.add), reads=[b_scr, b_rbias], writes=[b_sel])
                for g in range(8):
                    S.op("dve", lambda e, g=g: e.max(out=g8[:, g, :], in_=sel[:, g * 32:(g + 1) * 32]), reads=[b_sel],
                         writes=[b_g8] if g == 0 else (), accs=[b_g8] if g > 0 else ())
                S.op("dve", lambda e: e.tensor_tensor(out=gs[:, 0:8], in0=g8[:, :, 0], in1=g8[:, :, 1], op=ALU.add), reads=[b_g8], writes=[b_gs])
                S.op("dve", lambda e: e.max(out=gs[:, 8:16], in_=gs[:, 0:8]), accs=[b_gs])
                S.op("dve", lambda e: e.tensor_scalar(out=gs[:, 16:24], in0=gs[:, 0:8], scalar1=gs[:, 11:12], scalar2=None, op0=ALU.is_ge), accs=[b_gs])
                S.op("dve", lambda e: e.scalar_tensor_tensor(out=selm[:].rearrange("p (g i) -> p g i", g=8), in0=sel[:].rearrange("p (g i) -> p g i", g=8),
                                                             scalar=10.0, in1=gs[:, 16:24].unsqueeze(2).to_broadcast([128, 8, 32]), op0=ALU.add, op1=ALU.mult),
                     reads=[b_sel, b_gs], writes=[b_selm])
                S.op("dve", lambda e: e.max(out=gs[:, 24:32], in_=selm[:]), reads=[b_selm], accs=[b_gs])
                S.op("dve", lambda e: e.tensor_scalar(out=Mb[:], in0=selm[:], scalar1=gs[:, 31:32], scalar2=None, op0=ALU.is_ge),
                     reads=[b_selm, b_gs], writes=[b_Mb])
                S.op("dve", lambda e: e.tensor_tensor(out=wg[:], in0=scr[:], in1=Mb[:], op=ALU.mult), reads=[b_scr, b_Mb], writes=[b_wg])
                S.op("dve", lambda e: e.tensor_reduce(out=p8[:, 0:1], in_=wg[:], axis=AX.X, op=ALU.add), reads=[b_wg], writes=[b_p8])
                S.op("dve", lambda e: e.reciprocal(out=p8[:, 1:2], in_=p8[:, 0:1]), accs=[b_p8])
                S.op("dve", lambda e: e.tensor_scalar(out=wg[:], in0=wg[:], scalar1=p8[:, 1:2], scalar2=2.5, op0=ALU.mult, op1=ALU.mult),
                     reads=[b_p8], accs=[b_wg])
                S.op("pe", lambda e: e.matmul(PSB[5][:, 0:NE], lhsT=cx["tri_b"][:, :], rhs=Mb[:], start=True, stop=False),
                     reads=[cx["b_tri"], b_Mb], writes=[b_PSB[5]])
                S.op("pe", lambda e: e.matmul(PSB[5][:, 0:NE], lhsT=cx["ones_b"][:, :], rhs=Mcum[:], start=False, stop=True),
                     reads=[cx["b_ones_b"], b_Mcum], accs=[b_PSB[5]])
                S.op("dve", lambda e: e.tensor_tensor(out=pvm[:], in0=PSB[5][:, 0:NE], in1=E1[:], op=ALU.add), reads=[b_PSB[5], b_E1], writes=[b_pvm])
                S.op("dve", lambda e: e.tensor_tensor(out=pvm[:], in0=pvm[:], in1=Mb[:], op=ALU.mult), reads=[b_Mb], accs=[b_pvm])
                S.op("dve", lambda e: e.tensor_tensor(out=Mcum[:], in0=Mcum[:], in1=Mb[:], op=ALU.add), reads=[b_Mb], writes=[b_Mcum])
                S.op("dve", lambda e: e.max(out=p8[:, 0:8], in_=pvm[:]), reads=[b_pvm], writes=[b_p8])
                S.op("dve", lambda e: e.tensor_copy(p8i[:, 0:8], p8[:, 0:8]), reads=[b_p8], writes=[b_p8i])
                S.op("dve", lambda e: e.tensor_scalar(out=p8i[:, 8:16], in0=p8i[:, 0:8], scalar1=14, scalar2=None, op0=ALU.arith_shift_right), accs=[b_p8i])
                S.op("dve", lambda e, tg=tg: e.tensor_copy(E8[:, tg, :], p8i[:, 8:16]), reads=[b_p8i], accs=[b_E8])
                S.op("dve", lambda e: e.tensor_scalar(out=p8i[:, 8:16], in0=p8i[:, 0:8], scalar1=16383, scalar2=None, op0=ALU.bitwise_and), accs=[b_p8i])
                S.op("dve", lambda e, tg=tg: e.tensor_copy(R8[:, tg, :], p8i[:, 8:16]), reads=[b_p8i], accs=[b_R8])
                for k in range(8):
                    S.op("dve", lambda e, k=k, tg=tg: e.scalar_tensor_tensor(out=junk[:], in0=pvm[:], scalar=p8[:, k:k + 1], in1=wg[:], op0=ALU.is_equal, op1=ALU.mult,
                                                                             accum_out=G8[:, tg, k:k + 1]),
                         reads=[b_pvm, b_p8, b_wg], writes=[b_junk], accs=[b_G8])
                tg += 1
        tok0 += Sq
    cc = sb("cc", [128, 16], F32); b_cc = Buf("cc")
    cci = sb("cci", [128, 8], I32); b_cci = Buf("cci")
    dg = sb("dg", [128, 128], F32); b_dg = Buf("dg")
    PSrow = sb("PSrow", [128, NE], F32); b_PSrow = Buf("PSrow")
    for ec in range(2):
        S.op("pe", lambda e, ec=ec: e.matmul(PSB[0][:, ec:ec + 1], lhsT=Mcum[:, ec * 128:(ec + 1) * 128], rhs=cx["ones_b"][:, 0:1], start=True, stop=True),
             reads=[b_Mcum, cx["b_ones_b"]], writes=[b_PSB[0]] if ec == 0 else (), accs=[b_PSB[0]] if ec else ())
    S.op("dve", lambda e: e.tensor_scalar(out=cc[:, 0:2], in0=PSB[0][:, 0:2], scalar1=127.0, scalar2=None, op0=ALU.add), reads=[b_PSB[0]], writes=[b_cc])
    S.op("dve", lambda e: e.tensor_copy(cci[:, 0:2], cc[:, 0:2]), reads=[b_cc], writes=[b_cci])
    S.op("dve", lambda e: e.tensor_scalar(out=cci[:, 2:4], in0=cci[:, 0:2], scalar1=7, scalar2=None, op0=ALU.arith_shift_right), accs=[b_cci])
    S.op("dve", lambda e: e.tensor_copy(cc[:, 2:4], cci[:, 2:4]), reads=[b_cci], accs=[b_cc])
    for ec in range(2):
        S.op("pe", lambda e, ec=ec: e.matmul(PSB[1][:, ec:ec + 1], lhsT=cx["tril_f"][:, :], rhs=cc[:, 2 + ec:3 + ec], start=True, stop=(ec == 0)),
             reads=[cx["b_tril"], b_cc], writes=[b_PSB[1]] if ec == 0 else (), accs=[b_PSB[1]] if ec else ())
        if ec == 1:
            S.op("pe", lambda e: e.matmul(PSB[1][:, 1:2], lhsT=cx["ones_f"][:, :], rhs=cc[:, 2:3], start=False, stop=True),
                 reads=[cx["b_ones_f"], b_cc], accs=[b_PSB[1]])
    S.op("dve", lambda e: e.tensor_copy(cc[:, 4:6], PSB[1][:, 0:2]), reads=[b_PSB[1]], accs=[b_cc])
    S.op("dve", lambda e: e.tensor_tensor(out=cc[:, 6:8], in0=cc[:, 4:6], in1=cc[:, 2:4], op=ALU.subtract), accs=[b_cc])
    S.op("dve", lambda e: e.tensor_scalar(out=cc[:, 8:10], in0=cc[:, 6:8], scalar1=128.0, scalar2=None, op0=ALU.mult), accs=[b_cc])
    for ec in range(2):
        S.op("dve", lambda e, ec=ec: e.tensor_scalar(out=dg[:], in0=cx["ident_f"][:], scalar1=cc[:, 8 + ec:9 + ec], scalar2=None, op0=ALU.mult),
             reads=[cx["b_ident_f"], b_cc], writes=[b_dg])
        S.op("pe", lambda e, ec=ec: e.matmul(PSB[2][:, ec * 128:(ec + 1) * 128], lhsT=cx["ones_f"][:, :], rhs=dg[:], start=True, stop=True),
             reads=[cx["b_ones_f"], b_dg], writes=[b_PSB[2]] if ec == 0 else (), accs=[b_PSB[2]] if ec else ())
    S.op("dve", lambda e: e.tensor_copy(PSrow[:], PSB[2][:, 0:NE]), reads=[b_PSB[2]], writes=[b_PSrow])
    for t in range(NT):
        for k in range(8):
            S.op("dve", lambda e, k=k, t=t: e.scalar_tensor_tensor(out=junk[:], in0=cx["iota_e"][:], scalar=E8[:, t, k:k + 1], in1=PSrow[:], op0=ALU.is_equal, op1=ALU.mult,
                                                                   accum_out=p8[:, k:k + 1]),
                 reads=[cx["b_iota_e"], b_E8, b_PSrow], writes=[b_junk], accs=[b_p8])
        S.op("dve", lambda e, t=t: e.tensor_tensor(out=p8[:, 0:8], in0=p8[:, 0:8], in1=R8[:, t, :], op=ALU.add), reads=[b_R8], accs=[b_p8])
        S.op("dve", lambda e, t=t: e.tensor_scalar(out=SL8[:, t, :], in0=p8[:, 0:8], scalar1=-1.0, scalar2=None, op0=ALU.add), reads=[b_p8], accs=[b_SL8])
    NB1 = min(NBLK, 512)
    ind = sb("ind", [128, NBLK], BF16); b_ind = Buf("ind")
    EB = sb("EB", [128, NBLK], F32); b_EB = Buf("EB")
    CH = sb("CH", [128, NBLK], F32); b_CH = Buf("CH")
    for ec in range(2):
        S.op("dve", lambda e, ec=ec: e.tensor_scalar(out=ind[:], in0=cx["iota_b"][:, 0:NBLK], scalar1=cc[:, 4 + ec:5 + ec], scalar2=None, op0=ALU.is_ge),
             reads=[cx["b_iota_b"], b_cc], writes=[b_ind])
        for a0 in range(0, NBLK, 512):
            n_ = min(512, NBLK - a0)
            bk = 3 + a0 // 512
            S.op("pe", lambda e, ec=ec, a0=a0, n_=n_, bk=bk: e.matmul(PSB[bk][:, 0:n_], lhsT=cx["ones_b"][:, :], rhs=ind[:, a0:a0 + n_], start=(ec == 0), stop=(ec == 1)),
                 reads=[cx["b_ones_b"], b_ind], writes=[b_PSB[bk]] if ec == 0 else (), accs=[b_PSB[bk]] if ec else ())
    for a0 in range(0, NBLK, 512):
        n_ = min(512, NBLK - a0)
        bk = 3 + a0 // 512
        S.op("dve", lambda e, a0=a0, n_=n_, bk=bk: e.tensor_scalar(out=EB[:, a0:a0 + n_], in0=PSB[bk][:, 0:n_], scalar1=255.0, scalar2=None, op0=ALU.min),
             reads=[b_PSB[bk]], writes=[b_EB] if a0 == 0 else (), accs=[b_EB] if a0 else ())
    S.op("dve", lambda e: e.memset(CH[:, 0:4], 1.0), writes=[b_CH])
    S.op("dve", lambda e: e.tensor_tensor(out=CH[:, 4:NBLK], in0=EB[:, 4:NBLK], in1=EB[:, 0:NBLK - 4], op=ALU.not_equal), reads=[b_EB], accs=[b_CH])
    S.op("dve", lambda e: e.memset(CH[0:1, :], 1.0), accs=[b_CH])
    BIGI = 1.0e6
    EBs = sb("EBs", [128, NBLK], F32); b_EBs = Buf("EBs")
    S.op("dve", lambda e: e.tensor_scalar(out=EBs[:], in0=EB[:], scalar1=128.0, scalar2=cx["iota_p"][:, 0:1], op0=ALU.mult, op1=ALU.add),
         reads=[b_EB, cx["b_iota_p"]], writes=[b_EBs])
    S.op("dve", lambda e: e.scalar_tensor_tensor(out=EBs[:], in0=EBs[:], scalar=-BIGI, in1=CH[:], op0=ALU.add, op1=ALU.mult),
         reads=[b_CH], accs=[b_EBs])
    S.op("dve", lambda e: e.tensor_scalar(out=IDXW[:], in0=EBs[:], scalar1=BIGI, scalar2=None, op0=ALU.add), reads=[b_EBs], writes=[b_IDXW])
    b_XS = cx["b_XS"] = Buf("XS")
    for t in range(NT):
        hb_ = h2b[t % 2]; bh = b_h2b[t % 2]
        ld("sp", hb_[:], cx["H2"].ap()[t * 128:(t + 1) * 128, :], bh, "h2b%d_ld" % (t % 2), reads=[b_H2])
        for k in range(8):
            S.op("pool", lambda e, hb_=hb_, t=t, k=k: e.indirect_dma_start(out=cx["XS"].ap(), out_offset=bass.IndirectOffsetOnAxis(ap=SL8[:, t, k:k + 1], axis=0),
                                                                          in_=hb_[:], in_offset=None),
                 reads=[bh, b_SL8], accs=[b_XS], dma_key="h2b%d_sc" % (t % 2))
    sst.close(); cx["stk"].pop()
    S.barrier()


_REGS = {}


def breg(e, nc, val):
    k = (id(nc), val)
    if k not in _REGS:
        _REGS[k] = e.to_reg(val)
    return _REGS[k]


def stage_E(cx):
    nc, S, sb = cx["nc"], cx["S"], cx["sb"]
    PSB, b_PSB = cx["PSB"], cx["b_PSB"]
    W = cx["W"]; NBLK = cx["NBLK"]
    IDXW = cx["IDXW"]
    sst = ExitStack(); cx["stk"].append(sst)
    RW = 4
    wall = [sb("wall%d" % i, [128, 6144], BF16) for i in range(RW)]; b_wall = [Buf("wall%d" % i) for i in range(RW)]
    NXB = 3
    xb = [sb("xb%d" % i, [128, D], BF16) for i in range(NXB)]; b_xb = [Buf("xb%d" % i) for i in range(NXB)]
    xT = [sb("xTe%d" % i, [128, 8, 128], BF16) for i in range(2)]; b_xT = [Buf("xTe%d" % i) for i in range(2)]
    sg = [sb("sge%d" % i, [128, 256], F32) for i in range(2)]; b_sg = [Buf("sge%d" % i) for i in range(2)]
    aT = [sb("aTe%d" % i, [128, 2, 128], BF16) for i in range(2)]; b_aT = [Buf("aTe%d" % i) for i in range(2)]
    yb = [sb("yb%d" % i, [128, D], BF16) for i in range(2)]; b_yb = [Buf("yb%d" % i) for i in range(2)]
    b_YS = cx["b_YS"] = Buf("YS")
    NROW_W = NE * 128 - 1

    def load_w(b):
        i = b % RW
        S.op("pool", lambda e, b=b, i=i: e.indirect_dma_start(out=wall[i][:, 0:4096], out_offset=None, in_=cx["WBGU"].ap(),
                                                             in_offset=bass.IndirectOffsetOnAxis(ap=IDXW[:, b:b + 1], axis=0),
                                                             bounds_check=breg(e, nc, NROW_W), oob_is_err=False),
             reads=[cx["b_IDXW"], cx["b_WB"]], writes=[b_wall[i]], dma_key="wall%d" % i)
        S.op("pool", lambda e, b=b, i=i: e.indirect_dma_start(out=wall[i][:, 4096:6144], out_offset=None, in_=cx["WBD"].ap(),
                                                             in_offset=bass.IndirectOffsetOnAxis(ap=IDXW[:, b:b + 1], axis=0),
                                                             bounds_check=breg(e, nc, NROW_W), oob_is_err=False),
             reads=[cx["b_IDXW"], cx["b_WB"]], accs=[b_wall[i]], dma_key="wall%d" % i)

    def load_x(b):
        cx["ld"]("sp", xb[b % NXB][:], cx["XS"].ap()[b * 128:(b + 1) * 128, :], b_xb[b % NXB], "xb%d" % (b % NXB), reads=[cx["b_XS"]])

    def do_T(b):
        i = b % 2
        x_ = xb[b % NXB]; bx = b_xb[b % NXB]
        pX = PSB[i][:, :].bitcast(BF16).rearrange("p (kc t) -> p kc t", kc=8)
        xv = x_[:, :].rearrange("p (q kc) -> p kc q", kc=8)
        for kc in range(8):
            S.op("pe", lambda e, kc=kc, pX=pX, xv=xv: e.transpose(out=pX[:, kc, :], in_=xv[:, kc, :], identity=cx["ident_b"][:]),
                 reads=[bx, cx["b_ident_b"]], writes=[b_PSB[i]] if kc == 0 else (), accs=[b_PSB[i]] if kc > 0 else ())
        S.op("act", lambda e, pX=pX, i=i: e.activation(out=xT[i][:, 0:4, :], in_=pX[:, 0:4, :], func=AF.Identity), reads=[b_PSB[i]], writes=[b_xT[i]])
        S.op("dve", lambda e, pX=pX, i=i: e.tensor_copy(xT[i][:, 4:8, :], pX[:, 4:8, :]), reads=[b_PSB[i]], accs=[b_xT[i]])

    def do_GU(b):
        i = b % 2
        bank = 2 + i
        first = True
        wr_ = b % RW
        wgu = wall[wr_][:, 0:4096].rearrange("p (w kc f) -> p w kc f", w=2, kc=8)
        for which in range(2):
            wt = wgu[:, which]
            bw = b_wall[wr_]
            for fc in range(2):
                for kc in range(8):
                    S.op("pe", lambda e, wt=wt, fc=fc, kc=kc, which=which, bank=bank, i=i: e.matmul(
                        PSB[bank][:, which * 256 + fc * 128: which * 256 + (fc + 1) * 128], lhsT=wt[:, kc, fc:256:2], rhs=xT[i][:, kc, :],
                        start=(kc == 0), stop=(kc == 7)),
                        reads=[bw, b_xT[i]], writes=[b_PSB[bank]] if first else (), accs=[b_PSB[bank]] if not first else ())
                    first = False
        S.op("act", lambda e, bank=bank, i=i: e.activation(out=sg[i][:], in_=PSB[bank][:, 0:256], func=AF.Silu), reads=[b_PSB[bank]], writes=[b_sg[i]])
        S.op("dve", lambda e, bank=bank, i=i: e.tensor_tensor(out=aT[i][:].rearrange("p c t -> p (c t)"), in0=PSB[bank][:, 256:512], in1=sg[i][:], op=ALU.mult),
             reads=[b_PSB[bank], b_sg[i]], writes=[b_aT[i]])

    def do_D(b):
        i = b % 2
        y_ = yb[i]; by = b_yb[i]
        for half in range(2):
            bank = 4 + 2 * i + half
            for fc in range(2):
                wdv_ = wall[b % RW][:, 4096:6144].rearrange("p (fc d) -> p fc d", fc=2)
                S.op("pe", lambda e, half=half, fc=fc, bank=bank, i=i, wdv_=wdv_: e.matmul(PSB[bank][:, :], lhsT=aT[i][:, fc, :], rhs=wdv_[:, fc, half * 512:(half + 1) * 512],
                                                                                        start=(fc == 0), stop=(fc == 1)),
                     reads=[b_aT[i], b_wall[b % RW]], writes=[b_PSB[bank]] if fc == 0 else (), accs=[b_PSB[bank]] if fc else ())
            if half == 0:
                S.op("act", lambda e, y_=y_, bank=bank: e.activation(out=y_[:, 0:512], in_=PSB[bank][:, :], func=AF.Identity),
                     reads=[b_PSB[bank]], writes=[by])
            else:
                S.op("dve", lambda e, y_=y_, bank=bank: e.tensor_copy(y_[:, 512:1024], PSB[bank][:, :]), reads=[b_PSB[bank]], accs=[by])
        S.op("act", lambda e, y_=y_, b=b: e.dma_start(out=cx["YS"].ap()[b * 128:(b + 1) * 128, :], in_=y_[:]),
             reads=[by], accs=[b_YS], dma_key="yb%d_st" % i)

    for b0 in range(min(RW, NBLK)):
        load_w(b0)
    load_x(0); load_x(1)
    do_T(0)
    for b in range(NBLK):
        if b + 2 < NBLK:
            load_x(b + 2)
        if b + 1 < NBLK:
            do_T(b + 1)
        do_GU(b)
        if b >= 1:
            do_D(b - 1)
            if b - 1 + RW < NBLK:
                load_w(b - 1 + RW)
    do_D(NBLK - 1)
    sst.close(); cx["stk"].pop()
    S.barrier()


def stage_C(cx):
    nc, S, sb, jobs, J = cx["nc"], cx["S"], cx["sb"], cx["jobs"], cx["J"]
    NT = cx["NT"]
    SL8, G8 = cx["SL8"], cx["G8"]
    sst = ExitStack(); cx["stk"].append(sst)
    gfrow = sb("gfrow", [128, D], F32); b_gfrow = Buf("gfrow")
    cx["ld"]("sp", gfrow[:], bass.AP(cx["W"]["g_final"], 0, [[0, 128], [1, D]]), b_gfrow, "gfrow")
    gt2 = sb("gt2c", [128, D], F32); b_gt2 = Buf("gt2c")
    x1 = [sb("x1c%d" % i, [128, D], F32) for i in range(2)]; b_x1 = [Buf("x1c%d" % i) for i in range(2)]
    yg = [sb("yg%d" % i, [128, D], BF16) for i in range(8)]; b_yg = [Buf("yg%d" % i) for i in range(8)]
    acc = sb("accc", [128, D], F32); b_acc = Buf("accc")
    sq = sb("sqc", [128, D], BF16); b_sq = Buf("sqc")
    stc = sb("stc", [128, 8], F32); b_stc = Buf("stc")
    outs = []
    t = 0
    for j in range(J):
        Sk, q0, Sq = jobs[j][:3]
        cx["ld"]("sp", gt2[:], bass.AP(cx["MODR"], (j * 4 + 3) * D, [[0, 128], [1, D]]), b_gt2, "gt2c")
        for tl in range(Sq // 128):
            x_ = x1[t % 2]; bx = b_x1[t % 2]
            cx["ld"]("sp", x_[:], cx["X1"].ap()[t * 128:(t + 1) * 128, :], bx, "x1c%d" % (t % 2), reads=[cx["b_X1"]])
            for k in range(8):
                S.op("pool", lambda e, t=t, k=k: e.indirect_dma_start(out=yg[k][:], out_offset=None, in_=cx["YS"].ap(),
                                                                     in_offset=bass.IndirectOffsetOnAxis(ap=SL8[:, t, k:k + 1], axis=0)),
                     reads=[cx["b_SL8"], cx["b_YS"]], writes=[b_yg[k]], dma_key="yg%d" % k)
            S.op("dve", lambda e, t=t: e.tensor_scalar(out=acc[:], in0=yg[0][:], scalar1=G8[:, t, 0:1], scalar2=None, op0=ALU.mult),
                 reads=[b_yg[0], cx["b_G8"]], writes=[b_acc])
            for k in range(1, 8):
                S.op("dve", lambda e, t=t, k=k: e.scalar_tensor_tensor(out=acc[:], in0=yg[k][:], scalar=G8[:, t, k:k + 1], in1=acc[:], op0=ALU.mult, op1=ALU.add),
                     reads=[b_yg[k], cx["b_G8"]], accs=[b_acc])
            S.op("dve", lambda e: e.tensor_tensor(out=acc[:], in0=acc[:], in1=gt2[:], op=ALU.mult), reads=[b_gt2], accs=[b_acc])
            S.op("dve", lambda e, x_=x_: e.tensor_tensor(out=x_[:], in0=x_[:], in1=acc[:], op=ALU.add), reads=[b_acc], accs=[bx])
            S.op("act", lambda e, x_=x_: e.activation(out=sq[:], in_=x_[:], func=AF.Square, accum_out=stc[:, 0:1]), reads=[bx], writes=[b_sq, b_stc])
            S.op("act", lambda e: e.activation(out=stc[:, 1:2], in_=stc[:, 0:1], func=AF.Sqrt, scale=1.0 / D, bias=cx["epsb"][:, 0:1]),
                 reads=[cx["b_epsb"]], accs=[b_stc])
            S.op("dve", lambda e: e.reciprocal(out=stc[:, 2:3], in_=stc[:, 1:2]), accs=[b_stc])
            S.op("dve", lambda e, x_=x_: e.scalar_tensor_tensor(out=x_[:], in0=x_[:], scalar=stc[:, 2:3], in1=gfrow[:], op0=ALU.mult, op1=ALU.mult),
                 reads=[b_stc, b_gfrow], accs=[bx])
            o = S.op("act", lambda e, x_=x_, t=t: e.dma_start(out=cx["y_out"].ap()[t * 128:(t + 1) * 128, :], in_=x_[:]),
                     reads=[bx], dma_key="x1c%d_st" % (t % 2))
            outs.append(o)
            t += 1
    sst.close(); cx["stk"].pop()
    return outs


def build_all(jobs, dbg=False):
    cx = build(jobs, dbg=dbg)
    stage_P(cx)
    stage_T(cx)
    stage_O(cx)
    stage_E(cx)
    outs = stage_C(cx)
    cx["S"].emit(outs)
    return cx


def make_in_map(jobs, xseqs, cvecs, poss, hf, weights, consts):
    J = len(jobs)
    im = {}
    for j in range(J):
        im["xs%d" % j] = np.ascontiguousarray(xseqs[j], dtype=np.float32)
        im["rope%d" % j] = rope_table(poss[j])
    cT = np.stack([np.asarray(cv, np.float32).reshape(8, 128).T for cv in cvecs], axis=-1)
    im["cT"] = np.ascontiguousarray(cT)
    im.update(weights)
    for n, v in consts.items():
        im["c_" + n] = v
    im["c_hfv"] = np.broadcast_to(np.array([[float(hf), 1.0 - float(hf)]], np.float32), (128, 2)).copy()
    return im


def kernel(**inputs):
    jobs = [(2048, 0, 2048, False), (2048, 0, 2048, False), (8192, 0, 4096, True)]
    cx = build_all(jobs)
    weights = {}
    for n in WEIGHT_NAMES:
        a = np.asarray(inputs[n], dtype=np.float32)
        if n not in ("rel_bias", "g_final"):
            a = a[0]
        weights[n] = np.ascontiguousarray(a)
    consts = host_constants()
    xp = np.asarray(inputs["x_prompt"], np.float32)
    xsm = np.asarray(inputs["x_sample"], np.float32)
    cp = np.asarray(inputs["c_prompt"], np.float32)
    cs = np.asarray(inputs["c_sample"], np.float32)
    in_maps = []
    for c in range(8):
        sbi, hf = c // 2, c % 2
        seq = xsm[sbi]
        pos2 = np.arange(8192)
        if hf:
            seq = np.concatenate([seq[4096:], seq[:4096]], axis=0)
            pos2 = np.concatenate([pos2[4096:], pos2[:4096]])
        in_maps.append(make_in_map(jobs, [xp[2 * c], xp[2 * c + 1], seq], [cp[2 * c], cp[2 * c + 1], cs[sbi]],
                                   [np.arange(2048), np.arange(2048), pos2], hf, weights, consts))
    res = run_bass_kernel_spmd(cx["nc"], in_maps, core_ids=list(range(8)))
    y_prompt = np.empty((16, 2048, D), np.float32)
    y_sample = np.empty((4, 8192, D), np.float32)
    for c in range(8):
        y = np.asarray(res.results[c]["y"], dtype=np.float32)
        y_prompt[2 * c] = y[0:2048]
        y_prompt[2 * c + 1] = y[2048:4096]
        y_sample[c // 2, (c % 2) * 4096:(c % 2 + 1) * 4096] = y[4096:8192]
    return (y_prompt, y_sample)
```
